# Optimizing a Trainium2 kernel written in Bass

```python
import math
import jax
import jax.numpy as jnp
from jax import lax
import numpy as np

D_MODEL = 1024
BATCH = 8
SEQ = 2048
DEPTH = 2

GRID_W = 64
CTX_LEN = 256
A_HEADS = 4
A_HEAD_DIM = 64
A_WIDTH = 2 * A_HEADS * A_HEAD_DIM
ROPE_THETA = 10000.0
Q_BLOCK = 128
B_WIDTH = D_MODEL // 2
B_CONV = 3
C_WIDTH = D_MODEL
C_BLOCKS = 4
C_BLOCK = C_WIDTH // C_BLOCKS
C_CONV = 4
LRU_C = 8.0
N_EXPERTS = 32
TOP_K = 4
EXPERT_FF = D_MODEL
SWIGLU_ALPHA = 1.702
SWIGLU_LIMIT = 7.0
NORM_EPS = 1e-6
N_EVEN = (DEPTH + 1) // 2
N_ODD = DEPTH // 2
EVEN_IN = 3 * A_WIDTH + 3 * B_WIDTH
ODD_IN = 2 * C_WIDTH

kernel_name = "hybrid_diffattn_shortconv_rglru_moe_prefix_trunk"


def rmsnorm(x, g):
    xf = x.astype(jnp.float32)
    y = xf * lax.rsqrt(jnp.mean(xf * xf, axis=-1, keepdims=True) + NORM_EPS)
    return (y * g.astype(jnp.float32)).astype(x.dtype)


def modulate(h, shift, scale):
    return h * (1 + scale) + shift


def adaln(cvec, w_mod, b_mod, n_chunks):
    d = cvec.shape[-1]
    m = jax.nn.silu(cvec) @ w_mod[:, : n_chunks * d] + b_mod[: n_chunks * d]
    return jnp.split(m, n_chunks, axis=-1)


def depthwise_conv(x, w, pad):
    return lax.conv_general_dilated(
        x, w[:, None, :].astype(x.dtype), window_strides=(1,), padding=[pad],
        dimension_numbers=("NWC", "WIO", "NWC"), feature_group_count=x.shape[-1])


def axial_rope_tables(n_tokens, dtype):
    n_rows = n_tokens // GRID_W
    rows, cols = jnp.meshgrid(jnp.arange(n_rows), jnp.arange(GRID_W), indexing="ij")
    pos = jnp.stack([rows.reshape(-1), cols.reshape(-1)], axis=-1).astype(jnp.float32)
    n_freq = A_HEAD_DIM // 4
    inv = ROPE_THETA ** (-jnp.arange(n_freq, dtype=jnp.float32) / n_freq)
    ang = pos[:, :, None] * inv
    return jnp.cos(ang).astype(dtype), jnp.sin(ang).astype(dtype)


def apply_axial_rope(x, cos, sin):
    xs = x.reshape(x.shape[:-1] + (2, 2, A_HEAD_DIM // 4))
    x1, x2 = xs[..., 0, :], xs[..., 1, :]
    out = jnp.stack([x1 * cos - x2 * sin, x2 * cos + x1 * sin], axis=-2)
    return out.reshape(x.shape)


def qk_heads(t):
    b, l, _ = t.shape
    return t.reshape(b, l, A_HEADS, 2, A_HEAD_DIM).transpose(0, 2, 3, 1, 4)


def v_heads(t):
    b, l, _ = t.shape
    return t.reshape(b, l, A_HEADS, 2 * A_HEAD_DIM).transpose(0, 2, 1, 3)


def diff_attend(q, k, v, lam):
    s = jnp.einsum("bhmqd,bhmkd->bhmqk", q, k, preferred_element_type=jnp.float32) * (A_HEAD_DIM ** -0.5)
    p = jax.nn.softmax(s, axis=-1)
    w = p[:, :, 0] - lam * p[:, :, 1]
    return jnp.einsum("bhqk,bhkv->bhqv", w.astype(v.dtype), v)


def merge_heads(o, subln_g, lam_init):
    b, h, l, dv = o.shape
    o = rmsnorm(o, subln_g) * (1.0 - lam_init)
    return o.transpose(0, 2, 1, 3).reshape(b, l, h * dv)


def even_mixer(h_ctx, h_lat, w_in, w_out, lq1, lk1, lq2, lk2, subln_g, conv_w, lam_init, cos, sin, need_ctx):
    f32 = jnp.float32
    lam = (jnp.exp(jnp.sum(lq1.astype(f32) * lk1.astype(f32)))
           - jnp.exp(jnp.sum(lq2.astype(f32) * lk2.astype(f32))) + lam_init)
    splits = (A_WIDTH, 2 * A_WIDTH, 3 * A_WIDTH, 3 * A_WIDTH + B_WIDTH, 3 * A_WIDTH + 2 * B_WIDTH)
    q_l, k_l, v_l, bg_l, cg_l, u_l = jnp.split(h_lat @ w_in, splits, axis=-1)
    if need_ctx:
        q_c, k_c, v_c, bg_c, cg_c, u_c = jnp.split(h_ctx @ w_in, splits, axis=-1)
    else:
        k_c, v_c = jnp.split(h_ctx @ w_in[:, A_WIDTH:3 * A_WIDTH], 2, axis=-1)
    kh_c, vh_c = qk_heads(k_c), v_heads(v_c)
    q_l = apply_axial_rope(qk_heads(q_l), cos, sin)
    k_l = apply_axial_rope(qk_heads(k_l), cos, sin)
    k_all = jnp.concatenate([kh_c, k_l], axis=3)
    v_all = jnp.concatenate([vh_c, v_heads(v_l)], axis=2)
    b, h, _, s_len, d = q_l.shape
    nb = s_len // Q_BLOCK
    q_blocks = jnp.moveaxis(q_l.reshape(b, h, 2, nb, Q_BLOCK, d), 3, 0)
    o = lax.map(lambda qb: diff_attend(qb, k_all, v_all, lam), q_blocks)
    o = jnp.moveaxis(o, 0, 2).reshape(b, h, s_len, 2 * A_HEAD_DIM)
    attn_l = merge_heads(o, subln_g, lam_init)
    pad = (B_CONV // 2, B_CONV // 2)
    conv_l = bg_l * depthwise_conv(cg_l * u_l, conv_w, pad)
    y_lat = jnp.concatenate([attn_l, conv_l], axis=-1) @ w_out
    y_ctx = None
    if need_ctx:
        attn_c = merge_heads(diff_attend(qk_heads(q_c), kh_c, vh_c, lam), subln_g, lam_init)
        conv_c = bg_c * depthwise_conv(cg_c * u_c, conv_w, pad)
        y_ctx = jnp.concatenate([attn_c, conv_c], axis=-1) @ w_out
    return y_ctx, y_lat


def block_diag_linear(x, w, b):
    xb = x.reshape(x.shape[:-1] + (C_BLOCKS, C_BLOCK))
    return jnp.einsum("blgi,gio->blgo", xb, w).reshape(x.shape) + b


def linear_scan(a, u, h0, reverse, emit):
    def step(h, au):
        h = au[0] * h + au[1]
        return h, (h if emit else None)
    h_end, hs = lax.scan(step, h0, (jnp.swapaxes(a, 0, 1), jnp.swapaxes(u, 0, 1)), reverse=reverse)
    return h_end, (jnp.swapaxes(hs, 0, 1) if emit else None)


def rglru_direction(u_ctx, u_lat, conv_w, conv_b, ga_w, ga_b, gx_w, gx_b, lru_lam, reverse, need_ctx):
    f32 = jnp.float32
    pad = (0, C_CONV - 1) if reverse else (C_CONV - 1, 0)

    def gate_inputs(u):
        xc = depthwise_conv(u, conv_w, pad) + conv_b
        r = jax.nn.sigmoid(block_diag_linear(xc, ga_w, ga_b).astype(f32))
        i = jax.nn.sigmoid(block_diag_linear(xc, gx_w, gx_b).astype(f32))
        log_a = -LRU_C * r * jax.nn.softplus(-lru_lam.astype(f32))
        return jnp.exp(log_a), jnp.sqrt(-jnp.expm1(2.0 * log_a)) * (i * xc.astype(f32))

    a_c, b_c = gate_inputs(u_ctx)
    h0 = jnp.zeros((u_ctx.shape[0], C_WIDTH), f32)
    h_ctx_end, hs_c = linear_scan(a_c, b_c, h0, reverse, need_ctx)
    a_l, b_l = gate_inputs(u_lat)
    _, hs_l = linear_scan(a_l, b_l, h_ctx_end, reverse, True)
    return hs_c, hs_l


def odd_mixer(h_ctx, h_lat, w_in, w_out, conv_w, conv_b, ga_w, ga_b, gx_w, gx_b, lru_lam, need_ctx):
    gate_l, u_l = jnp.split(h_lat @ w_in, 2, axis=-1)
    if need_ctx:
        gate_c, u_c = jnp.split(h_ctx @ w_in, 2, axis=-1)
    else:
        u_c = h_ctx @ w_in[:, C_WIDTH:]
    outs = [rglru_direction(u_c, u_l, conv_w[d], conv_b[d], ga_w[d], ga_b[d], gx_w[d], gx_b[d],
                            lru_lam[d], d == 1, need_ctx) for d in range(2)]
    rec_l = outs[0][1] + outs[1][1]
    y_lat = (rec_l.astype(h_lat.dtype) * jax.nn.gelu(gate_l)) @ w_out
    y_ctx = None
    if need_ctx:
        rec_c = outs[0][0] + outs[1][0]
        y_ctx = (rec_c.astype(h_ctx.dtype) * jax.nn.gelu(gate_c)) @ w_out
    return y_ctx, y_lat


def moe_ffn(x, w_r, b_r, w1, b1, w2, b2):
    logits = (x @ w_r + b_r).astype(jnp.float32)
    top_v, top_i = lax.top_k(logits, TOP_K)
    probs = jax.nn.softmax(top_v, axis=-1)
    gates = jnp.sum(jax.nn.one_hot(top_i, N_EXPERTS, dtype=jnp.float32) * probs[..., None], axis=1)

    def expert(acc, ew):
        w1e, b1e, w2e, b2e, ge = ew
        h = x @ w1e + b1e
        glu = jnp.minimum(h[:, :EXPERT_FF], SWIGLU_LIMIT)
        lin = jnp.clip(h[:, EXPERT_FF:], -SWIGLU_LIMIT, SWIGLU_LIMIT)
        act = glu * jax.nn.sigmoid(SWIGLU_ALPHA * glu) * (lin + 1)
        return acc + ge[:, None] * (act @ w2e + b2e), None

    y, _ = lax.scan(expert, jnp.zeros_like(x), (w1, b1, w2, b2, gates.T.astype(x.dtype)))
    return y


def setup_inputs(seed: int = 0) -> dict:
    key = jax.random.key(seed)
    ks = iter(jax.random.split(key, 40))
    D, E, F = D_MODEL, N_EXPERTS, EXPERT_FF

    def nrm(shape, scale):
        return jax.random.normal(next(ks), shape, jnp.float32) * scale

    u = jax.random.uniform(next(ks), (N_ODD, 2, C_WIDTH), jnp.float32, 0.9, 0.999)
    s = u ** (1.0 / LRU_C)
    lru_lambda = jnp.log(s) - jnp.log1p(-s)
    return {
        "x": nrm((BATCH, SEQ, D), 1.0),
        "c": nrm((BATCH, D), 1.0),
        "ctx": nrm((BATCH, CTX_LEN, D), 1.0),
        "c_ctx": nrm((D,), 1.0),
        "w_mod": nrm((DEPTH, D, 6 * D), 0.5 * D ** -0.5),
        "b_mod": nrm((DEPTH, 6 * D), 0.02),
        "norm_mix": 1.0 + nrm((DEPTH, D), 0.05),
        "norm_ffn": 1.0 + nrm((DEPTH, D), 0.05),
        "ev_w_in": nrm((N_EVEN, D, EVEN_IN), D ** -0.5),
        "ev_w_out": nrm((N_EVEN, A_WIDTH + B_WIDTH, D), (A_WIDTH + B_WIDTH) ** -0.5),
        "ev_lambda_q1": nrm((N_EVEN, A_HEAD_DIM), 0.1),
        "ev_lambda_k1": nrm((N_EVEN, A_HEAD_DIM), 0.1),
        "ev_lambda_q2": nrm((N_EVEN, A_HEAD_DIM), 0.1),
        "ev_lambda_k2": nrm((N_EVEN, A_HEAD_DIM), 0.1),
        "ev_subln": 1.0 + nrm((N_EVEN, 2 * A_HEAD_DIM), 0.05),
        "ev_conv_w": nrm((N_EVEN, B_CONV, B_WIDTH), B_CONV ** -0.5),
        "od_w_in": nrm((N_ODD, D, ODD_IN), D ** -0.5),
        "od_w_out": nrm((N_ODD, C_WIDTH, D), C_WIDTH ** -0.5),
        "od_conv_w": nrm((N_ODD, 2, C_CONV, C_WIDTH), C_CONV ** -0.5),
        "od_conv_b": nrm((N_ODD, 2, C_WIDTH), 0.02),
        "od_gate_a_w": nrm((N_ODD, 2, C_BLOCKS, C_BLOCK, C_BLOCK), C_BLOCK ** -0.5),
        "od_gate_a_b": nrm((N_ODD, 2, C_WIDTH), 0.02),
        "od_gate_x_w": nrm((N_ODD, 2, C_BLOCKS, C_BLOCK, C_BLOCK), C_BLOCK ** -0.5),
        "od_gate_x_b": nrm((N_ODD, 2, C_WIDTH), 0.02),
        "od_lru_lambda": lru_lambda,
        "moe_w_router": nrm((DEPTH, D, E), D ** -0.5),
        "moe_b_router": nrm((DEPTH, E), 0.01),
        "moe_w1": nrm((DEPTH, E, D, 2 * F), D ** -0.5),
        "moe_b1": nrm((DEPTH, E, 2 * F), 0.02),
        "moe_w2": nrm((DEPTH, E, F, D), F ** -0.5),
        "moe_b2": nrm((DEPTH, E, D), 0.02),
        "final_norm": 1.0 + nrm((D,), 0.05),
    }


def reference(x, c, ctx, c_ctx, w_mod, b_mod, norm_mix, norm_ffn,
              ev_w_in, ev_w_out, ev_lambda_q1, ev_lambda_k1, ev_lambda_q2, ev_lambda_k2, ev_subln, ev_conv_w,
              od_w_in, od_w_out, od_conv_w, od_conv_b, od_gate_a_w, od_gate_a_b, od_gate_x_w, od_gate_x_b,
              od_lru_lambda, moe_w_router, moe_b_router, moe_w1, moe_b1, moe_w2, moe_b2, final_norm):
    b, s_len, d_model = x.shape
    n_ctx_tok = b * ctx.shape[1]
    cos, sin = axial_rope_tables(s_len, x.dtype)
    for i in range(DEPTH):
        last = i == DEPTH - 1
        need_ctx = not last
        sh1, sc1, g1, sh2, sc2, g2 = [m[:, None, :] for m in adaln(c, w_mod[i], b_mod[i], 6)]
        mc = adaln(c_ctx, w_mod[i], b_mod[i], 2 if last else 6)
        h_lat = modulate(rmsnorm(x, norm_mix[i]), sh1, sc1)
        h_ctx = modulate(rmsnorm(ctx, norm_mix[i]), mc[0], mc[1])
        j = i // 2
        if i % 2 == 0:
            lam_init = 0.8 - 0.6 * math.exp(-0.3 * i)
            y_ctx, y_lat = even_mixer(h_ctx, h_lat, ev_w_in[j], ev_w_out[j], ev_lambda_q1[j], ev_lambda_k1[j],
                                      ev_lambda_q2[j], ev_lambda_k2[j], ev_subln[j], ev_conv_w[j], lam_init,
                                      cos, sin, need_ctx)
        else:
            y_ctx, y_lat = odd_mixer(h_ctx, h_lat, od_w_in[j], od_w_out[j], od_conv_w[j], od_conv_b[j],
                                     od_gate_a_w[j], od_gate_a_b[j], od_gate_x_w[j], od_gate_x_b[j],
                                     od_lru_lambda[j], need_ctx)
        x = x + g1 * y_lat
        h_lat2 = modulate(rmsnorm(x, norm_ffn[i]), sh2, sc2)
        moe_p = (moe_w_router[i], moe_b_router[i], moe_w1[i], moe_b1[i], moe_w2[i], moe_b2[i])
        if last:
            x = x + g2 * moe_ffn(h_lat2.reshape(-1, d_model), *moe_p).reshape(x.shape)
        else:
            ctx = ctx + mc[2] * y_ctx
            h_ctx2 = modulate(rmsnorm(ctx, norm_ffn[i]), mc[3], mc[4])
            tok = jnp.concatenate([h_ctx2.reshape(-1, d_model), h_lat2.reshape(-1, d_model)], axis=0)
            out = moe_ffn(tok, *moe_p)
            ctx = ctx + mc[5] * out[:n_ctx_tok].reshape(ctx.shape)
            x = x + g2 * out[n_ctx_tok:].reshape(x.shape)
    return rmsnorm(x, final_norm)
```

```python
import math
from contextlib import ExitStack

import numpy as np
import concourse.bass as bass
import concourse.mybir as mybir
from concourse.bass_utils import run_bass_kernel_spmd

F32 = mybir.dt.float32
F32R = mybir.dt.float32r
BF16 = mybir.dt.bfloat16
AF = mybir.ActivationFunctionType
ALU = mybir.AluOpType
AX = mybir.AxisListType

D = 1024
NCH = 8
TC = 256
TL = 2048
T = TC + TL
TOFF = [0, 256, 768, 1280, 1792]
TN = [256, 512, 512, 512, 512]
NTT = 5
NKT = T // 128
EPS = 1e-6
SELF_SYNC = True
CAP = 512
NCHUNK = (T + CAP - 1) // CAP
NJ = CAP // 128
TS = NCHUNK * CAP
I32 = mybir.dt.int32
ET = mybir.EngineType
ENG_KEY = {ET.PE: "pe", ET.Activation: "act", ET.DVE: "dve", ET.Pool: "pool", ET.SP: "sp"}


def tsl(tt):
    return slice(TOFF[tt], TOFF[tt] + TN[tt])


def vec_layout(NE):
    ents = [("c", 8), ("cctx", 8), ("bmod", 96), ("nmix", 16), ("nffn", 16), ("fnorm", 8), ("evconv", 12),
            ("subln", 1), ("odconvw", 64), ("odconvb", 16), ("odgab", 16), ("odgxb", 16), ("odlam", 16),
            ("b1", 2 * NE * 16)]
    off = {}
    r = 0
    for name, n in ents:
        off[name] = r
        r += n
    rpad = ((r + 127) // 128) * 128
    return off, r, rpad


class Dep:
    __slots__ = ("w", "r", "sem", "tot")

    def __init__(self):
        self.w = None
        self.r = {}
        self.sem = None
        self.tot = 0


class G:
    def __init__(self, nc, es):
        self.nc = nc
        self.es = es
        self.eng = {"pe": nc.tensor, "act": nc.scalar, "dve": nc.vector, "pool": nc.gpsimd, "sp": nc.sync}
        self.semh = []
        self.esem = {}
        self.cnt = {}
        self.known = {}
        for k in self.eng:
            self.esem[k] = self.new_sem("e_" + k)
            self.cnt[k] = 0
            self.known[k] = {}
        self.dma_sems = []
        self.nsem = 0
        self.alldeps = []

    def new_sem(self, name):
        h = self.es.enter_context(self.nc.semaphore(name))
        self.semh.append(h)
        return len(self.semh) - 1

    def snapshot(self):
        deps = [(d, d.w, dict(d.r), d.tot) for d in self.alldeps]
        return (deps, dict(self.cnt), {k: dict(v) for k, v in self.known.items()})

    def restore(self, snap):
        deps, cnt, known = snap
        for d, w, r, tot in deps:
            d.w = w
            d.r = dict(r)
            d.tot = tot
        self.cnt = dict(cnt)
        self.known = {k: dict(v) for k, v in known.items()}

    def pad_to(self, big):
        deps, cnt, _ = big
        for e in self.eng:
            diff = cnt[e] - self.cnt[e]
            assert diff >= 0, (e, diff)
            if diff > 0:
                self.eng[e].wait_ge(self.semh[self.esem[e]], self.cnt[e])
                self.eng[e].sem_inc(self.semh[self.esem[e]], diff)
                self.cnt[e] = cnt[e]
        for d, w, r, tot in deps:
            if d.sem is None:
                continue
            diff = tot - d.tot
            assert diff >= 0
            if diff > 0:
                self.eng["sp"].wait_ge(self.semh[d.sem], d.tot)
                self.eng["sp"].sem_inc(self.semh[d.sem], diff)
                d.tot = tot

    def dep(self, dma=False):
        d = Dep()
        self.alldeps.append(d)
        if dma:
            self.nsem += 1
            d.sem = self.new_sem("d%d" % self.nsem)
            self.dma_sems.append(d)
        return d

    def _wait(self, e, deps):
        need = {}
        kn = self.known[e]
        for d in deps:
            if d is None:
                continue
            s, v = d
            if s == self.esem[e] and (e == "pe" or not SELF_SYNC):
                continue
            if kn.get(s, 0) >= v:
                continue
            if need.get(s, 0) < v:
                need[s] = v
        for s, v in need.items():
            self.eng[e].wait_ge(self.semh[s], v)
            kn[s] = v

    def op(self, e, fn, reads=(), writes=()):
        deps = []
        for t in reads:
            deps.append(t.w)
        for t in writes:
            deps.append(t.w)
            deps.extend(t.r.items())
        self._wait(e, deps)
        ins = fn()
        self.cnt[e] += 1
        s = self.esem[e]
        ins.then_inc(self.semh[s], 1)
        me = (s, self.cnt[e])
        for t in reads:
            t.r[s] = self.cnt[e]
        for t in writes:
            t.w = me
            t.r = {}
        return ins

    def dma(self, q, out, in_, reads=(), writes=(), sem_dep=None):
        deps = []
        for t in reads:
            deps.append(t.w)
        for t in writes:
            deps.append(t.w)
            deps.extend(t.r.items())
        self._wait(q, deps)
        sd = sem_dep if sem_dep is not None else writes[0]
        assert sd.sem is not None
        ins = self.eng[q].dma_start(out=out, in_=in_)
        sd.tot += 16
        ins.then_inc(self.semh[sd.sem], 16)
        me = (sd.sem, sd.tot)
        for t in reads:
            t.r[sd.sem] = sd.tot
        for t in writes:
            t.w = me
            t.r = {}
        return ins

    def idma(self, out, off_ap, in_, bound, reads=(), writes=()):
        deps = []
        for t in reads:
            deps.append(t.w)
        for t in writes:
            deps.append(t.w)
            deps.extend(t.r.items())
        self._wait("pool", deps)
        sd = writes[0]
        ins = self.nc.gpsimd.indirect_dma_start(out=out, out_offset=bass.IndirectOffsetOnAxis(ap=off_ap, axis=0), in_=in_, in_offset=None,
                                                bounds_check=bound, oob_is_err=False)
        sd.tot += 16
        ins.then_inc(self.semh[sd.sem], 16)
        me = (sd.sem, sd.tot)
        for t in reads:
            t.r[sd.sem] = sd.tot
        for t in writes:
            t.w = me
            t.r = {}
        return ins

    def barrier(self):
        for e in self.eng:
            deps = [(self.esem[o], self.cnt[o]) for o in self.eng if o != e and self.cnt[o] > 0]
            deps += [(d.sem, d.tot) for d in self.dma_sems if d.tot > 0]
            self._wait(e, deps)


def build(NE=32, dbg=None):
    nc = bass.Bass("TRN2", target_bir_lowering=False)
    voff, vrows, vpad = vec_layout(NE)
    NVB = vpad // 128

    def din(name, shape, dt=F32):
        return nc.dram_tensor(name, list(shape), dt, kind="ExternalInput").ap()

    xin = din("xin", [TL, D])
    ctxin = din("ctxin", [TC, D])
    vecs = din("vecs", [vpad, 128])
    w_mod = din("w_mod", [2, D, 6 * D])
    ev_w_in = din("ev_w_in", [D, 3 * D])
    ev_w_out = din("ev_w_out", [D, D])
    od_w_in = din("od_w_in", [D, 2 * D])
    od_w_out = din("od_w_out", [D, D])
    od_ga = din("od_ga", [2, 4, 256, 256])
    od_gx = din("od_gx", [2, 4, 256, 256])
    w_router = din("w_router", [2, D, NE])
    b_router = din("b_router", [2, NE])
    moe_w1 = din("moe_w1", [2, NE, D, 2 * D])
    moe_w2 = din("moe_w2", [2, NE, D, D])
    moe_b2 = din("moe_b2", [2, NE, D])
    lams = din("lams", [4, 64])
    ident_d = din("ident", [128, 128])
    perm_d = din("perm", [128, 128])
    cos_d = din("cosT", [128, T])
    sin_d = din("sinT", [128, T])
    utri_d = din("utri", [128, 128])
    offrow_d = din("offrow", [128, NE])
    tok4f_d = din("tok4f", [128, 4])
    out_d = nc.dram_tensor("out", [TL, D], F32, kind="ExternalOutput").ap()
    xs_d = nc.dram_tensor("xs_scratch", [128, NCH, T], F32, kind="Internal").ap()
    gsc_d = nc.dram_tensor("g_scratch", [2, NE, T], F32, kind="Internal").ap()
    psc_d = nc.dram_tensor("p_scratch", [2, NE, T], F32, kind="Internal").ap()
    cnt_d = nc.dram_tensor("cnt_scratch", [2, NE], I32, kind="Internal").ap()
    hs_d = nc.dram_tensor("hs_scratch", [NE * TS, D], BF16, kind="Internal").ap()
    idx_d = nc.dram_tensor("idx_scratch", [NE * TS, 1], I32, kind="Internal").ap()
    y4_d = nc.dram_tensor("y4_scratch", [4 * T, D], F32, kind="Internal").ap()
    dbg_d = None
    if dbg is not None:
        dbg_d = nc.dram_tensor("dbg", [128, NCH, T], F32, kind="ExternalOutput").ap()

    es = ExitStack()
    with es:
        g = G(nc, es)
        xs_dep = g.dep(dma=True)
        gsc_dep = [g.dep(dma=True), g.dep(dma=True)]
        psc_dep = [g.dep(dma=True), g.dep(dma=True)]
        flag_dep = [g.dep(dma=True), g.dep(dma=True)]
        hs_dep = g.dep(dma=True)
        bnd_hs = nc.gpsimd.alloc_register("bnd_hs")
        nc.gpsimd.reg_mov(bnd_hs, NE * TS - 1)
        bnd_y4 = nc.gpsimd.alloc_register("bnd_y4")
        nc.gpsimd.reg_mov(bnd_y4, 4 * T - 1)
        idx_dep = g.dep(dma=True)
        y4_dep = g.dep(dma=True)
        flag_regs = nc.alloc_registers("ovf", [ET.PE, ET.Activation, ET.DVE, ET.Pool, ET.SP])
        out_dep = g.dep(dma=True)

        _uid = [0]

        def sb(es_, name, shape, dt, side=None):
            _uid[0] += 1
            return es_.enter_context(nc.sbuf_tensor("sb%d_%s" % (_uid[0], name), list(shape), dt, side=side))

        ps = [es.enter_context(nc.psum_tensor("ps%d" % i, [128, 512], F32)) for i in range(8)]
        psd = [g.dep() for _ in range(8)]

        ident = sb(es, "ident", [128, 128], F32)
        perm = sb(es, "perm", [128, 128], F32)
        ones32 = sb(es, "ones32", [128, 128], F32)
        ones16 = sb(es, "ones16", [128, 128], BF16)
        V = sb(es, "V", [128, vpad], F32)
        modT = sb(es, "modT", [128, 2, 48, 2], F32)
        S = sb(es, "S", [128, 160], F32)
        cdep = g.dep(dma=True)
        Vd = g.dep()
        modd = g.dep()
        Sd = g.dep()
        onesd = g.dep()
        g.dma("sp", ident[:], ident_d, writes=[cdep])
        g.dma("sp", perm[:], perm_d, writes=[cdep])
        identb = sb(es, "identb", [128, 128], BF16)
        utri = sb(es, "utri", [128, 128], F32)
        offrow = sb(es, "offrow", [128, NE], F32)
        tok4f = sb(es, "tok4f", [128, 4], F32)
        cdep2 = g.dep(dma=True)
        g.dma("pool", identb[:], ident_d, writes=[cdep2])
        g.dma("sp", utri[:], utri_d, writes=[cdep2])
        g.dma("sp", offrow[:], offrow_d, writes=[cdep2])
        g.dma("sp", tok4f[:], tok4f_d, writes=[cdep2])
        g.op("dve", lambda: nc.vector.memset(ones32[:], 1.0), writes=[onesd])
        g.op("dve", lambda: nc.vector.memset(ones16[:], 1.0), writes=[onesd])

        SC = {}
        _sc = [0]

        def scol(name, n):
            SC[name] = _sc[0]
            _sc[0] += n
            return SC[name]

        for l in range(2):
            for cls in range(2):
                scol("gs1_%d_%d" % (l, cls), 8)
                scol("gs2_%d_%d" % (l, cls), 8)
        scol("sg", 1)
        scol("neglam", 1)
        scol("c8", 16)
        scol("c8x2", 16)
        scol("tmp", 16)
        assert _sc[0] <= 160

        def Scol(name, i=0):
            return S[:, SC[name] + i:SC[name] + i + 1]

        def Vcol(name, i=0):
            return V[:, voff[name] + i:voff[name] + i + 1]

        def mod(l, k, c, cls):
            return modT[:, l, k * 8 + c, cls:cls + 1]

        pes = ExitStack()
        with pes:
            vstg = [sb(pes, "vstg%d" % i, [128, 128], F32) for i in range(2)]
            vstd = [g.dep(dma=True) for _ in range(2)]
            for blk in range(NVB):
                s = blk % 2
                g.dma("sp", vstg[s][:], vecs[blk * 128:(blk + 1) * 128, :], writes=[vstd[s]])
                pb = blk % 2
                g.op("pe", lambda: nc.tensor.transpose(out=ps[pb][:, 0:128], in_=vstg[s][:], identity=ident[:]),
                     reads=[vstd[s], cdep], writes=[psd[pb]])
                g.op("act", lambda: nc.scalar.copy(out=V[:, blk * 128:(blk + 1) * 128], in_=ps[pb][:, 0:128]),
                     reads=[psd[pb]], writes=[Vd])
            b1v = V[:, voff["b1"]:voff["b1"] + 2 * NE * 16].rearrange("p (a j) -> p a j", j=16)
            g.op("dve", lambda: nc.vector.tensor_scalar(out=b1v[:, :, 8:16], in0=b1v[:, :, 8:16], scalar1=1.0,
                                                        scalar2=None, op0=ALU.add), reads=[Vd], writes=[Vd])
            scT = sb(pes, "scT", [128, 8, 2], F32R)
            scd = g.dep()
            g.op("act", lambda: nc.scalar.activation(out=scT[:, :, 0], in_=V[:, voff["c"]:voff["c"] + 8],
                                                     func=AF.Silu), reads=[Vd], writes=[scd])
            g.op("act", lambda: nc.scalar.activation(out=scT[:, :, 1], in_=V[:, voff["cctx"]:voff["cctx"] + 8],
                                                     func=AF.Silu), reads=[Vd], writes=[scd])
            wm = [sb(pes, "wm%d" % i, [128, 8, 512], F32R) for i in range(2)]
            wmd = [g.dep(dma=True) for _ in range(2)]
            it = 0
            for l in range(2):
                for blk in range(12):
                    s = it % 2
                    it += 1
                    src = w_mod[l, :, blk * 512:(blk + 1) * 512].rearrange("(c p) f -> p c f", p=128)
                    g.dma("pool", wm[s][:], src, writes=[wmd[s]])
                    for fcl in range(4):
                        pb = (blk * 4 + fcl) % 2
                        for dc in range(8):
                            g.op("pe", lambda: nc.tensor.matmul(ps[pb][:, 0:2], lhsT=wm[s][:, dc, fcl * 128:(fcl + 1) * 128],
                                                                rhs=scT[:, dc, :], start=(dc == 0), stop=(dc == 7)),
                                 reads=[wmd[s], scd], writes=[psd[pb]])
                        if True:
                            kk = blk * 4 + fcl
                            g.op("dve", lambda: nc.vector.tensor_scalar(out=modT[:, l, kk, :], in0=ps[pb][:, 0:2],
                                                                        scalar1=Vcol("bmod", l * 48 + kk), scalar2=None,
                                                                        op0=ALU.add), reads=[psd[pb], Vd], writes=[modd])
            for l in range(2):
                for cls in range(2):
                    for (nm, kc, vn) in (("gs1", 1, "nmix"), ("gs2", 4, "nffn")):
                        c0 = SC["%s_%d_%d" % (nm, l, cls)]
                        g.op("dve", lambda: nc.vector.scalar_tensor_tensor(
                            out=S[:, c0:c0 + 8], in0=modT[:, l, kc * 8:kc * 8 + 8, cls], scalar=1.0,
                            in1=V[:, voff[vn] + l * 8:voff[vn] + l * 8 + 8], op0=ALU.add, op1=ALU.mult),
                            reads=[modd, Vd], writes=[Sd])
            lam_init0 = 0.8 - 0.6 * math.exp(-0.3 * 0)
            g.op("dve", lambda: nc.vector.tensor_scalar(out=Scol("sg"), in0=Vcol("subln"), scalar1=(1.0 - lam_init0),
                                                        scalar2=None, op0=ALU.mult), reads=[Vd], writes=[Sd])
            lb = sb(pes, "lamb", [128, 4, 64], F32)
            lbd = g.dep(dma=True)
            for i in range(4):
                g.dma("sp", lb[:, i, :], lams[i:i + 1, :].to_broadcast([128, 64]), writes=[lbd])
            lt = sb(pes, "lamt", [128, 2, 64], F32)
            ltd = g.dep()
            g.op("dve", lambda: nc.vector.tensor_tensor(out=lt[:, 0, :], in0=lb[:, 0, :], in1=lb[:, 1, :], op=ALU.mult),
                 reads=[lbd], writes=[ltd])
            g.op("dve", lambda: nc.vector.tensor_tensor(out=lt[:, 1, :], in0=lb[:, 2, :], in1=lb[:, 3, :], op=ALU.mult),
                 reads=[lbd], writes=[ltd])
            tm = SC["tmp"]
            g.op("dve", lambda: nc.vector.tensor_reduce(out=S[:, tm:tm + 2], in_=lt[:], axis=AX.X, op=ALU.add),
                 reads=[ltd], writes=[Sd])
            g.op("act", lambda: nc.scalar.activation(out=S[:, tm + 2:tm + 4], in_=S[:, tm:tm + 2], func=AF.Exp),
                 reads=[Sd], writes=[Sd])
            g.op("dve", lambda: nc.vector.scalar_tensor_tensor(out=Scol("neglam"), in0=S[:, tm + 3:tm + 4],
                                                               scalar=-lam_init0, in1=S[:, tm + 2:tm + 3],
                                                               op0=ALU.add, op1=ALU.subtract), reads=[Sd], writes=[Sd])
            g.op("act", lambda: nc.scalar.activation(out=S[:, tm:tm + 16], in_=V[:, voff["odlam"]:voff["odlam"] + 16],
                                                     func=AF.Exp, scale=-1.0), reads=[Vd, Sd], writes=[Sd])
            g.op("act", lambda: nc.scalar.activation(out=S[:, tm:tm + 16], in_=S[:, tm:tm + 16], func=AF.Ln, bias=1.0),
                 reads=[Sd], writes=[Sd])
            g.op("dve", lambda: nc.vector.tensor_scalar(out=S[:, SC["c8"]:SC["c8"] + 16], in0=S[:, tm:tm + 16],
                                                        scalar1=-8.0, scalar2=None, op0=ALU.mult), reads=[Sd], writes=[Sd])
            g.op("dve", lambda: nc.vector.tensor_scalar(out=S[:, SC["c8x2"]:SC["c8x2"] + 16], in0=S[:, tm:tm + 16],
                                                        scalar1=-16.0, scalar2=None, op0=ALU.mult), reads=[Sd], writes=[Sd])
            g.barrier()
        hb = sb(es, "hb", [128, NCH, T], BF16)
        hbd = [[g.dep() for _ in range(NTT)] for _ in range(NCH)]
        xes = ExitStack()
        xstate = {}

        def alloc_x():
            xstate["x"] = xes.enter_context(nc.sbuf_tensor("xres%d" % len(xstate), [128, NCH, T], F32, side="right"))
            xstate["d"] = [[g.dep() for _ in range(NTT)] for _ in range(NCH)]

        def load_tok_tile(pes_bufs, tt, l0_src=True):
            xst, xstd, tstg, tstgd, cnt = pes_bufs
            s = cnt[0] % 2
            cnt[0] += 1
            n = TN[tt]
            for sub in range(n // 128):
                k = cnt[1] % 2
                cnt[1] += 1
                if tt == 0:
                    src = ctxin[sub * 128:(sub + 1) * 128, :]
                else:
                    r0 = TOFF[tt] - TC + sub * 128
                    src = xin[r0:r0 + 128, :]
                g.dma("sp", tstg[k][:], src, writes=[tstgd[k]])
                for half in range(2):
                    pb = 6 + half
                    for cc in range(4):
                        c = half * 4 + cc
                        g.op("pe", lambda: nc.tensor.transpose(out=ps[pb][:, cc * 128:(cc + 1) * 128],
                                                               in_=tstg[k][:, c * 128:(c + 1) * 128], identity=ident[:]),
                             reads=[tstgd[k], cdep], writes=[psd[pb]])
                    dst = xst[s][:, half * 4:half * 4 + 4, sub * 128:(sub + 1) * 128]
                    srcp = ps[pb][:, :].rearrange("p (c t) -> p c t", t=128)
                    if half == 0:
                        g.op("act", lambda: nc.scalar.copy(out=dst, in_=srcp), reads=[psd[pb]], writes=[xstd[s]])
                    else:
                        g.op("dve", lambda: nc.vector.tensor_copy(out=dst, in_=srcp), reads=[psd[pb]], writes=[xstd[s]])
            return xst[s], xstd[s]

        def rmsnorm_tile(nb, xsrc, xdeps, tt, gsname, l, k_sh, out32=None, out32d=None, hbout=None):
            sq, sqd, tmp, tmpd, rs, rsd, cnt = nb
            n = TN[tt]
            cls = 1 if tt == 0 else 0
            pb = 5
            for c in range(NCH):
                s = cnt[0] % 2
                cnt[0] += 1
                g.op("act", lambda: nc.scalar.activation(out=sq[s][:, 0:n], in_=xsrc(c), func=AF.Square),
                     reads=[xdeps(c)], writes=[sqd[s]])
                g.op("pe", lambda: nc.tensor.matmul(ps[pb][:, 0:n], lhsT=ones32[:], rhs=sq[s][:, 0:n],
                                                    start=(c == 0), stop=(c == NCH - 1)),
                     reads=[sqd[s], onesd], writes=[psd[pb]])
            g.op("act", lambda: nc.scalar.activation(out=rs[:, 0:n], in_=ps[pb][:, 0:n], func=AF.Sqrt,
                                                     scale=1.0 / D, bias=EPS), reads=[psd[pb]], writes=[rsd])
            g.op("dve", lambda: nc.vector.reciprocal(out=rs[:, 0:n], in_=rs[:, 0:n]), reads=[rsd], writes=[rsd])
            c0 = SC["%s_%d_%d" % (gsname, l, cls)]
            for c in range(NCH):
                s = cnt[1] % 2
                cnt[1] += 1
                g.op("dve", lambda: nc.vector.tensor_tensor(out=tmp[s][:, 0:n], in0=xsrc(c), in1=rs[:, 0:n], op=ALU.mult),
                     reads=[xdeps(c), rsd], writes=[tmpd[s]])
                ho, hod = (hb[:, c, tsl(tt)], hbd[c][tt]) if hbout is None else hbout(c)
                g.op("act", lambda: nc.scalar.activation(out=ho, in_=tmp[s][:, 0:n], func=AF.Identity,
                                                         scale=S[:, c0 + c:c0 + c + 1], bias=mod(l, k_sh, c, cls)),
                     reads=[tmpd[s], Sd, modd], writes=[hod])
                if out32 is not None:
                    g.op("dve", lambda: nc.vector.tensor_scalar(out=out32[:, c, 0:n], in0=tmp[s][:, 0:n],
                                                                scalar1=S[:, c0 + c:c0 + c + 1],
                                                                scalar2=mod(l, k_sh, c, cls), op0=ALU.mult, op1=ALU.add),
                         reads=[tmpd[s], Sd, modd], writes=[out32d])

        def norm_bufs(es_):
            sq = [sb(es_, "nsq%d" % i, [128, 512], F32) for i in range(2)]
            tmp = [sb(es_, "ntmp%d" % i, [128, 512], F32) for i in range(2)]
            rs = sb(es_, "nrs", [128, 512], F32)
            return (sq, [g.dep(), g.dep()], tmp, [g.dep(), g.dep()], rs, g.dep(), [0, 0])

        def stage_bufs(es_):
            xst = [sb(es_, "xst%d" % i, [128, NCH, 512], F32) for i in range(2)]
            tstg = [sb(es_, "tstg%d" % i, [128, D], F32) for i in range(2)]
            return (xst, [g.dep(dma=True), g.dep(dma=True)], tstg, [g.dep(dma=True), g.dep(dma=True)], [0, 0])

        def out_proj(l, wout_d, catfn, catdeps, tiles, xold_fn, es_):
            wo = sb(es_, "wo%d" % l, [128, NCH, D], BF16)
            wod = g.dep(dma=True)
            for hh in range(2):
                g.dma("pool", wo[:, :, hh * 512:(hh + 1) * 512],
                      wout_d[:, hh * 512:(hh + 1) * 512].rearrange("(c p) f -> p c f", p=128), writes=[wod])
            x = xstate["x"]
            xd = xstate["d"]
            it = 0
            for tt in tiles:
                n = TN[tt]
                cls = 1 if tt == 0 else 0
                xo, xod = xold_fn(tt)
                for oc in range(NCH):
                    pb = it % 2
                    it += 1
                    for c in range(NCH):
                        g.op("pe", lambda: nc.tensor.matmul(ps[pb][:, 0:n], lhsT=wo[:, c, oc * 128:(oc + 1) * 128],
                                                            rhs=catfn(c, tt), start=(c == 0), stop=(c == NCH - 1)),
                             reads=[wod, catdeps(c, tt)], writes=[psd[pb]])
                    g.op("dve", lambda: nc.vector.scalar_tensor_tensor(out=x[:, oc, tsl(tt)], in0=ps[pb][:, 0:n],
                                                                       scalar=mod(l, 2, oc, cls), in1=xo[:, oc, 0:n],
                                                                       op0=ALU.mult, op1=ALU.add),
                         reads=[psd[pb], xod, modd], writes=[xd[oc][tt]])

        L0 = ExitStack()
        with L0:
            catc = sb(L0, "catc", [128, 4, T], BF16)
            catcd = [[g.dep() for _ in range(NTT)] for _ in range(4)]
            A0 = ExitStack()
            with A0:
                stg = stage_bufs(A0)
                nb = norm_bufs(A0)
                for tt in range(NTT):
                    xt, xtd = load_tok_tile(stg, tt)
                    rmsnorm_tile(nb, lambda c: xt[:, c, 0:TN[tt]], lambda c: xtd, tt, "gs1", 0, 0)
                g.barrier()
            M0 = ExitStack()
            with M0:
                wr = [sb(M0, "w0r%d" % i, [128, NCH, 512], BF16) for i in range(3)]
                wrd = [g.dep(dma=True) for _ in range(3)]

                def load_win(slot, blk):
                    g.dma("pool", wr[slot][:], ev_w_in[:, blk * 512:(blk + 1) * 512].rearrange("(c p) f -> p c f", p=128),
                          writes=[wrd[slot]])

                load_win(0, 3)
                load_win(1, 4)
                load_win(2, 5)
                CV = ExitStack()
                with CV:
                    pj = sb(CV, "pj", [128, T], F32)
                    accj = sb(CV, "accj", [128, T], F32)
                    ctmp = [sb(CV, "ctmp%d" % i, [128, 512], F32) for i in range(2)]
                    pjd = g.dep()
                    accd = g.dep()
                    ctd = [g.dep(), g.dep()]
                    it = 0
                    for j in range(4):
                        for tt in range(NTT):
                            n = TN[tt]
                            for which, pb in ((1, 0), (2, 1)):
                                for c in range(NCH):
                                    g.op("pe", lambda: nc.tensor.matmul(ps[pb][:, 0:n], lhsT=wr[which][:, c, j * 128:(j + 1) * 128],
                                                                        rhs=hb[:, c, tsl(tt)], start=(c == 0), stop=(c == NCH - 1)),
                                         reads=[wrd[which], hbd[c][tt]], writes=[psd[pb]])
                            s = it % 2
                            it += 1
                            g.op("act", lambda: nc.scalar.copy(out=ctmp[s][:, 0:n], in_=ps[0][:, 0:n]), reads=[psd[0]], writes=[ctd[s]])
                            g.op("dve", lambda: nc.vector.tensor_tensor(out=pj[:, tsl(tt)], in0=ps[1][:, 0:n], in1=ctmp[s][:, 0:n],
                                                                        op=ALU.mult), reads=[psd[1], ctd[s]], writes=[pjd])
                        for (a, b) in ((0, TC), (TC, T)):
                            g.op("dve", lambda: nc.vector.tensor_scalar(out=accj[:, a:b], in0=pj[:, a:b], scalar1=Vcol("evconv", 1 * 4 + j),
                                                                        scalar2=None, op0=ALU.mult), reads=[pjd, Vd], writes=[accd])
                            g.op("dve", lambda: nc.vector.scalar_tensor_tensor(out=accj[:, a + 1:b], in0=pj[:, a:b - 1],
                                                                               scalar=Vcol("evconv", 0 * 4 + j), in1=accj[:, a + 1:b],
                                                                               op0=ALU.mult, op1=ALU.add), reads=[pjd, Vd, accd], writes=[accd])
                            g.op("dve", lambda: nc.vector.scalar_tensor_tensor(out=accj[:, a:b - 1], in0=pj[:, a + 1:b],
                                                                               scalar=Vcol("evconv", 2 * 4 + j), in1=accj[:, a:b - 1],
                                                                               op0=ALU.mult, op1=ALU.add), reads=[pjd, Vd, accd], writes=[accd])
                        for tt in range(NTT):
                            n = TN[tt]
                            pb = 2 + (tt % 2)
                            for c in range(NCH):
                                g.op("pe", lambda: nc.tensor.matmul(ps[pb][:, 0:n], lhsT=wr[0][:, c, j * 128:(j + 1) * 128],
                                                                    rhs=hb[:, c, tsl(tt)], start=(c == 0), stop=(c == NCH - 1)),
                                     reads=[wrd[0], hbd[c][tt]], writes=[psd[pb]])
                            g.op("dve", lambda: nc.vector.tensor_tensor(out=catc[:, j, tsl(tt)], in0=ps[pb][:, 0:n], in1=accj[:, tsl(tt)],
                                                                        op=ALU.mult), reads=[psd[pb], accd], writes=[catcd[j][tt]])
                    g.barrier()
                load_win(0, 0)
                load_win(1, 1)
                load_win(2, 2)
                qk = [sb(M0, "q", [128, 4, T], BF16), sb(M0, "k", [128, 4, T], BF16)]
                qkd = [[[g.dep() for _ in range(NTT)] for _ in range(4)] for _ in range(2)]
                vt = sb(M0, "v", [128, NKT, 512], BF16)
                vtd = [g.dep() for _ in range(NKT)]
                cosT = sb(M0, "cosT", [128, T], F32)
                sinT = sb(M0, "sinT", [128, T], F32)
                tabd = g.dep(dma=True)
                g.dma("sp", cosT[:], cos_d, writes=[tabd])
                g.dma("sp", sinT[:], sin_d, writes=[tabd])
                RP = ExitStack()
                with RP:
                    qf = [sb(RP, "qf%d" % i, [128, 512], F32) for i in range(2)]
                    qfd = [g.dep(), g.dep()]
                    t1 = [sb(RP, "rt1%d" % i, [128, 512], F32) for i in range(2)]
                    t1d = [g.dep(), g.dep()]
                    t2 = [sb(RP, "rt2%d" % i, [128, 512], F32) for i in range(2)]
                    t2d = [g.dep(), g.dep()]
                    it = 0
                    for which in range(2):
                        for hc in range(4):
                            for tt in range(NTT):
                                n = TN[tt]
                                s = it % 2
                                it += 1
                                pb = s
                                pr = 2 + s
                                for c in range(NCH):
                                    g.op("pe", lambda: nc.tensor.matmul(ps[pb][:, 0:n], lhsT=wr[which][:, c, hc * 128:(hc + 1) * 128],
                                                                        rhs=hb[:, c, tsl(tt)], start=(c == 0), stop=(c == NCH - 1)),
                                         reads=[wrd[which], hbd[c][tt]], writes=[psd[pb]])
                                g.op("act", lambda: nc.scalar.copy(out=qf[s][:, 0:n], in_=ps[pb][:, 0:n]), reads=[psd[pb]], writes=[qfd[s]])
                                g.op("pe", lambda: nc.tensor.matmul(ps[pr][:, 0:n], lhsT=perm[:], rhs=qf[s][:, 0:n], start=True, stop=True),
                                     reads=[qfd[s], cdep], writes=[psd[pr]])
                                g.op("dve", lambda: nc.vector.tensor_tensor(out=t1[s][:, 0:n], in0=qf[s][:, 0:n], in1=cosT[:, tsl(tt)], op=ALU.mult),
                                     reads=[qfd[s], tabd], writes=[t1d[s]])
                                g.op("dve", lambda: nc.vector.tensor_tensor(out=t2[s][:, 0:n], in0=ps[pr][:, 0:n], in1=sinT[:, tsl(tt)], op=ALU.mult),
                                     reads=[psd[pr], tabd], writes=[t2d[s]])
                                g.op("pool", lambda: nc.gpsimd.tensor_tensor(out=qk[which][:, hc, tsl(tt)], in0=t1[s][:, 0:n], in1=t2[s][:, 0:n], op=ALU.add),
                                     reads=[t1d[s], t2d[s]], writes=[qkd[which][hc][tt]])
                    for kt in range(NKT):
                        tt = 0 if kt < 2 else 1 + (kt - 2) // 4
                        pb = 4 + kt % 2
                        for c in range(NCH):
                            g.op("pe", lambda: nc.tensor.matmul(ps[pb][:, :], lhsT=hb[:, c, kt * 128:(kt + 1) * 128], rhs=wr[2][:, c, :],
                                                                start=(c == 0), stop=(c == NCH - 1)),
                                 reads=[wrd[2], hbd[c][tt]], writes=[psd[pb]])
                        g.op("act", lambda: nc.scalar.copy(out=vt[:, kt, :], in_=ps[pb][:, :]), reads=[psd[pb]], writes=[vtd[kt]])
                    g.barrier()
                AT = ExitStack()
                with AT:
                    eb = [[sb(AT, "e%d_%d" % (m, i), [128, 512], BF16) for i in range(2)] for m in range(2)]
                    ebd = [[g.dep(), g.dep()] for _ in range(2)]
                    rz = [sb(AT, "rz%d" % m, [128, 512], F32) for m in range(2)]
                    rzd = [g.dep(), g.dep()]
                    to = [sb(AT, "to%d" % m, [128, 512], F32) for m in range(2)]
                    tod = [g.dep(), g.dep()]
                    osb = sb(AT, "osb", [128, 512], F32)
                    osd = g.dep()
                    osq = sb(AT, "osq", [128, 512], F32)
                    osqd = g.dep()
                    ors = sb(AT, "ors", [128, 512], F32)
                    orsd = g.dep()
                    for h in range(4):
                        for qt in range(NTT):
                            n = TN[qt]
                            nkt = 2 if qt == 0 else NKT
                            for kt in range(nkt):
                                ktt = 0 if kt < 2 else 1 + (kt - 2) // 4
                                sl = kt % 2
                                for m in range(2):
                                    pbs = 4 + 2 * m + sl
                                    g.op("pe", lambda: nc.tensor.matmul(ps[pbs][:, 0:n], lhsT=qk[1][m * 64:(m + 1) * 64, h, kt * 128:(kt + 1) * 128],
                                                                        rhs=qk[0][m * 64:(m + 1) * 64, h, tsl(qt)], start=True, stop=True),
                                         reads=[qkd[1][h][ktt], qkd[0][h][qt]], writes=[psd[pbs]])
                                    g.op("act", lambda: nc.scalar.activation(out=eb[m][sl][:, 0:n], in_=ps[pbs][:, 0:n], func=AF.Exp, scale=0.125),
                                         reads=[psd[pbs]], writes=[ebd[m][sl]])
                                for m in range(2):
                                    g.op("pe", lambda: nc.tensor.matmul(ps[2 * m][:, 0:n], lhsT=vt[:, kt, h * 128:(h + 1) * 128], rhs=eb[m][sl][:, 0:n],
                                                                        start=(kt == 0), stop=(kt == nkt - 1)),
                                         reads=[vtd[kt], ebd[m][sl]], writes=[psd[2 * m]])
                                    g.op("pe", lambda: nc.tensor.matmul(ps[2 * m + 1][:, 0:n], lhsT=ones16[:], rhs=eb[m][sl][:, 0:n],
                                                                        start=(kt == 0), stop=(kt == nkt - 1)),
                                         reads=[onesd, ebd[m][sl]], writes=[psd[2 * m + 1]])
                            for m in range(2):
                                g.op("dve", lambda: nc.vector.reciprocal(out=rz[m][:, 0:n], in_=ps[2 * m + 1][:, 0:n]), reads=[psd[2 * m + 1]], writes=[rzd[m]])
                                g.op("dve", lambda: nc.vector.tensor_tensor(out=to[m][:, 0:n], in0=ps[2 * m][:, 0:n], in1=rz[m][:, 0:n], op=ALU.mult),
                                     reads=[psd[2 * m], rzd[m]], writes=[tod[m]])
                            g.op("dve", lambda: nc.vector.scalar_tensor_tensor(out=osb[:, 0:n], in0=to[1][:, 0:n], scalar=Scol("neglam"), in1=to[0][:, 0:n],
                                                                               op0=ALU.mult, op1=ALU.add), reads=[tod[0], tod[1], Sd], writes=[osd])
                            g.op("act", lambda: nc.scalar.activation(out=osq[:, 0:n], in_=osb[:, 0:n], func=AF.Square), reads=[osd], writes=[osqd])
                            g.op("pe", lambda: nc.tensor.matmul(ps[4][:, 0:n], lhsT=ones32[:], rhs=osq[:, 0:n], start=True, stop=True),
                                 reads=[osqd, onesd], writes=[psd[4]])
                            g.op("act", lambda: nc.scalar.activation(out=ors[:, 0:n], in_=ps[4][:, 0:n], func=AF.Sqrt, scale=1.0 / 128, bias=EPS),
                                 reads=[psd[4]], writes=[orsd])
                            g.op("dve", lambda: nc.vector.reciprocal(out=ors[:, 0:n], in_=ors[:, 0:n]), reads=[orsd], writes=[orsd])
                            g.op("dve", lambda: nc.vector.tensor_tensor(out=osb[:, 0:n], in0=osb[:, 0:n], in1=ors[:, 0:n], op=ALU.mult),
                                 reads=[orsd, osd], writes=[osd])
                            g.op("act", lambda: nc.scalar.activation(out=hb[:, h, tsl(qt)], in_=osb[:, 0:n], func=AF.Identity, scale=Scol("sg")),
                                 reads=[osd, Sd], writes=[hbd[h][qt]])
                    g.barrier()
            alloc_x()
            C0 = ExitStack()
            with C0:
                stg = stage_bufs(C0)

                def xold0(tt):
                    return load_tok_tile(stg, tt)

                def cat0(c, tt):
                    return hb[:, c, tsl(tt)] if c < 4 else catc[:, c - 4, tsl(tt)]

                def cat0d(c, tt):
                    return hbd[c][tt] if c < 4 else catcd[c - 4][tt]

                out_proj(0, ev_w_out, cat0, cat0d, range(NTT), xold0, C0)
                g.barrier()

        def moe_layer(l, tiles):
            x = xstate["x"]
            xd = xstate["d"]
            subs = []
            for tt in tiles:
                for sub in range(TN[tt] // 128):
                    subs.append((TOFF[tt] // 128 + sub, tt, sub))
            hbTok = hb[:].rearrange("p c t -> p (c t)").rearrange("p (i f) -> p i f", f=D)
            hbtokd = [g.dep() for _ in range(NKT)]
            ML = ExitStack()
            with ML:
                GK = sb(ML, "GK", [128, NKT, 4], F32)
                GKd = g.dep()
                DD = ExitStack()
                with DD:
                    nb = norm_bufs(DD)
                    h32 = sb(DD, "h32", [128, NCH, 512], F32)
                    h32d = g.dep()
                    hbt = sb(DD, "hbt", [128, NCH, 512], BF16)
                    hbtd = g.dep()
                    wrt = sb(DD, "wrt", [128, NCH, NE], F32)
                    brt = sb(DD, "brt", [1, NE], F32)
                    b2n = sb(DD, "b2n", [NE, D], F32)
                    rtd = g.dep(dma=True)
                    g.dma("sp", wrt[:], w_router[l].rearrange("(c p) e -> p c e", p=128), writes=[rtd])
                    g.dma("sp", brt[:], b_router[l:l + 1, :], writes=[rtd])
                    g.dma("sp", b2n[:], moe_b2[l], writes=[rtd])
                    gT = sb(DD, "gT", [NE, T], F32)
                    gTd = g.dep()
                    carry = sb(DD, "carry", [128, NE], F32)
                    card = g.dep()
                    g.op("dve", lambda: nc.vector.memset(carry[:], 0.0), writes=[card])
                    oob = sb(DD, "oob", [128, NE * TS // 128], I32)
                    oobd = g.dep()
                    g.op("pool", lambda: nc.gpsimd.memset(oob[:], 2000000000), writes=[oobd])
                    g.dma("sp", idx_d[:, :].rearrange("(p r) o -> p (r o)", p=128), oob[:], reads=[oobd], writes=[idx_dep])
                    lg = sb(DD, "lg", [128, NE], F32)
                    ex = sb(DD, "ex", [128, NE], F32)
                    mk = sb(DD, "mk", [128, NE], F32)
                    gt_ = sb(DD, "gt_", [128, NE], F32)
                    ngd = sb(DD, "ngd", [128, NE], F32)
                    t8 = sb(DD, "t8", [128, 8], F32)
                    t8b = sb(DD, "t8b", [128, 8], F32)
                    sm = sb(DD, "sm", [128, 4], F32)
                    fli = sb(DD, "fli", [128, NE], I32)
                    NIX = 3
                    idx4 = [sb(DD, "idx4_%d" % i, [128, 4], I32) for i in range(NIX)]
                    val4 = [sb(DD, "val4_%d" % i, [128, 4], I32) for i in range(NIX)]
                    ixd = [g.dep() for _ in range(NIX)]
                    rd = g.dep()
                    mkd = g.dep()
                    ixc = 0
                    for tt in tiles:
                        n = TN[tt]
                        rmsnorm_tile(nb, lambda c: x[:, c, tsl(tt)], lambda c: xd[c][tt], tt, "gs2", l, 3, out32=h32, out32d=h32d,
                                     hbout=lambda c: (hbt[:, c, 0:n], hbtd))
                        for sub in range(n // 128):
                            t0 = TOFF[tt] + sub * 128
                            i = t0 // 128
                            psb = ps[6][:, :].bitcast(BF16)
                            for c in range(NCH):
                                g.op("pe", lambda: nc.tensor.transpose(out=psb[:, c * 128:(c + 1) * 128], in_=hbt[:, c, sub * 128:(sub + 1) * 128], identity=identb[:]),
                                     reads=[hbtd, cdep2], writes=[psd[6]])
                            g.op("act", lambda: nc.scalar.copy(out=hbTok[:, i, :], in_=psb), reads=[psd[6]], writes=[hbtokd[i]])
                            pb = 0
                            for c in range(NCH):
                                g.op("pe", lambda: nc.tensor.matmul(ps[pb][:, 0:NE], lhsT=h32[:, c, sub * 128:(sub + 1) * 128], rhs=wrt[:, c, :],
                                                                    start=(c == 0), stop=False),
                                     reads=[h32d, rtd], writes=[psd[pb]])
                            g.op("pe", lambda: nc.tensor.matmul(ps[pb][:, 0:NE], lhsT=ones32[0:1, :], rhs=brt[0:1, :], start=False, stop=True),
                                 reads=[rtd, onesd], writes=[psd[pb]])
                            g.op("act", lambda: nc.scalar.copy(out=lg[:], in_=ps[pb][:, 0:NE]), reads=[psd[pb]], writes=[rd])
                            g.op("dve", lambda: nc.vector.max(out=t8[:], in_=lg[:]), reads=[rd], writes=[rd])
                            g.op("dve", lambda: nc.vector.tensor_scalar(out=sm[:, 0:1], in0=t8[:, 0:1], scalar1=-1.0, scalar2=None, op0=ALU.mult),
                                 reads=[rd], writes=[rd])
                            g.op("act", lambda: nc.scalar.activation(out=ex[:], in_=lg[:], func=AF.Exp, bias=sm[:, 0:1]), reads=[rd], writes=[rd])
                            g.op("dve", lambda: nc.vector.tensor_scalar(out=mk[:], in0=lg[:], scalar1=t8[:, 3:4], scalar2=None, op0=ALU.is_ge),
                                 reads=[rd, mkd], writes=[rd, mkd])
                            g.op("dve", lambda: nc.vector.tensor_tensor(out=ex[:], in0=ex[:], in1=mk[:], op=ALU.mult), reads=[rd], writes=[rd])
                            g.op("dve", lambda: nc.vector.tensor_reduce(out=sm[:, 1:2], in_=ex[:], axis=AX.X, op=ALU.add), reads=[rd], writes=[rd])
                            g.op("dve", lambda: nc.vector.reciprocal(out=sm[:, 2:3], in_=sm[:, 1:2]), reads=[rd], writes=[rd])
                            g.op("dve", lambda: nc.vector.tensor_scalar(out=gt_[:], in0=ex[:], scalar1=sm[:, 2:3], scalar2=None, op0=ALU.mult),
                                 reads=[rd], writes=[rd])
                            g.op("pe", lambda: nc.tensor.transpose(out=ps[1][0:NE, 0:128], in_=gt_[:], identity=ident[:]),
                                 reads=[rd, cdep], writes=[psd[1]])
                            g.op("act", lambda: nc.scalar.copy(out=gT[:, t0:t0 + 128], in_=ps[1][0:NE, 0:128]), reads=[psd[1]], writes=[gTd])
                            g.op("pe", lambda: nc.tensor.matmul(ps[2][:, 0:NE], lhsT=utri[:], rhs=mk[:], start=True, stop=True),
                                 reads=[mkd, cdep2], writes=[psd[2]])
                            g.op("pe", lambda: nc.tensor.matmul(ps[3][:, 0:NE], lhsT=ones32[:], rhs=mk[:], start=True, stop=True),
                                 reads=[mkd, onesd], writes=[psd[3]])
                            g.op("dve", lambda: nc.vector.tensor_tensor(out=ex[:], in0=ps[2][:, 0:NE], in1=carry[:], op=ALU.add), reads=[psd[2], card, rd], writes=[rd])
                            g.op("dve", lambda: nc.vector.tensor_tensor(out=ex[:], in0=ex[:], in1=offrow[:], op=ALU.add), reads=[rd, cdep2], writes=[rd])
                            g.op("dve", lambda: nc.vector.tensor_tensor(out=ex[:], in0=ex[:], in1=mk[:], op=ALU.mult), reads=[rd], writes=[rd])
                            g.op("dve", lambda: nc.vector.tensor_scalar(out=ngd[:], in0=mk[:], scalar1=1.0e6, scalar2=-1.0e6, op0=ALU.mult, op1=ALU.add), reads=[rd], writes=[rd])
                            g.op("dve", lambda: nc.vector.tensor_tensor(out=ngd[:], in0=ngd[:], in1=ex[:], op=ALU.subtract), reads=[rd], writes=[rd])
                            g.op("dve", lambda: nc.vector.tensor_tensor(out=carry[:], in0=ps[3][:, 0:NE], in1=carry[:], op=ALU.add), reads=[psd[3], rd], writes=[card])
                            g.op("dve", lambda: nc.vector.max(out=t8b[:], in_=ngd[:]), reads=[rd], writes=[rd])
                            si = ixc % NIX
                            ixc += 1
                            g.op("dve", lambda: nc.vector.tensor_scalar(out=idx4[si][:], in0=t8b[:, 0:4], scalar1=-1.0, scalar2=None, op0=ALU.mult), reads=[rd], writes=[ixd[si]])
                            g.op("dve", lambda: nc.vector.tensor_scalar(out=val4[si][:], in0=tok4f[:], scalar1=float(4 * t0), scalar2=None, op0=ALU.add), reads=[cdep2], writes=[ixd[si]])
                            for k in range(4):
                                g.op("dve", lambda: nc.vector.scalar_tensor_tensor(out=ex[:], in0=ngd[:], scalar=t8b[:, k:k + 1], in1=gt_[:], op0=ALU.is_equal, op1=ALU.mult,
                                                                                   accum_out=GK[:, i, k:k + 1]), reads=[rd], writes=[rd, GKd])
                            for k in range(4):
                                g.idma(hs_d[:, :], idx4[si][:, k:k + 1], hbTok[:, i, :], bnd_hs, reads=[ixd[si], hbtokd[i]], writes=[hs_dep])
                                g.idma(idx_d[:, :], idx4[si][:, k:k + 1], val4[si][:, k:k + 1], bnd_hs, reads=[ixd[si]], writes=[idx_dep])
                    g.op("dve", lambda: nc.vector.tensor_copy(out=fli[:], in_=carry[:]), reads=[card, rd], writes=[rd])
                    g.dma("sp", cnt_d[l:l + 1, :], fli[0:1, :], reads=[rd], writes=[flag_dep[l]])
                    it = 0
                    for tt in tiles:
                        n = TN[tt]
                        for oc in range(NCH):
                            pb = 2 + it % 2
                            it += 1
                            g.op("pe", lambda: nc.tensor.matmul(ps[pb][:, 0:n], lhsT=b2n[:, oc * 128:(oc + 1) * 128], rhs=gT[:, tsl(tt)], start=True, stop=True),
                                 reads=[rtd, gTd], writes=[psd[pb]])
                            g.op("dve", lambda: nc.vector.scalar_tensor_tensor(out=x[:, oc, tsl(tt)], in0=ps[pb][:, 0:n], scalar=mod(l, 5, oc, 1 if tt == 0 else 0),
                                                                               in1=x[:, oc, tsl(tt)], op0=ALU.mult, op1=ALU.add),
                                 reads=[psd[pb], modd], writes=[xd[oc][tt]])
                    g.barrier()
                EE = ExitStack()
                with EE:
                    NSLOT = 4
                    hbflat = hb[:].rearrange("p c t -> p (c t)")
                    ring = [hbflat[:, k * 4096:(k + 1) * 4096].rearrange("p (c f) -> p c f", f=512) for k in range(NSLOT)]
                    ringd = [g.dep(dma=True) for _ in range(NSLOT)]
                    hgToks = [sb(EE, "hgTok%d" % i, [128, NJ, D], BF16) for i in range(2)]
                    hgds = [g.dep(dma=True) for _ in range(2)]
                    idxts = [sb(EE, "idxt%d" % i, [128, NJ, 1], I32) for i in range(2)]
                    idxtds = [g.dep(dma=True) for _ in range(2)]
                    hc_ = [0]
                    hbg = sb(EE, "hbg", [128, NCH, CAP], BF16)
                    hbgd = [g.dep() for _ in range(NCH)]
                    actT = sb(EE, "actT", [128, NCH, CAP], BF16)
                    actd = [g.dep() for _ in range(NCH)]
                    yTok = sb(EE, "yTok", [128, NJ, D], F32)
                    yTd = [g.dep() for _ in range(NJ)]
                    At = [sb(EE, "mA%d" % i, [128, CAP], F32) for i in range(2)]
                    St = [sb(EE, "mS%d" % i, [128, CAP], F32) for i in range(2)]
                    Lt = [sb(EE, "mL%d" % i, [128, CAP], F32) for i in range(2)]
                    Ad = [g.dep(), g.dep()]
                    Sdp = [g.dep(), g.dep()]
                    Ld = [g.dep(), g.dep()]
                    rc = [0]
                    ec = [0]
                    pc_ = [0]

                    def load_unit(src):
                        s = rc[0] % NSLOT
                        rc[0] += 1
                        g.dma("pool", ring[s][:], src.rearrange("(c p) f -> p c f", p=128), writes=[ringd[s]])
                        return s

                    def sparse_chunk(e, c0):
                        b1o = voff["b1"] + (l * NE + e) * 16
                        r0 = e * TS + c0 * CAP
                        hsel = hc_[0] % 2
                        hc_[0] += 1
                        hgTok, hgd, idxt, idxtd = hgToks[hsel], hgds[hsel], idxts[hsel], idxtds[hsel]
                        g.dma("sp", hgTok[:], hs_d[r0:r0 + CAP, :].rearrange("(j p) f -> p j f", p=128), reads=[hs_dep], writes=[hgd])
                        for j3 in range(NJ):
                            g.dma("sp", idxt[:, j3, :], idx_d[r0 + j3 * 128:r0 + (j3 + 1) * 128, :], reads=[idx_dep], writes=[idxtd])
                        w1s = {}
                        w1s[0] = (load_unit(moe_w1[l, e, :, 0:512]), load_unit(moe_w1[l, e, :, D:D + 512]))
                        for c in range(NCH):
                            pb = 6 + pc_[0] % 2
                            pc_[0] += 1
                            psb = ps[pb][:, :].bitcast(BF16)
                            for j3 in range(NJ):
                                g.op("pe", lambda: nc.tensor.transpose(out=psb[:, j3 * 128:(j3 + 1) * 128], in_=hgTok[:, j3, c * 128:(c + 1) * 128], identity=identb[:]),
                                     reads=[hgd, cdep2], writes=[psd[pb]])
                            if c % 2 == 0:
                                g.op("act", lambda: nc.scalar.copy(out=hbg[:, c, :], in_=psb[:, 0:CAP]), reads=[psd[pb]], writes=[hbgd[c]])
                            else:
                                g.op("dve", lambda: nc.vector.tensor_copy(out=hbg[:, c, :], in_=psb[:, 0:CAP]), reads=[psd[pb]], writes=[hbgd[c]])
                        pend = []
                        for u in range(2):
                            if u == 0:
                                w1s[1] = (load_unit(moe_w1[l, e, :, 512:1024]), load_unit(moe_w1[l, e, :, D + 512:D + 1024]))
                            sg_, sl_ = w1s[u]
                            for jj in range(4):
                                j = u * 4 + jj
                                s = ec[0] % 2
                                s3 = ec[0] % 3
                                ec[0] += 1
                                pg = s3
                                pl = 3 + s3
                                for c in range(NCH):
                                    g.op("pe", lambda: nc.tensor.matmul(ps[pg][:, 0:CAP], lhsT=ring[sg_][:, c, jj * 128:(jj + 1) * 128], rhs=hbg[:, c, :],
                                                                        start=(c == 0), stop=(c == NCH - 1)),
                                         reads=[ringd[sg_], hbgd[c]], writes=[psd[pg]])
                                for c in range(NCH):
                                    g.op("pe", lambda: nc.tensor.matmul(ps[pl][:, 0:CAP], lhsT=ring[sl_][:, c, jj * 128:(jj + 1) * 128], rhs=hbg[:, c, :],
                                                                        start=(c == 0), stop=(c == NCH - 1)),
                                         reads=[ringd[sl_], hbgd[c]], writes=[psd[pl]])
                                g.op("dve", lambda: nc.vector.tensor_scalar(out=At[s][:], in0=ps[pg][:, 0:CAP], scalar1=V[:, b1o + j:b1o + j + 1], scalar2=7.0,
                                                                            op0=ALU.add, op1=ALU.min), reads=[psd[pg], Vd], writes=[Ad[s]])
                                g.op("act", lambda: nc.scalar.activation(out=St[s][:], in_=At[s][:], func=AF.Sigmoid, scale=1.702),
                                     reads=[Ad[s]], writes=[Sdp[s]])
                                g.op("dve", lambda: nc.vector.tensor_scalar(out=Lt[s][:], in0=ps[pl][:, 0:CAP], scalar1=V[:, b1o + 8 + j:b1o + 8 + j + 1], scalar2=-6.0,
                                                                            op0=ALU.add, op1=ALU.max), reads=[psd[pl], Vd], writes=[Ld[s]])
                                if pend:
                                    pend.pop()()

                                def _fin(s=s, j=j):
                                    g.op("dve", lambda: nc.vector.tensor_tensor(out=St[s][:], in0=At[s][:], in1=St[s][:], op=ALU.mult),
                                         reads=[Ad[s], Sdp[s]], writes=[Sdp[s]])
                                    g.op("dve", lambda: nc.vector.scalar_tensor_tensor(out=actT[:, j, :], in0=Lt[s][:], scalar=8.0, in1=St[s][:],
                                                                                       op0=ALU.min, op1=ALU.mult), reads=[Ld[s], Sdp[s]], writes=[actd[j]])
                                pend.append(_fin)
                            if u == 0:
                                w2s = [load_unit(moe_w2[l, e, :, 0:512])]
                        if pend:
                            pend.pop()()
                        w2s.append(load_unit(moe_w2[l, e, :, 512:1024]))
                        for j3 in range(NJ):
                            for hh in range(2):
                                sw_ = w2s[hh]
                                pb = 6 + pc_[0] % 2
                                pc_[0] += 1
                                for fc in range(NCH):
                                    g.op("pe", lambda: nc.tensor.matmul(ps[pb][:, :], lhsT=actT[:, fc, j3 * 128:(j3 + 1) * 128], rhs=ring[sw_][:, fc, :],
                                                                        start=(fc == 0), stop=(fc == NCH - 1)),
                                         reads=[ringd[sw_], actd[fc]], writes=[psd[pb]])
                                g.op("act", lambda: nc.scalar.copy(out=yTok[:, j3, hh * 512:(hh + 1) * 512], in_=ps[pb][:, :]), reads=[psd[pb]], writes=[yTd[j3]])
                            g.idma(y4_d[:, :], idxt[:, j3, :], yTok[:, j3, :], bnd_y4, reads=[idxtd, yTd[j3]], writes=[y4_dep])

                    for e in range(NE):
                        for reg in flag_regs:
                            ek = ENG_KEY[reg.engine]
                            g._wait(ek, [(flag_dep[l].sem, flag_dep[l].tot)])
                            g.eng[ek].reg_load(reg, cnt_d[l:l + 1, e:e + 1])
                        for c0 in range(NCHUNK):
                            snap = g.snapshot()
                            with nc.If_cmp(flag_regs, c0 * CAP, "IS_GT"):
                                sparse_chunk(e, c0)
                            big = g.snapshot()
                            g.restore(snap)
                            with nc.Else():
                                g.pad_to(big)
                            g.restore(big)
                            g.known = {k: dict(v) for k, v in snap[2].items()}
                    g.barrier()
                CB = ExitStack()
                with CB:
                    y4t = [sb(CB, "y4t%d" % i, [128, 4, D], F32) for i in range(2)]
                    y4td = [g.dep(dma=True) for _ in range(2)]
                    acc = [sb(CB, "cacc%d" % i, [128, D], F32) for i in range(2)]
                    accd = [g.dep(), g.dep()]
                    for n_, (i, tt, sub) in enumerate(subs):
                        s = n_ % 2
                        t0 = i * 128
                        cls = 1 if tt == 0 else 0
                        g.dma("sp", y4t[s][:], y4_d[4 * t0:4 * t0 + 512, :].rearrange("(p k) f -> p k f", k=4), reads=[y4_dep], writes=[y4td[s]])
                        g.op("dve", lambda: nc.vector.tensor_scalar(out=acc[s][:], in0=y4t[s][:, 0, :], scalar1=GK[:, i, 0:1], scalar2=None, op0=ALU.mult),
                             reads=[y4td[s], GKd], writes=[accd[s]])
                        for k in range(1, 4):
                            g.op("dve", lambda: nc.vector.scalar_tensor_tensor(out=acc[s][:], in0=y4t[s][:, k, :], scalar=GK[:, i, k:k + 1], in1=acc[s][:],
                                                                               op0=ALU.mult, op1=ALU.add), reads=[y4td[s], GKd, accd[s]], writes=[accd[s]])
                        for half in range(2):
                            pbt = 2 * s + half
                            for cc in range(4):
                                c = half * 4 + cc
                                g.op("pe", lambda: nc.tensor.transpose(out=ps[pbt][:, cc * 128:(cc + 1) * 128], in_=acc[s][:, c * 128:(c + 1) * 128], identity=ident[:]),
                                     reads=[accd[s], cdep], writes=[psd[pbt]])
                            for cc in range(4):
                                c = half * 4 + cc
                                g.op("dve", lambda: nc.vector.scalar_tensor_tensor(out=x[:, c, t0:t0 + 128], in0=ps[pbt][:, cc * 128:(cc + 1) * 128], scalar=mod(l, 5, c, cls),
                                                                                   in1=x[:, c, t0:t0 + 128], op0=ALU.mult, op1=ALU.add),
                                     reads=[psd[pbt], modd], writes=[xd[c][tt]])
                    g.barrier()

        moe_layer(0, list(range(NTT)))

        if dbg == "x1":
            for c in range(NCH):
                g.dma("sp", dbg_d[:, c, :], xstate["x"][:, c, :], reads=[xstate["d"][c][tt] for tt in range(NTT)], writes=[out_dep])

        LAT = [1, 2, 3, 4]
        L1 = ExitStack()
        with L1:
            cat1 = sb(L1, "cat1", [128, NCH, TL], BF16)
            cat1d = [[g.dep() for _ in range(NTT)] for _ in range(NCH)]
            x = xstate["x"]
            xd = xstate["d"]
            A1 = ExitStack()
            with A1:
                nb = norm_bufs(A1)
                for tt in range(NTT):
                    rmsnorm_tile(nb, lambda c: x[:, c, tsl(tt)], lambda c: xd[c][tt], tt, "gs1", 1, 0)
                for c in range(NCH):
                    g.dma("sp", xs_d[:, c, :], x[:, c, :], reads=[xd[c][tt] for tt in range(NTT)], writes=[xs_dep])
                g.barrier()
            xes.close()
            M1 = ExitStack()
            with M1:
                gw = sb(M1, "gw", [128, 16, 2, 256], BF16)
                gwd = g.dep(dma=True)
                for gi_, src in ((0, od_ga), (1, od_gx)):
                    for d_ in range(2):
                        g.dma("pool", gw[:, gi_ * 8 + d_ * 4:gi_ * 8 + d_ * 4 + 4, :, :],
                              src[d_].rearrange("b (c p) o -> p b c o", p=128), writes=[gwd])
                wr1 = [sb(M1, "w1r%d" % i, [128, NCH, 512], BF16) for i in range(2)]
                wr1d = [g.dep(dma=True) for _ in range(2)]
                ug = sb(M1, "ug", [128, 2, T], F32)
                ugd = [g.dep(), g.dep()]
                xc = sb(M1, "xc", [128, 2, T], F32)
                xcd = [g.dep(), g.dep()]
                xcb = sb(M1, "xcb", [128, 2, T], BF16)
                xcbd = [g.dep(), g.dep()]
                rec = sb(M1, "rec", [128, 2, T], F32)
                recd = [[g.dep() for _ in range(NTT)] for _ in range(2)]
                NT_ = 9
                tb = [[sb(M1, "tb%d_%d" % (k, i), [128, 512], F32) for i in range(2 if k < 7 else 1)] for k in range(NT_)]
                tb[7].append(tb[7][0])
                tb[8].append(tb[8][0])
                tbd = [[g.dep(), g.dep()] for _ in range(NT_)]
                tbd[7][1] = tbd[7][0]
                tbd[8][1] = tbd[8][0]
                cnt1 = [0]
                pcnt = [0]
                for blk in range(4):
                    sW = blk % 2
                    g.dma("pool", wr1[sW][:, :, 0:256], od_w_in[:, blk * 256:(blk + 1) * 256].rearrange("(c p) f -> p c f", p=128), writes=[wr1d[sW]])
                    g.dma("pool", wr1[sW][:, :, 256:512], od_w_in[:, D + blk * 256:D + (blk + 1) * 256].rearrange("(c p) f -> p c f", p=128), writes=[wr1d[sW]])
                    for cc in range(2):
                        for tt in range(NTT):
                            n = TN[tt]
                            pb = pcnt[0] % 2
                            pcnt[0] += 1
                            for c in range(NCH):
                                g.op("pe", lambda: nc.tensor.matmul(ps[pb][:, 0:n], lhsT=wr1[sW][:, c, 256 + cc * 128:256 + (cc + 1) * 128], rhs=hb[:, c, tsl(tt)],
                                                                    start=(c == 0), stop=(c == NCH - 1)),
                                     reads=[wr1d[sW], hbd[c][tt]], writes=[psd[pb]])
                            g.op("act", lambda: nc.scalar.copy(out=ug[:, cc, tsl(tt)], in_=ps[pb][:, 0:n]), reads=[psd[pb]], writes=[ugd[cc]])
                    for d_ in range(2):
                        for cc in range(2):
                            ch = blk * 2 + cc
                            wv = lambda k: Vcol("odconvw", (d_ * 4 + k) * 8 + ch)
                            bv = Vcol("odconvb", d_ * 8 + ch)
                            for (a, b) in ((0, TC), (TC, T)):
                                if d_ == 0:
                                    g.op("dve", lambda: nc.vector.tensor_scalar(out=xc[:, cc, a:b], in0=ug[:, cc, a:b], scalar1=wv(3), scalar2=bv, op0=ALU.mult, op1=ALU.add),
                                         reads=[ugd[cc], Vd], writes=[xcd[cc]])
                                    for k in range(3):
                                        sh = 3 - k
                                        g.op("dve", lambda: nc.vector.scalar_tensor_tensor(out=xc[:, cc, a + sh:b], in0=ug[:, cc, a:b - sh], scalar=wv(k), in1=xc[:, cc, a + sh:b],
                                                                                           op0=ALU.mult, op1=ALU.add), reads=[ugd[cc], Vd, xcd[cc]], writes=[xcd[cc]])
                                else:
                                    g.op("dve", lambda: nc.vector.tensor_scalar(out=xc[:, cc, a:b], in0=ug[:, cc, a:b], scalar1=wv(0), scalar2=bv, op0=ALU.mult, op1=ALU.add),
                                         reads=[ugd[cc], Vd], writes=[xcd[cc]])
                                    for k in range(1, 4):
                                        g.op("dve", lambda: nc.vector.scalar_tensor_tensor(out=xc[:, cc, a:b - k], in0=ug[:, cc, a + k:b], scalar=wv(k), in1=xc[:, cc, a:b - k],
                                                                                           op0=ALU.mult, op1=ALU.add), reads=[ugd[cc], Vd, xcd[cc]], writes=[xcd[cc]])
                            g.op("act", lambda: nc.scalar.copy(out=xcb[:, cc, :], in_=xc[:, cc, :]), reads=[xcd[cc]], writes=[xcbd[cc]])
                        order = [0, 1, 2, 3, 4] if d_ == 0 else [0, 4, 3, 2, 1]
                        for oc in range(2):
                            ch = blk * 2 + oc
                            prev = None
                            for tt in order:
                                n = TN[tt]
                                s = cnt1[0] % 2
                                cnt1[0] += 1
                                pa = 2 + s
                                px = 4 + s
                                for gi_, pb in ((0, pa), (1, px)):
                                    for ic in range(2):
                                        g.op("pe", lambda: nc.tensor.matmul(ps[pb][:, 0:n], lhsT=gw[:, gi_ * 8 + d_ * 4 + blk, ic, oc * 128:(oc + 1) * 128], rhs=xcb[:, ic, tsl(tt)],
                                                                            start=(ic == 0), stop=(ic == 1)),
                                             reads=[gwd, xcbd[ic]], writes=[psd[pb]])
                                R, I_, A_, A2, TH, GI, HS = 0, 1, 2, 3, 4, 5, 6
                                c8 = Scol("c8", d_ * 8 + ch)
                                c8x2 = Scol("c8x2", d_ * 8 + ch)
                                g.op("act", lambda: nc.scalar.activation(out=tb[R][s][:, 0:n], in_=ps[pa][:, 0:n], func=AF.Sigmoid, bias=Vcol("odgab", d_ * 8 + ch)),
                                     reads=[psd[pa], Vd], writes=[tbd[R][s]])
                                g.op("act", lambda: nc.scalar.activation(out=tb[I_][s][:, 0:n], in_=ps[px][:, 0:n], func=AF.Sigmoid, bias=Vcol("odgxb", d_ * 8 + ch)),
                                     reads=[psd[px], Vd], writes=[tbd[I_][s]])
                                g.op("act", lambda: nc.scalar.activation(out=tb[A_][s][:, 0:n], in_=tb[R][s][:, 0:n], func=AF.Exp, scale=c8),
                                     reads=[tbd[R][s], Sd], writes=[tbd[A_][s]])
                                g.op("act", lambda: nc.scalar.activation(out=tb[A2][s][:, 0:n], in_=tb[R][s][:, 0:n], func=AF.Exp, scale=c8x2),
                                     reads=[tbd[R][s], Sd], writes=[tbd[A2][s]])
                                g.op("act", lambda: nc.scalar.activation(out=tb[TH][s][:, 0:n], in_=tb[R][s][:, 0:n], func=AF.Tanh, scale=c8),
                                     reads=[tbd[R][s], Sd], writes=[tbd[TH][s]])
                                g.op("dve", lambda: nc.vector.scalar_tensor_tensor(out=tb[A2][s][:, 0:n], in0=tb[A2][s][:, 0:n], scalar=1.0, in1=tb[TH][s][:, 0:n],
                                                                                   op0=ALU.add, op1=ALU.mult), reads=[tbd[A2][s], tbd[TH][s]], writes=[tbd[A2][s]])
                                g.op("act", lambda: nc.scalar.activation(out=tb[A2][s][:, 0:n], in_=tb[A2][s][:, 0:n], func=AF.Sqrt, scale=-1.0),
                                     reads=[tbd[A2][s]], writes=[tbd[A2][s]])
                                g.op("pool", lambda: nc.gpsimd.tensor_tensor(out=tb[GI][s][:, 0:n], in0=tb[I_][s][:, 0:n], in1=xc[:, oc, tsl(tt)], op=ALU.mult),
                                     reads=[tbd[I_][s], xcd[oc]], writes=[tbd[GI][s]])
                                g.op("dve", lambda: nc.vector.tensor_tensor(out=tb[GI][s][:, 0:n], in0=tb[GI][s][:, 0:n], in1=tb[A2][s][:, 0:n], op=ALU.mult),
                                     reads=[tbd[GI][s], tbd[A2][s]], writes=[tbd[GI][s]])
                                if d_ == 0:
                                    dst = rec[:, oc, tsl(tt)]
                                    dstd = recd[oc][tt]
                                    init = 0.0 if prev is None else prev[0][:, TN[prev[2]] - 1:TN[prev[2]]]
                                    g.op("dve", lambda: nc.vector.tensor_tensor_scan(out=dst, data0=tb[A_][s][:, 0:n], data1=tb[GI][s][:, 0:n], initial=init,
                                                                                     op0=ALU.mult, op1=ALU.add),
                                         reads=[tbd[A_][s], tbd[GI][s]] + ([prev[1]] if prev else []), writes=[dstd])
                                    prev = (dst, dstd, tt)
                                else:
                                    dst = tb[HS][s][:, 0:n]
                                    dstd = tbd[HS][s]
                                    init = 0.0 if prev is None else prev[0][:, 0:1]
                                    g.op("dve", lambda: nc.vector.tensor_tensor_scan(out=dst[:, ::-1], data0=tb[A_][s][:, 0:n][:, ::-1], data1=tb[GI][s][:, 0:n][:, ::-1],
                                                                                     initial=init, op0=ALU.mult, op1=ALU.add),
                                         reads=[tbd[A_][s], tbd[GI][s]] + ([prev[1]] if prev else []), writes=[dstd])
                                    prev = (dst, dstd, tt)
                                    if tt != 0:
                                        pgt = 6 + s
                                        for c in range(NCH):
                                            g.op("pe", lambda: nc.tensor.matmul(ps[pgt][:, 0:n], lhsT=wr1[sW][:, c, oc * 128:(oc + 1) * 128], rhs=hb[:, c, tsl(tt)],
                                                                                start=(c == 0), stop=(c == NCH - 1)),
                                                 reads=[wr1d[sW], hbd[c][tt]], writes=[psd[pgt]])
                                        GL, SM = 7, 8
                                        g.op("act", lambda: nc.scalar.activation(out=tb[GL][s][:, 0:n], in_=ps[pgt][:, 0:n], func=AF.Gelu), reads=[psd[pgt]], writes=[tbd[GL][s]])
                                        g.op("pool", lambda: nc.gpsimd.tensor_tensor(out=tb[SM][s][:, 0:n], in0=dst, in1=rec[:, oc, tsl(tt)], op=ALU.add),
                                             reads=[dstd, recd[oc][tt]], writes=[tbd[SM][s]])
                                        g.op("dve", lambda: nc.vector.tensor_tensor(out=cat1[:, ch, TOFF[tt] - TC:TOFF[tt] - TC + n], in0=tb[SM][s][:, 0:n], in1=tb[GL][s][:, 0:n], op=ALU.mult),
                                             reads=[tbd[SM][s], tbd[GL][s]], writes=[cat1d[ch][tt]])
                g.barrier()
            alloc_x()
            C1 = ExitStack()
            with C1:
                xst = [sb(C1, "x1st%d" % i, [128, NCH, 512], F32) for i in range(2)]
                xstd = [g.dep(dma=True), g.dep(dma=True)]
                c1c = [0]

                def xold1(tt):
                    s = c1c[0] % 2
                    c1c[0] += 1
                    for c in range(NCH):
                        g.dma("sp", xst[s][:, c, 0:TN[tt]], xs_d[:, c, tsl(tt)], reads=[xs_dep], writes=[xstd[s]])
                    return xst[s], xstd[s]

                out_proj(1, od_w_out, lambda c, tt: cat1[:, c, TOFF[tt] - TC:TOFF[tt] - TC + TN[tt]], lambda c, tt: cat1d[c][tt], LAT, xold1, C1)
                g.barrier()
        moe_layer(1, LAT)

        FN = ExitStack()
        with FN:
            x = xstate["x"]
            xd = xstate["d"]
            nb = norm_bufs(FN)
            sq, sqd, tmp, tmpd, rs, rsd, cnt = nb
            yb = [sb(FN, "yb%d" % i, [128, NCH, 128], F32) for i in range(2)]
            ybd = [g.dep(), g.dep()]
            ot = [sb(FN, "ot%d" % i, [128, D], F32) for i in range(2)]
            otd = [g.dep(dma=True), g.dep(dma=True)]
            oc_ = [0]
            for tt in LAT:
                n = TN[tt]
                pb = 5
                for c in range(NCH):
                    s = cnt[0] % 2
                    cnt[0] += 1
                    g.op("act", lambda: nc.scalar.activation(out=sq[s][:, 0:n], in_=x[:, c, tsl(tt)], func=AF.Square), reads=[xd[c][tt]], writes=[sqd[s]])
                    g.op("pe", lambda: nc.tensor.matmul(ps[pb][:, 0:n], lhsT=ones32[:], rhs=sq[s][:, 0:n], start=(c == 0), stop=(c == NCH - 1)),
                         reads=[sqd[s], onesd], writes=[psd[pb]])
                g.op("act", lambda: nc.scalar.activation(out=rs[:, 0:n], in_=ps[pb][:, 0:n], func=AF.Sqrt, scale=1.0 / D, bias=EPS), reads=[psd[pb]], writes=[rsd])
                g.op("dve", lambda: nc.vector.reciprocal(out=rs[:, 0:n], in_=rs[:, 0:n]), reads=[rsd], writes=[rsd])
                for sub in range(n // 128):
                    s = oc_[0] % 2
                    oc_[0] += 1
                    for c in range(NCH):
                        g.op("dve", lambda: nc.vector.scalar_tensor_tensor(out=yb[s][:, c, :], in0=x[:, c, TOFF[tt] + sub * 128:TOFF[tt] + (sub + 1) * 128],
                                                                           scalar=Vcol("fnorm", c), in1=rs[:, sub * 128:(sub + 1) * 128], op0=ALU.mult, op1=ALU.mult),
                             reads=[xd[c][tt], rsd, Vd], writes=[ybd[s]])
                    for half in range(2):
                        pbt = 6 + half
                        for cc in range(4):
                            c = half * 4 + cc
                            g.op("pe", lambda: nc.tensor.transpose(out=ps[pbt][:, cc * 128:(cc + 1) * 128], in_=yb[s][:, c, :], identity=ident[:]),
                                 reads=[ybd[s], cdep], writes=[psd[pbt]])
                        if half == 0:
                            g.op("act", lambda: nc.scalar.copy(out=ot[s][:, 0:512], in_=ps[pbt][:, :]), reads=[psd[pbt]], writes=[otd[s]])
                        else:
                            g.op("dve", lambda: nc.vector.tensor_copy(out=ot[s][:, 512:1024], in_=ps[pbt][:, :]), reads=[psd[pbt]], writes=[otd[s]])
                    r0 = TOFF[tt] - TC + sub * 128
                    g.dma("sp", out_d[r0:r0 + 128, :], ot[s][:], reads=[otd[s]], writes=[out_dep])
            g.barrier()
        xes.close()
    return nc


def _consts():
    ident = np.eye(128, dtype=np.float32)
    perm = np.zeros((128, 128), np.float32)
    for m in range(128):
        blk = m // 16
        partner = (blk ^ 1) * 16 + (m % 16)
        perm[partner, m] = 1.0
    n_rows = TL // 64
    rows, cols = np.meshgrid(np.arange(n_rows), np.arange(64), indexing="ij")
    pos = np.stack([rows.reshape(-1), cols.reshape(-1)], axis=-1).astype(np.float32)
    inv = (np.float32(10000.0) ** (-np.arange(16, dtype=np.float32) / np.float32(16))).astype(np.float32)
    ang = (pos[:, :, None] * inv).astype(np.float32)
    cos = np.cos(ang).astype(np.float32)
    sin = np.sin(ang).astype(np.float32)
    cosT = np.ones((128, T), np.float32)
    sinT = np.zeros((128, T), np.float32)
    for p in range(128):
        dd = p % 64
        axis = dd // 32
        half = (dd % 32) // 16
        f = dd % 16
        cosT[p, TC:] = cos[:, axis, f]
        sinT[p, TC:] = (-sin[:, axis, f]) if half == 0 else sin[:, axis, f]
    return ident, perm, cosT, sinT


def _consts2(NE):
    utri = np.triu(np.ones((128, 128), np.float32), k=1)
    offrow = np.tile((np.arange(NE, dtype=np.float32) * TS)[None, :], (128, 1))
    tok4f = (4.0 * np.arange(128, dtype=np.float32)[:, None] + np.arange(4, dtype=np.float32)[None, :]).astype(np.float32)
    return utri, offrow, tok4f


def make_in_maps(inp, NE, ncores):
    voff, vrows, vpad = vec_layout(NE)
    ident, perm, cosT, sinT = _consts()
    f = lambda a: np.ascontiguousarray(np.asarray(a, dtype=np.float32))
    shared = {
        "w_mod": f(inp["w_mod"]), "ev_w_in": f(inp["ev_w_in"][0]), "ev_w_out": f(inp["ev_w_out"][0]),
        "od_w_in": f(inp["od_w_in"][0]), "od_w_out": f(inp["od_w_out"][0]),
        "od_ga": f(inp["od_gate_a_w"][0]), "od_gx": f(inp["od_gate_x_w"][0]),
        "w_router": f(inp["moe_w_router"]), "b_router": f(inp["moe_b_router"]),
        "moe_w1": f(inp["moe_w1"]), "moe_w2": f(inp["moe_w2"]), "moe_b2": f(inp["moe_b2"]),
        "lams": f(np.stack([inp["ev_lambda_q1"][0], inp["ev_lambda_k1"][0], inp["ev_lambda_q2"][0], inp["ev_lambda_k2"][0]])),
        "ident": ident, "perm": perm, "cosT": cosT, "sinT": sinT,
    }
    shared["utri"], shared["offrow"], shared["tok4f"] = _consts2(NE)
    maps = []
    for b in range(ncores):
        rows = [
            f(inp["c"][b]).reshape(8, 128), f(inp["c_ctx"]).reshape(8, 128), f(inp["b_mod"]).reshape(96, 128),
            f(inp["norm_mix"]).reshape(16, 128), f(inp["norm_ffn"]).reshape(16, 128), f(inp["final_norm"]).reshape(8, 128),
            f(inp["ev_conv_w"][0]).reshape(12, 128), f(inp["ev_subln"][0]).reshape(1, 128),
            f(inp["od_conv_w"][0]).reshape(64, 128), f(inp["od_conv_b"][0]).reshape(16, 128),
            f(inp["od_gate_a_b"][0]).reshape(16, 128), f(inp["od_gate_x_b"][0]).reshape(16, 128),
            f(inp["od_lru_lambda"][0]).reshape(16, 128), f(inp["moe_b1"]).reshape(2 * NE * 16, 128),
        ]
        v = np.concatenate(rows, axis=0)
        assert v.shape[0] == vrows
        vp = np.zeros((vpad, 128), np.float32)
        vp[:vrows] = v
        m = dict(shared)
        m["xin"] = f(inp["x"][b])
        m["ctxin"] = f(inp["ctx"][b])
        m["vecs"] = vp
        maps.append(m)
    return maps


def kernel(**inputs):
    NE = inputs["moe_w1"].shape[1]
    B = inputs["x"].shape[0]
    nc = build(NE)
    maps = make_in_maps(inputs, NE, B)
    res = run_bass_kernel_spmd(nc, maps, core_ids=list(range(B)))
    return np.stack([np.asarray(r["out"], dtype=np.float32) for r in res.results], axis=0)
```

```python
import math
from contextlib import ExitStack

import numpy as np
import concourse.bass as bass
import concourse.mybir as mybir
from concourse.bass_utils import run_bass_kernel_spmd

F32 = mybir.dt.float32
F32R = mybir.dt.float32r
BF16 = mybir.dt.bfloat16
AF = mybir.ActivationFunctionType
ALU = mybir.AluOpType
AX = mybir.AxisListType

D = 1024
NCH = 8
TC = 256
TL = 2048
T = TC + TL
TOFF = [0, 256, 768, 1280, 1792]
TN = [256, 512, 512, 512, 512]
NTT = 5
NKT = T // 128
EPS = 1e-6
SELF_SYNC = True
CAP = 384
NCHUNK = (T + CAP - 1) // CAP
I32 = mybir.dt.int32
ET = mybir.EngineType
ENG_KEY = {ET.PE: "pe", ET.Activation: "act", ET.DVE: "dve", ET.Pool: "pool", ET.SP: "sp"}


def tsl(tt):
    return slice(TOFF[tt], TOFF[tt] + TN[tt])


def vec_layout(NE):
    ents = [("c", 8), ("cctx", 8), ("bmod", 96), ("nmix", 16), ("nffn", 16), ("fnorm", 8), ("evconv", 12),
            ("subln", 1), ("odconvw", 64), ("odconvb", 16), ("odgab", 16), ("odgxb", 16), ("odlam", 16),
            ("b1", 2 * NE * 16)]
    off = {}
    r = 0
    for name, n in ents:
        off[name] = r
        r += n
    rpad = ((r + 127) // 128) * 128
    return off, r, rpad


class Dep:
    __slots__ = ("w", "r", "sem", "tot")

    def __init__(self):
        self.w = None
        self.r = {}
        self.sem = None
        self.tot = 0


class G:
    def __init__(self, nc, es):
        self.nc = nc
        self.es = es
        self.eng = {"pe": nc.tensor, "act": nc.scalar, "dve": nc.vector, "pool": nc.gpsimd, "sp": nc.sync}
        self.semh = []
        self.esem = {}
        self.cnt = {}
        self.known = {}
        for k in self.eng:
            self.esem[k] = self.new_sem("e_" + k)
            self.cnt[k] = 0
            self.known[k] = {}
        self.dma_sems = []
        self.nsem = 0
        self.alldeps = []

    def new_sem(self, name):
        h = self.es.enter_context(self.nc.semaphore(name))
        self.semh.append(h)
        return len(self.semh) - 1

    def snapshot(self):
        deps = [(d, d.w, dict(d.r), d.tot) for d in self.alldeps]
        return (deps, dict(self.cnt), {k: dict(v) for k, v in self.known.items()})

    def restore(self, snap):
        deps, cnt, known = snap
        for d, w, r, tot in deps:
            d.w = w
            d.r = dict(r)
            d.tot = tot
        self.cnt = dict(cnt)
        self.known = {k: dict(v) for k, v in known.items()}

    def pad_to(self, big):
        deps, cnt, _ = big
        for e in self.eng:
            diff = cnt[e] - self.cnt[e]
            assert diff >= 0, (e, diff)
            if diff > 0:
                self.eng[e].wait_ge(self.semh[self.esem[e]], self.cnt[e])
                self.eng[e].sem_inc(self.semh[self.esem[e]], diff)
                self.cnt[e] = cnt[e]
        for d, w, r, tot in deps:
            if d.sem is None:
                continue
            diff = tot - d.tot
            assert diff >= 0
            if diff > 0:
                self.eng["sp"].wait_ge(self.semh[d.sem], d.tot)
                self.eng["sp"].sem_inc(self.semh[d.sem], diff)
                d.tot = tot

    def dep(self, dma=False):
        d = Dep()
        self.alldeps.append(d)
        if dma:
            self.nsem += 1
            d.sem = self.new_sem("d%d" % self.nsem)
            self.dma_sems.append(d)
        return d

    def _wait(self, e, deps):
        need = {}
        kn = self.known[e]
        for d in deps:
            if d is None:
                continue
            s, v = d
            if s == self.esem[e] and (e == "pe" or not SELF_SYNC):
                continue
            if kn.get(s, 0) >= v:
                continue
            if need.get(s, 0) < v:
                need[s] = v
        for s, v in need.items():
            self.eng[e].wait_ge(self.semh[s], v)
            kn[s] = v

    def op(self, e, fn, reads=(), writes=()):
        deps = []
        for t in reads:
            deps.append(t.w)
        for t in writes:
            deps.append(t.w)
            deps.extend(t.r.items())
        self._wait(e, deps)
        ins = fn()
        self.cnt[e] += 1
        s = self.esem[e]
        ins.then_inc(self.semh[s], 1)
        me = (s, self.cnt[e])
        for t in reads:
            t.r[s] = self.cnt[e]
        for t in writes:
            t.w = me
            t.r = {}
        return ins

    def dma(self, q, out, in_, reads=(), writes=(), sem_dep=None):
        deps = []
        for t in reads:
            deps.append(t.w)
        for t in writes:
            deps.append(t.w)
            deps.extend(t.r.items())
        self._wait(q, deps)
        sd = sem_dep if sem_dep is not None else writes[0]
        assert sd.sem is not None
        ins = self.eng[q].dma_start(out=out, in_=in_)
        sd.tot += 16
        ins.then_inc(self.semh[sd.sem], 16)
        me = (sd.sem, sd.tot)
        for t in reads:
            t.r[sd.sem] = sd.tot
        for t in writes:
            t.w = me
            t.r = {}
        return ins

    def idma(self, out, off_ap, in_, bound, reads=(), writes=()):
        deps = []
        for t in reads:
            deps.append(t.w)
        for t in writes:
            deps.append(t.w)
            deps.extend(t.r.items())
        self._wait("pool", deps)
        sd = writes[0]
        ins = self.nc.gpsimd.indirect_dma_start(out=out, out_offset=bass.IndirectOffsetOnAxis(ap=off_ap, axis=0), in_=in_, in_offset=None,
                                                bounds_check=bound, oob_is_err=False)
        sd.tot += 16
        ins.then_inc(self.semh[sd.sem], 16)
        me = (sd.sem, sd.tot)
        for t in reads:
            t.r[sd.sem] = sd.tot
        for t in writes:
            t.w = me
            t.r = {}
        return ins

    def barrier(self):
        for e in self.eng:
            deps = [(self.esem[o], self.cnt[o]) for o in self.eng if o != e and self.cnt[o] > 0]
            deps += [(d.sem, d.tot) for d in self.dma_sems if d.tot > 0]
            self._wait(e, deps)


def build(NE=32, dbg=None):
    nc = bass.Bass("TRN2", target_bir_lowering=False)
    voff, vrows, vpad = vec_layout(NE)
    NVB = vpad // 128

    def din(name, shape, dt=F32):
        return nc.dram_tensor(name, list(shape), dt, kind="ExternalInput").ap()

    xin = din("xin", [TL, D])
    ctxin = din("ctxin", [TC, D])
    vecs = din("vecs", [vpad, 128])
    w_mod = din("w_mod", [2, D, 6 * D])
    ev_w_in = din("ev_w_in", [D, 3 * D])
    ev_w_out = din("ev_w_out", [D, D])
    od_w_in = din("od_w_in", [D, 2 * D])
    od_w_out = din("od_w_out", [D, D])
    od_ga = din("od_ga", [2, 4, 256, 256])
    od_gx = din("od_gx", [2, 4, 256, 256])
    w_router = din("w_router", [2, D, NE])
    b_router = din("b_router", [2, NE])
    moe_w1 = din("moe_w1", [2, NE, D, 2 * D])
    moe_w2 = din("moe_w2", [2, NE, D, D])
    moe_b2 = din("moe_b2", [2, NE, D])
    lams = din("lams", [4, 64])
    ident_d = din("ident", [128, 128])
    perm_d = din("perm", [128, 128])
    cos_d = din("cosT", [128, T])
    sin_d = din("sinT", [128, T])
    utri_d = din("utri", [128, 128])
    offrow_d = din("offrow", [128, NE])
    tok4f_d = din("tok4f", [128, 4])
    out_d = nc.dram_tensor("out", [TL, D], F32, kind="ExternalOutput").ap()
    xs_d = nc.dram_tensor("xs_scratch", [128, NCH, T], F32, kind="Internal").ap()
    gsc_d = nc.dram_tensor("g_scratch", [2, NE, T], F32, kind="Internal").ap()
    psc_d = nc.dram_tensor("p_scratch", [2, NE, T], F32, kind="Internal").ap()
    cnt_d = nc.dram_tensor("cnt_scratch", [2, NE], I32, kind="Internal").ap()
    hs_d = nc.dram_tensor("hs_scratch", [NE * T, D], BF16, kind="Internal").ap()
    idx_d = nc.dram_tensor("idx_scratch", [NE * T, 1], I32, kind="Internal").ap()
    y4_d = nc.dram_tensor("y4_scratch", [4 * T, D], F32, kind="Internal").ap()
    dbg_d = None
    if dbg is not None:
        dbg_d = nc.dram_tensor("dbg", [128, NCH, T], F32, kind="ExternalOutput").ap()

    es = ExitStack()
    with es:
        g = G(nc, es)
        xs_dep = g.dep(dma=True)
        gsc_dep = [g.dep(dma=True), g.dep(dma=True)]
        psc_dep = [g.dep(dma=True), g.dep(dma=True)]
        flag_dep = [g.dep(dma=True), g.dep(dma=True)]
        hs_dep = g.dep(dma=True)
        bnd_hs = nc.gpsimd.alloc_register("bnd_hs")
        nc.gpsimd.reg_mov(bnd_hs, NE * T - 1)
        bnd_y4 = nc.gpsimd.alloc_register("bnd_y4")
        nc.gpsimd.reg_mov(bnd_y4, 4 * T - 1)
        idx_dep = g.dep(dma=True)
        y4_dep = g.dep(dma=True)
        flag_regs = nc.alloc_registers("ovf", [ET.PE, ET.Activation, ET.DVE, ET.Pool, ET.SP])
        out_dep = g.dep(dma=True)

        _uid = [0]

        def sb(es_, name, shape, dt, side=None):
            _uid[0] += 1
            return es_.enter_context(nc.sbuf_tensor("sb%d_%s" % (_uid[0], name), list(shape), dt, side=side))

        ps = [es.enter_context(nc.psum_tensor("ps%d" % i, [128, 512], F32)) for i in range(8)]
        psd = [g.dep() for _ in range(8)]

        ident = sb(es, "ident", [128, 128], F32)
        perm = sb(es, "perm", [128, 128], F32)
        ones32 = sb(es, "ones32", [128, 128], F32)
        ones16 = sb(es, "ones16", [128, 128], BF16)
        V = sb(es, "V", [128, vpad], F32)
        modT = sb(es, "modT", [128, 2, 48, 2], F32)
        S = sb(es, "S", [128, 160], F32)
        cdep = g.dep(dma=True)
        Vd = g.dep()
        modd = g.dep()
        Sd = g.dep()
        onesd = g.dep()
        g.dma("sp", ident[:], ident_d, writes=[cdep])
        g.dma("sp", perm[:], perm_d, writes=[cdep])
        identb = sb(es, "identb", [128, 128], BF16)
        utri = sb(es, "utri", [128, 128], F32)
        offrow = sb(es, "offrow", [128, NE], F32)
        tok4f = sb(es, "tok4f", [128, 4], F32)
        cdep2 = g.dep(dma=True)
        g.dma("pool", identb[:], ident_d, writes=[cdep2])
        g.dma("sp", utri[:], utri_d, writes=[cdep2])
        g.dma("sp", offrow[:], offrow_d, writes=[cdep2])
        g.dma("sp", tok4f[:], tok4f_d, writes=[cdep2])
        g.op("dve", lambda: nc.vector.memset(ones32[:], 1.0), writes=[onesd])
        g.op("dve", lambda: nc.vector.memset(ones16[:], 1.0), writes=[onesd])

        SC = {}
        _sc = [0]

        def scol(name, n):
            SC[name] = _sc[0]
            _sc[0] += n
            return SC[name]

        for l in range(2):
            for cls in range(2):
                scol("gs1_%d_%d" % (l, cls), 8)
                scol("gs2_%d_%d" % (l, cls), 8)
        scol("sg", 1)
        scol("neglam", 1)
        scol("c8", 16)
        scol("c8x2", 16)
        scol("tmp", 16)
        assert _sc[0] <= 160

        def Scol(name, i=0):
            return S[:, SC[name] + i:SC[name] + i + 1]

        def Vcol(name, i=0):
            return V[:, voff[name] + i:voff[name] + i + 1]

        def mod(l, k, c, cls):
            return modT[:, l, k * 8 + c, cls:cls + 1]

        pes = ExitStack()
        with pes:
            vstg = [sb(pes, "vstg%d" % i, [128, 128], F32) for i in range(2)]
            vstd = [g.dep(dma=True) for _ in range(2)]
            for blk in range(NVB):
                s = blk % 2
                g.dma("sp", vstg[s][:], vecs[blk * 128:(blk + 1) * 128, :], writes=[vstd[s]])
                pb = blk % 2
                g.op("pe", lambda: nc.tensor.transpose(out=ps[pb][:, 0:128], in_=vstg[s][:], identity=ident[:]),
                     reads=[vstd[s], cdep], writes=[psd[pb]])
                g.op("act", lambda: nc.scalar.copy(out=V[:, blk * 128:(blk + 1) * 128], in_=ps[pb][:, 0:128]),
                     reads=[psd[pb]], writes=[Vd])
            b1v = V[:, voff["b1"]:voff["b1"] + 2 * NE * 16].rearrange("p (a j) -> p a j", j=16)
            g.op("dve", lambda: nc.vector.tensor_scalar(out=b1v[:, :, 8:16], in0=b1v[:, :, 8:16], scalar1=1.0,
                                                        scalar2=None, op0=ALU.add), reads=[Vd], writes=[Vd])
            scT = sb(pes, "scT", [128, 8, 2], F32R)
            scd = g.dep()
            g.op("act", lambda: nc.scalar.activation(out=scT[:, :, 0], in_=V[:, voff["c"]:voff["c"] + 8],
                                                     func=AF.Silu), reads=[Vd], writes=[scd])
            g.op("act", lambda: nc.scalar.activation(out=scT[:, :, 1], in_=V[:, voff["cctx"]:voff["cctx"] + 8],
                                                     func=AF.Silu), reads=[Vd], writes=[scd])
            wm = [sb(pes, "wm%d" % i, [128, 8, 512], F32R) for i in range(2)]
            wmd = [g.dep(dma=True) for _ in range(2)]
            it = 0
            for l in range(2):
                for blk in range(12):
                    s = it % 2
                    it += 1
                    src = w_mod[l, :, blk * 512:(blk + 1) * 512].rearrange("(c p) f -> p c f", p=128)
                    g.dma("pool", wm[s][:], src, writes=[wmd[s]])
                    for fcl in range(4):
                        pb = (blk * 4 + fcl) % 2
                        for dc in range(8):
                            g.op("pe", lambda: nc.tensor.matmul(ps[pb][:, 0:2], lhsT=wm[s][:, dc, fcl * 128:(fcl + 1) * 128],
                                                                rhs=scT[:, dc, :], start=(dc == 0), stop=(dc == 7)),
                                 reads=[wmd[s], scd], writes=[psd[pb]])
                        if True:
                            kk = blk * 4 + fcl
                            g.op("dve", lambda: nc.vector.tensor_scalar(out=modT[:, l, kk, :], in0=ps[pb][:, 0:2],
                                                                        scalar1=Vcol("bmod", l * 48 + kk), scalar2=None,
                                                                        op0=ALU.add), reads=[psd[pb], Vd], writes=[modd])
            for l in range(2):
                for cls in range(2):
                    for (nm, kc, vn) in (("gs1", 1, "nmix"), ("gs2", 4, "nffn")):
                        c0 = SC["%s_%d_%d" % (nm, l, cls)]
                        g.op("dve", lambda: nc.vector.scalar_tensor_tensor(
                            out=S[:, c0:c0 + 8], in0=modT[:, l, kc * 8:kc * 8 + 8, cls], scalar=1.0,
                            in1=V[:, voff[vn] + l * 8:voff[vn] + l * 8 + 8], op0=ALU.add, op1=ALU.mult),
                            reads=[modd, Vd], writes=[Sd])
            lam_init0 = 0.8 - 0.6 * math.exp(-0.3 * 0)
            g.op("dve", lambda: nc.vector.tensor_scalar(out=Scol("sg"), in0=Vcol("subln"), scalar1=(1.0 - lam_init0),
                                                        scalar2=None, op0=ALU.mult), reads=[Vd], writes=[Sd])
            lb = sb(pes, "lamb", [128, 4, 64], F32)
            lbd = g.dep(dma=True)
            for i in range(4):
                g.dma("sp", lb[:, i, :], lams[i:i + 1, :].to_broadcast([128, 64]), writes=[lbd])
            lt = sb(pes, "lamt", [128, 2, 64], F32)
            ltd = g.dep()
            g.op("dve", lambda: nc.vector.tensor_tensor(out=lt[:, 0, :], in0=lb[:, 0, :], in1=lb[:, 1, :], op=ALU.mult),
                 reads=[lbd], writes=[ltd])
            g.op("dve", lambda: nc.vector.tensor_tensor(out=lt[:, 1, :], in0=lb[:, 2, :], in1=lb[:, 3, :], op=ALU.mult),
                 reads=[lbd], writes=[ltd])
            tm = SC["tmp"]
            g.op("dve", lambda: nc.vector.tensor_reduce(out=S[:, tm:tm + 2], in_=lt[:], axis=AX.X, op=ALU.add),
                 reads=[ltd], writes=[Sd])
            g.op("act", lambda: nc.scalar.activation(out=S[:, tm + 2:tm + 4], in_=S[:, tm:tm + 2], func=AF.Exp),
                 reads=[Sd], writes=[Sd])
            g.op("dve", lambda: nc.vector.scalar_tensor_tensor(out=Scol("neglam"), in0=S[:, tm + 3:tm + 4],
                                                               scalar=-lam_init0, in1=S[:, tm + 2:tm + 3],
                                                               op0=ALU.add, op1=ALU.subtract), reads=[Sd], writes=[Sd])
            g.op("act", lambda: nc.scalar.activation(out=S[:, tm:tm + 16], in_=V[:, voff["odlam"]:voff["odlam"] + 16],
                                                     func=AF.Exp, scale=-1.0), reads=[Vd, Sd], writes=[Sd])
            g.op("act", lambda: nc.scalar.activation(out=S[:, tm:tm + 16], in_=S[:, tm:tm + 16], func=AF.Ln, bias=1.0),
                 reads=[Sd], writes=[Sd])
            g.op("dve", lambda: nc.vector.tensor_scalar(out=S[:, SC["c8"]:SC["c8"] + 16], in0=S[:, tm:tm + 16],
                                                        scalar1=-8.0, scalar2=None, op0=ALU.mult), reads=[Sd], writes=[Sd])
            g.op("dve", lambda: nc.vector.tensor_scalar(out=S[:, SC["c8x2"]:SC["c8x2"] + 16], in0=S[:, tm:tm + 16],
                                                        scalar1=-16.0, scalar2=None, op0=ALU.mult), reads=[Sd], writes=[Sd])
            g.barrier()
        hb = sb(es, "hb", [128, NCH, T], BF16)
        hbd = [[g.dep() for _ in range(NTT)] for _ in range(NCH)]
        xes = ExitStack()
        xstate = {}

        def alloc_x():
            xstate["x"] = xes.enter_context(nc.sbuf_tensor("xres%d" % len(xstate), [128, NCH, T], F32, side="right"))
            xstate["d"] = [[g.dep() for _ in range(NTT)] for _ in range(NCH)]

        def load_tok_tile(pes_bufs, tt, l0_src=True):
            xst, xstd, tstg, tstgd, cnt = pes_bufs
            s = cnt[0] % 2
            cnt[0] += 1
            n = TN[tt]
            for sub in range(n // 128):
                k = cnt[1] % 2
                cnt[1] += 1
                if tt == 0:
                    src = ctxin[sub * 128:(sub + 1) * 128, :]
                else:
                    r0 = TOFF[tt] - TC + sub * 128
                    src = xin[r0:r0 + 128, :]
                g.dma("sp", tstg[k][:], src, writes=[tstgd[k]])
                for half in range(2):
                    pb = 6 + half
                    for cc in range(4):
                        c = half * 4 + cc
                        g.op("pe", lambda: nc.tensor.transpose(out=ps[pb][:, cc * 128:(cc + 1) * 128],
                                                               in_=tstg[k][:, c * 128:(c + 1) * 128], identity=ident[:]),
                             reads=[tstgd[k], cdep], writes=[psd[pb]])
                    dst = xst[s][:, half * 4:half * 4 + 4, sub * 128:(sub + 1) * 128]
                    srcp = ps[pb][:, :].rearrange("p (c t) -> p c t", t=128)
                    if half == 0:
                        g.op("act", lambda: nc.scalar.copy(out=dst, in_=srcp), reads=[psd[pb]], writes=[xstd[s]])
                    else:
                        g.op("dve", lambda: nc.vector.tensor_copy(out=dst, in_=srcp), reads=[psd[pb]], writes=[xstd[s]])
            return xst[s], xstd[s]

        def rmsnorm_tile(nb, xsrc, xdeps, tt, gsname, l, k_sh, out32=None, out32d=None, hbout=None):
            sq, sqd, tmp, tmpd, rs, rsd, cnt = nb
            n = TN[tt]
            cls = 1 if tt == 0 else 0
            pb = 5
            for c in range(NCH):
                s = cnt[0] % 2
                cnt[0] += 1
                g.op("act", lambda: nc.scalar.activation(out=sq[s][:, 0:n], in_=xsrc(c), func=AF.Square),
                     reads=[xdeps(c)], writes=[sqd[s]])
                g.op("pe", lambda: nc.tensor.matmul(ps[pb][:, 0:n], lhsT=ones32[:], rhs=sq[s][:, 0:n],
                                                    start=(c == 0), stop=(c == NCH - 1)),
                     reads=[sqd[s], onesd], writes=[psd[pb]])
            g.op("act", lambda: nc.scalar.activation(out=rs[:, 0:n], in_=ps[pb][:, 0:n], func=AF.Sqrt,
                                                     scale=1.0 / D, bias=EPS), reads=[psd[pb]], writes=[rsd])
            g.op("dve", lambda: nc.vector.reciprocal(out=rs[:, 0:n], in_=rs[:, 0:n]), reads=[rsd], writes=[rsd])
            c0 = SC["%s_%d_%d" % (gsname, l, cls)]
            for c in range(NCH):
                s = cnt[1] % 2
                cnt[1] += 1
                g.op("dve", lambda: nc.vector.tensor_tensor(out=tmp[s][:, 0:n], in0=xsrc(c), in1=rs[:, 0:n], op=ALU.mult),
                     reads=[xdeps(c), rsd], writes=[tmpd[s]])
                ho, hod = (hb[:, c, tsl(tt)], hbd[c][tt]) if hbout is None else hbout(c)
                g.op("act", lambda: nc.scalar.activation(out=ho, in_=tmp[s][:, 0:n], func=AF.Identity,
                                                         scale=S[:, c0 + c:c0 + c + 1], bias=mod(l, k_sh, c, cls)),
                     reads=[tmpd[s], Sd, modd], writes=[hod])
                if out32 is not None:
                    g.op("dve", lambda: nc.vector.tensor_scalar(out=out32[:, c, 0:n], in0=tmp[s][:, 0:n],
                                                                scalar1=S[:, c0 + c:c0 + c + 1],
                                                                scalar2=mod(l, k_sh, c, cls), op0=ALU.mult, op1=ALU.add),
                         reads=[tmpd[s], Sd, modd], writes=[out32d])

        def norm_bufs(es_):
            sq = [sb(es_, "nsq%d" % i, [128, 512], F32) for i in range(2)]
            tmp = [sb(es_, "ntmp%d" % i, [128, 512], F32) for i in range(2)]
            rs = sb(es_, "nrs", [128, 512], F32)
            return (sq, [g.dep(), g.dep()], tmp, [g.dep(), g.dep()], rs, g.dep(), [0, 0])

        def stage_bufs(es_):
            xst = [sb(es_, "xst%d" % i, [128, NCH, 512], F32) for i in range(2)]
            tstg = [sb(es_, "tstg%d" % i, [128, D], F32) for i in range(2)]
            return (xst, [g.dep(dma=True), g.dep(dma=True)], tstg, [g.dep(dma=True), g.dep(dma=True)], [0, 0])

        def out_proj(l, wout_d, catfn, catdeps, tiles, xold_fn, es_):
            wo = sb(es_, "wo%d" % l, [128, NCH, D], BF16)
            wod = g.dep(dma=True)
            for hh in range(2):
                g.dma("pool", wo[:, :, hh * 512:(hh + 1) * 512],
                      wout_d[:, hh * 512:(hh + 1) * 512].rearrange("(c p) f -> p c f", p=128), writes=[wod])
            x = xstate["x"]
            xd = xstate["d"]
            it = 0
            for tt in tiles:
                n = TN[tt]
                cls = 1 if tt == 0 else 0
                xo, xod = xold_fn(tt)
                for oc in range(NCH):
                    pb = it % 2
                    it += 1
                    for c in range(NCH):
                        g.op("pe", lambda: nc.tensor.matmul(ps[pb][:, 0:n], lhsT=wo[:, c, oc * 128:(oc + 1) * 128],
                                                            rhs=catfn(c, tt), start=(c == 0), stop=(c == NCH - 1)),
                             reads=[wod, catdeps(c, tt)], writes=[psd[pb]])
                    g.op("dve", lambda: nc.vector.scalar_tensor_tensor(out=x[:, oc, tsl(tt)], in0=ps[pb][:, 0:n],
                                                                       scalar=mod(l, 2, oc, cls), in1=xo[:, oc, 0:n],
                                                                       op0=ALU.mult, op1=ALU.add),
                         reads=[psd[pb], xod, modd], writes=[xd[oc][tt]])

        L0 = ExitStack()
        with L0:
            catc = sb(L0, "catc", [128, 4, T], BF16)
            catcd = [[g.dep() for _ in range(NTT)] for _ in range(4)]
            A0 = ExitStack()
            with A0:
                stg = stage_bufs(A0)
                nb = norm_bufs(A0)
                for tt in range(NTT):
                    xt, xtd = load_tok_tile(stg, tt)
                    rmsnorm_tile(nb, lambda c: xt[:, c, 0:TN[tt]], lambda c: xtd, tt, "gs1", 0, 0)
                g.barrier()
            M0 = ExitStack()
            with M0:
                wr = [sb(M0, "w0r%d" % i, [128, NCH, 512], BF16) for i in range(3)]
                wrd = [g.dep(dma=True) for _ in range(3)]

                def load_win(slot, blk):
                    g.dma("pool", wr[slot][:], ev_w_in[:, blk * 512:(blk + 1) * 512].rearrange("(c p) f -> p c f", p=128),
                          writes=[wrd[slot]])

                load_win(0, 3)
                load_win(1, 4)
                load_win(2, 5)
                CV = ExitStack()
                with CV:
                    pj = sb(CV, "pj", [128, T], F32)
                    accj = sb(CV, "accj", [128, T], F32)
                    ctmp = [sb(CV, "ctmp%d" % i, [128, 512], F32) for i in range(2)]
                    pjd = g.dep()
                    accd = g.dep()
                    ctd = [g.dep(), g.dep()]
                    it = 0
                    for j in range(4):
                        for tt in range(NTT):
                            n = TN[tt]
                            for which, pb in ((1, 0), (2, 1)):
                                for c in range(NCH):
                                    g.op("pe", lambda: nc.tensor.matmul(ps[pb][:, 0:n], lhsT=wr[which][:, c, j * 128:(j + 1) * 128],
                                                                        rhs=hb[:, c, tsl(tt)], start=(c == 0), stop=(c == NCH - 1)),
                                         reads=[wrd[which], hbd[c][tt]], writes=[psd[pb]])
                            s = it % 2
                            it += 1
                            g.op("act", lambda: nc.scalar.copy(out=ctmp[s][:, 0:n], in_=ps[0][:, 0:n]), reads=[psd[0]], writes=[ctd[s]])
                            g.op("dve", lambda: nc.vector.tensor_tensor(out=pj[:, tsl(tt)], in0=ps[1][:, 0:n], in1=ctmp[s][:, 0:n],
                                                                        op=ALU.mult), reads=[psd[1], ctd[s]], writes=[pjd])
                        for (a, b) in ((0, TC), (TC, T)):
                            g.op("dve", lambda: nc.vector.tensor_scalar(out=accj[:, a:b], in0=pj[:, a:b], scalar1=Vcol("evconv", 1 * 4 + j),
                                                                        scalar2=None, op0=ALU.mult), reads=[pjd, Vd], writes=[accd])
                            g.op("dve", lambda: nc.vector.scalar_tensor_tensor(out=accj[:, a + 1:b], in0=pj[:, a:b - 1],
                                                                               scalar=Vcol("evconv", 0 * 4 + j), in1=accj[:, a + 1:b],
                                                                               op0=ALU.mult, op1=ALU.add), reads=[pjd, Vd, accd], writes=[accd])
                            g.op("dve", lambda: nc.vector.scalar_tensor_tensor(out=accj[:, a:b - 1], in0=pj[:, a + 1:b],
                                                                               scalar=Vcol("evconv", 2 * 4 + j), in1=accj[:, a:b - 1],
                                                                               op0=ALU.mult, op1=ALU.add), reads=[pjd, Vd, accd], writes=[accd])
                        for tt in range(NTT):
                            n = TN[tt]
                            pb = 2 + (tt % 2)
                            for c in range(NCH):
                                g.op("pe", lambda: nc.tensor.matmul(ps[pb][:, 0:n], lhsT=wr[0][:, c, j * 128:(j + 1) * 128],
                                                                    rhs=hb[:, c, tsl(tt)], start=(c == 0), stop=(c == NCH - 1)),
                                     reads=[wrd[0], hbd[c][tt]], writes=[psd[pb]])
                            g.op("dve", lambda: nc.vector.tensor_tensor(out=catc[:, j, tsl(tt)], in0=ps[pb][:, 0:n], in1=accj[:, tsl(tt)],
                                                                        op=ALU.mult), reads=[psd[pb], accd], writes=[catcd[j][tt]])
                    g.barrier()
                load_win(0, 0)
                load_win(1, 1)
                load_win(2, 2)
                qk = [sb(M0, "q", [128, 4, T], BF16), sb(M0, "k", [128, 4, T], BF16)]
                qkd = [[[g.dep() for _ in range(NTT)] for _ in range(4)] for _ in range(2)]
                vt = sb(M0, "v", [128, NKT, 512], BF16)
                vtd = [g.dep() for _ in range(NKT)]
                cosT = sb(M0, "cosT", [128, T], F32)
                sinT = sb(M0, "sinT", [128, T], F32)
                tabd = g.dep(dma=True)
                g.dma("sp", cosT[:], cos_d, writes=[tabd])
                g.dma("sp", sinT[:], sin_d, writes=[tabd])
                RP = ExitStack()
                with RP:
                    qf = [sb(RP, "qf%d" % i, [128, 512], F32) for i in range(2)]
                    qfd = [g.dep(), g.dep()]
                    t1 = [sb(RP, "rt1%d" % i, [128, 512], F32) for i in range(2)]
                    t1d = [g.dep(), g.dep()]
                    t2 = [sb(RP, "rt2%d" % i, [128, 512], F32) for i in range(2)]
                    t2d = [g.dep(), g.dep()]
                    it = 0
                    for which in range(2):
                        for hc in range(4):
                            for tt in range(NTT):
                                n = TN[tt]
                                s = it % 2
                                it += 1
                                pb = s
                                pr = 2 + s
                                for c in range(NCH):
                                    g.op("pe", lambda: nc.tensor.matmul(ps[pb][:, 0:n], lhsT=wr[which][:, c, hc * 128:(hc + 1) * 128],
                                                                        rhs=hb[:, c, tsl(tt)], start=(c == 0), stop=(c == NCH - 1)),
                                         reads=[wrd[which], hbd[c][tt]], writes=[psd[pb]])
                                g.op("act", lambda: nc.scalar.copy(out=qf[s][:, 0:n], in_=ps[pb][:, 0:n]), reads=[psd[pb]], writes=[qfd[s]])
                                g.op("pe", lambda: nc.tensor.matmul(ps[pr][:, 0:n], lhsT=perm[:], rhs=qf[s][:, 0:n], start=True, stop=True),
                                     reads=[qfd[s], cdep], writes=[psd[pr]])
                                g.op("dve", lambda: nc.vector.tensor_tensor(out=t1[s][:, 0:n], in0=qf[s][:, 0:n], in1=cosT[:, tsl(tt)], op=ALU.mult),
                                     reads=[qfd[s], tabd], writes=[t1d[s]])
                                g.op("dve", lambda: nc.vector.tensor_tensor(out=t2[s][:, 0:n], in0=ps[pr][:, 0:n], in1=sinT[:, tsl(tt)], op=ALU.mult),
                                     reads=[psd[pr], tabd], writes=[t2d[s]])
                                g.op("pool", lambda: nc.gpsimd.tensor_tensor(out=qk[which][:, hc, tsl(tt)], in0=t1[s][:, 0:n], in1=t2[s][:, 0:n], op=ALU.add),
                                     reads=[t1d[s], t2d[s]], writes=[qkd[which][hc][tt]])
                    for kt in range(NKT):
                        tt = 0 if kt < 2 else 1 + (kt - 2) // 4
                        pb = 4 + kt % 2
                        for c in range(NCH):
                            g.op("pe", lambda: nc.tensor.matmul(ps[pb][:, :], lhsT=hb[:, c, kt * 128:(kt + 1) * 128], rhs=wr[2][:, c, :],
                                                                start=(c == 0), stop=(c == NCH - 1)),
                                 reads=[wrd[2], hbd[c][tt]], writes=[psd[pb]])
                        g.op("act", lambda: nc.scalar.copy(out=vt[:, kt, :], in_=ps[pb][:, :]), reads=[psd[pb]], writes=[vtd[kt]])
                    g.barrier()
                AT = ExitStack()
                with AT:
                    eb = [[sb(AT, "e%d_%d" % (m, i), [128, 512], BF16) for i in range(2)] for m in range(2)]
                    ebd = [[g.dep(), g.dep()] for _ in range(2)]
                    rz = [sb(AT, "rz%d" % m, [128, 512], F32) for m in range(2)]
                    rzd = [g.dep(), g.dep()]
                    to = [sb(AT, "to%d" % m, [128, 512], F32) for m in range(2)]
                    tod = [g.dep(), g.dep()]
                    osb = sb(AT, "osb", [128, 512], F32)
                    osd = g.dep()
                    osq = sb(AT, "osq", [128, 512], F32)
                    osqd = g.dep()
                    ors = sb(AT, "ors", [128, 512], F32)
                    orsd = g.dep()
                    for h in range(4):
                        for qt in range(NTT):
                            n = TN[qt]
                            nkt = 2 if qt == 0 else NKT
                            for kt in range(nkt):
                                ktt = 0 if kt < 2 else 1 + (kt - 2) // 4
                                sl = kt % 2
                                for m in range(2):
                                    pbs = 4 + 2 * m + sl
                                    g.op("pe", lambda: nc.tensor.matmul(ps[pbs][:, 0:n], lhsT=qk[1][m * 64:(m + 1) * 64, h, kt * 128:(kt + 1) * 128],
                                                                        rhs=qk[0][m * 64:(m + 1) * 64, h, tsl(qt)], start=True, stop=True),
                                         reads=[qkd[1][h][ktt], qkd[0][h][qt]], writes=[psd[pbs]])
                                    g.op("act", lambda: nc.scalar.activation(out=eb[m][sl][:, 0:n], in_=ps[pbs][:, 0:n], func=AF.Exp, scale=0.125),
                                         reads=[psd[pbs]], writes=[ebd[m][sl]])
                                for m in range(2):
                                    g.op("pe", lambda: nc.tensor.matmul(ps[2 * m][:, 0:n], lhsT=vt[:, kt, h * 128:(h + 1) * 128], rhs=eb[m][sl][:, 0:n],
                                                                        start=(kt == 0), stop=(kt == nkt - 1)),
                                         reads=[vtd[kt], ebd[m][sl]], writes=[psd[2 * m]])
                                    g.op("pe", lambda: nc.tensor.matmul(ps[2 * m + 1][:, 0:n], lhsT=ones16[:], rhs=eb[m][sl][:, 0:n],
                                                                        start=(kt == 0), stop=(kt == nkt - 1)),
                                         reads=[onesd, ebd[m][sl]], writes=[psd[2 * m + 1]])
                            for m in range(2):
                                g.op("dve", lambda: nc.vector.reciprocal(out=rz[m][:, 0:n], in_=ps[2 * m + 1][:, 0:n]), reads=[psd[2 * m + 1]], writes=[rzd[m]])
                                g.op("dve", lambda: nc.vector.tensor_tensor(out=to[m][:, 0:n], in0=ps[2 * m][:, 0:n], in1=rz[m][:, 0:n], op=ALU.mult),
                                     reads=[psd[2 * m], rzd[m]], writes=[tod[m]])
                            g.op("dve", lambda: nc.vector.scalar_tensor_tensor(out=osb[:, 0:n], in0=to[1][:, 0:n], scalar=Scol("neglam"), in1=to[0][:, 0:n],
                                                                               op0=ALU.mult, op1=ALU.add), reads=[tod[0], tod[1], Sd], writes=[osd])
                            g.op("act", lambda: nc.scalar.activation(out=osq[:, 0:n], in_=osb[:, 0:n], func=AF.Square), reads=[osd], writes=[osqd])
                            g.op("pe", lambda: nc.tensor.matmul(ps[4][:, 0:n], lhsT=ones32[:], rhs=osq[:, 0:n], start=True, stop=True),
                                 reads=[osqd, onesd], writes=[psd[4]])
                            g.op("act", lambda: nc.scalar.activation(out=ors[:, 0:n], in_=ps[4][:, 0:n], func=AF.Sqrt, scale=1.0 / 128, bias=EPS),
                                 reads=[psd[4]], writes=[orsd])
                            g.op("dve", lambda: nc.vector.reciprocal(out=ors[:, 0:n], in_=ors[:, 0:n]), reads=[orsd], writes=[orsd])
                            g.op("dve", lambda: nc.vector.tensor_tensor(out=osb[:, 0:n], in0=osb[:, 0:n], in1=ors[:, 0:n], op=ALU.mult),
                                 reads=[orsd, osd], writes=[osd])
                            g.op("act", lambda: nc.scalar.activation(out=hb[:, h, tsl(qt)], in_=osb[:, 0:n], func=AF.Identity, scale=Scol("sg")),
                                 reads=[osd, Sd], writes=[hbd[h][qt]])
                    g.barrier()
            alloc_x()
            C0 = ExitStack()
            with C0:
                stg = stage_bufs(C0)

                def xold0(tt):
                    return load_tok_tile(stg, tt)

                def cat0(c, tt):
                    return hb[:, c, tsl(tt)] if c < 4 else catc[:, c - 4, tsl(tt)]

                def cat0d(c, tt):
                    return hbd[c][tt] if c < 4 else catcd[c - 4][tt]

                out_proj(0, ev_w_out, cat0, cat0d, range(NTT), xold0, C0)
                g.barrier()

        def moe_layer(l, tiles):
            x = xstate["x"]
            xd = xstate["d"]
            subs = []
            for tt in tiles:
                for sub in range(TN[tt] // 128):
                    subs.append((TOFF[tt] // 128 + sub, tt, sub))
            hbTok = hb[:].rearrange("p c t -> p (c t)").rearrange("p (i f) -> p i f", f=D)
            hbtokd = [g.dep() for _ in range(NKT)]
            ML = ExitStack()
            with ML:
                GK = sb(ML, "GK", [128, NKT, 4], F32)
                GKd = g.dep()
                DD = ExitStack()
                with DD:
                    nb = norm_bufs(DD)
                    h32 = sb(DD, "h32", [128, NCH, 512], F32)
                    h32d = g.dep()
                    hbt = sb(DD, "hbt", [128, NCH, 512], BF16)
                    hbtd = g.dep()
                    wrt = sb(DD, "wrt", [128, NCH, NE], F32)
                    brt = sb(DD, "brt", [1, NE], F32)
                    b2n = sb(DD, "b2n", [NE, D], F32)
                    rtd = g.dep(dma=True)
                    g.dma("sp", wrt[:], w_router[l].rearrange("(c p) e -> p c e", p=128), writes=[rtd])
                    g.dma("sp", brt[:], b_router[l:l + 1, :], writes=[rtd])
                    g.dma("sp", b2n[:], moe_b2[l], writes=[rtd])
                    gT = sb(DD, "gT", [NE, T], F32)
                    gTd = g.dep()
                    carry = sb(DD, "carry", [128, NE], F32)
                    card = g.dep()
                    g.op("dve", lambda: nc.vector.memset(carry[:], 0.0), writes=[card])
                    oob = sb(DD, "oob", [128, NE * T // 128], I32)
                    oobd = g.dep()
                    g.op("pool", lambda: nc.gpsimd.memset(oob[:], 2000000000), writes=[oobd])
                    g.dma("sp", idx_d[:, :].rearrange("(p r) o -> p (r o)", p=128), oob[:], reads=[oobd], writes=[idx_dep])
                    lg = sb(DD, "lg", [128, NE], F32)
                    ex = sb(DD, "ex", [128, NE], F32)
                    mk = sb(DD, "mk", [128, NE], F32)
                    gt_ = sb(DD, "gt_", [128, NE], F32)
                    ngd = sb(DD, "ngd", [128, NE], F32)
                    t8 = sb(DD, "t8", [128, 8], F32)
                    t8b = sb(DD, "t8b", [128, 8], F32)
                    sm = sb(DD, "sm", [128, 4], F32)
                    fli = sb(DD, "fli", [128, NE], I32)
                    NIX = 3
                    idx4 = [sb(DD, "idx4_%d" % i, [128, 4], I32) for i in range(NIX)]
                    val4 = [sb(DD, "val4_%d" % i, [128, 4], I32) for i in range(NIX)]
                    ixd = [g.dep() for _ in range(NIX)]
                    rd = g.dep()
                    mkd = g.dep()
                    ixc = 0
                    for tt in tiles:
                        n = TN[tt]
                        rmsnorm_tile(nb, lambda c: x[:, c, tsl(tt)], lambda c: xd[c][tt], tt, "gs2", l, 3, out32=h32, out32d=h32d,
                                     hbout=lambda c: (hbt[:, c, 0:n], hbtd))
                        for sub in range(n // 128):
                            t0 = TOFF[tt] + sub * 128
                            i = t0 // 128
                            psb = ps[6][:, :].bitcast(BF16)
                            for c in range(NCH):
                                g.op("pe", lambda: nc.tensor.transpose(out=psb[:, c * 128:(c + 1) * 128], in_=hbt[:, c, sub * 128:(sub + 1) * 128], identity=identb[:]),
                                     reads=[hbtd, cdep2], writes=[psd[6]])
                            g.op("act", lambda: nc.scalar.copy(out=hbTok[:, i, :], in_=psb), reads=[psd[6]], writes=[hbtokd[i]])
                            pb = 0
                            for c in range(NCH):
                                g.op("pe", lambda: nc.tensor.matmul(ps[pb][:, 0:NE], lhsT=h32[:, c, sub * 128:(sub + 1) * 128], rhs=wrt[:, c, :],
                                                                    start=(c == 0), stop=False),
                                     reads=[h32d, rtd], writes=[psd[pb]])
                            g.op("pe", lambda: nc.tensor.matmul(ps[pb][:, 0:NE], lhsT=ones32[0:1, :], rhs=brt[0:1, :], start=False, stop=True),
                                 reads=[rtd, onesd], writes=[psd[pb]])
                            g.op("act", lambda: nc.scalar.copy(out=lg[:], in_=ps[pb][:, 0:NE]), reads=[psd[pb]], writes=[rd])
                            g.op("dve", lambda: nc.vector.max(out=t8[:], in_=lg[:]), reads=[rd], writes=[rd])
                            g.op("dve", lambda: nc.vector.tensor_scalar(out=sm[:, 0:1], in0=t8[:, 0:1], scalar1=-1.0, scalar2=None, op0=ALU.mult),
                                 reads=[rd], writes=[rd])
                            g.op("act", lambda: nc.scalar.activation(out=ex[:], in_=lg[:], func=AF.Exp, bias=sm[:, 0:1]), reads=[rd], writes=[rd])
                            g.op("dve", lambda: nc.vector.tensor_scalar(out=mk[:], in0=lg[:], scalar1=t8[:, 3:4], scalar2=None, op0=ALU.is_ge),
                                 reads=[rd, mkd], writes=[rd, mkd])
                            g.op("dve", lambda: nc.vector.tensor_tensor(out=ex[:], in0=ex[:], in1=mk[:], op=ALU.mult), reads=[rd], writes=[rd])
                            g.op("dve", lambda: nc.vector.tensor_reduce(out=sm[:, 1:2], in_=ex[:], axis=AX.X, op=ALU.add), reads=[rd], writes=[rd])
                            g.op("dve", lambda: nc.vector.reciprocal(out=sm[:, 2:3], in_=sm[:, 1:2]), reads=[rd], writes=[rd])
                            g.op("dve", lambda: nc.vector.tensor_scalar(out=gt_[:], in0=ex[:], scalar1=sm[:, 2:3], scalar2=None, op0=ALU.mult),
                                 reads=[rd], writes=[rd])
                            g.op("pe", lambda: nc.tensor.transpose(out=ps[1][0:NE, 0:128], in_=gt_[:], identity=ident[:]),
                                 reads=[rd, cdep], writes=[psd[1]])
                            g.op("act", lambda: nc.scalar.copy(out=gT[:, t0:t0 + 128], in_=ps[1][0:NE, 0:128]), reads=[psd[1]], writes=[gTd])
                            g.op("pe", lambda: nc.tensor.matmul(ps[2][:, 0:NE], lhsT=utri[:], rhs=mk[:], start=True, stop=True),
                                 reads=[mkd, cdep2], writes=[psd[2]])
                            g.op("pe", lambda: nc.tensor.matmul(ps[3][:, 0:NE], lhsT=ones32[:], rhs=mk[:], start=True, stop=True),
                                 reads=[mkd, onesd], writes=[psd[3]])
                            g.op("dve", lambda: nc.vector.tensor_tensor(out=ex[:], in0=ps[2][:, 0:NE], in1=carry[:], op=ALU.add), reads=[psd[2], card, rd], writes=[rd])
                            g.op("dve", lambda: nc.vector.tensor_tensor(out=ex[:], in0=ex[:], in1=offrow[:], op=ALU.add), reads=[rd, cdep2], writes=[rd])
                            g.op("dve", lambda: nc.vector.tensor_tensor(out=ex[:], in0=ex[:], in1=mk[:], op=ALU.mult), reads=[rd], writes=[rd])
                            g.op("dve", lambda: nc.vector.tensor_scalar(out=ngd[:], in0=mk[:], scalar1=1.0e6, scalar2=-1.0e6, op0=ALU.mult, op1=ALU.add), reads=[rd], writes=[rd])
                            g.op("dve", lambda: nc.vector.tensor_tensor(out=ngd[:], in0=ngd[:], in1=ex[:], op=ALU.subtract), reads=[rd], writes=[rd])
                            g.op("dve", lambda: nc.vector.tensor_tensor(out=carry[:], in0=ps[3][:, 0:NE], in1=carry[:], op=ALU.add), reads=[psd[3], rd], writes=[card])
                            g.op("dve", lambda: nc.vector.max(out=t8b[:], in_=ngd[:]), reads=[rd], writes=[rd])
                            si = ixc % NIX
                            ixc += 1
                            g.op("dve", lambda: nc.vector.tensor_scalar(out=idx4[si][:], in0=t8b[:, 0:4], scalar1=-1.0, scalar2=None, op0=ALU.mult), reads=[rd], writes=[ixd[si]])
                            g.op("dve", lambda: nc.vector.tensor_scalar(out=val4[si][:], in0=tok4f[:], scalar1=float(4 * t0), scalar2=None, op0=ALU.add), reads=[cdep2], writes=[ixd[si]])
                            for k in range(4):
                                g.op("dve", lambda: nc.vector.scalar_tensor_tensor(out=ex[:], in0=ngd[:], scalar=t8b[:, k:k + 1], in1=gt_[:], op0=ALU.is_equal, op1=ALU.mult,
                                                                                   accum_out=GK[:, i, k:k + 1]), reads=[rd], writes=[rd, GKd])
                            for k in range(4):
                                g.idma(hs_d[:, :], idx4[si][:, k:k + 1], hbTok[:, i, :], bnd_hs, reads=[ixd[si], hbtokd[i]], writes=[hs_dep])
                                g.idma(idx_d[:, :], idx4[si][:, k:k + 1], val4[si][:, k:k + 1], bnd_hs, reads=[ixd[si]], writes=[idx_dep])
                    g.op("dve", lambda: nc.vector.tensor_copy(out=fli[:], in_=carry[:]), reads=[card, rd], writes=[rd])
                    g.dma("sp", cnt_d[l:l + 1, :], fli[0:1, :], reads=[rd], writes=[flag_dep[l]])
                    it = 0
                    for tt in tiles:
                        n = TN[tt]
                        for oc in range(NCH):
                            pb = 2 + it % 2
                            it += 1
                            g.op("pe", lambda: nc.tensor.matmul(ps[pb][:, 0:n], lhsT=b2n[:, oc * 128:(oc + 1) * 128], rhs=gT[:, tsl(tt)], start=True, stop=True),
                                 reads=[rtd, gTd], writes=[psd[pb]])
                            g.op("dve", lambda: nc.vector.scalar_tensor_tensor(out=x[:, oc, tsl(tt)], in0=ps[pb][:, 0:n], scalar=mod(l, 5, oc, 1 if tt == 0 else 0),
                                                                               in1=x[:, oc, tsl(tt)], op0=ALU.mult, op1=ALU.add),
                                 reads=[psd[pb], modd], writes=[xd[oc][tt]])
                    g.barrier()
                EE = ExitStack()
                with EE:
                    NSLOT = 4
                    hbflat = hb[:].rearrange("p c t -> p (c t)")
                    ring = [hbflat[:, k * 4096:(k + 1) * 4096].rearrange("p (c f) -> p c f", f=512) for k in range(NSLOT)]
                    ringd = [g.dep(dma=True) for _ in range(NSLOT)]
                    pf = [sb(EE, "pf%d" % i, [128, NCH, 512], BF16) for i in range(4)]
                    pfd = [g.dep(dma=True) for _ in range(4)]
                    hgToks = [sb(EE, "hgTok%d" % i, [128, 3, D], BF16) for i in range(3)]
                    hgds = [g.dep(dma=True) for _ in range(3)]
                    idxts = [sb(EE, "idxt%d" % i, [128, 3, 1], I32) for i in range(3)]
                    idxtds = [g.dep(dma=True) for _ in range(3)]

                    def gather_load(e, c0, hsel):
                        r0 = e * T + c0 * CAP
                        g.dma("sp", hgToks[hsel][:], hs_d[r0:r0 + CAP, :].rearrange("(j p) f -> p j f", p=128), reads=[hs_dep], writes=[hgds[hsel]])
                        for j3 in range(3):
                            g.dma("sp", idxts[hsel][:, j3, :], idx_d[r0 + j3 * 128:r0 + (j3 + 1) * 128, :], reads=[idx_dep], writes=[idxtds[hsel]])
                    hbg = sb(EE, "hbg", [128, NCH, CAP], BF16)
                    hbgd = [g.dep() for _ in range(NCH)]
                    actT = sb(EE, "actT", [128, NCH, CAP], BF16)
                    actd = [g.dep() for _ in range(NCH)]
                    yTok = sb(EE, "yTok", [128, 3, D], F32)
                    yTd = [g.dep() for _ in range(3)]
                    At = [sb(EE, "mA%d" % i, [128, CAP], F32) for i in range(2)]
                    St = [sb(EE, "mS%d" % i, [128, CAP], F32) for i in range(2)]
                    Lt = [sb(EE, "mL%d" % i, [128, CAP], F32) for i in range(2)]
                    Ad = [g.dep(), g.dep()]
                    Sdp = [g.dep(), g.dep()]
                    Ld = [g.dep(), g.dep()]
                    rc = [0]
                    ec = [0]
                    pc_ = [0]

                    def wsrc(ap):
                        return ap.rearrange("(c p) f -> p c f", p=128)

                    def expert_loads(e):
                        g.dma("pool", ring[0][:], wsrc(moe_w1[l, e, :, 512:1024]), writes=[ringd[0]])
                        g.dma("pool", ring[1][:], wsrc(moe_w1[l, e, :, D + 512:D + 1024]), writes=[ringd[1]])
                        g.dma("pool", ring[2][:], wsrc(moe_w2[l, e, :, 0:512]), writes=[ringd[2]])
                        g.dma("pool", ring[3][:], wsrc(moe_w2[l, e, :, 512:1024]), writes=[ringd[3]])

                    def prefetchA(e):
                        par = (e % 2) * 2
                        g.dma("pool", pf[par][:], wsrc(moe_w1[l, e, :, 0:512]), writes=[pfd[par]])
                        g.dma("pool", pf[par + 1][:], wsrc(moe_w1[l, e, :, D:D + 512]), writes=[pfd[par + 1]])

                    def sparse_chunk(e, c0):
                        b1o = voff["b1"] + (l * NE + e) * 16
                        r0 = e * T + c0 * CAP
                        if c0 == 0:
                            hsel = e % 2
                        else:
                            hsel = 2
                            gather_load(e, c0, hsel)
                        hgTok, hgd, idxt, idxtd = hgToks[hsel], hgds[hsel], idxts[hsel], idxtds[hsel]
                        par = (e % 2) * 2
                        w1s = {0: ((pf[par], pfd[par]), (pf[par + 1], pfd[par + 1])), 1: ((ring[0], ringd[0]), (ring[1], ringd[1]))}
                        w2s = [(ring[2], ringd[2]), (ring[3], ringd[3])]
                        for c in range(NCH):
                            pb = 6 + pc_[0] % 2
                            pc_[0] += 1
                            psb = ps[pb][:, :].bitcast(BF16)
                            for j3 in range(3):
                                g.op("pe", lambda: nc.tensor.transpose(out=psb[:, j3 * 128:(j3 + 1) * 128], in_=hgTok[:, j3, c * 128:(c + 1) * 128], identity=identb[:]),
                                     reads=[hgd, cdep2], writes=[psd[pb]])
                            if c % 2 == 0:
                                g.op("act", lambda: nc.scalar.copy(out=hbg[:, c, :], in_=psb[:, 0:CAP]), reads=[psd[pb]], writes=[hbgd[c]])
                            else:
                                g.op("dve", lambda: nc.vector.tensor_copy(out=hbg[:, c, :], in_=psb[:, 0:CAP]), reads=[psd[pb]], writes=[hbgd[c]])
                        pend = []
                        for u in range(2):
                            (sgA, sgD), (slA, slD) = w1s[u]
                            for jj in range(4):
                                j = u * 4 + jj
                                s = ec[0] % 2
                                s3 = ec[0] % 3
                                ec[0] += 1
                                pg = s3
                                pl = 3 + s3
                                for c in range(NCH):
                                    g.op("pe", lambda: nc.tensor.matmul(ps[pg][:, 0:CAP], lhsT=sgA[:, c, jj * 128:(jj + 1) * 128], rhs=hbg[:, c, :],
                                                                        start=(c == 0), stop=(c == NCH - 1)),
                                         reads=[sgD, hbgd[c]], writes=[psd[pg]])
                                for c in range(NCH):
                                    g.op("pe", lambda: nc.tensor.matmul(ps[pl][:, 0:CAP], lhsT=slA[:, c, jj * 128:(jj + 1) * 128], rhs=hbg[:, c, :],
                                                                        start=(c == 0), stop=(c == NCH - 1)),
                                         reads=[slD, hbgd[c]], writes=[psd[pl]])
                                g.op("dve", lambda: nc.vector.tensor_scalar(out=At[s][:], in0=ps[pg][:, 0:CAP], scalar1=V[:, b1o + j:b1o + j + 1], scalar2=7.0,
                                                                            op0=ALU.add, op1=ALU.min), reads=[psd[pg], Vd], writes=[Ad[s]])
                                g.op("act", lambda: nc.scalar.activation(out=St[s][:], in_=At[s][:], func=AF.Sigmoid, scale=1.702),
                                     reads=[Ad[s]], writes=[Sdp[s]])
                                g.op("dve", lambda: nc.vector.tensor_scalar(out=Lt[s][:], in0=ps[pl][:, 0:CAP], scalar1=V[:, b1o + 8 + j:b1o + 8 + j + 1], scalar2=-6.0,
                                                                            op0=ALU.add, op1=ALU.max), reads=[psd[pl], Vd], writes=[Ld[s]])
                                if pend:
                                    pend.pop()()

                                def _fin(s=s, j=j):
                                    g.op("dve", lambda: nc.vector.tensor_tensor(out=St[s][:], in0=At[s][:], in1=St[s][:], op=ALU.mult),
                                         reads=[Ad[s], Sdp[s]], writes=[Sdp[s]])
                                    g.op("dve", lambda: nc.vector.scalar_tensor_tensor(out=actT[:, j, :], in0=Lt[s][:], scalar=8.0, in1=St[s][:],
                                                                                       op0=ALU.min, op1=ALU.mult), reads=[Ld[s], Sdp[s]], writes=[actd[j]])
                                pend.append(_fin)
                        if pend:
                            pend.pop()()
                        for j3 in range(3):
                            for hh in range(2):
                                swA, swD = w2s[hh]
                                pb = 6 + pc_[0] % 2
                                pc_[0] += 1
                                for fc in range(NCH):
                                    g.op("pe", lambda: nc.tensor.matmul(ps[pb][:, :], lhsT=actT[:, fc, j3 * 128:(j3 + 1) * 128], rhs=swA[:, fc, :],
                                                                        start=(fc == 0), stop=(fc == NCH - 1)),
                                         reads=[swD, actd[fc]], writes=[psd[pb]])
                                g.op("act", lambda: nc.scalar.copy(out=yTok[:, j3, hh * 512:(hh + 1) * 512], in_=ps[pb][:, :]), reads=[psd[pb]], writes=[yTd[j3]])
                            g.idma(y4_d[:, :], idxt[:, j3, :], yTok[:, j3, :], bnd_y4, reads=[idxtd, yTd[j3]], writes=[y4_dep])

                    prefetchA(0)
                    gather_load(0, 0, 0)
                    for e in range(NE):
                        expert_loads(e)
                        if e + 1 < NE:
                            prefetchA(e + 1)
                            gather_load(e + 1, 0, (e + 1) % 2)
                        for reg in flag_regs:
                            ek = ENG_KEY[reg.engine]
                            g._wait(ek, [(flag_dep[l].sem, flag_dep[l].tot)])
                            g.eng[ek].reg_load(reg, cnt_d[l:l + 1, e:e + 1])
                        for c0 in range(NCHUNK):
                            snap = g.snapshot()
                            with nc.If_cmp(flag_regs, c0 * CAP, "IS_GT"):
                                sparse_chunk(e, c0)
                            big = g.snapshot()
                            g.restore(snap)
                            with nc.Else():
                                g.pad_to(big)
                            g.restore(big)
                            g.known = {k: dict(v) for k, v in snap[2].items()}
                    g.barrier()
                CB = ExitStack()
                with CB:
                    y4t = [sb(CB, "y4t%d" % i, [128, 4, D], F32) for i in range(2)]
                    y4td = [g.dep(dma=True) for _ in range(2)]
                    acc = [sb(CB, "cacc%d" % i, [128, D], F32) for i in range(2)]
                    accd = [g.dep(), g.dep()]
                    for n_, (i, tt, sub) in enumerate(subs):
                        s = n_ % 2
                        t0 = i * 128
                        cls = 1 if tt == 0 else 0
                        g.dma("sp", y4t[s][:], y4_d[4 * t0:4 * t0 + 512, :].rearrange("(p k) f -> p k f", k=4), reads=[y4_dep], writes=[y4td[s]])
                        g.op("dve", lambda: nc.vector.tensor_scalar(out=acc[s][:], in0=y4t[s][:, 0, :], scalar1=GK[:, i, 0:1], scalar2=None, op0=ALU.mult),
                             reads=[y4td[s], GKd], writes=[accd[s]])
                        for k in range(1, 4):
                            g.op("dve", lambda: nc.vector.scalar_tensor_tensor(out=acc[s][:], in0=y4t[s][:, k, :], scalar=GK[:, i, k:k + 1], in1=acc[s][:],
                                                                               op0=ALU.mult, op1=ALU.add), reads=[y4td[s], GKd, accd[s]], writes=[accd[s]])
                        for half in range(2):
                            pbt = 2 * s + half
                            for cc in range(4):
                                c = half * 4 + cc
                                g.op("pe", lambda: nc.tensor.transpose(out=ps[pbt][:, cc * 128:(cc + 1) * 128], in_=acc[s][:, c * 128:(c + 1) * 128], identity=ident[:]),
                                     reads=[accd[s], cdep], writes=[psd[pbt]])
                            for cc in range(4):
                                c = half * 4 + cc
                                g.op("dve", lambda: nc.vector.scalar_tensor_tensor(out=x[:, c, t0:t0 + 128], in0=ps[pbt][:, cc * 128:(cc + 1) * 128], scalar=mod(l, 5, c, cls),
                                                                                   in1=x[:, c, t0:t0 + 128], op0=ALU.mult, op1=ALU.add),
                                     reads=[psd[pbt], modd], writes=[xd[c][tt]])
                    g.barrier()

        moe_layer(0, list(range(NTT)))

        if dbg == "x1":
            for c in range(NCH):
                g.dma("sp", dbg_d[:, c, :], xstate["x"][:, c, :], reads=[xstate["d"][c][tt] for tt in range(NTT)], writes=[out_dep])

        LAT = [1, 2, 3, 4]
        L1 = ExitStack()
        with L1:
            cat1 = sb(L1, "cat1", [128, NCH, TL], BF16)
            cat1d = [[g.dep() for _ in range(NTT)] for _ in range(NCH)]
            x = xstate["x"]
            xd = xstate["d"]
            A1 = ExitStack()
            with A1:
                nb = norm_bufs(A1)
                for tt in range(NTT):
                    rmsnorm_tile(nb, lambda c: x[:, c, tsl(tt)], lambda c: xd[c][tt], tt, "gs1", 1, 0)
                for c in range(NCH):
                    g.dma("sp", xs_d[:, c, :], x[:, c, :], reads=[xd[c][tt] for tt in range(NTT)], writes=[xs_dep])
                g.barrier()
            xes.close()
            M1 = ExitStack()
            with M1:
                gw = sb(M1, "gw", [128, 16, 2, 256], BF16)
                gwd = g.dep(dma=True)
                for gi_, src in ((0, od_ga), (1, od_gx)):
                    for d_ in range(2):
                        g.dma("pool", gw[:, gi_ * 8 + d_ * 4:gi_ * 8 + d_ * 4 + 4, :, :],
                              src[d_].rearrange("b (c p) o -> p b c o", p=128), writes=[gwd])
                wr1 = [sb(M1, "w1r%d" % i, [128, NCH, 512], BF16) for i in range(2)]
                wr1d = [g.dep(dma=True) for _ in range(2)]
                ug = sb(M1, "ug", [128, 2, T], F32)
                ugd = [g.dep(), g.dep()]
                xc = sb(M1, "xc", [128, 2, T], F32)
                xcd = [g.dep(), g.dep()]
                xcb = sb(M1, "xcb", [128, 2, T], BF16)
                xcbd = [g.dep(), g.dep()]
                rec = sb(M1, "rec", [128, 2, T], F32)
                recd = [[g.dep() for _ in range(NTT)] for _ in range(2)]
                NT_ = 9
                tb = [[sb(M1, "tb%d_%d" % (k, i), [128, 512], F32) for i in range(2 if k < 7 else 1)] for k in range(NT_)]
                tb[7].append(tb[7][0])
                tb[8].append(tb[8][0])
                tbd = [[g.dep(), g.dep()] for _ in range(NT_)]
                tbd[7][1] = tbd[7][0]
                tbd[8][1] = tbd[8][0]
                cnt1 = [0]
                pcnt = [0]
                for blk in range(4):
                    sW = blk % 2
                    g.dma("pool", wr1[sW][:, :, 0:256], od_w_in[:, blk * 256:(blk + 1) * 256].rearrange("(c p) f -> p c f", p=128), writes=[wr1d[sW]])
                    g.dma("pool", wr1[sW][:, :, 256:512], od_w_in[:, D + blk * 256:D + (blk + 1) * 256].rearrange("(c p) f -> p c f", p=128), writes=[wr1d[sW]])
                    for cc in range(2):
                        for tt in range(NTT):
                            n = TN[tt]
                            pb = pcnt[0] % 2
                            pcnt[0] += 1
                            for c in range(NCH):
                                g.op("pe", lambda: nc.tensor.matmul(ps[pb][:, 0:n], lhsT=wr1[sW][:, c, 256 + cc * 128:256 + (cc + 1) * 128], rhs=hb[:, c, tsl(tt)],
                                                                    start=(c == 0), stop=(c == NCH - 1)),
                                     reads=[wr1d[sW], hbd[c][tt]], writes=[psd[pb]])
                            g.op("act", lambda: nc.scalar.copy(out=ug[:, cc, tsl(tt)], in_=ps[pb][:, 0:n]), reads=[psd[pb]], writes=[ugd[cc]])
                    for d_ in range(2):
                        for cc in range(2):
                            ch = blk * 2 + cc
                            wv = lambda k: Vcol("odconvw", (d_ * 4 + k) * 8 + ch)
                            bv = Vcol("odconvb", d_ * 8 + ch)
                            for (a, b) in ((0, TC), (TC, T)):
                                if d_ == 0:
                                    g.op("dve", lambda: nc.vector.tensor_scalar(out=xc[:, cc, a:b], in0=ug[:, cc, a:b], scalar1=wv(3), scalar2=bv, op0=ALU.mult, op1=ALU.add),
                                         reads=[ugd[cc], Vd], writes=[xcd[cc]])
                                    for k in range(3):
                                        sh = 3 - k
                                        g.op("dve", lambda: nc.vector.scalar_tensor_tensor(out=xc[:, cc, a + sh:b], in0=ug[:, cc, a:b - sh], scalar=wv(k), in1=xc[:, cc, a + sh:b],
                                                                                           op0=ALU.mult, op1=ALU.add), reads=[ugd[cc], Vd, xcd[cc]], writes=[xcd[cc]])
                                else:
                                    g.op("dve", lambda: nc.vector.tensor_scalar(out=xc[:, cc, a:b], in0=ug[:, cc, a:b], scalar1=wv(0), scalar2=bv, op0=ALU.mult, op1=ALU.add),
                                         reads=[ugd[cc], Vd], writes=[xcd[cc]])
                                    for k in range(1, 4):
                                        g.op("dve", lambda: nc.vector.scalar_tensor_tensor(out=xc[:, cc, a:b - k], in0=ug[:, cc, a + k:b], scalar=wv(k), in1=xc[:, cc, a:b - k],
                                                                                           op0=ALU.mult, op1=ALU.add), reads=[ugd[cc], Vd, xcd[cc]], writes=[xcd[cc]])
                            g.op("act", lambda: nc.scalar.copy(out=xcb[:, cc, :], in_=xc[:, cc, :]), reads=[xcd[cc]], writes=[xcbd[cc]])
                        order = [0, 1, 2, 3, 4] if d_ == 0 else [0, 4, 3, 2, 1]
                        for oc in range(2):
                            ch = blk * 2 + oc
                            prev = None
                            for tt in order:
                                n = TN[tt]
                                s = cnt1[0] % 2
                                cnt1[0] += 1
                                pa = 2 + s
                                px = 4 + s
                                for gi_, pb in ((0, pa), (1, px)):
                                    for ic in range(2):
                                        g.op("pe", lambda: nc.tensor.matmul(ps[pb][:, 0:n], lhsT=gw[:, gi_ * 8 + d_ * 4 + blk, ic, oc * 128:(oc + 1) * 128], rhs=xcb[:, ic, tsl(tt)],
                                                                            start=(ic == 0), stop=(ic == 1)),
                                             reads=[gwd, xcbd[ic]], writes=[psd[pb]])
                                R, I_, A_, A2, TH, GI, HS = 0, 1, 2, 3, 4, 5, 6
                                c8 = Scol("c8", d_ * 8 + ch)
                                c8x2 = Scol("c8x2", d_ * 8 + ch)
                                g.op("act", lambda: nc.scalar.activation(out=tb[R][s][:, 0:n], in_=ps[pa][:, 0:n], func=AF.Sigmoid, bias=Vcol("odgab", d_ * 8 + ch)),
                                     reads=[psd[pa], Vd], writes=[tbd[R][s]])
                                g.op("act", lambda: nc.scalar.activation(out=tb[I_][s][:, 0:n], in_=ps[px][:, 0:n], func=AF.Sigmoid, bias=Vcol("odgxb", d_ * 8 + ch)),
                                     reads=[psd[px], Vd], writes=[tbd[I_][s]])
                                g.op("act", lambda: nc.scalar.activation(out=tb[A_][s][:, 0:n], in_=tb[R][s][:, 0:n], func=AF.Exp, scale=c8),
                                     reads=[tbd[R][s], Sd], writes=[tbd[A_][s]])
                                g.op("act", lambda: nc.scalar.activation(out=tb[A2][s][:, 0:n], in_=tb[R][s][:, 0:n], func=AF.Exp, scale=c8x2),
                                     reads=[tbd[R][s], Sd], writes=[tbd[A2][s]])
                                g.op("act", lambda: nc.scalar.activation(out=tb[TH][s][:, 0:n], in_=tb[R][s][:, 0:n], func=AF.Tanh, scale=c8),
                                     reads=[tbd[R][s], Sd], writes=[tbd[TH][s]])
                                g.op("dve", lambda: nc.vector.scalar_tensor_tensor(out=tb[A2][s][:, 0:n], in0=tb[A2][s][:, 0:n], scalar=1.0, in1=tb[TH][s][:, 0:n],
                                                                                   op0=ALU.add, op1=ALU.mult), reads=[tbd[A2][s], tbd[TH][s]], writes=[tbd[A2][s]])
                                g.op("act", lambda: nc.scalar.activation(out=tb[A2][s][:, 0:n], in_=tb[A2][s][:, 0:n], func=AF.Sqrt, scale=-1.0),
                                     reads=[tbd[A2][s]], writes=[tbd[A2][s]])
                                g.op("pool", lambda: nc.gpsimd.tensor_tensor(out=tb[GI][s][:, 0:n], in0=tb[I_][s][:, 0:n], in1=xc[:, oc, tsl(tt)], op=ALU.mult),
                                     reads=[tbd[I_][s], xcd[oc]], writes=[tbd[GI][s]])
                                g.op("dve", lambda: nc.vector.tensor_tensor(out=tb[GI][s][:, 0:n], in0=tb[GI][s][:, 0:n], in1=tb[A2][s][:, 0:n], op=ALU.mult),
                                     reads=[tbd[GI][s], tbd[A2][s]], writes=[tbd[GI][s]])
                                if d_ == 0:
                                    dst = rec[:, oc, tsl(tt)]
                                    dstd = recd[oc][tt]
                                    init = 0.0 if prev is None else prev[0][:, TN[prev[2]] - 1:TN[prev[2]]]
                                    g.op("dve", lambda: nc.vector.tensor_tensor_scan(out=dst, data0=tb[A_][s][:, 0:n], data1=tb[GI][s][:, 0:n], initial=init,
                                                                                     op0=ALU.mult, op1=ALU.add),
                                         reads=[tbd[A_][s], tbd[GI][s]] + ([prev[1]] if prev else []), writes=[dstd])
                                    prev = (dst, dstd, tt)
                                else:
                                    dst = tb[HS][s][:, 0:n]
                                    dstd = tbd[HS][s]
                                    init = 0.0 if prev is None else prev[0][:, 0:1]
                                    g.op("dve", lambda: nc.vector.tensor_tensor_scan(out=dst[:, ::-1], data0=tb[A_][s][:, 0:n][:, ::-1], data1=tb[GI][s][:, 0:n][:, ::-1],
                                                                                     initial=init, op0=ALU.mult, op1=ALU.add),
                                         reads=[tbd[A_][s], tbd[GI][s]] + ([prev[1]] if prev else []), writes=[dstd])
                                    prev = (dst, dstd, tt)
                                    if tt != 0:
                                        pgt = 6 + s
                                        for c in range(NCH):
                                            g.op("pe", lambda: nc.tensor.matmul(ps[pgt][:, 0:n], lhsT=wr1[sW][:, c, oc * 128:(oc + 1) * 128], rhs=hb[:, c, tsl(tt)],
                                                                                start=(c == 0), stop=(c == NCH - 1)),
                                                 reads=[wr1d[sW], hbd[c][tt]], writes=[psd[pgt]])
                                        GL, SM = 7, 8
                                        g.op("act", lambda: nc.scalar.activation(out=tb[GL][s][:, 0:n], in_=ps[pgt][:, 0:n], func=AF.Gelu), reads=[psd[pgt]], writes=[tbd[GL][s]])
                                        g.op("pool", lambda: nc.gpsimd.tensor_tensor(out=tb[SM][s][:, 0:n], in0=dst, in1=rec[:, oc, tsl(tt)], op=ALU.add),
                                             reads=[dstd, recd[oc][tt]], writes=[tbd[SM][s]])
                                        g.op("dve", lambda: nc.vector.tensor_tensor(out=cat1[:, ch, TOFF[tt] - TC:TOFF[tt] - TC + n], in0=tb[SM][s][:, 0:n], in1=tb[GL][s][:, 0:n], op=ALU.mult),
                                             reads=[tbd[SM][s], tbd[GL][s]], writes=[cat1d[ch][tt]])
                g.barrier()
            alloc_x()
            C1 = ExitStack()
            with C1:
                xst = [sb(C1, "x1st%d" % i, [128, NCH, 512], F32) for i in range(2)]
                xstd = [g.dep(dma=True), g.dep(dma=True)]
                c1c = [0]

                def xold1(tt):
                    s = c1c[0] % 2
                    c1c[0] += 1
                    for c in range(NCH):
                        g.dma("sp", xst[s][:, c, 0:TN[tt]], xs_d[:, c, tsl(tt)], reads=[xs_dep], writes=[xstd[s]])
                    return xst[s], xstd[s]

                out_proj(1, od_w_out, lambda c, tt: cat1[:, c, TOFF[tt] - TC:TOFF[tt] - TC + TN[tt]], lambda c, tt: cat1d[c][tt], LAT, xold1, C1)
                g.barrier()
        moe_layer(1, LAT)

        FN = ExitStack()
        with FN:
            x = xstate["x"]
            xd = xstate["d"]
            nb = norm_bufs(FN)
            sq, sqd, tmp, tmpd, rs, rsd, cnt = nb
            yb = [sb(FN, "yb%d" % i, [128, NCH, 128], F32) for i in range(2)]
            ybd = [g.dep(), g.dep()]
            ot = [sb(FN, "ot%d" % i, [128, D], F32) for i in range(2)]
            otd = [g.dep(dma=True), g.dep(dma=True)]
            oc_ = [0]
            for tt in LAT:
                n = TN[tt]
                pb = 5
                for c in range(NCH):
                    s = cnt[0] % 2
                    cnt[0] += 1
                    g.op("act", lambda: nc.scalar.activation(out=sq[s][:, 0:n], in_=x[:, c, tsl(tt)], func=AF.Square), reads=[xd[c][tt]], writes=[sqd[s]])
                    g.op("pe", lambda: nc.tensor.matmul(ps[pb][:, 0:n], lhsT=ones32[:], rhs=sq[s][:, 0:n], start=(c == 0), stop=(c == NCH - 1)),
                         reads=[sqd[s], onesd], writes=[psd[pb]])
                g.op("act", lambda: nc.scalar.activation(out=rs[:, 0:n], in_=ps[pb][:, 0:n], func=AF.Sqrt, scale=1.0 / D, bias=EPS), reads=[psd[pb]], writes=[rsd])
                g.op("dve", lambda: nc.vector.reciprocal(out=rs[:, 0:n], in_=rs[:, 0:n]), reads=[rsd], writes=[rsd])
                for sub in range(n // 128):
                    s = oc_[0] % 2
                    oc_[0] += 1
                    for c in range(NCH):
                        g.op("dve", lambda: nc.vector.scalar_tensor_tensor(out=yb[s][:, c, :], in0=x[:, c, TOFF[tt] + sub * 128:TOFF[tt] + (sub + 1) * 128],
                                                                           scalar=Vcol("fnorm", c), in1=rs[:, sub * 128:(sub + 1) * 128], op0=ALU.mult, op1=ALU.mult),
                             reads=[xd[c][tt], rsd, Vd], writes=[ybd[s]])
                    for half in range(2):
                        pbt = 6 + half
                        for cc in range(4):
                            c = half * 4 + cc
                            g.op("pe", lambda: nc.tensor.transpose(out=ps[pbt][:, cc * 128:(cc + 1) * 128], in_=yb[s][:, c, :], identity=ident[:]),
                                 reads=[ybd[s], cdep], writes=[psd[pbt]])
                        if half == 0:
                            g.op("act", lambda: nc.scalar.copy(out=ot[s][:, 0:512], in_=ps[pbt][:, :]), reads=[psd[pbt]], writes=[otd[s]])
                        else:
                            g.op("dve", lambda: nc.vector.tensor_copy(out=ot[s][:, 512:1024], in_=ps[pbt][:, :]), reads=[psd[pbt]], writes=[otd[s]])
                    r0 = TOFF[tt] - TC + sub * 128
                    g.dma("sp", out_d[r0:r0 + 128, :], ot[s][:], reads=[otd[s]], writes=[out_dep])
            g.barrier()
        xes.close()
    return nc


def _consts():
    ident = np.eye(128, dtype=np.float32)
    perm = np.zeros((128, 128), np.float32)
    for m in range(128):
        blk = m // 16
        partner = (blk ^ 1) * 16 + (m % 16)
        perm[partner, m] = 1.0
    n_rows = TL // 64
    rows, cols = np.meshgrid(np.arange(n_rows), np.arange(64), indexing="ij")
    pos = np.stack([rows.reshape(-1), cols.reshape(-1)], axis=-1).astype(np.float32)
    inv = (np.float32(10000.0) ** (-np.arange(16, dtype=np.float32) / np.float32(16))).astype(np.float32)
    ang = (pos[:, :, None] * inv).astype(np.float32)
    cos = np.cos(ang).astype(np.float32)
    sin = np.sin(ang).astype(np.float32)
    cosT = np.ones((128, T), np.float32)
    sinT = np.zeros((128, T), np.float32)
    for p in range(128):
        dd = p % 64
        axis = dd // 32
        half = (dd % 32) // 16
        f = dd % 16
        cosT[p, TC:] = cos[:, axis, f]
        sinT[p, TC:] = (-sin[:, axis, f]) if half == 0 else sin[:, axis, f]
    return ident, perm, cosT, sinT


def _consts2(NE):
    utri = np.triu(np.ones((128, 128), np.float32), k=1)
    offrow = np.tile((np.arange(NE, dtype=np.float32) * T)[None, :], (128, 1))
    tok4f = (4.0 * np.arange(128, dtype=np.float32)[:, None] + np.arange(4, dtype=np.float32)[None, :]).astype(np.float32)
    return utri, offrow, tok4f


def make_in_maps(inp, NE, ncores):
    voff, vrows, vpad = vec_layout(NE)
    ident, perm, cosT, sinT = _consts()
    f = lambda a: np.ascontiguousarray(np.asarray(a, dtype=np.float32))
    shared = {
        "w_mod": f(inp["w_mod"]), "ev_w_in": f(inp["ev_w_in"][0]), "ev_w_out": f(inp["ev_w_out"][0]),
        "od_w_in": f(inp["od_w_in"][0]), "od_w_out": f(inp["od_w_out"][0]),
        "od_ga": f(inp["od_gate_a_w"][0]), "od_gx": f(inp["od_gate_x_w"][0]),
        "w_router": f(inp["moe_w_router"]), "b_router": f(inp["moe_b_router"]),
        "moe_w1": f(inp["moe_w1"]), "moe_w2": f(inp["moe_w2"]), "moe_b2": f(inp["moe_b2"]),
        "lams": f(np.stack([inp["ev_lambda_q1"][0], inp["ev_lambda_k1"][0], inp["ev_lambda_q2"][0], inp["ev_lambda_k2"][0]])),
        "ident": ident, "perm": perm, "cosT": cosT, "sinT": sinT,
    }
    shared["utri"], shared["offrow"], shared["tok4f"] = _consts2(NE)
    maps = []
    for b in range(ncores):
        rows = [
            f(inp["c"][b]).reshape(8, 128), f(inp["c_ctx"]).reshape(8, 128), f(inp["b_mod"]).reshape(96, 128),
            f(inp["norm_mix"]).reshape(16, 128), f(inp["norm_ffn"]).reshape(16, 128), f(inp["final_norm"]).reshape(8, 128),
            f(inp["ev_conv_w"][0]).reshape(12, 128), f(inp["ev_subln"][0]).reshape(1, 128),
            f(inp["od_conv_w"][0]).reshape(64, 128), f(inp["od_conv_b"][0]).reshape(16, 128),
            f(inp["od_gate_a_b"][0]).reshape(16, 128), f(inp["od_gate_x_b"][0]).reshape(16, 128),
            f(inp["od_lru_lambda"][0]).reshape(16, 128), f(inp["moe_b1"]).reshape(2 * NE * 16, 128),
        ]
        v = np.concatenate(rows, axis=0)
        assert v.shape[0] == vrows
        vp = np.zeros((vpad, 128), np.float32)
        vp[:vrows] = v
        m = dict(shared)
        m["xin"] = f(inp["x"][b])
        m["ctxin"] = f(inp["ctx"][b])
        m["vecs"] = vp
        maps.append(m)
    return maps


def kernel(**inputs):
    NE = inputs["moe_w1"].shape[1]
    B = inputs["x"].shape[0]
    nc = build(NE)
    maps = make_in_maps(inputs, NE, B)
    res = run_bass_kernel_spmd(nc, maps, core_ids=list(range(B)))
    return np.stack([np.asarray(r["out"], dtype=np.float32) for r in res.results], axis=0)
```

```python
import math
from contextlib import ExitStack

import numpy as np
import concourse.bass as bass
import concourse.mybir as mybir
from concourse.bass_utils import run_bass_kernel_spmd

F32 = mybir.dt.float32
F32R = mybir.dt.float32r
BF16 = mybir.dt.bfloat16
AF = mybir.ActivationFunctionType
ALU = mybir.AluOpType
AX = mybir.AxisListType

D = 1024
NCH = 8
TC = 256
TL = 2048
T = TC + TL
TOFF = [0, 256, 768, 1280, 1792]
TN = [256, 512, 512, 512, 512]
NTT = 5
NKT = T // 128
EPS = 1e-6
SELF_SYNC = True
CAP = 384
NCHUNK = (T + CAP - 1) // CAP
I32 = mybir.dt.int32
ET = mybir.EngineType
ENG_KEY = {ET.PE: "pe", ET.Activation: "act", ET.DVE: "dve", ET.Pool: "pool", ET.SP: "sp"}


def tsl(tt):
    return slice(TOFF[tt], TOFF[tt] + TN[tt])


def vec_layout(NE):
    ents = [("c", 8), ("cctx", 8), ("bmod", 96), ("nmix", 16), ("nffn", 16), ("fnorm", 8), ("evconv", 12),
            ("subln", 1), ("odconvw", 64), ("odconvb", 16), ("odgab", 16), ("odgxb", 16), ("odlam", 16),
            ("b1", 2 * NE * 16)]
    off = {}
    r = 0
    for name, n in ents:
        off[name] = r
        r += n
    rpad = ((r + 127) // 128) * 128
    return off, r, rpad


class Dep:
    __slots__ = ("w", "r", "sem", "tot")

    def __init__(self):
        self.w = None
        self.r = {}
        self.sem = None
        self.tot = 0


class G:
    def __init__(self, nc, es):
        self.nc = nc
        self.es = es
        self.eng = {"pe": nc.tensor, "act": nc.scalar, "dve": nc.vector, "pool": nc.gpsimd, "sp": nc.sync}
        self.semh = []
        self.esem = {}
        self.cnt = {}
        self.known = {}
        for k in self.eng:
            self.esem[k] = self.new_sem("e_" + k)
            self.cnt[k] = 0
            self.known[k] = {}
        self.dma_sems = []
        self.nsem = 0
        self.alldeps = []

    def new_sem(self, name):
        h = self.es.enter_context(self.nc.semaphore(name))
        self.semh.append(h)
        return len(self.semh) - 1

    def snapshot(self):
        deps = [(d, d.w, dict(d.r), d.tot) for d in self.alldeps]
        return (deps, dict(self.cnt), {k: dict(v) for k, v in self.known.items()})

    def restore(self, snap):
        deps, cnt, known = snap
        for d, w, r, tot in deps:
            d.w = w
            d.r = dict(r)
            d.tot = tot
        self.cnt = dict(cnt)
        self.known = {k: dict(v) for k, v in known.items()}

    def pad_to(self, big):
        deps, cnt, _ = big
        for e in self.eng:
            diff = cnt[e] - self.cnt[e]
            assert diff >= 0, (e, diff)
            if diff > 0:
                self.eng[e].wait_ge(self.semh[self.esem[e]], self.cnt[e])
                self.eng[e].sem_inc(self.semh[self.esem[e]], diff)
                self.cnt[e] = cnt[e]
        for d, w, r, tot in deps:
            if d.sem is None:
                continue
            diff = tot - d.tot
            assert diff >= 0
            if diff > 0:
                self.eng["sp"].wait_ge(self.semh[d.sem], d.tot)
                self.eng["sp"].sem_inc(self.semh[d.sem], diff)
                d.tot = tot

    def dep(self, dma=False):
        d = Dep()
        self.alldeps.append(d)
        if dma:
            self.nsem += 1
            d.sem = self.new_sem("d%d" % self.nsem)
            self.dma_sems.append(d)
        return d

    def _wait(self, e, deps):
        need = {}
        kn = self.known[e]
        for d in deps:
            if d is None:
                continue
            s, v = d
            if s == self.esem[e] and (e == "pe" or not SELF_SYNC):
                continue
            if kn.get(s, 0) >= v:
                continue
            if need.get(s, 0) < v:
                need[s] = v
        for s, v in need.items():
            self.eng[e].wait_ge(self.semh[s], v)
            kn[s] = v

    def op(self, e, fn, reads=(), writes=()):
        deps = []
        for t in reads:
            deps.append(t.w)
        for t in writes:
            deps.append(t.w)
            deps.extend(t.r.items())
        self._wait(e, deps)
        ins = fn()
        self.cnt[e] += 1
        s = self.esem[e]
        ins.then_inc(self.semh[s], 1)
        me = (s, self.cnt[e])
        for t in reads:
            t.r[s] = self.cnt[e]
        for t in writes:
            t.w = me
            t.r = {}
        return ins

    def dma(self, q, out, in_, reads=(), writes=(), sem_dep=None):
        deps = []
        for t in reads:
            deps.append(t.w)
        for t in writes:
            deps.append(t.w)
            deps.extend(t.r.items())
        self._wait(q, deps)
        sd = sem_dep if sem_dep is not None else writes[0]
        assert sd.sem is not None
        ins = self.eng[q].dma_start(out=out, in_=in_)
        sd.tot += 16
        ins.then_inc(self.semh[sd.sem], 16)
        me = (sd.sem, sd.tot)
        for t in reads:
            t.r[sd.sem] = sd.tot
        for t in writes:
            t.w = me
            t.r = {}
        return ins

    def idma(self, out, off_ap, in_, bound, reads=(), writes=()):
        deps = []
        for t in reads:
            deps.append(t.w)
        for t in writes:
            deps.append(t.w)
            deps.extend(t.r.items())
        self._wait("pool", deps)
        sd = writes[0]
        ins = self.nc.gpsimd.indirect_dma_start(out=out, out_offset=bass.IndirectOffsetOnAxis(ap=off_ap, axis=0), in_=in_, in_offset=None,
                                                bounds_check=bound, oob_is_err=False)
        sd.tot += 16
        ins.then_inc(self.semh[sd.sem], 16)
        me = (sd.sem, sd.tot)
        for t in reads:
            t.r[sd.sem] = sd.tot
        for t in writes:
            t.w = me
            t.r = {}
        return ins

    def barrier(self):
        for e in self.eng:
            deps = [(self.esem[o], self.cnt[o]) for o in self.eng if o != e and self.cnt[o] > 0]
            deps += [(d.sem, d.tot) for d in self.dma_sems if d.tot > 0]
            self._wait(e, deps)


def build(NE=32, dbg=None):
    nc = bass.Bass("TRN2", target_bir_lowering=False)
    voff, vrows, vpad = vec_layout(NE)
    NVB = vpad // 128

    def din(name, shape, dt=F32):
        return nc.dram_tensor(name, list(shape), dt, kind="ExternalInput").ap()

    xin = din("xin", [TL, D])
    ctxin = din("ctxin", [TC, D])
    vecs = din("vecs", [vpad, 128])
    w_mod = din("w_mod", [2, D, 6 * D])
    ev_w_in = din("ev_w_in", [D, 3 * D])
    ev_w_out = din("ev_w_out", [D, D])
    od_w_in = din("od_w_in", [D, 2 * D])
    od_w_out = din("od_w_out", [D, D])
    od_ga = din("od_ga", [2, 4, 256, 256])
    od_gx = din("od_gx", [2, 4, 256, 256])
    w_router = din("w_router", [2, D, NE])
    b_router = din("b_router", [2, NE])
    moe_w1 = din("moe_w1", [2, NE, D, 2 * D])
    moe_w2 = din("moe_w2", [2, NE, D, D])
    moe_b2 = din("moe_b2", [2, NE, D])
    lams = din("lams", [4, 64])
    ident_d = din("ident", [128, 128])
    perm_d = din("perm", [128, 128])
    cos_d = din("cosT", [128, T])
    sin_d = din("sinT", [128, T])
    utri_d = din("utri", [128, 128])
    offrow_d = din("offrow", [128, NE])
    tok4f_d = din("tok4f", [128, 4])
    out_d = nc.dram_tensor("out", [TL, D], F32, kind="ExternalOutput").ap()
    xs_d = nc.dram_tensor("xs_scratch", [128, NCH, T], F32, kind="Internal").ap()
    gsc_d = nc.dram_tensor("g_scratch", [2, NE, T], F32, kind="Internal").ap()
    psc_d = nc.dram_tensor("p_scratch", [2, NE, T], F32, kind="Internal").ap()
    cnt_d = nc.dram_tensor("cnt_scratch", [2, NE], I32, kind="Internal").ap()
    hs_d = nc.dram_tensor("hs_scratch", [NE * T, D], BF16, kind="Internal").ap()
    idx_d = nc.dram_tensor("idx_scratch", [NE * T, 1], I32, kind="Internal").ap()
    y4_d = nc.dram_tensor("y4_scratch", [4 * T, D], F32, kind="Internal").ap()
    dbg_d = None
    if dbg is not None:
        dbg_d = nc.dram_tensor("dbg", [128, NCH, T], F32, kind="ExternalOutput").ap()

    es = ExitStack()
    with es:
        g = G(nc, es)
        xs_dep = g.dep(dma=True)
        gsc_dep = [g.dep(dma=True), g.dep(dma=True)]
        psc_dep = [g.dep(dma=True), g.dep(dma=True)]
        flag_dep = [g.dep(dma=True), g.dep(dma=True)]
        hs_dep = g.dep(dma=True)
        bnd_hs = nc.gpsimd.alloc_register("bnd_hs")
        nc.gpsimd.reg_mov(bnd_hs, NE * T - 1)
        bnd_y4 = nc.gpsimd.alloc_register("bnd_y4")
        nc.gpsimd.reg_mov(bnd_y4, 4 * T - 1)
        idx_dep = g.dep(dma=True)
        y4_dep = g.dep(dma=True)
        flag_regs = nc.alloc_registers("ovf", [ET.PE, ET.Activation, ET.DVE, ET.Pool, ET.SP])
        out_dep = g.dep(dma=True)

        _uid = [0]

        def sb(es_, name, shape, dt, side=None):
            _uid[0] += 1
            return es_.enter_context(nc.sbuf_tensor("sb%d_%s" % (_uid[0], name), list(shape), dt, side=side))

        ps = [es.enter_context(nc.psum_tensor("ps%d" % i, [128, 512], F32)) for i in range(8)]
        psd = [g.dep() for _ in range(8)]

        ident = sb(es, "ident", [128, 128], F32)
        perm = sb(es, "perm", [128, 128], F32)
        ones32 = sb(es, "ones32", [128, 128], F32)
        ones16 = sb(es, "ones16", [128, 128], BF16)
        V = sb(es, "V", [128, vpad], F32)
        modT = sb(es, "modT", [128, 2, 48, 2], F32)
        S = sb(es, "S", [128, 160], F32)
        cdep = g.dep(dma=True)
        Vd = g.dep()
        modd = g.dep()
        Sd = g.dep()
        onesd = g.dep()
        g.dma("sp", ident[:], ident_d, writes=[cdep])
        g.dma("sp", perm[:], perm_d, writes=[cdep])
        identb = sb(es, "identb", [128, 128], BF16)
        utri = sb(es, "utri", [128, 128], F32)
        offrow = sb(es, "offrow", [128, NE], F32)
        tok4f = sb(es, "tok4f", [128, 4], F32)
        cdep2 = g.dep(dma=True)
        g.dma("pool", identb[:], ident_d, writes=[cdep2])
        g.dma("sp", utri[:], utri_d, writes=[cdep2])
        g.dma("sp", offrow[:], offrow_d, writes=[cdep2])
        g.dma("sp", tok4f[:], tok4f_d, writes=[cdep2])
        g.op("dve", lambda: nc.vector.memset(ones32[:], 1.0), writes=[onesd])
        g.op("dve", lambda: nc.vector.memset(ones16[:], 1.0), writes=[onesd])

        SC = {}
        _sc = [0]

        def scol(name, n):
            SC[name] = _sc[0]
            _sc[0] += n
            return SC[name]

        for l in range(2):
            for cls in range(2):
                scol("gs1_%d_%d" % (l, cls), 8)
                scol("gs2_%d_%d" % (l, cls), 8)
        scol("sg", 1)
        scol("neglam", 1)
        scol("c8", 16)
        scol("c8x2", 16)
        scol("tmp", 16)
        assert _sc[0] <= 160

        def Scol(name, i=0):
            return S[:, SC[name] + i:SC[name] + i + 1]

        def Vcol(name, i=0):
            return V[:, voff[name] + i:voff[name] + i + 1]

        def mod(l, k, c, cls):
            return modT[:, l, k * 8 + c, cls:cls + 1]

        pes = ExitStack()
        with pes:
            vstg = [sb(pes, "vstg%d" % i, [128, 128], F32) for i in range(2)]
            vstd = [g.dep(dma=True) for _ in range(2)]
            for blk in range(NVB):
                s = blk % 2
                g.dma("sp", vstg[s][:], vecs[blk * 128:(blk + 1) * 128, :], writes=[vstd[s]])
                pb = blk % 2
                g.op("pe", lambda: nc.tensor.transpose(out=ps[pb][:, 0:128], in_=vstg[s][:], identity=ident[:]),
                     reads=[vstd[s], cdep], writes=[psd[pb]])
                g.op("act", lambda: nc.scalar.copy(out=V[:, blk * 128:(blk + 1) * 128], in_=ps[pb][:, 0:128]),
                     reads=[psd[pb]], writes=[Vd])
            b1v = V[:, voff["b1"]:voff["b1"] + 2 * NE * 16].rearrange("p (a j) -> p a j", j=16)
            g.op("dve", lambda: nc.vector.tensor_scalar(out=b1v[:, :, 8:16], in0=b1v[:, :, 8:16], scalar1=1.0,
                                                        scalar2=None, op0=ALU.add), reads=[Vd], writes=[Vd])
            scT = sb(pes, "scT", [128, 8, 2], F32R)
            scd = g.dep()
            g.op("act", lambda: nc.scalar.activation(out=scT[:, :, 0], in_=V[:, voff["c"]:voff["c"] + 8],
                                                     func=AF.Silu), reads=[Vd], writes=[scd])
            g.op("act", lambda: nc.scalar.activation(out=scT[:, :, 1], in_=V[:, voff["cctx"]:voff["cctx"] + 8],
                                                     func=AF.Silu), reads=[Vd], writes=[scd])
            wm = [sb(pes, "wm%d" % i, [128, 8, 512], F32R) for i in range(2)]
            wmd = [g.dep(dma=True) for _ in range(2)]
            it = 0
            for l in range(2):
                for blk in range(12):
                    s = it % 2
                    it += 1
                    src = w_mod[l, :, blk * 512:(blk + 1) * 512].rearrange("(c p) f -> p c f", p=128)
                    g.dma("pool", wm[s][:], src, writes=[wmd[s]])
                    for fcl in range(4):
                        pb = (blk * 4 + fcl) % 2
                        for dc in range(8):
                            g.op("pe", lambda: nc.tensor.matmul(ps[pb][:, 0:2], lhsT=wm[s][:, dc, fcl * 128:(fcl + 1) * 128],
                                                                rhs=scT[:, dc, :], start=(dc == 0), stop=(dc == 7)),
                                 reads=[wmd[s], scd], writes=[psd[pb]])
                        if True:
                            kk = blk * 4 + fcl
                            g.op("dve", lambda: nc.vector.tensor_scalar(out=modT[:, l, kk, :], in0=ps[pb][:, 0:2],
                                                                        scalar1=Vcol("bmod", l * 48 + kk), scalar2=None,
                                                                        op0=ALU.add), reads=[psd[pb], Vd], writes=[modd])
            for l in range(2):
                for cls in range(2):
                    for (nm, kc, vn) in (("gs1", 1, "nmix"), ("gs2", 4, "nffn")):
                        c0 = SC["%s_%d_%d" % (nm, l, cls)]
                        g.op("dve", lambda: nc.vector.scalar_tensor_tensor(
                            out=S[:, c0:c0 + 8], in0=modT[:, l, kc * 8:kc * 8 + 8, cls], scalar=1.0,
                            in1=V[:, voff[vn] + l * 8:voff[vn] + l * 8 + 8], op0=ALU.add, op1=ALU.mult),
                            reads=[modd, Vd], writes=[Sd])
            lam_init0 = 0.8 - 0.6 * math.exp(-0.3 * 0)
            g.op("dve", lambda: nc.vector.tensor_scalar(out=Scol("sg"), in0=Vcol("subln"), scalar1=(1.0 - lam_init0),
                                                        scalar2=None, op0=ALU.mult), reads=[Vd], writes=[Sd])
            lb = sb(pes, "lamb", [128, 4, 64], F32)
            lbd = g.dep(dma=True)
            for i in range(4):
                g.dma("sp", lb[:, i, :], lams[i:i + 1, :].to_broadcast([128, 64]), writes=[lbd])
            lt = sb(pes, "lamt", [128, 2, 64], F32)
            ltd = g.dep()
            g.op("dve", lambda: nc.vector.tensor_tensor(out=lt[:, 0, :], in0=lb[:, 0, :], in1=lb[:, 1, :], op=ALU.mult),
                 reads=[lbd], writes=[ltd])
            g.op("dve", lambda: nc.vector.tensor_tensor(out=lt[:, 1, :], in0=lb[:, 2, :], in1=lb[:, 3, :], op=ALU.mult),
                 reads=[lbd], writes=[ltd])
            tm = SC["tmp"]
            g.op("dve", lambda: nc.vector.tensor_reduce(out=S[:, tm:tm + 2], in_=lt[:], axis=AX.X, op=ALU.add),
                 reads=[ltd], writes=[Sd])
            g.op("act", lambda: nc.scalar.activation(out=S[:, tm + 2:tm + 4], in_=S[:, tm:tm + 2], func=AF.Exp),
                 reads=[Sd], writes=[Sd])
            g.op("dve", lambda: nc.vector.scalar_tensor_tensor(out=Scol("neglam"), in0=S[:, tm + 3:tm + 4],
                                                               scalar=-lam_init0, in1=S[:, tm + 2:tm + 3],
                                                               op0=ALU.add, op1=ALU.subtract), reads=[Sd], writes=[Sd])
            g.op("act", lambda: nc.scalar.activation(out=S[:, tm:tm + 16], in_=V[:, voff["odlam"]:voff["odlam"] + 16],
                                                     func=AF.Exp, scale=-1.0), reads=[Vd, Sd], writes=[Sd])
            g.op("act", lambda: nc.scalar.activation(out=S[:, tm:tm + 16], in_=S[:, tm:tm + 16], func=AF.Ln, bias=1.0),
                 reads=[Sd], writes=[Sd])
            g.op("dve", lambda: nc.vector.tensor_scalar(out=S[:, SC["c8"]:SC["c8"] + 16], in0=S[:, tm:tm + 16],
                                                        scalar1=-8.0, scalar2=None, op0=ALU.mult), reads=[Sd], writes=[Sd])
            g.op("dve", lambda: nc.vector.tensor_scalar(out=S[:, SC["c8x2"]:SC["c8x2"] + 16], in0=S[:, tm:tm + 16],
                                                        scalar1=-16.0, scalar2=None, op0=ALU.mult), reads=[Sd], writes=[Sd])
            g.barrier()
        hb = sb(es, "hb", [128, NCH, T], BF16)
        hbd = [[g.dep() for _ in range(NTT)] for _ in range(NCH)]
        xes = ExitStack()
        xstate = {}

        def alloc_x():
            xstate["x"] = xes.enter_context(nc.sbuf_tensor("xres%d" % len(xstate), [128, NCH, T], F32, side="right"))
            xstate["d"] = [[g.dep() for _ in range(NTT)] for _ in range(NCH)]

        def load_tok_tile(pes_bufs, tt, l0_src=True):
            xst, xstd, tstg, tstgd, cnt = pes_bufs
            s = cnt[0] % 2
            cnt[0] += 1
            n = TN[tt]
            for sub in range(n // 128):
                k = cnt[1] % 2
                cnt[1] += 1
                if tt == 0:
                    src = ctxin[sub * 128:(sub + 1) * 128, :]
                else:
                    r0 = TOFF[tt] - TC + sub * 128
                    src = xin[r0:r0 + 128, :]
                g.dma("sp", tstg[k][:], src, writes=[tstgd[k]])
                for half in range(2):
                    pb = 6 + half
                    for cc in range(4):
                        c = half * 4 + cc
                        g.op("pe", lambda: nc.tensor.transpose(out=ps[pb][:, cc * 128:(cc + 1) * 128],
                                                               in_=tstg[k][:, c * 128:(c + 1) * 128], identity=ident[:]),
                             reads=[tstgd[k], cdep], writes=[psd[pb]])
                    dst = xst[s][:, half * 4:half * 4 + 4, sub * 128:(sub + 1) * 128]
                    srcp = ps[pb][:, :].rearrange("p (c t) -> p c t", t=128)
                    if half == 0:
                        g.op("act", lambda: nc.scalar.copy(out=dst, in_=srcp), reads=[psd[pb]], writes=[xstd[s]])
                    else:
                        g.op("dve", lambda: nc.vector.tensor_copy(out=dst, in_=srcp), reads=[psd[pb]], writes=[xstd[s]])
            return xst[s], xstd[s]

        def rmsnorm_tile(nb, xsrc, xdeps, tt, gsname, l, k_sh, out32=None, out32d=None, hbout=None):
            sq, sqd, tmp, tmpd, rs, rsd, cnt = nb
            n = TN[tt]
            cls = 1 if tt == 0 else 0
            pb = 5
            for c in range(NCH):
                s = cnt[0] % 2
                cnt[0] += 1
                g.op("act", lambda: nc.scalar.activation(out=sq[s][:, 0:n], in_=xsrc(c), func=AF.Square),
                     reads=[xdeps(c)], writes=[sqd[s]])
                g.op("pe", lambda: nc.tensor.matmul(ps[pb][:, 0:n], lhsT=ones32[:], rhs=sq[s][:, 0:n],
                                                    start=(c == 0), stop=(c == NCH - 1)),
                     reads=[sqd[s], onesd], writes=[psd[pb]])
            g.op("act", lambda: nc.scalar.activation(out=rs[:, 0:n], in_=ps[pb][:, 0:n], func=AF.Sqrt,
                                                     scale=1.0 / D, bias=EPS), reads=[psd[pb]], writes=[rsd])
            g.op("dve", lambda: nc.vector.reciprocal(out=rs[:, 0:n], in_=rs[:, 0:n]), reads=[rsd], writes=[rsd])
            c0 = SC["%s_%d_%d" % (gsname, l, cls)]
            for c in range(NCH):
                s = cnt[1] % 2
                cnt[1] += 1
                g.op("dve", lambda: nc.vector.tensor_tensor(out=tmp[s][:, 0:n], in0=xsrc(c), in1=rs[:, 0:n], op=ALU.mult),
                     reads=[xdeps(c), rsd], writes=[tmpd[s]])
                ho, hod = (hb[:, c, tsl(tt)], hbd[c][tt]) if hbout is None else hbout(c)
                g.op("act", lambda: nc.scalar.activation(out=ho, in_=tmp[s][:, 0:n], func=AF.Identity,
                                                         scale=S[:, c0 + c:c0 + c + 1], bias=mod(l, k_sh, c, cls)),
                     reads=[tmpd[s], Sd, modd], writes=[hod])
                if out32 is not None:
                    g.op("dve", lambda: nc.vector.tensor_scalar(out=out32[:, c, 0:n], in0=tmp[s][:, 0:n],
                                                                scalar1=S[:, c0 + c:c0 + c + 1],
                                                                scalar2=mod(l, k_sh, c, cls), op0=ALU.mult, op1=ALU.add),
                         reads=[tmpd[s], Sd, modd], writes=[out32d])

        def norm_bufs(es_):
            sq = [sb(es_, "nsq%d" % i, [128, 512], F32) for i in range(2)]
            tmp = [sb(es_, "ntmp%d" % i, [128, 512], F32) for i in range(2)]
            rs = sb(es_, "nrs", [128, 512], F32)
            return (sq, [g.dep(), g.dep()], tmp, [g.dep(), g.dep()], rs, g.dep(), [0, 0])

        def stage_bufs(es_):
            xst = [sb(es_, "xst%d" % i, [128, NCH, 512], F32) for i in range(2)]
            tstg = [sb(es_, "tstg%d" % i, [128, D], F32) for i in range(2)]
            return (xst, [g.dep(dma=True), g.dep(dma=True)], tstg, [g.dep(dma=True), g.dep(dma=True)], [0, 0])

        def out_proj(l, wout_d, catfn, catdeps, tiles, xold_fn, es_):
            wo = sb(es_, "wo%d" % l, [128, NCH, D], BF16)
            wod = g.dep(dma=True)
            for hh in range(2):
                g.dma("pool", wo[:, :, hh * 512:(hh + 1) * 512],
                      wout_d[:, hh * 512:(hh + 1) * 512].rearrange("(c p) f -> p c f", p=128), writes=[wod])
            x = xstate["x"]
            xd = xstate["d"]
            it = 0
            for tt in tiles:
                n = TN[tt]
                cls = 1 if tt == 0 else 0
                xo, xod = xold_fn(tt)
                for oc in range(NCH):
                    pb = it % 2
                    it += 1
                    for c in range(NCH):
                        g.op("pe", lambda: nc.tensor.matmul(ps[pb][:, 0:n], lhsT=wo[:, c, oc * 128:(oc + 1) * 128],
                                                            rhs=catfn(c, tt), start=(c == 0), stop=(c == NCH - 1)),
                             reads=[wod, catdeps(c, tt)], writes=[psd[pb]])
                    g.op("dve", lambda: nc.vector.scalar_tensor_tensor(out=x[:, oc, tsl(tt)], in0=ps[pb][:, 0:n],
                                                                       scalar=mod(l, 2, oc, cls), in1=xo[:, oc, 0:n],
                                                                       op0=ALU.mult, op1=ALU.add),
                         reads=[psd[pb], xod, modd], writes=[xd[oc][tt]])

        L0 = ExitStack()
        with L0:
            catc = sb(L0, "catc", [128, 4, T], BF16)
            catcd = [[g.dep() for _ in range(NTT)] for _ in range(4)]
            A0 = ExitStack()
            with A0:
                stg = stage_bufs(A0)
                nb = norm_bufs(A0)
                for tt in range(NTT):
                    xt, xtd = load_tok_tile(stg, tt)
                    rmsnorm_tile(nb, lambda c: xt[:, c, 0:TN[tt]], lambda c: xtd, tt, "gs1", 0, 0)
                g.barrier()
            M0 = ExitStack()
            with M0:
                wr = [sb(M0, "w0r%d" % i, [128, NCH, 512], BF16) for i in range(3)]
                wrd = [g.dep(dma=True) for _ in range(3)]

                def load_win(slot, blk):
                    g.dma("pool", wr[slot][:], ev_w_in[:, blk * 512:(blk + 1) * 512].rearrange("(c p) f -> p c f", p=128),
                          writes=[wrd[slot]])

                load_win(0, 3)
                load_win(1, 4)
                load_win(2, 5)
                CV = ExitStack()
                with CV:
                    pj = sb(CV, "pj", [128, T], F32)
                    accj = sb(CV, "accj", [128, T], F32)
                    ctmp = [sb(CV, "ctmp%d" % i, [128, 512], F32) for i in range(2)]
                    pjd = g.dep()
                    accd = g.dep()
                    ctd = [g.dep(), g.dep()]
                    it = 0
                    for j in range(4):
                        for tt in range(NTT):
                            n = TN[tt]
                            for which, pb in ((1, 0), (2, 1)):
                                for c in range(NCH):
                                    g.op("pe", lambda: nc.tensor.matmul(ps[pb][:, 0:n], lhsT=wr[which][:, c, j * 128:(j + 1) * 128],
                                                                        rhs=hb[:, c, tsl(tt)], start=(c == 0), stop=(c == NCH - 1)),
                                         reads=[wrd[which], hbd[c][tt]], writes=[psd[pb]])
                            s = it % 2
                            it += 1
                            g.op("act", lambda: nc.scalar.copy(out=ctmp[s][:, 0:n], in_=ps[0][:, 0:n]), reads=[psd[0]], writes=[ctd[s]])
                            g.op("dve", lambda: nc.vector.tensor_tensor(out=pj[:, tsl(tt)], in0=ps[1][:, 0:n], in1=ctmp[s][:, 0:n],
                                                                        op=ALU.mult), reads=[psd[1], ctd[s]], writes=[pjd])
                        for (a, b) in ((0, TC), (TC, T)):
                            g.op("dve", lambda: nc.vector.tensor_scalar(out=accj[:, a:b], in0=pj[:, a:b], scalar1=Vcol("evconv", 1 * 4 + j),
                                                                        scalar2=None, op0=ALU.mult), reads=[pjd, Vd], writes=[accd])
                            g.op("dve", lambda: nc.vector.scalar_tensor_tensor(out=accj[:, a + 1:b], in0=pj[:, a:b - 1],
                                                                               scalar=Vcol("evconv", 0 * 4 + j), in1=accj[:, a + 1:b],
                                                                               op0=ALU.mult, op1=ALU.add), reads=[pjd, Vd, accd], writes=[accd])
                            g.op("dve", lambda: nc.vector.scalar_tensor_tensor(out=accj[:, a:b - 1], in0=pj[:, a + 1:b],
                                                                               scalar=Vcol("evconv", 2 * 4 + j), in1=accj[:, a:b - 1],
                                                                               op0=ALU.mult, op1=ALU.add), reads=[pjd, Vd, accd], writes=[accd])
                        for tt in range(NTT):
                            n = TN[tt]
                            pb = 2 + (tt % 2)
                            for c in range(NCH):
                                g.op("pe", lambda: nc.tensor.matmul(ps[pb][:, 0:n], lhsT=wr[0][:, c, j * 128:(j + 1) * 128],
                                                                    rhs=hb[:, c, tsl(tt)], start=(c == 0), stop=(c == NCH - 1)),
                                     reads=[wrd[0], hbd[c][tt]], writes=[psd[pb]])
                            g.op("dve", lambda: nc.vector.tensor_tensor(out=catc[:, j, tsl(tt)], in0=ps[pb][:, 0:n], in1=accj[:, tsl(tt)],
                                                                        op=ALU.mult), reads=[psd[pb], accd], writes=[catcd[j][tt]])
                    g.barrier()
                load_win(0, 0)
                load_win(1, 1)
                load_win(2, 2)
                qk = [sb(M0, "q", [128, 4, T], BF16), sb(M0, "k", [128, 4, T], BF16)]
                qkd = [[[g.dep() for _ in range(NTT)] for _ in range(4)] for _ in range(2)]
                vt = sb(M0, "v", [128, NKT, 512], BF16)
                vtd = [g.dep() for _ in range(NKT)]
                cosT = sb(M0, "cosT", [128, T], F32)
                sinT = sb(M0, "sinT", [128, T], F32)
                tabd = g.dep(dma=True)
                g.dma("sp", cosT[:], cos_d, writes=[tabd])
                g.dma("sp", sinT[:], sin_d, writes=[tabd])
                RP = ExitStack()
                with RP:
                    qf = [sb(RP, "qf%d" % i, [128, 512], F32) for i in range(2)]
                    qfd = [g.dep(), g.dep()]
                    t1 = [sb(RP, "rt1%d" % i, [128, 512], F32) for i in range(2)]
                    t1d = [g.dep(), g.dep()]
                    t2 = [sb(RP, "rt2%d" % i, [128, 512], F32) for i in range(2)]
                    t2d = [g.dep(), g.dep()]
                    it = 0
                    for which in range(2):
                        for hc in range(4):
                            for tt in range(NTT):
                                n = TN[tt]
                                s = it % 2
                                it += 1
                                pb = s
                                pr = 2 + s
                                for c in range(NCH):
                                    g.op("pe", lambda: nc.tensor.matmul(ps[pb][:, 0:n], lhsT=wr[which][:, c, hc * 128:(hc + 1) * 128],
                                                                        rhs=hb[:, c, tsl(tt)], start=(c == 0), stop=(c == NCH - 1)),
                                         reads=[wrd[which], hbd[c][tt]], writes=[psd[pb]])
                                g.op("act", lambda: nc.scalar.copy(out=qf[s][:, 0:n], in_=ps[pb][:, 0:n]), reads=[psd[pb]], writes=[qfd[s]])
                                g.op("pe", lambda: nc.tensor.matmul(ps[pr][:, 0:n], lhsT=perm[:], rhs=qf[s][:, 0:n], start=True, stop=True),
                                     reads=[qfd[s], cdep], writes=[psd[pr]])
                                g.op("dve", lambda: nc.vector.tensor_tensor(out=t1[s][:, 0:n], in0=qf[s][:, 0:n], in1=cosT[:, tsl(tt)], op=ALU.mult),
                                     reads=[qfd[s], tabd], writes=[t1d[s]])
                                g.op("dve", lambda: nc.vector.tensor_tensor(out=t2[s][:, 0:n], in0=ps[pr][:, 0:n], in1=sinT[:, tsl(tt)], op=ALU.mult),
                                     reads=[psd[pr], tabd], writes=[t2d[s]])
                                g.op("pool", lambda: nc.gpsimd.tensor_tensor(out=qk[which][:, hc, tsl(tt)], in0=t1[s][:, 0:n], in1=t2[s][:, 0:n], op=ALU.add),
                                     reads=[t1d[s], t2d[s]], writes=[qkd[which][hc][tt]])
                    for kt in range(NKT):
                        tt = 0 if kt < 2 else 1 + (kt - 2) // 4
                        pb = 4 + kt % 2
                        for c in range(NCH):
                            g.op("pe", lambda: nc.tensor.matmul(ps[pb][:, :], lhsT=hb[:, c, kt * 128:(kt + 1) * 128], rhs=wr[2][:, c, :],
                                                                start=(c == 0), stop=(c == NCH - 1)),
                                 reads=[wrd[2], hbd[c][tt]], writes=[psd[pb]])
                        g.op("act", lambda: nc.scalar.copy(out=vt[:, kt, :], in_=ps[pb][:, :]), reads=[psd[pb]], writes=[vtd[kt]])
                    g.barrier()
                AT = ExitStack()
                with AT:
                    eb = [[sb(AT, "e%d_%d" % (m, i), [128, 512], BF16) for i in range(2)] for m in range(2)]
                    ebd = [[g.dep(), g.dep()] for _ in range(2)]
                    rz = [sb(AT, "rz%d" % m, [128, 512], F32) for m in range(2)]
                    rzd = [g.dep(), g.dep()]
                    to = [sb(AT, "to%d" % m, [128, 512], F32) for m in range(2)]
                    tod = [g.dep(), g.dep()]
                    osb = sb(AT, "osb", [128, 512], F32)
                    osd = g.dep()
                    osq = sb(AT, "osq", [128, 512], F32)
                    osqd = g.dep()
                    ors = sb(AT, "ors", [128, 512], F32)
                    orsd = g.dep()
                    for h in range(4):
                        for qt in range(NTT):
                            n = TN[qt]
                            nkt = 2 if qt == 0 else NKT
                            for kt in range(nkt):
                                ktt = 0 if kt < 2 else 1 + (kt - 2) // 4
                                sl = kt % 2
                                for m in range(2):
                                    pbs = 4 + 2 * m + sl
                                    g.op("pe", lambda: nc.tensor.matmul(ps[pbs][:, 0:n], lhsT=qk[1][m * 64:(m + 1) * 64, h, kt * 128:(kt + 1) * 128],
                                                                        rhs=qk[0][m * 64:(m + 1) * 64, h, tsl(qt)], start=True, stop=True),
                                         reads=[qkd[1][h][ktt], qkd[0][h][qt]], writes=[psd[pbs]])
                                    g.op("act", lambda: nc.scalar.activation(out=eb[m][sl][:, 0:n], in_=ps[pbs][:, 0:n], func=AF.Exp, scale=0.125),
                                         reads=[psd[pbs]], writes=[ebd[m][sl]])
                                for m in range(2):
                                    g.op("pe", lambda: nc.tensor.matmul(ps[2 * m][:, 0:n], lhsT=vt[:, kt, h * 128:(h + 1) * 128], rhs=eb[m][sl][:, 0:n],
                                                                        start=(kt == 0), stop=(kt == nkt - 1)),
                                         reads=[vtd[kt], ebd[m][sl]], writes=[psd[2 * m]])
                                    g.op("pe", lambda: nc.tensor.matmul(ps[2 * m + 1][:, 0:n], lhsT=ones16[:], rhs=eb[m][sl][:, 0:n],
                                                                        start=(kt == 0), stop=(kt == nkt - 1)),
                                         reads=[onesd, ebd[m][sl]], writes=[psd[2 * m + 1]])
                            for m in range(2):
                                g.op("dve", lambda: nc.vector.reciprocal(out=rz[m][:, 0:n], in_=ps[2 * m + 1][:, 0:n]), reads=[psd[2 * m + 1]], writes=[rzd[m]])
                                g.op("dve", lambda: nc.vector.tensor_tensor(out=to[m][:, 0:n], in0=ps[2 * m][:, 0:n], in1=rz[m][:, 0:n], op=ALU.mult),
                                     reads=[psd[2 * m], rzd[m]], writes=[tod[m]])
                            g.op("dve", lambda: nc.vector.scalar_tensor_tensor(out=osb[:, 0:n], in0=to[1][:, 0:n], scalar=Scol("neglam"), in1=to[0][:, 0:n],
                                                                               op0=ALU.mult, op1=ALU.add), reads=[tod[0], tod[1], Sd], writes=[osd])
                            g.op("act", lambda: nc.scalar.activation(out=osq[:, 0:n], in_=osb[:, 0:n], func=AF.Square), reads=[osd], writes=[osqd])
                            g.op("pe", lambda: nc.tensor.matmul(ps[4][:, 0:n], lhsT=ones32[:], rhs=osq[:, 0:n], start=True, stop=True),
                                 reads=[osqd, onesd], writes=[psd[4]])
                            g.op("act", lambda: nc.scalar.activation(out=ors[:, 0:n], in_=ps[4][:, 0:n], func=AF.Sqrt, scale=1.0 / 128, bias=EPS),
                                 reads=[psd[4]], writes=[orsd])
                            g.op("dve", lambda: nc.vector.reciprocal(out=ors[:, 0:n], in_=ors[:, 0:n]), reads=[orsd], writes=[orsd])
                            g.op("dve", lambda: nc.vector.tensor_tensor(out=osb[:, 0:n], in0=osb[:, 0:n], in1=ors[:, 0:n], op=ALU.mult),
                                 reads=[orsd, osd], writes=[osd])
                            g.op("act", lambda: nc.scalar.activation(out=hb[:, h, tsl(qt)], in_=osb[:, 0:n], func=AF.Identity, scale=Scol("sg")),
                                 reads=[osd, Sd], writes=[hbd[h][qt]])
                    g.barrier()
            alloc_x()
            C0 = ExitStack()
            with C0:
                stg = stage_bufs(C0)

                def xold0(tt):
                    return load_tok_tile(stg, tt)

                def cat0(c, tt):
                    return hb[:, c, tsl(tt)] if c < 4 else catc[:, c - 4, tsl(tt)]

                def cat0d(c, tt):
                    return hbd[c][tt] if c < 4 else catcd[c - 4][tt]

                out_proj(0, ev_w_out, cat0, cat0d, range(NTT), xold0, C0)
                g.barrier()

        def moe_layer(l, tiles):
            x = xstate["x"]
            xd = xstate["d"]
            subs = []
            for tt in tiles:
                for sub in range(TN[tt] // 128):
                    subs.append((TOFF[tt] // 128 + sub, tt, sub))
            hbTok = hb[:].rearrange("p c t -> p (c t)").rearrange("p (i f) -> p i f", f=D)
            hbtokd = [g.dep() for _ in range(NKT)]
            ML = ExitStack()
            with ML:
                GK = sb(ML, "GK", [128, NKT, 4], F32)
                GKd = g.dep()
                DD = ExitStack()
                with DD:
                    nb = norm_bufs(DD)
                    h32 = sb(DD, "h32", [128, NCH, 512], F32)
                    h32d = g.dep()
                    hbt = sb(DD, "hbt", [128, NCH, 512], BF16)
                    hbtd = g.dep()
                    wrt = sb(DD, "wrt", [128, NCH, NE], F32)
                    brt = sb(DD, "brt", [1, NE], F32)
                    b2n = sb(DD, "b2n", [NE, D], F32)
                    rtd = g.dep(dma=True)
                    g.dma("sp", wrt[:], w_router[l].rearrange("(c p) e -> p c e", p=128), writes=[rtd])
                    g.dma("sp", brt[:], b_router[l:l + 1, :], writes=[rtd])
                    g.dma("sp", b2n[:], moe_b2[l], writes=[rtd])
                    gT = sb(DD, "gT", [NE, T], F32)
                    gTd = g.dep()
                    carry = sb(DD, "carry", [128, NE], F32)
                    card = g.dep()
                    g.op("dve", lambda: nc.vector.memset(carry[:], 0.0), writes=[card])
                    oob = sb(DD, "oob", [128, NE * T // 128], I32)
                    oobd = g.dep()
                    g.op("pool", lambda: nc.gpsimd.memset(oob[:], 2000000000), writes=[oobd])
                    g.dma("sp", idx_d[:, :].rearrange("(p r) o -> p (r o)", p=128), oob[:], reads=[oobd], writes=[idx_dep])
                    lg = sb(DD, "lg", [128, NE], F32)
                    ex = sb(DD, "ex", [128, NE], F32)
                    mk = sb(DD, "mk", [128, NE], F32)
                    gt_ = sb(DD, "gt_", [128, NE], F32)
                    ngd = sb(DD, "ngd", [128, NE], F32)
                    t8 = sb(DD, "t8", [128, 8], F32)
                    t8b = sb(DD, "t8b", [128, 8], F32)
                    sm = sb(DD, "sm", [128, 4], F32)
                    fli = sb(DD, "fli", [128, NE], I32)
                    NIX = 3
                    idx4 = [sb(DD, "idx4_%d" % i, [128, 4], I32) for i in range(NIX)]
                    val4 = [sb(DD, "val4_%d" % i, [128, 4], I32) for i in range(NIX)]
                    ixd = [g.dep() for _ in range(NIX)]
                    rd = g.dep()
                    mkd = g.dep()
                    ixc = 0
                    for tt in tiles:
                        n = TN[tt]
                        rmsnorm_tile(nb, lambda c: x[:, c, tsl(tt)], lambda c: xd[c][tt], tt, "gs2", l, 3, out32=h32, out32d=h32d,
                                     hbout=lambda c: (hbt[:, c, 0:n], hbtd))
                        for sub in range(n // 128):
                            t0 = TOFF[tt] + sub * 128
                            i = t0 // 128
                            psb = ps[6][:, :].bitcast(BF16)
                            for c in range(NCH):
                                g.op("pe", lambda: nc.tensor.transpose(out=psb[:, c * 128:(c + 1) * 128], in_=hbt[:, c, sub * 128:(sub + 1) * 128], identity=identb[:]),
                                     reads=[hbtd, cdep2], writes=[psd[6]])
                            g.op("act", lambda: nc.scalar.copy(out=hbTok[:, i, :], in_=psb), reads=[psd[6]], writes=[hbtokd[i]])
                            pb = 0
                            for c in range(NCH):
                                g.op("pe", lambda: nc.tensor.matmul(ps[pb][:, 0:NE], lhsT=h32[:, c, sub * 128:(sub + 1) * 128], rhs=wrt[:, c, :],
                                                                    start=(c == 0), stop=False),
                                     reads=[h32d, rtd], writes=[psd[pb]])
                            g.op("pe", lambda: nc.tensor.matmul(ps[pb][:, 0:NE], lhsT=ones32[0:1, :], rhs=brt[0:1, :], start=False, stop=True),
                                 reads=[rtd, onesd], writes=[psd[pb]])
                            g.op("act", lambda: nc.scalar.copy(out=lg[:], in_=ps[pb][:, 0:NE]), reads=[psd[pb]], writes=[rd])
                            g.op("dve", lambda: nc.vector.max(out=t8[:], in_=lg[:]), reads=[rd], writes=[rd])
                            g.op("dve", lambda: nc.vector.tensor_scalar(out=sm[:, 0:1], in0=t8[:, 0:1], scalar1=-1.0, scalar2=None, op0=ALU.mult),
                                 reads=[rd], writes=[rd])
                            g.op("act", lambda: nc.scalar.activation(out=ex[:], in_=lg[:], func=AF.Exp, bias=sm[:, 0:1]), reads=[rd], writes=[rd])
                            g.op("dve", lambda: nc.vector.tensor_scalar(out=mk[:], in0=lg[:], scalar1=t8[:, 3:4], scalar2=None, op0=ALU.is_ge),
                                 reads=[rd, mkd], writes=[rd, mkd])
                            g.op("dve", lambda: nc.vector.tensor_tensor(out=ex[:], in0=ex[:], in1=mk[:], op=ALU.mult), reads=[rd], writes=[rd])
                            g.op("dve", lambda: nc.vector.tensor_reduce(out=sm[:, 1:2], in_=ex[:], axis=AX.X, op=ALU.add), reads=[rd], writes=[rd])
                            g.op("dve", lambda: nc.vector.reciprocal(out=sm[:, 2:3], in_=sm[:, 1:2]), reads=[rd], writes=[rd])
                            g.op("dve", lambda: nc.vector.tensor_scalar(out=gt_[:], in0=ex[:], scalar1=sm[:, 2:3], scalar2=None, op0=ALU.mult),
                                 reads=[rd], writes=[rd])
                            g.op("pe", lambda: nc.tensor.transpose(out=ps[1][0:NE, 0:128], in_=gt_[:], identity=ident[:]),
                                 reads=[rd, cdep], writes=[psd[1]])
                            g.op("act", lambda: nc.scalar.copy(out=gT[:, t0:t0 + 128], in_=ps[1][0:NE, 0:128]), reads=[psd[1]], writes=[gTd])
                            g.op("pe", lambda: nc.tensor.matmul(ps[2][:, 0:NE], lhsT=utri[:], rhs=mk[:], start=True, stop=True),
                                 reads=[mkd, cdep2], writes=[psd[2]])
                            g.op("pe", lambda: nc.tensor.matmul(ps[3][:, 0:NE], lhsT=ones32[:], rhs=mk[:], start=True, stop=True),
                                 reads=[mkd, onesd], writes=[psd[3]])
                            g.op("dve", lambda: nc.vector.tensor_tensor(out=ex[:], in0=ps[2][:, 0:NE], in1=carry[:], op=ALU.add), reads=[psd[2], card, rd], writes=[rd])
                            g.op("dve", lambda: nc.vector.tensor_tensor(out=ex[:], in0=ex[:], in1=offrow[:], op=ALU.add), reads=[rd, cdep2], writes=[rd])
                            g.op("dve", lambda: nc.vector.tensor_tensor(out=ex[:], in0=ex[:], in1=mk[:], op=ALU.mult), reads=[rd], writes=[rd])
                            g.op("dve", lambda: nc.vector.tensor_scalar(out=ngd[:], in0=mk[:], scalar1=1.0e6, scalar2=-1.0e6, op0=ALU.mult, op1=ALU.add), reads=[rd], writes=[rd])
                            g.op("dve", lambda: nc.vector.tensor_tensor(out=ngd[:], in0=ngd[:], in1=ex[:], op=ALU.subtract), reads=[rd], writes=[rd])
                            g.op("dve", lambda: nc.vector.tensor_tensor(out=carry[:], in0=ps[3][:, 0:NE], in1=carry[:], op=ALU.add), reads=[psd[3], rd], writes=[card])
                            g.op("dve", lambda: nc.vector.max(out=t8b[:], in_=ngd[:]), reads=[rd], writes=[rd])
                            si = ixc % NIX
                            ixc += 1
                            g.op("dve", lambda: nc.vector.tensor_scalar(out=idx4[si][:], in0=t8b[:, 0:4], scalar1=-1.0, scalar2=None, op0=ALU.mult), reads=[rd], writes=[ixd[si]])
                            g.op("dve", lambda: nc.vector.tensor_scalar(out=val4[si][:], in0=tok4f[:], scalar1=float(4 * t0), scalar2=None, op0=ALU.add), reads=[cdep2], writes=[ixd[si]])
                            for k in range(4):
                                g.op("dve", lambda: nc.vector.scalar_tensor_tensor(out=ex[:], in0=ngd[:], scalar=t8b[:, k:k + 1], in1=gt_[:], op0=ALU.is_equal, op1=ALU.mult,
                                                                                   accum_out=GK[:, i, k:k + 1]), reads=[rd], writes=[rd, GKd])
                            for k in range(4):
                                g.idma(hs_d[:, :], idx4[si][:, k:k + 1], hbTok[:, i, :], bnd_hs, reads=[ixd[si], hbtokd[i]], writes=[hs_dep])
                                g.idma(idx_d[:, :], idx4[si][:, k:k + 1], val4[si][:, k:k + 1], bnd_hs, reads=[ixd[si]], writes=[idx_dep])
                    g.op("dve", lambda: nc.vector.tensor_copy(out=fli[:], in_=carry[:]), reads=[card, rd], writes=[rd])
                    g.dma("sp", cnt_d[l:l + 1, :], fli[0:1, :], reads=[rd], writes=[flag_dep[l]])
                    it = 0
                    for tt in tiles:
                        n = TN[tt]
                        for oc in range(NCH):
                            pb = 2 + it % 2
                            it += 1
                            g.op("pe", lambda: nc.tensor.matmul(ps[pb][:, 0:n], lhsT=b2n[:, oc * 128:(oc + 1) * 128], rhs=gT[:, tsl(tt)], start=True, stop=True),
                                 reads=[rtd, gTd], writes=[psd[pb]])
                            g.op("dve", lambda: nc.vector.scalar_tensor_tensor(out=x[:, oc, tsl(tt)], in0=ps[pb][:, 0:n], scalar=mod(l, 5, oc, 1 if tt == 0 else 0),
                                                                               in1=x[:, oc, tsl(tt)], op0=ALU.mult, op1=ALU.add),
                                 reads=[psd[pb], modd], writes=[xd[oc][tt]])
                    g.barrier()
                EE = ExitStack()
                with EE:
                    NSLOT = 4
                    hbflat = hb[:].rearrange("p c t -> p (c t)")
                    ring = [hbflat[:, k * 4096:(k + 1) * 4096].rearrange("p (c f) -> p c f", f=512) for k in range(NSLOT)]
                    ringd = [g.dep(dma=True) for _ in range(NSLOT)]
                    pf = [sb(EE, "pf%d" % i, [128, NCH, 512], BF16) for i in range(4)]
                    pfd = [g.dep(dma=True) for _ in range(4)]
                    hgToks = [sb(EE, "hgTok%d" % i, [128, 3, D], BF16) for i in range(3)]
                    hgds = [g.dep(dma=True) for _ in range(3)]
                    idxts = [sb(EE, "idxt%d" % i, [128, 3, 1], I32) for i in range(3)]
                    idxtds = [g.dep(dma=True) for _ in range(3)]

                    def gather_load(e, c0, hsel):
                        r0 = e * T + c0 * CAP
                        g.dma("sp", hgToks[hsel][:], hs_d[r0:r0 + CAP, :].rearrange("(j p) f -> p j f", p=128), reads=[hs_dep], writes=[hgds[hsel]])
                        for j3 in range(3):
                            g.dma("sp", idxts[hsel][:, j3, :], idx_d[r0 + j3 * 128:r0 + (j3 + 1) * 128, :], reads=[idx_dep], writes=[idxtds[hsel]])
                    hbg = sb(EE, "hbg", [128, NCH, CAP], BF16)
                    hbgd = [g.dep() for _ in range(NCH)]
                    actT = sb(EE, "actT", [128, NCH, CAP], BF16)
                    actd = [g.dep() for _ in range(NCH)]
                    yTok = sb(EE, "yTok", [128, 3, D], F32)
                    yTd = [g.dep() for _ in range(3)]
                    At = [sb(EE, "mA%d" % i, [128, CAP], F32) for i in range(2)]
                    St = [sb(EE, "mS%d" % i, [128, CAP], F32) for i in range(2)]
                    Lt = [sb(EE, "mL%d" % i, [128, CAP], F32) for i in range(2)]
                    Ad = [g.dep(), g.dep()]
                    Sdp = [g.dep(), g.dep()]
                    Ld = [g.dep(), g.dep()]
                    rc = [0]
                    ec = [0]
                    pc_ = [0]

                    def wsrc(ap):
                        return ap.rearrange("(c p) f -> p c f", p=128)

                    def expert_loads(e):
                        g.dma("pool", ring[0][:], wsrc(moe_w1[l, e, :, 512:1024]), writes=[ringd[0]])
                        g.dma("pool", ring[1][:], wsrc(moe_w1[l, e, :, D + 512:D + 1024]), writes=[ringd[1]])
                        g.dma("pool", ring[2][:], wsrc(moe_w2[l, e, :, 0:512]), writes=[ringd[2]])
                        g.dma("pool", ring[3][:], wsrc(moe_w2[l, e, :, 512:1024]), writes=[ringd[3]])

                    def prefetchA(e):
                        par = (e % 2) * 2
                        g.dma("pool", pf[par][:], wsrc(moe_w1[l, e, :, 0:512]), writes=[pfd[par]])
                        g.dma("pool", pf[par + 1][:], wsrc(moe_w1[l, e, :, D:D + 512]), writes=[pfd[par + 1]])

                    def sparse_chunk(e, c0):
                        b1o = voff["b1"] + (l * NE + e) * 16
                        r0 = e * T + c0 * CAP
                        if c0 == 0:
                            hsel = e % 2
                        else:
                            hsel = 2
                            gather_load(e, c0, hsel)
                        hgTok, hgd, idxt, idxtd = hgToks[hsel], hgds[hsel], idxts[hsel], idxtds[hsel]
                        par = (e % 2) * 2
                        w1s = {0: ((pf[par], pfd[par]), (pf[par + 1], pfd[par + 1])), 1: ((ring[0], ringd[0]), (ring[1], ringd[1]))}
                        w2s = [(ring[2], ringd[2]), (ring[3], ringd[3])]
                        for c in range(NCH):
                            pb = 6 + pc_[0] % 2
                            pc_[0] += 1
                            psb = ps[pb][:, :].bitcast(BF16)
                            for j3 in range(3):
                                g.op("pe", lambda: nc.tensor.transpose(out=psb[:, j3 * 128:(j3 + 1) * 128], in_=hgTok[:, j3, c * 128:(c + 1) * 128], identity=identb[:]),
                                     reads=[hgd, cdep2], writes=[psd[pb]])
                            if c % 2 == 0:
                                g.op("act", lambda: nc.scalar.copy(out=hbg[:, c, :], in_=psb[:, 0:CAP]), reads=[psd[pb]], writes=[hbgd[c]])
                            else:
                                g.op("dve", lambda: nc.vector.tensor_copy(out=hbg[:, c, :], in_=psb[:, 0:CAP]), reads=[psd[pb]], writes=[hbgd[c]])
                        pend = []
                        for u in range(2):
                            (sgA, sgD), (slA, slD) = w1s[u]
                            for jj in range(4):
                                j = u * 4 + jj
                                s = ec[0] % 2
                                s3 = ec[0] % 3
                                ec[0] += 1
                                pg = s3
                                pl = 3 + s3
                                for c in range(NCH):
                                    g.op("pe", lambda: nc.tensor.matmul(ps[pg][:, 0:CAP], lhsT=sgA[:, c, jj * 128:(jj + 1) * 128], rhs=hbg[:, c, :],
                                                                        start=(c == 0), stop=(c == NCH - 1)),
                                         reads=[sgD, hbgd[c]], writes=[psd[pg]])
                                for c in range(NCH):
                                    g.op("pe", lambda: nc.tensor.matmul(ps[pl][:, 0:CAP], lhsT=slA[:, c, jj * 128:(jj + 1) * 128], rhs=hbg[:, c, :],
                                                                        start=(c == 0), stop=(c == NCH - 1)),
                                         reads=[slD, hbgd[c]], writes=[psd[pl]])
                                g.op("dve", lambda: nc.vector.tensor_scalar(out=At[s][:], in0=ps[pg][:, 0:CAP], scalar1=V[:, b1o + j:b1o + j + 1], scalar2=7.0,
                                                                            op0=ALU.add, op1=ALU.min), reads=[psd[pg], Vd], writes=[Ad[s]])
                                g.op("act", lambda: nc.scalar.activation(out=St[s][:], in_=At[s][:], func=AF.Sigmoid, scale=1.702),
                                     reads=[Ad[s]], writes=[Sdp[s]])
                                g.op("dve", lambda: nc.vector.tensor_scalar(out=Lt[s][:], in0=ps[pl][:, 0:CAP], scalar1=V[:, b1o + 8 + j:b1o + 8 + j + 1], scalar2=-6.0,
                                                                            op0=ALU.add, op1=ALU.max), reads=[psd[pl], Vd], writes=[Ld[s]])
                                if pend:
                                    pend.pop()()

                                def _fin(s=s, j=j):
                                    g.op("dve", lambda: nc.vector.tensor_tensor(out=St[s][:], in0=At[s][:], in1=St[s][:], op=ALU.mult),
                                         reads=[Ad[s], Sdp[s]], writes=[Sdp[s]])
                                    g.op("dve", lambda: nc.vector.scalar_tensor_tensor(out=actT[:, j, :], in0=Lt[s][:], scalar=8.0, in1=St[s][:],
                                                                                       op0=ALU.min, op1=ALU.mult), reads=[Ld[s], Sdp[s]], writes=[actd[j]])
                                pend.append(_fin)
                        if pend:
                            pend.pop()()
                        for j3 in range(3):
                            for hh in range(2):
                                swA, swD = w2s[hh]
                                pb = 6 + pc_[0] % 2
                                pc_[0] += 1
                                for fc in range(NCH):
                                    g.op("pe", lambda: nc.tensor.matmul(ps[pb][:, :], lhsT=actT[:, fc, j3 * 128:(j3 + 1) * 128], rhs=swA[:, fc, :],
                                                                        start=(fc == 0), stop=(fc == NCH - 1)),
                                         reads=[swD, actd[fc]], writes=[psd[pb]])
                                g.op("act", lambda: nc.scalar.copy(out=yTok[:, j3, hh * 512:(hh + 1) * 512], in_=ps[pb][:, :]), reads=[psd[pb]], writes=[yTd[j3]])
                            g.idma(y4_d[:, :], idxt[:, j3, :], yTok[:, j3, :], bnd_y4, reads=[idxtd, yTd[j3]], writes=[y4_dep])

                    prefetchA(0)
                    gather_load(0, 0, 0)
                    for e in range(NE):
                        expert_loads(e)
                        if e + 1 < NE:
                            prefetchA(e + 1)
                            gather_load(e + 1, 0, (e + 1) % 2)
                        for reg in flag_regs:
                            ek = ENG_KEY[reg.engine]
                            g._wait(ek, [(flag_dep[l].sem, flag_dep[l].tot)])
                            g.eng[ek].reg_load(reg, cnt_d[l:l + 1, e:e + 1])
                        def emit_chunks(c0):
                            snap = g.snapshot()
                            with nc.If_cmp(flag_regs, c0 * CAP, "IS_GT"):
                                sparse_chunk(e, c0)
                                if c0 + 1 < NCHUNK:
                                    emit_chunks(c0 + 1)
                            big = g.snapshot()
                            g.restore(snap)
                            with nc.Else():
                                g.pad_to(big)
                            g.restore(big)
                            g.known = {k: dict(v) for k, v in snap[2].items()}

                        emit_chunks(0)
                    g.barrier()
                CB = ExitStack()
                with CB:
                    y4t = [sb(CB, "y4t%d" % i, [128, 4, D], F32) for i in range(2)]
                    y4td = [g.dep(dma=True) for _ in range(2)]
                    acc = [sb(CB, "cacc%d" % i, [128, D], F32) for i in range(2)]
                    accd = [g.dep(), g.dep()]
                    for n_, (i, tt, sub) in enumerate(subs):
                        s = n_ % 2
                        t0 = i * 128
                        cls = 1 if tt == 0 else 0
                        g.dma("sp", y4t[s][:], y4_d[4 * t0:4 * t0 + 512, :].rearrange("(p k) f -> p k f", k=4), reads=[y4_dep], writes=[y4td[s]])
                        g.op("dve", lambda: nc.vector.tensor_scalar(out=acc[s][:], in0=y4t[s][:, 0, :], scalar1=GK[:, i, 0:1], scalar2=None, op0=ALU.mult),
                             reads=[y4td[s], GKd], writes=[accd[s]])
                        for k in range(1, 4):
                            g.op("dve", lambda: nc.vector.scalar_tensor_tensor(out=acc[s][:], in0=y4t[s][:, k, :], scalar=GK[:, i, k:k + 1], in1=acc[s][:],
                                                                               op0=ALU.mult, op1=ALU.add), reads=[y4td[s], GKd, accd[s]], writes=[accd[s]])
                        for half in range(2):
                            pbt = 2 * s + half
                            for cc in range(4):
                                c = half * 4 + cc
                                g.op("pe", lambda: nc.tensor.transpose(out=ps[pbt][:, cc * 128:(cc + 1) * 128], in_=acc[s][:, c * 128:(c + 1) * 128], identity=ident[:]),
                                     reads=[accd[s], cdep], writes=[psd[pbt]])
                            for cc in range(4):
                                c = half * 4 + cc
                                g.op("dve", lambda: nc.vector.scalar_tensor_tensor(out=x[:, c, t0:t0 + 128], in0=ps[pbt][:, cc * 128:(cc + 1) * 128], scalar=mod(l, 5, c, cls),
                                                                                   in1=x[:, c, t0:t0 + 128], op0=ALU.mult, op1=ALU.add),
                                     reads=[psd[pbt], modd], writes=[xd[c][tt]])
                    g.barrier()

        moe_layer(0, list(range(NTT)))

        if dbg == "x1":
            for c in range(NCH):
                g.dma("sp", dbg_d[:, c, :], xstate["x"][:, c, :], reads=[xstate["d"][c][tt] for tt in range(NTT)], writes=[out_dep])

        LAT = [1, 2, 3, 4]
        L1 = ExitStack()
        with L1:
            cat1 = sb(L1, "cat1", [128, NCH, TL], BF16)
            cat1d = [[g.dep() for _ in range(NTT)] for _ in range(NCH)]
            x = xstate["x"]
            xd = xstate["d"]
            A1 = ExitStack()
            with A1:
                nb = norm_bufs(A1)
                for tt in range(NTT):
                    rmsnorm_tile(nb, lambda c: x[:, c, tsl(tt)], lambda c: xd[c][tt], tt, "gs1", 1, 0)
                for c in range(NCH):
                    g.dma("sp", xs_d[:, c, :], x[:, c, :], reads=[xd[c][tt] for tt in range(NTT)], writes=[xs_dep])
                g.barrier()
            xes.close()
            M1 = ExitStack()
            with M1:
                gw = sb(M1, "gw", [128, 16, 2, 256], BF16)
                gwd = g.dep(dma=True)
                for gi_, src in ((0, od_ga), (1, od_gx)):
                    for d_ in range(2):
                        g.dma("pool", gw[:, gi_ * 8 + d_ * 4:gi_ * 8 + d_ * 4 + 4, :, :],
                              src[d_].rearrange("b (c p) o -> p b c o", p=128), writes=[gwd])
                wr1 = [sb(M1, "w1r%d" % i, [128, NCH, 512], BF16) for i in range(2)]
                wr1d = [g.dep(dma=True) for _ in range(2)]
                ug = sb(M1, "ug", [128, 2, T], F32)
                ugd = [g.dep(), g.dep()]
                xc = sb(M1, "xc", [128, 2, T], F32)
                xcd = [g.dep(), g.dep()]
                xcb = sb(M1, "xcb", [128, 2, T], BF16)
                xcbd = [g.dep(), g.dep()]
                rec = sb(M1, "rec", [128, 2, T], F32)
                recd = [[g.dep() for _ in range(NTT)] for _ in range(2)]
                NT_ = 9
                tb = [[sb(M1, "tb%d_%d" % (k, i), [128, 512], F32) for i in range(2 if k < 7 else 1)] for k in range(NT_)]
                tb[7].append(tb[7][0])
                tb[8].append(tb[8][0])
                tbd = [[g.dep(), g.dep()] for _ in range(NT_)]
                tbd[7][1] = tbd[7][0]
                tbd[8][1] = tbd[8][0]
                cnt1 = [0]
                pcnt = [0]
                for blk in range(4):
                    sW = blk % 2
                    g.dma("pool", wr1[sW][:, :, 0:256], od_w_in[:, blk * 256:(blk + 1) * 256].rearrange("(c p) f -> p c f", p=128), writes=[wr1d[sW]])
                    g.dma("pool", wr1[sW][:, :, 256:512], od_w_in[:, D + blk * 256:D + (blk + 1) * 256].rearrange("(c p) f -> p c f", p=128), writes=[wr1d[sW]])
                    for cc in range(2):
                        for tt in range(NTT):
                            n = TN[tt]
                            pb = pcnt[0] % 2
                            pcnt[0] += 1
                            for c in range(NCH):
                                g.op("pe", lambda: nc.tensor.matmul(ps[pb][:, 0:n], lhsT=wr1[sW][:, c, 256 + cc * 128:256 + (cc + 1) * 128], rhs=hb[:, c, tsl(tt)],
                                                                    start=(c == 0), stop=(c == NCH - 1)),
                                     reads=[wr1d[sW], hbd[c][tt]], writes=[psd[pb]])
                            g.op("act", lambda: nc.scalar.copy(out=ug[:, cc, tsl(tt)], in_=ps[pb][:, 0:n]), reads=[psd[pb]], writes=[ugd[cc]])
                    for d_ in range(2):
                        for cc in range(2):
                            ch = blk * 2 + cc
                            wv = lambda k: Vcol("odconvw", (d_ * 4 + k) * 8 + ch)
                            bv = Vcol("odconvb", d_ * 8 + ch)
                            for (a, b) in ((0, TC), (TC, T)):
                                if d_ == 0:
                                    g.op("dve", lambda: nc.vector.tensor_scalar(out=xc[:, cc, a:b], in0=ug[:, cc, a:b], scalar1=wv(3), scalar2=bv, op0=ALU.mult, op1=ALU.add),
                                         reads=[ugd[cc], Vd], writes=[xcd[cc]])
                                    for k in range(3):
                                        sh = 3 - k
                                        g.op("dve", lambda: nc.vector.scalar_tensor_tensor(out=xc[:, cc, a + sh:b], in0=ug[:, cc, a:b - sh], scalar=wv(k), in1=xc[:, cc, a + sh:b],
                                                                                           op0=ALU.mult, op1=ALU.add), reads=[ugd[cc], Vd, xcd[cc]], writes=[xcd[cc]])
                                else:
                                    g.op("dve", lambda: nc.vector.tensor_scalar(out=xc[:, cc, a:b], in0=ug[:, cc, a:b], scalar1=wv(0), scalar2=bv, op0=ALU.mult, op1=ALU.add),
                                         reads=[ugd[cc], Vd], writes=[xcd[cc]])
                                    for k in range(1, 4):
                                        g.op("dve", lambda: nc.vector.scalar_tensor_tensor(out=xc[:, cc, a:b - k], in0=ug[:, cc, a + k:b], scalar=wv(k), in1=xc[:, cc, a:b - k],
                                                                                           op0=ALU.mult, op1=ALU.add), reads=[ugd[cc], Vd, xcd[cc]], writes=[xcd[cc]])
                            g.op("act", lambda: nc.scalar.copy(out=xcb[:, cc, :], in_=xc[:, cc, :]), reads=[xcd[cc]], writes=[xcbd[cc]])
                        order = [0, 1, 2, 3, 4] if d_ == 0 else [0, 4, 3, 2, 1]
                        for oc in range(2):
                            ch = blk * 2 + oc
                            prev = None
                            for tt in order:
                                n = TN[tt]
                                s = cnt1[0] % 2
                                cnt1[0] += 1
                                pa = 2 + s
                                px = 4 + s
                                for gi_, pb in ((0, pa), (1, px)):
                                    for ic in range(2):
                                        g.op("pe", lambda: nc.tensor.matmul(ps[pb][:, 0:n], lhsT=gw[:, gi_ * 8 + d_ * 4 + blk, ic, oc * 128:(oc + 1) * 128], rhs=xcb[:, ic, tsl(tt)],
                                                                            start=(ic == 0), stop=(ic == 1)),
                                             reads=[gwd, xcbd[ic]], writes=[psd[pb]])
                                R, I_, A_, A2, TH, GI, HS = 0, 1, 2, 3, 4, 5, 6
                                c8 = Scol("c8", d_ * 8 + ch)
                                c8x2 = Scol("c8x2", d_ * 8 + ch)
                                g.op("act", lambda: nc.scalar.activation(out=tb[R][s][:, 0:n], in_=ps[pa][:, 0:n], func=AF.Sigmoid, bias=Vcol("odgab", d_ * 8 + ch)),
                                     reads=[psd[pa], Vd], writes=[tbd[R][s]])
                                g.op("act", lambda: nc.scalar.activation(out=tb[I_][s][:, 0:n], in_=ps[px][:, 0:n], func=AF.Sigmoid, bias=Vcol("odgxb", d_ * 8 + ch)),
                                     reads=[psd[px], Vd], writes=[tbd[I_][s]])
                                g.op("act", lambda: nc.scalar.activation(out=tb[A_][s][:, 0:n], in_=tb[R][s][:, 0:n], func=AF.Exp, scale=c8),
                                     reads=[tbd[R][s], Sd], writes=[tbd[A_][s]])
                                g.op("act", lambda: nc.scalar.activation(out=tb[A2][s][:, 0:n], in_=tb[R][s][:, 0:n], func=AF.Exp, scale=c8x2),
                                     reads=[tbd[R][s], Sd], writes=[tbd[A2][s]])
                                g.op("act", lambda: nc.scalar.activation(out=tb[TH][s][:, 0:n], in_=tb[R][s][:, 0:n], func=AF.Tanh, scale=c8),
                                     reads=[tbd[R][s], Sd], writes=[tbd[TH][s]])
                                g.op("dve", lambda: nc.vector.scalar_tensor_tensor(out=tb[A2][s][:, 0:n], in0=tb[A2][s][:, 0:n], scalar=1.0, in1=tb[TH][s][:, 0:n],
                                                                                   op0=ALU.add, op1=ALU.mult), reads=[tbd[A2][s], tbd[TH][s]], writes=[tbd[A2][s]])
                                g.op("act", lambda: nc.scalar.activation(out=tb[A2][s][:, 0:n], in_=tb[A2][s][:, 0:n], func=AF.Sqrt, scale=-1.0),
                                     reads=[tbd[A2][s]], writes=[tbd[A2][s]])
                                g.op("pool", lambda: nc.gpsimd.tensor_tensor(out=tb[GI][s][:, 0:n], in0=tb[I_][s][:, 0:n], in1=xc[:, oc, tsl(tt)], op=ALU.mult),
                                     reads=[tbd[I_][s], xcd[oc]], writes=[tbd[GI][s]])
                                g.op("dve", lambda: nc.vector.tensor_tensor(out=tb[GI][s][:, 0:n], in0=tb[GI][s][:, 0:n], in1=tb[A2][s][:, 0:n], op=ALU.mult),
                                     reads=[tbd[GI][s], tbd[A2][s]], writes=[tbd[GI][s]])
                                if d_ == 0:
                                    dst = rec[:, oc, tsl(tt)]
                                    dstd = recd[oc][tt]
                                    init = 0.0 if prev is None else prev[0][:, TN[prev[2]] - 1:TN[prev[2]]]
                                    g.op("dve", lambda: nc.vector.tensor_tensor_scan(out=dst, data0=tb[A_][s][:, 0:n], data1=tb[GI][s][:, 0:n], initial=init,
                                                                                     op0=ALU.mult, op1=ALU.add),
                                         reads=[tbd[A_][s], tbd[GI][s]] + ([prev[1]] if prev else []), writes=[dstd])
                                    prev = (dst, dstd, tt)
                                else:
                                    dst = tb[HS][s][:, 0:n]
                                    dstd = tbd[HS][s]
                                    init = 0.0 if prev is None else prev[0][:, 0:1]
                                    g.op("dve", lambda: nc.vector.tensor_tensor_scan(out=dst[:, ::-1], data0=tb[A_][s][:, 0:n][:, ::-1], data1=tb[GI][s][:, 0:n][:, ::-1],
                                                                                     initial=init, op0=ALU.mult, op1=ALU.add),
                                         reads=[tbd[A_][s], tbd[GI][s]] + ([prev[1]] if prev else []), writes=[dstd])
                                    prev = (dst, dstd, tt)
                                    if tt != 0:
                                        pgt = 6 + s
                                        for c in range(NCH):
                                            g.op("pe", lambda: nc.tensor.matmul(ps[pgt][:, 0:n], lhsT=wr1[sW][:, c, oc * 128:(oc + 1) * 128], rhs=hb[:, c, tsl(tt)],
                                                                                start=(c == 0), stop=(c == NCH - 1)),
                                                 reads=[wr1d[sW], hbd[c][tt]], writes=[psd[pgt]])
                                        GL, SM = 7, 8
                                        g.op("act", lambda: nc.scalar.activation(out=tb[GL][s][:, 0:n], in_=ps[pgt][:, 0:n], func=AF.Gelu), reads=[psd[pgt]], writes=[tbd[GL][s]])
                                        g.op("pool", lambda: nc.gpsimd.tensor_tensor(out=tb[SM][s][:, 0:n], in0=dst, in1=rec[:, oc, tsl(tt)], op=ALU.add),
                                             reads=[dstd, recd[oc][tt]], writes=[tbd[SM][s]])
                                        g.op("dve", lambda: nc.vector.tensor_tensor(out=cat1[:, ch, TOFF[tt] - TC:TOFF[tt] - TC + n], in0=tb[SM][s][:, 0:n], in1=tb[GL][s][:, 0:n], op=ALU.mult),
                                             reads=[tbd[SM][s], tbd[GL][s]], writes=[cat1d[ch][tt]])
                g.barrier()
            alloc_x()
            C1 = ExitStack()
            with C1:
                xst = [sb(C1, "x1st%d" % i, [128, NCH, 512], F32) for i in range(2)]
                xstd = [g.dep(dma=True), g.dep(dma=True)]
                c1c = [0]

                def xold1(tt):
                    s = c1c[0] % 2
                    c1c[0] += 1
                    for c in range(NCH):
                        g.dma("sp", xst[s][:, c, 0:TN[tt]], xs_d[:, c, tsl(tt)], reads=[xs_dep], writes=[xstd[s]])
                    return xst[s], xstd[s]

                out_proj(1, od_w_out, lambda c, tt: cat1[:, c, TOFF[tt] - TC:TOFF[tt] - TC + TN[tt]], lambda c, tt: cat1d[c][tt], LAT, xold1, C1)
                g.barrier()
        moe_layer(1, LAT)

        FN = ExitStack()
        with FN:
            x = xstate["x"]
            xd = xstate["d"]
            nb = norm_bufs(FN)
            sq, sqd, tmp, tmpd, rs, rsd, cnt = nb
            yb = [sb(FN, "yb%d" % i, [128, NCH, 128], F32) for i in range(2)]
            ybd = [g.dep(), g.dep()]
            ot = [sb(FN, "ot%d" % i, [128, D], F32) for i in range(2)]
            otd = [g.dep(dma=True), g.dep(dma=True)]
            oc_ = [0]
            for tt in LAT:
                n = TN[tt]
                pb = 5
                for c in range(NCH):
                    s = cnt[0] % 2
                    cnt[0] += 1
                    g.op("act", lambda: nc.scalar.activation(out=sq[s][:, 0:n], in_=x[:, c, tsl(tt)], func=AF.Square), reads=[xd[c][tt]], writes=[sqd[s]])
                    g.op("pe", lambda: nc.tensor.matmul(ps[pb][:, 0:n], lhsT=ones32[:], rhs=sq[s][:, 0:n], start=(c == 0), stop=(c == NCH - 1)),
                         reads=[sqd[s], onesd], writes=[psd[pb]])
                g.op("act", lambda: nc.scalar.activation(out=rs[:, 0:n], in_=ps[pb][:, 0:n], func=AF.Sqrt, scale=1.0 / D, bias=EPS), reads=[psd[pb]], writes=[rsd])
                g.op("dve", lambda: nc.vector.reciprocal(out=rs[:, 0:n], in_=rs[:, 0:n]), reads=[rsd], writes=[rsd])
                for sub in range(n // 128):
                    s = oc_[0] % 2
                    oc_[0] += 1
                    for c in range(NCH):
                        g.op("dve", lambda: nc.vector.scalar_tensor_tensor(out=yb[s][:, c, :], in0=x[:, c, TOFF[tt] + sub * 128:TOFF[tt] + (sub + 1) * 128],
                                                                           scalar=Vcol("fnorm", c), in1=rs[:, sub * 128:(sub + 1) * 128], op0=ALU.mult, op1=ALU.mult),
                             reads=[xd[c][tt], rsd, Vd], writes=[ybd[s]])
                    for half in range(2):
                        pbt = 6 + half
                        for cc in range(4):
                            c = half * 4 + cc
                            g.op("pe", lambda: nc.tensor.transpose(out=ps[pbt][:, cc * 128:(cc + 1) * 128], in_=yb[s][:, c, :], identity=ident[:]),
                                 reads=[ybd[s], cdep], writes=[psd[pbt]])
                        if half == 0:
                            g.op("act", lambda: nc.scalar.copy(out=ot[s][:, 0:512], in_=ps[pbt][:, :]), reads=[psd[pbt]], writes=[otd[s]])
                        else:
                            g.op("dve", lambda: nc.vector.tensor_copy(out=ot[s][:, 512:1024], in_=ps[pbt][:, :]), reads=[psd[pbt]], writes=[otd[s]])
                    r0 = TOFF[tt] - TC + sub * 128
                    g.dma("sp", out_d[r0:r0 + 128, :], ot[s][:], reads=[otd[s]], writes=[out_dep])
            g.barrier()
        xes.close()
    return nc


def _consts():
    ident = np.eye(128, dtype=np.float32)
    perm = np.zeros((128, 128), np.float32)
    for m in range(128):
        blk = m // 16
        partner = (blk ^ 1) * 16 + (m % 16)
        perm[partner, m] = 1.0
    n_rows = TL // 64
    rows, cols = np.meshgrid(np.arange(n_rows), np.arange(64), indexing="ij")
    pos = np.stack([rows.reshape(-1), cols.reshape(-1)], axis=-1).astype(np.float32)
    inv = (np.float32(10000.0) ** (-np.arange(16, dtype=np.float32) / np.float32(16))).astype(np.float32)
    ang = (pos[:, :, None] * inv).astype(np.float32)
    cos = np.cos(ang).astype(np.float32)
    sin = np.sin(ang).astype(np.float32)
    cosT = np.ones((128, T), np.float32)
    sinT = np.zeros((128, T), np.float32)
    for p in range(128):
        dd = p % 64
        axis = dd // 32
        half = (dd % 32) // 16
        f = dd % 16
        cosT[p, TC:] = cos[:, axis, f]
        sinT[p, TC:] = (-sin[:, axis, f]) if half == 0 else sin[:, axis, f]
    return ident, perm, cosT, sinT


def _consts2(NE):
    utri = np.triu(np.ones((128, 128), np.float32), k=1)
    offrow = np.tile((np.arange(NE, dtype=np.float32) * T)[None, :], (128, 1))
    tok4f = (4.0 * np.arange(128, dtype=np.float32)[:, None] + np.arange(4, dtype=np.float32)[None, :]).astype(np.float32)
    return utri, offrow, tok4f


def make_in_maps(inp, NE, ncores):
    voff, vrows, vpad = vec_layout(NE)
    ident, perm, cosT, sinT = _consts()
    f = lambda a: np.ascontiguousarray(np.asarray(a, dtype=np.float32))
    shared = {
        "w_mod": f(inp["w_mod"]), "ev_w_in": f(inp["ev_w_in"][0]), "ev_w_out": f(inp["ev_w_out"][0]),
        "od_w_in": f(inp["od_w_in"][0]), "od_w_out": f(inp["od_w_out"][0]),
        "od_ga": f(inp["od_gate_a_w"][0]), "od_gx": f(inp["od_gate_x_w"][0]),
        "w_router": f(inp["moe_w_router"]), "b_router": f(inp["moe_b_router"]),
        "moe_w1": f(inp["moe_w1"]), "moe_w2": f(inp["moe_w2"]), "moe_b2": f(inp["moe_b2"]),
        "lams": f(np.stack([inp["ev_lambda_q1"][0], inp["ev_lambda_k1"][0], inp["ev_lambda_q2"][0], inp["ev_lambda_k2"][0]])),
        "ident": ident, "perm": perm, "cosT": cosT, "sinT": sinT,
    }
    shared["utri"], shared["offrow"], shared["tok4f"] = _consts2(NE)
    maps = []
    for b in range(ncores):
        rows = [
            f(inp["c"][b]).reshape(8, 128), f(inp["c_ctx"]).reshape(8, 128), f(inp["b_mod"]).reshape(96, 128),
            f(inp["norm_mix"]).reshape(16, 128), f(inp["norm_ffn"]).reshape(16, 128), f(inp["final_norm"]).reshape(8, 128),
            f(inp["ev_conv_w"][0]).reshape(12, 128), f(inp["ev_subln"][0]).reshape(1, 128),
            f(inp["od_conv_w"][0]).reshape(64, 128), f(inp["od_conv_b"][0]).reshape(16, 128),
            f(inp["od_gate_a_b"][0]).reshape(16, 128), f(inp["od_gate_x_b"][0]).reshape(16, 128),
            f(inp["od_lru_lambda"][0]).reshape(16, 128), f(inp["moe_b1"]).reshape(2 * NE * 16, 128),
        ]
        v = np.concatenate(rows, axis=0)
        assert v.shape[0] == vrows
        vp = np.zeros((vpad, 128), np.float32)
        vp[:vrows] = v
        m = dict(shared)
        m["xin"] = f(inp["x"][b])
        m["ctxin"] = f(inp["ctx"][b])
        m["vecs"] = vp
        maps.append(m)
    return maps


def kernel(**inputs):
    NE = inputs["moe_w1"].shape[1]
    B = inputs["x"].shape[0]
    nc = build(NE)
    maps = make_in_maps(inputs, NE, B)
    res = run_bass_kernel_spmd(nc, maps, core_ids=list(range(B)))
    return np.stack([np.asarray(r["out"], dtype=np.float32) for r in res.results], axis=0)
```

```python
import math
from contextlib import ExitStack

import numpy as np
import concourse.bass as bass
import concourse.mybir as mybir
from concourse.bass_utils import run_bass_kernel_spmd

F32 = mybir.dt.float32
F32R = mybir.dt.float32r
BF16 = mybir.dt.bfloat16
AF = mybir.ActivationFunctionType
ALU = mybir.AluOpType
AX = mybir.AxisListType

D = 1024
NCH = 8
TC = 256
TL = 2048
T = TC + TL
TOFF = [0, 256, 768, 1280, 1792]
TN = [256, 512, 512, 512, 512]
NTT = 5
NKT = T // 128
EPS = 1e-6
SELF_SYNC = True
CAP = 384
NCHUNK = (T + CAP - 1) // CAP
I32 = mybir.dt.int32
ET = mybir.EngineType
ENG_KEY = {ET.PE: "pe", ET.Activation: "act", ET.DVE: "dve", ET.Pool: "pool", ET.SP: "sp"}


def tsl(tt):
    return slice(TOFF[tt], TOFF[tt] + TN[tt])


def vec_layout(NE):
    ents = [("c", 8), ("cctx", 8), ("bmod", 96), ("nmix", 16), ("nffn", 16), ("fnorm", 8), ("evconv", 12),
            ("subln", 1), ("odconvw", 64), ("odconvb", 16), ("odgab", 16), ("odgxb", 16), ("odlam", 16),
            ("b1", 2 * NE * 16)]
    off = {}
    r = 0
    for name, n in ents:
        off[name] = r
        r += n
    rpad = ((r + 127) // 128) * 128
    return off, r, rpad


class Dep:
    __slots__ = ("w", "r", "sem", "tot")

    def __init__(self):
        self.w = None
        self.r = {}
        self.sem = None
        self.tot = 0


class G:
    def __init__(self, nc, es):
        self.nc = nc
        self.es = es
        self.eng = {"pe": nc.tensor, "act": nc.scalar, "dve": nc.vector, "pool": nc.gpsimd, "sp": nc.sync}
        self.semh = []
        self.esem = {}
        self.cnt = {}
        self.known = {}
        for k in self.eng:
            self.esem[k] = self.new_sem("e_" + k)
            self.cnt[k] = 0
            self.known[k] = {}
        self.dma_sems = []
        self.nsem = 0
        self.alldeps = []

    def new_sem(self, name):
        h = self.es.enter_context(self.nc.semaphore(name))
        self.semh.append(h)
        return len(self.semh) - 1

    def snapshot(self):
        deps = [(d, d.w, dict(d.r), d.tot) for d in self.alldeps]
        return (deps, dict(self.cnt), {k: dict(v) for k, v in self.known.items()})

    def restore(self, snap):
        deps, cnt, known = snap
        for d, w, r, tot in deps:
            d.w = w
            d.r = dict(r)
            d.tot = tot
        self.cnt = dict(cnt)
        self.known = {k: dict(v) for k, v in known.items()}

    def pad_to(self, big):
        deps, cnt, _ = big
        for e in self.eng:
            diff = cnt[e] - self.cnt[e]
            assert diff >= 0, (e, diff)
            if diff > 0:
                self.eng[e].wait_ge(self.semh[self.esem[e]], self.cnt[e])
                self.eng[e].sem_inc(self.semh[self.esem[e]], diff)
                self.cnt[e] = cnt[e]
        for d, w, r, tot in deps:
            if d.sem is None:
                continue
            diff = tot - d.tot
            assert diff >= 0
            if diff > 0:
                self.eng["sp"].wait_ge(self.semh[d.sem], d.tot)
                self.eng["sp"].sem_inc(self.semh[d.sem], diff)
                d.tot = tot

    def dep(self, dma=False):
        d = Dep()
        self.alldeps.append(d)
        if dma:
            self.nsem += 1
            d.sem = self.new_sem("d%d" % self.nsem)
            self.dma_sems.append(d)
        return d

    def _wait(self, e, deps):
        need = {}
        kn = self.known[e]
        for d in deps:
            if d is None:
                continue
            s, v = d
            if s == self.esem[e] and (e == "pe" or not SELF_SYNC):
                continue
            if kn.get(s, 0) >= v:
                continue
            if need.get(s, 0) < v:
                need[s] = v
        for s, v in need.items():
            self.eng[e].wait_ge(self.semh[s], v)
            kn[s] = v

    def op(self, e, fn, reads=(), writes=()):
        deps = []
        for t in reads:
            deps.append(t.w)
        for t in writes:
            deps.append(t.w)
            deps.extend(t.r.items())
        self._wait(e, deps)
        ins = fn()
        self.cnt[e] += 1
        s = self.esem[e]
        ins.then_inc(self.semh[s], 1)
        me = (s, self.cnt[e])
        for t in reads:
            t.r[s] = self.cnt[e]
        for t in writes:
            t.w = me
            t.r = {}
        return ins

    def dma(self, q, out, in_, reads=(), writes=(), sem_dep=None):
        deps = []
        for t in reads:
            deps.append(t.w)
        for t in writes:
            deps.append(t.w)
            deps.extend(t.r.items())
        self._wait(q, deps)
        sd = sem_dep if sem_dep is not None else writes[0]
        assert sd.sem is not None
        ins = self.eng[q].dma_start(out=out, in_=in_)
        sd.tot += 16
        ins.then_inc(self.semh[sd.sem], 16)
        me = (sd.sem, sd.tot)
        for t in reads:
            t.r[sd.sem] = sd.tot
        for t in writes:
            t.w = me
            t.r = {}
        return ins

    def idma(self, out, off_ap, in_, bound, reads=(), writes=()):
        deps = []
        for t in reads:
            deps.append(t.w)
        for t in writes:
            deps.append(t.w)
            deps.extend(t.r.items())
        self._wait("pool", deps)
        sd = writes[0]
        ins = self.nc.gpsimd.indirect_dma_start(out=out, out_offset=bass.IndirectOffsetOnAxis(ap=off_ap, axis=0), in_=in_, in_offset=None,
                                                bounds_check=bound, oob_is_err=False)
        sd.tot += 16
        ins.then_inc(self.semh[sd.sem], 16)
        me = (sd.sem, sd.tot)
        for t in reads:
            t.r[sd.sem] = sd.tot
        for t in writes:
            t.w = me
            t.r = {}
        return ins

    def barrier(self):
        for e in self.eng:
            deps = [(self.esem[o], self.cnt[o]) for o in self.eng if o != e and self.cnt[o] > 0]
            deps += [(d.sem, d.tot) for d in self.dma_sems if d.tot > 0]
            self._wait(e, deps)


def build(NE=32, dbg=None):
    nc = bass.Bass("TRN2", target_bir_lowering=False)
    voff, vrows, vpad = vec_layout(NE)
    NVB = vpad // 128

    def din(name, shape, dt=F32):
        return nc.dram_tensor(name, list(shape), dt, kind="ExternalInput").ap()

    xin = din("xin", [TL, D])
    ctxin = din("ctxin", [TC, D])
    vecs = din("vecs", [vpad, 128])
    w_mod = din("w_mod", [2, D, 6 * D])
    ev_w_in = din("ev_w_in", [D, 3 * D])
    ev_w_out = din("ev_w_out", [D, D])
    od_w_in = din("od_w_in", [D, 2 * D])
    od_w_out = din("od_w_out", [D, D])
    od_ga = din("od_ga", [2, 4, 256, 256])
    od_gx = din("od_gx", [2, 4, 256, 256])
    w_router = din("w_router", [2, D, NE])
    b_router = din("b_router", [2, NE])
    moe_w1 = din("moe_w1", [2, NE, D, 2 * D])
    moe_w2 = din("moe_w2", [2, NE, D, D])
    moe_b2 = din("moe_b2", [2, NE, D])
    lams = din("lams", [4, 64])
    ident_d = din("ident", [128, 128])
    perm_d = din("perm", [128, 128])
    cos_d = din("cosT", [128, T])
    sin_d = din("sinT", [128, T])
    utri_d = din("utri", [128, 128])
    offrow_d = din("offrow", [128, NE])
    tok4f_d = din("tok4f", [128, 4])
    out_d = nc.dram_tensor("out", [TL, D], F32, kind="ExternalOutput").ap()
    xs_d = nc.dram_tensor("xs_scratch", [128, NCH, T], F32, kind="Internal").ap()
    gsc_d = nc.dram_tensor("g_scratch", [2, NE, T], F32, kind="Internal").ap()
    psc_d = nc.dram_tensor("p_scratch", [2, NE, T], F32, kind="Internal").ap()
    cnt_d = nc.dram_tensor("cnt_scratch", [2, NE], I32, kind="Internal").ap()
    hs_d = nc.dram_tensor("hs_scratch", [NE * T, D], BF16, kind="Internal").ap()
    idx_d = nc.dram_tensor("idx_scratch", [NE * T, 1], I32, kind="Internal").ap()
    y4_d = nc.dram_tensor("y4_scratch", [4 * T, D], F32, kind="Internal").ap()
    dbg_d = None
    if dbg is not None:
        dbg_d = nc.dram_tensor("dbg", [128, NCH, T], F32, kind="ExternalOutput").ap()

    es = ExitStack()
    with es:
        g = G(nc, es)
        xs_dep = g.dep(dma=True)
        gsc_dep = [g.dep(dma=True), g.dep(dma=True)]
        psc_dep = [g.dep(dma=True), g.dep(dma=True)]
        flag_dep = [g.dep(dma=True), g.dep(dma=True)]
        hs_dep = g.dep(dma=True)
        bnd_hs = nc.gpsimd.alloc_register("bnd_hs")
        nc.gpsimd.reg_mov(bnd_hs, NE * T - 1)
        bnd_y4 = nc.gpsimd.alloc_register("bnd_y4")
        nc.gpsimd.reg_mov(bnd_y4, 4 * T - 1)
        idx_dep = g.dep(dma=True)
        y4_dep = g.dep(dma=True)
        flag_regs = nc.alloc_registers("ovf", [ET.PE, ET.Activation, ET.DVE, ET.Pool, ET.SP])
        out_dep = g.dep(dma=True)

        _uid = [0]

        def sb(es_, name, shape, dt, side=None):
            _uid[0] += 1
            return es_.enter_context(nc.sbuf_tensor("sb%d_%s" % (_uid[0], name), list(shape), dt, side=side))

        ps = [es.enter_context(nc.psum_tensor("ps%d" % i, [128, 512], F32)) for i in range(8)]
        psd = [g.dep() for _ in range(8)]

        ident = sb(es, "ident", [128, 128], F32)
        perm = sb(es, "perm", [128, 128], F32)
        ones32 = sb(es, "ones32", [128, 128], F32)
        ones16 = sb(es, "ones16", [128, 128], BF16)
        V = sb(es, "V", [128, vpad], F32)
        modT = sb(es, "modT", [128, 2, 48, 2], F32)
        S = sb(es, "S", [128, 160], F32)
        cdep = g.dep(dma=True)
        Vd = g.dep()
        modd = g.dep()
        Sd = g.dep()
        onesd = g.dep()
        g.dma("sp", ident[:], ident_d, writes=[cdep])
        g.dma("sp", perm[:], perm_d, writes=[cdep])
        identb = sb(es, "identb", [128, 128], BF16)
        utri = sb(es, "utri", [128, 128], F32)
        offrow = sb(es, "offrow", [128, NE], F32)
        tok4f = sb(es, "tok4f", [128, 4], F32)
        cdep2 = g.dep(dma=True)
        g.dma("pool", identb[:], ident_d, writes=[cdep2])
        g.dma("sp", utri[:], utri_d, writes=[cdep2])
        g.dma("sp", offrow[:], offrow_d, writes=[cdep2])
        g.dma("sp", tok4f[:], tok4f_d, writes=[cdep2])
        g.op("dve", lambda: nc.vector.memset(ones32[:], 1.0), writes=[onesd])
        g.op("dve", lambda: nc.vector.memset(ones16[:], 1.0), writes=[onesd])

        SC = {}
        _sc = [0]

        def scol(name, n):
            SC[name] = _sc[0]
            _sc[0] += n
            return SC[name]

        for l in range(2):
            for cls in range(2):
                scol("gs1_%d_%d" % (l, cls), 8)
                scol("gs2_%d_%d" % (l, cls), 8)
        scol("sg", 1)
        scol("neglam", 1)
        scol("c8", 16)
        scol("c8x2", 16)
        scol("tmp", 16)
        assert _sc[0] <= 160

        def Scol(name, i=0):
            return S[:, SC[name] + i:SC[name] + i + 1]

        def Vcol(name, i=0):
            return V[:, voff[name] + i:voff[name] + i + 1]

        def mod(l, k, c, cls):
            return modT[:, l, k * 8 + c, cls:cls + 1]

        pes = ExitStack()
        with pes:
            vstg = [sb(pes, "vstg%d" % i, [128, 128], F32) for i in range(2)]
            vstd = [g.dep(dma=True) for _ in range(2)]
            for blk in range(NVB):
                s = blk % 2
                g.dma("sp", vstg[s][:], vecs[blk * 128:(blk + 1) * 128, :], writes=[vstd[s]])
                pb = blk % 2
                g.op("pe", lambda: nc.tensor.transpose(out=ps[pb][:, 0:128], in_=vstg[s][:], identity=ident[:]),
                     reads=[vstd[s], cdep], writes=[psd[pb]])
                g.op("act", lambda: nc.scalar.copy(out=V[:, blk * 128:(blk + 1) * 128], in_=ps[pb][:, 0:128]),
                     reads=[psd[pb]], writes=[Vd])
            b1v = V[:, voff["b1"]:voff["b1"] + 2 * NE * 16].rearrange("p (a j) -> p a j", j=16)
            g.op("dve", lambda: nc.vector.tensor_scalar(out=b1v[:, :, 8:16], in0=b1v[:, :, 8:16], scalar1=1.0,
                                                        scalar2=None, op0=ALU.add), reads=[Vd], writes=[Vd])
            scT = sb(pes, "scT", [128, 8, 2], F32R)
            scd = g.dep()
            g.op("act", lambda: nc.scalar.activation(out=scT[:, :, 0], in_=V[:, voff["c"]:voff["c"] + 8],
                                                     func=AF.Silu), reads=[Vd], writes=[scd])
            g.op("act", lambda: nc.scalar.activation(out=scT[:, :, 1], in_=V[:, voff["cctx"]:voff["cctx"] + 8],
                                                     func=AF.Silu), reads=[Vd], writes=[scd])
            wm = [sb(pes, "wm%d" % i, [128, 8, 512], F32R) for i in range(2)]
            wmd = [g.dep(dma=True) for _ in range(2)]
            it = 0
            for l in range(2):
                for blk in range(12):
                    s = it % 2
                    it += 1
                    src = w_mod[l, :, blk * 512:(blk + 1) * 512].rearrange("(c p) f -> p c f", p=128)
                    g.dma("pool", wm[s][:], src, writes=[wmd[s]])
                    for fcl in range(4):
                        pb = (blk * 4 + fcl) % 2
                        for dc in range(8):
                            g.op("pe", lambda: nc.tensor.matmul(ps[pb][:, 0:2], lhsT=wm[s][:, dc, fcl * 128:(fcl + 1) * 128],
                                                                rhs=scT[:, dc, :], start=(dc == 0), stop=(dc == 7)),
                                 reads=[wmd[s], scd], writes=[psd[pb]])
                        if True:
                            kk = blk * 4 + fcl
                            g.op("dve", lambda: nc.vector.tensor_scalar(out=modT[:, l, kk, :], in0=ps[pb][:, 0:2],
                                                                        scalar1=Vcol("bmod", l * 48 + kk), scalar2=None,
                                                                        op0=ALU.add), reads=[psd[pb], Vd], writes=[modd])
            for l in range(2):
                for cls in range(2):
                    for (nm, kc, vn) in (("gs1", 1, "nmix"), ("gs2", 4, "nffn")):
                        c0 = SC["%s_%d_%d" % (nm, l, cls)]
                        g.op("dve", lambda: nc.vector.scalar_tensor_tensor(
                            out=S[:, c0:c0 + 8], in0=modT[:, l, kc * 8:kc * 8 + 8, cls], scalar=1.0,
                            in1=V[:, voff[vn] + l * 8:voff[vn] + l * 8 + 8], op0=ALU.add, op1=ALU.mult),
                            reads=[modd, Vd], writes=[Sd])
            lam_init0 = 0.8 - 0.6 * math.exp(-0.3 * 0)
            g.op("dve", lambda: nc.vector.tensor_scalar(out=Scol("sg"), in0=Vcol("subln"), scalar1=(1.0 - lam_init0),
                                                        scalar2=None, op0=ALU.mult), reads=[Vd], writes=[Sd])
            lb = sb(pes, "lamb", [128, 4, 64], F32)
            lbd = g.dep(dma=True)
            for i in range(4):
                g.dma("sp", lb[:, i, :], lams[i:i + 1, :].to_broadcast([128, 64]), writes=[lbd])
            lt = sb(pes, "lamt", [128, 2, 64], F32)
            ltd = g.dep()
            g.op("dve", lambda: nc.vector.tensor_tensor(out=lt[:, 0, :], in0=lb[:, 0, :], in1=lb[:, 1, :], op=ALU.mult),
                 reads=[lbd], writes=[ltd])
            g.op("dve", lambda: nc.vector.tensor_tensor(out=lt[:, 1, :], in0=lb[:, 2, :], in1=lb[:, 3, :], op=ALU.mult),
                 reads=[lbd], writes=[ltd])
            tm = SC["tmp"]
            g.op("dve", lambda: nc.vector.tensor_reduce(out=S[:, tm:tm + 2], in_=lt[:], axis=AX.X, op=ALU.add),
                 reads=[ltd], writes=[Sd])
            g.op("act", lambda: nc.scalar.activation(out=S[:, tm + 2:tm + 4], in_=S[:, tm:tm + 2], func=AF.Exp),
                 reads=[Sd], writes=[Sd])
            g.op("dve", lambda: nc.vector.scalar_tensor_tensor(out=Scol("neglam"), in0=S[:, tm + 3:tm + 4],
                                                               scalar=-lam_init0, in1=S[:, tm + 2:tm + 3],
                                                               op0=ALU.add, op1=ALU.subtract), reads=[Sd], writes=[Sd])
            g.op("act", lambda: nc.scalar.activation(out=S[:, tm:tm + 16], in_=V[:, voff["odlam"]:voff["odlam"] + 16],
                                                     func=AF.Exp, scale=-1.0), reads=[Vd, Sd], writes=[Sd])
            g.op("act", lambda: nc.scalar.activation(out=S[:, tm:tm + 16], in_=S[:, tm:tm + 16], func=AF.Ln, bias=1.0),
                 reads=[Sd], writes=[Sd])
            g.op("dve", lambda: nc.vector.tensor_scalar(out=S[:, SC["c8"]:SC["c8"] + 16], in0=S[:, tm:tm + 16],
                                                        scalar1=-8.0, scalar2=None, op0=ALU.mult), reads=[Sd], writes=[Sd])
            g.op("dve", lambda: nc.vector.tensor_scalar(out=S[:, SC["c8x2"]:SC["c8x2"] + 16], in0=S[:, tm:tm + 16],
                                                        scalar1=-16.0, scalar2=None, op0=ALU.mult), reads=[Sd], writes=[Sd])
            g.barrier()
        hb = sb(es, "hb", [128, NCH, T], BF16)
        hbd = [[g.dep() for _ in range(NTT)] for _ in range(NCH)]
        xes = ExitStack()
        xstate = {}

        def alloc_x():
            xstate["x"] = xes.enter_context(nc.sbuf_tensor("xres%d" % len(xstate), [128, NCH, T], F32, side="right"))
            xstate["d"] = [[g.dep() for _ in range(NTT)] for _ in range(NCH)]

        def load_tok_tile(pes_bufs, tt, l0_src=True):
            xst, xstd, tstg, tstgd, cnt = pes_bufs
            s = cnt[0] % 2
            cnt[0] += 1
            n = TN[tt]
            for sub in range(n // 128):
                k = cnt[1] % 2
                cnt[1] += 1
                if tt == 0:
                    src = ctxin[sub * 128:(sub + 1) * 128, :]
                else:
                    r0 = TOFF[tt] - TC + sub * 128
                    src = xin[r0:r0 + 128, :]
                g.dma("sp", tstg[k][:], src, writes=[tstgd[k]])
                for half in range(2):
                    pb = 6 + half
                    for cc in range(4):
                        c = half * 4 + cc
                        g.op("pe", lambda: nc.tensor.transpose(out=ps[pb][:, cc * 128:(cc + 1) * 128],
                                                               in_=tstg[k][:, c * 128:(c + 1) * 128], identity=ident[:]),
                             reads=[tstgd[k], cdep], writes=[psd[pb]])
                    dst = xst[s][:, half * 4:half * 4 + 4, sub * 128:(sub + 1) * 128]
                    srcp = ps[pb][:, :].rearrange("p (c t) -> p c t", t=128)
                    if half == 0:
                        g.op("act", lambda: nc.scalar.copy(out=dst, in_=srcp), reads=[psd[pb]], writes=[xstd[s]])
                    else:
                        g.op("dve", lambda: nc.vector.tensor_copy(out=dst, in_=srcp), reads=[psd[pb]], writes=[xstd[s]])
            return xst[s], xstd[s]

        def rmsnorm_tile(nb, xsrc, xdeps, tt, gsname, l, k_sh, out32=None, out32d=None, hbout=None):
            sq, sqd, tmp, tmpd, rs, rsd, cnt = nb
            n = TN[tt]
            cls = 1 if tt == 0 else 0
            pb = 5
            for c in range(NCH):
                s = cnt[0] % 2
                cnt[0] += 1
                g.op("act", lambda: nc.scalar.activation(out=sq[s][:, 0:n], in_=xsrc(c), func=AF.Square),
                     reads=[xdeps(c)], writes=[sqd[s]])
                g.op("pe", lambda: nc.tensor.matmul(ps[pb][:, 0:n], lhsT=ones32[:], rhs=sq[s][:, 0:n],
                                                    start=(c == 0), stop=(c == NCH - 1)),
                     reads=[sqd[s], onesd], writes=[psd[pb]])
            g.op("act", lambda: nc.scalar.activation(out=rs[:, 0:n], in_=ps[pb][:, 0:n], func=AF.Sqrt,
                                                     scale=1.0 / D, bias=EPS), reads=[psd[pb]], writes=[rsd])
            g.op("dve", lambda: nc.vector.reciprocal(out=rs[:, 0:n], in_=rs[:, 0:n]), reads=[rsd], writes=[rsd])
            c0 = SC["%s_%d_%d" % (gsname, l, cls)]
            for c in range(NCH):
                s = cnt[1] % 2
                cnt[1] += 1
                g.op("dve", lambda: nc.vector.tensor_tensor(out=tmp[s][:, 0:n], in0=xsrc(c), in1=rs[:, 0:n], op=ALU.mult),
                     reads=[xdeps(c), rsd], writes=[tmpd[s]])
                ho, hod = (hb[:, c, tsl(tt)], hbd[c][tt]) if hbout is None else hbout(c)
                g.op("act", lambda: nc.scalar.activation(out=ho, in_=tmp[s][:, 0:n], func=AF.Identity,
                                                         scale=S[:, c0 + c:c0 + c + 1], bias=mod(l, k_sh, c, cls)),
                     reads=[tmpd[s], Sd, modd], writes=[hod])
                if out32 is not None:
                    g.op("dve", lambda: nc.vector.tensor_scalar(out=out32[:, c, 0:n], in0=tmp[s][:, 0:n],
                                                                scalar1=S[:, c0 + c:c0 + c + 1],
                                                                scalar2=mod(l, k_sh, c, cls), op0=ALU.mult, op1=ALU.add),
                         reads=[tmpd[s], Sd, modd], writes=[out32d])

        def norm_bufs(es_):
            sq = [sb(es_, "nsq%d" % i, [128, 512], F32) for i in range(2)]
            tmp = [sb(es_, "ntmp%d" % i, [128, 512], F32) for i in range(2)]
            rs = sb(es_, "nrs", [128, 512], F32)
            return (sq, [g.dep(), g.dep()], tmp, [g.dep(), g.dep()], rs, g.dep(), [0, 0])

        def stage_bufs(es_):
            xst = [sb(es_, "xst%d" % i, [128, NCH, 512], F32) for i in range(2)]
            tstg = [sb(es_, "tstg%d" % i, [128, D], F32) for i in range(2)]
            return (xst, [g.dep(dma=True), g.dep(dma=True)], tstg, [g.dep(dma=True), g.dep(dma=True)], [0, 0])

        def out_proj(l, wout_d, catfn, catdeps, tiles, xold_fn, es_):
            wo = sb(es_, "wo%d" % l, [128, NCH, D], BF16)
            wod = g.dep(dma=True)
            for hh in range(2):
                g.dma("pool", wo[:, :, hh * 512:(hh + 1) * 512],
                      wout_d[:, hh * 512:(hh + 1) * 512].rearrange("(c p) f -> p c f", p=128), writes=[wod])
            x = xstate["x"]
            xd = xstate["d"]
            it = 0
            for tt in tiles:
                n = TN[tt]
                cls = 1 if tt == 0 else 0
                xo, xod = xold_fn(tt)
                for oc in range(NCH):
                    pb = it % 2
                    it += 1
                    for c in range(NCH):
                        g.op("pe", lambda: nc.tensor.matmul(ps[pb][:, 0:n], lhsT=wo[:, c, oc * 128:(oc + 1) * 128],
                                                            rhs=catfn(c, tt), start=(c == 0), stop=(c == NCH - 1)),
                             reads=[wod, catdeps(c, tt)], writes=[psd[pb]])
                    g.op("dve", lambda: nc.vector.scalar_tensor_tensor(out=x[:, oc, tsl(tt)], in0=ps[pb][:, 0:n],
                                                                       scalar=mod(l, 2, oc, cls), in1=xo[:, oc, 0:n],
                                                                       op0=ALU.mult, op1=ALU.add),
                         reads=[psd[pb], xod, modd], writes=[xd[oc][tt]])

        L0 = ExitStack()
        with L0:
            catc = sb(L0, "catc", [128, 4, T], BF16)
            catcd = [[g.dep() for _ in range(NTT)] for _ in range(4)]
            A0 = ExitStack()
            with A0:
                stg = stage_bufs(A0)
                nb = norm_bufs(A0)
                for tt in range(NTT):
                    xt, xtd = load_tok_tile(stg, tt)
                    rmsnorm_tile(nb, lambda c: xt[:, c, 0:TN[tt]], lambda c: xtd, tt, "gs1", 0, 0)
                g.barrier()
            M0 = ExitStack()
            with M0:
                wr = [sb(M0, "w0r%d" % i, [128, NCH, 512], BF16) for i in range(3)]
                wrd = [g.dep(dma=True) for _ in range(3)]

                def load_win(slot, blk):
                    g.dma("pool", wr[slot][:], ev_w_in[:, blk * 512:(blk + 1) * 512].rearrange("(c p) f -> p c f", p=128),
                          writes=[wrd[slot]])

                load_win(0, 3)
                load_win(1, 4)
                load_win(2, 5)
                CV = ExitStack()
                with CV:
                    pj = sb(CV, "pj", [128, T], F32)
                    accj = sb(CV, "accj", [128, T], F32)
                    ctmp = [sb(CV, "ctmp%d" % i, [128, 512], F32) for i in range(2)]
                    pjd = g.dep()
                    accd = g.dep()
                    ctd = [g.dep(), g.dep()]
                    it = 0
                    for j in range(4):
                        for tt in range(NTT):
                            n = TN[tt]
                            for which, pb in ((1, 0), (2, 1)):
                                for c in range(NCH):
                                    g.op("pe", lambda: nc.tensor.matmul(ps[pb][:, 0:n], lhsT=wr[which][:, c, j * 128:(j + 1) * 128],
                                                                        rhs=hb[:, c, tsl(tt)], start=(c == 0), stop=(c == NCH - 1)),
                                         reads=[wrd[which], hbd[c][tt]], writes=[psd[pb]])
                            s = it % 2
                            it += 1
                            g.op("act", lambda: nc.scalar.copy(out=ctmp[s][:, 0:n], in_=ps[0][:, 0:n]), reads=[psd[0]], writes=[ctd[s]])
                            g.op("dve", lambda: nc.vector.tensor_tensor(out=pj[:, tsl(tt)], in0=ps[1][:, 0:n], in1=ctmp[s][:, 0:n],
                                                                        op=ALU.mult), reads=[psd[1], ctd[s]], writes=[pjd])
                        for (a, b) in ((0, TC), (TC, T)):
                            g.op("dve", lambda: nc.vector.tensor_scalar(out=accj[:, a:b], in0=pj[:, a:b], scalar1=Vcol("evconv", 1 * 4 + j),
                                                                        scalar2=None, op0=ALU.mult), reads=[pjd, Vd], writes=[accd])
                            g.op("dve", lambda: nc.vector.scalar_tensor_tensor(out=accj[:, a + 1:b], in0=pj[:, a:b - 1],
                                                                               scalar=Vcol("evconv", 0 * 4 + j), in1=accj[:, a + 1:b],
                                                                               op0=ALU.mult, op1=ALU.add), reads=[pjd, Vd, accd], writes=[accd])
                            g.op("dve", lambda: nc.vector.scalar_tensor_tensor(out=accj[:, a:b - 1], in0=pj[:, a + 1:b],
                                                                               scalar=Vcol("evconv", 2 * 4 + j), in1=accj[:, a:b - 1],
                                                                               op0=ALU.mult, op1=ALU.add), reads=[pjd, Vd, accd], writes=[accd])
                        for tt in range(NTT):
                            n = TN[tt]
                            pb = 2 + (tt % 2)
                            for c in range(NCH):
                                g.op("pe", lambda: nc.tensor.matmul(ps[pb][:, 0:n], lhsT=wr[0][:, c, j * 128:(j + 1) * 128],
                                                                    rhs=hb[:, c, tsl(tt)], start=(c == 0), stop=(c == NCH - 1)),
                                     reads=[wrd[0], hbd[c][tt]], writes=[psd[pb]])
                            g.op("dve", lambda: nc.vector.tensor_tensor(out=catc[:, j, tsl(tt)], in0=ps[pb][:, 0:n], in1=accj[:, tsl(tt)],
                                                                        op=ALU.mult), reads=[psd[pb], accd], writes=[catcd[j][tt]])
                    g.barrier()
                load_win(0, 0)
                load_win(1, 1)
                load_win(2, 2)
                qk = [sb(M0, "q", [128, 4, T], BF16), sb(M0, "k", [128, 4, T], BF16)]
                qkd = [[[g.dep() for _ in range(NTT)] for _ in range(4)] for _ in range(2)]
                vt = sb(M0, "v", [128, NKT, 512], BF16)
                vtd = [g.dep() for _ in range(NKT)]
                cosT = sb(M0, "cosT", [128, T], F32)
                sinT = sb(M0, "sinT", [128, T], F32)
                tabd = g.dep(dma=True)
                g.dma("sp", cosT[:], cos_d, writes=[tabd])
                g.dma("sp", sinT[:], sin_d, writes=[tabd])
                RP = ExitStack()
                with RP:
                    qf = [sb(RP, "qf%d" % i, [128, 512], F32) for i in range(2)]
                    qfd = [g.dep(), g.dep()]
                    t1 = [sb(RP, "rt1%d" % i, [128, 512], F32) for i in range(2)]
                    t1d = [g.dep(), g.dep()]
                    t2 = [sb(RP, "rt2%d" % i, [128, 512], F32) for i in range(2)]
                    t2d = [g.dep(), g.dep()]
                    it = 0
                    for which in range(2):
                        for hc in range(4):
                            for tt in range(NTT):
                                n = TN[tt]
                                s = it % 2
                                it += 1
                                pb = s
                                pr = 2 + s
                                for c in range(NCH):
                                    g.op("pe", lambda: nc.tensor.matmul(ps[pb][:, 0:n], lhsT=wr[which][:, c, hc * 128:(hc + 1) * 128],
                                                                        rhs=hb[:, c, tsl(tt)], start=(c == 0), stop=(c == NCH - 1)),
                                         reads=[wrd[which], hbd[c][tt]], writes=[psd[pb]])
                                g.op("act", lambda: nc.scalar.copy(out=qf[s][:, 0:n], in_=ps[pb][:, 0:n]), reads=[psd[pb]], writes=[qfd[s]])
                                g.op("pe", lambda: nc.tensor.matmul(ps[pr][:, 0:n], lhsT=perm[:], rhs=qf[s][:, 0:n], start=True, stop=True),
                                     reads=[qfd[s], cdep], writes=[psd[pr]])
                                g.op("dve", lambda: nc.vector.tensor_tensor(out=t1[s][:, 0:n], in0=qf[s][:, 0:n], in1=cosT[:, tsl(tt)], op=ALU.mult),
                                     reads=[qfd[s], tabd], writes=[t1d[s]])
                                g.op("dve", lambda: nc.vector.tensor_tensor(out=t2[s][:, 0:n], in0=ps[pr][:, 0:n], in1=sinT[:, tsl(tt)], op=ALU.mult),
                                     reads=[psd[pr], tabd], writes=[t2d[s]])
                                g.op("pool", lambda: nc.gpsimd.tensor_tensor(out=qk[which][:, hc, tsl(tt)], in0=t1[s][:, 0:n], in1=t2[s][:, 0:n], op=ALU.add),
                                     reads=[t1d[s], t2d[s]], writes=[qkd[which][hc][tt]])
                    for kt in range(NKT):
                        tt = 0 if kt < 2 else 1 + (kt - 2) // 4
                        pb = 4 + kt % 2
                        for c in range(NCH):
                            g.op("pe", lambda: nc.tensor.matmul(ps[pb][:, :], lhsT=hb[:, c, kt * 128:(kt + 1) * 128], rhs=wr[2][:, c, :],
                                                                start=(c == 0), stop=(c == NCH - 1)),
                                 reads=[wrd[2], hbd[c][tt]], writes=[psd[pb]])
                        g.op("act", lambda: nc.scalar.copy(out=vt[:, kt, :], in_=ps[pb][:, :]), reads=[psd[pb]], writes=[vtd[kt]])
                    g.barrier()
                AT = ExitStack()
                with AT:
                    eb = [[sb(AT, "e%d_%d" % (m, i), [128, 512], BF16) for i in range(2)] for m in range(2)]
                    ebd = [[g.dep(), g.dep()] for _ in range(2)]
                    rz = [sb(AT, "rz%d" % m, [128, 512], F32) for m in range(2)]
                    rzd = [g.dep(), g.dep()]
                    to = [sb(AT, "to%d" % m, [128, 512], F32) for m in range(2)]
                    tod = [g.dep(), g.dep()]
                    osb = sb(AT, "osb", [128, 512], F32)
                    osd = g.dep()
                    osq = sb(AT, "osq", [128, 512], F32)
                    osqd = g.dep()
                    ors = sb(AT, "ors", [128, 512], F32)
                    orsd = g.dep()
                    for h in range(4):
                        for qt in range(NTT):
                            n = TN[qt]
                            nkt = 2 if qt == 0 else NKT
                            def scores(kt):
                                ktt = 0 if kt < 2 else 1 + (kt - 2) // 4
                                sl = kt % 2
                                for m in range(2):
                                    pbs = 4 + 2 * m + sl
                                    g.op("pe", lambda: nc.tensor.matmul(ps[pbs][:, 0:n], lhsT=qk[1][m * 64:(m + 1) * 64, h, kt * 128:(kt + 1) * 128],
                                                                        rhs=qk[0][m * 64:(m + 1) * 64, h, tsl(qt)], start=True, stop=True),
                                         reads=[qkd[1][h][ktt], qkd[0][h][qt]], writes=[psd[pbs]])
                                    g.op("act", lambda: nc.scalar.activation(out=eb[m][sl][:, 0:n], in_=ps[pbs][:, 0:n], func=AF.Exp, scale=0.125),
                                         reads=[psd[pbs]], writes=[ebd[m][sl]])

                            scores(0)
                            for kt in range(nkt):
                                sl = kt % 2
                                if kt + 1 < nkt:
                                    scores(kt + 1)
                                for m in range(2):
                                    g.op("pe", lambda: nc.tensor.matmul(ps[2 * m][:, 0:n], lhsT=vt[:, kt, h * 128:(h + 1) * 128], rhs=eb[m][sl][:, 0:n],
                                                                        start=(kt == 0), stop=(kt == nkt - 1)),
                                         reads=[vtd[kt], ebd[m][sl]], writes=[psd[2 * m]])
                                    g.op("pe", lambda: nc.tensor.matmul(ps[2 * m + 1][:, 0:n], lhsT=ones16[:], rhs=eb[m][sl][:, 0:n],
                                                                        start=(kt == 0), stop=(kt == nkt - 1)),
                                         reads=[onesd, ebd[m][sl]], writes=[psd[2 * m + 1]])
                            for m in range(2):
                                g.op("dve", lambda: nc.vector.reciprocal(out=rz[m][:, 0:n], in_=ps[2 * m + 1][:, 0:n]), reads=[psd[2 * m + 1]], writes=[rzd[m]])
                                g.op("dve", lambda: nc.vector.tensor_tensor(out=to[m][:, 0:n], in0=ps[2 * m][:, 0:n], in1=rz[m][:, 0:n], op=ALU.mult),
                                     reads=[psd[2 * m], rzd[m]], writes=[tod[m]])
                            g.op("dve", lambda: nc.vector.scalar_tensor_tensor(out=osb[:, 0:n], in0=to[1][:, 0:n], scalar=Scol("neglam"), in1=to[0][:, 0:n],
                                                                               op0=ALU.mult, op1=ALU.add), reads=[tod[0], tod[1], Sd], writes=[osd])
                            g.op("act", lambda: nc.scalar.activation(out=osq[:, 0:n], in_=osb[:, 0:n], func=AF.Square), reads=[osd], writes=[osqd])
                            g.op("pe", lambda: nc.tensor.matmul(ps[4][:, 0:n], lhsT=ones32[:], rhs=osq[:, 0:n], start=True, stop=True),
                                 reads=[osqd, onesd], writes=[psd[4]])
                            g.op("act", lambda: nc.scalar.activation(out=ors[:, 0:n], in_=ps[4][:, 0:n], func=AF.Sqrt, scale=1.0 / 128, bias=EPS),
                                 reads=[psd[4]], writes=[orsd])
                            g.op("dve", lambda: nc.vector.reciprocal(out=ors[:, 0:n], in_=ors[:, 0:n]), reads=[orsd], writes=[orsd])
                            g.op("dve", lambda: nc.vector.tensor_tensor(out=osb[:, 0:n], in0=osb[:, 0:n], in1=ors[:, 0:n], op=ALU.mult),
                                 reads=[orsd, osd], writes=[osd])
                            g.op("act", lambda: nc.scalar.activation(out=hb[:, h, tsl(qt)], in_=osb[:, 0:n], func=AF.Identity, scale=Scol("sg")),
                                 reads=[osd, Sd], writes=[hbd[h][qt]])
                    g.barrier()
            alloc_x()
            C0 = ExitStack()
            with C0:
                stg = stage_bufs(C0)

                def xold0(tt):
                    return load_tok_tile(stg, tt)

                def cat0(c, tt):
                    return hb[:, c, tsl(tt)] if c < 4 else catc[:, c - 4, tsl(tt)]

                def cat0d(c, tt):
                    return hbd[c][tt] if c < 4 else catcd[c - 4][tt]

                out_proj(0, ev_w_out, cat0, cat0d, range(NTT), xold0, C0)
                g.barrier()

        def moe_layer(l, tiles):
            x = xstate["x"]
            xd = xstate["d"]
            subs = []
            for tt in tiles:
                for sub in range(TN[tt] // 128):
                    subs.append((TOFF[tt] // 128 + sub, tt, sub))
            hbTok = hb[:].rearrange("p c t -> p (c t)").rearrange("p (i f) -> p i f", f=D)
            hbtokd = [g.dep() for _ in range(NKT)]
            ML = ExitStack()
            with ML:
                GK = sb(ML, "GK", [128, NKT, 4], F32)
                GKd = g.dep()
                DD = ExitStack()
                with DD:
                    nb = norm_bufs(DD)
                    h32 = sb(DD, "h32", [128, NCH, 512], F32)
                    h32d = g.dep()
                    hbt = sb(DD, "hbt", [128, NCH, 512], BF16)
                    hbtd = g.dep()
                    wrt = sb(DD, "wrt", [128, NCH, NE], F32)
                    brt = sb(DD, "brt", [1, NE], F32)
                    b2n = sb(DD, "b2n", [NE, D], F32)
                    rtd = g.dep(dma=True)
                    g.dma("sp", wrt[:], w_router[l].rearrange("(c p) e -> p c e", p=128), writes=[rtd])
                    g.dma("sp", brt[:], b_router[l:l + 1, :], writes=[rtd])
                    g.dma("sp", b2n[:], moe_b2[l], writes=[rtd])
                    gT = sb(DD, "gT", [NE, T], F32)
                    gTd = g.dep()
                    carry = sb(DD, "carry", [128, NE], F32)
                    card = g.dep()
                    g.op("dve", lambda: nc.vector.memset(carry[:], 0.0), writes=[card])
                    oob = sb(DD, "oob", [128, NE * T // 128], I32)
                    oobd = g.dep()
                    g.op("pool", lambda: nc.gpsimd.memset(oob[:], 2000000000), writes=[oobd])
                    g.dma("sp", idx_d[:, :].rearrange("(p r) o -> p (r o)", p=128), oob[:], reads=[oobd], writes=[idx_dep])
                    lg = sb(DD, "lg", [128, NE], F32)
                    ex = sb(DD, "ex", [128, NE], F32)
                    mk = sb(DD, "mk", [128, NE], F32)
                    gt_ = sb(DD, "gt_", [128, NE], F32)
                    ngd = sb(DD, "ngd", [128, NE], F32)
                    t8 = sb(DD, "t8", [128, 8], F32)
                    t8b = sb(DD, "t8b", [128, 8], F32)
                    sm = sb(DD, "sm", [128, 4], F32)
                    fli = sb(DD, "fli", [128, NE], I32)
                    NIX = 3
                    idx4 = [sb(DD, "idx4_%d" % i, [128, 4], I32) for i in range(NIX)]
                    val4 = [sb(DD, "val4_%d" % i, [128, 4], I32) for i in range(NIX)]
                    ixd = [g.dep() for _ in range(NIX)]
                    rd = g.dep()
                    mkd = g.dep()
                    ixc = 0
                    for tt in tiles:
                        n = TN[tt]
                        rmsnorm_tile(nb, lambda c: x[:, c, tsl(tt)], lambda c: xd[c][tt], tt, "gs2", l, 3, out32=h32, out32d=h32d,
                                     hbout=lambda c: (hbt[:, c, 0:n], hbtd))
                        for sub in range(n // 128):
                            t0 = TOFF[tt] + sub * 128
                            i = t0 // 128
                            psb = ps[6][:, :].bitcast(BF16)
                            for c in range(NCH):
                                g.op("pe", lambda: nc.tensor.transpose(out=psb[:, c * 128:(c + 1) * 128], in_=hbt[:, c, sub * 128:(sub + 1) * 128], identity=identb[:]),
                                     reads=[hbtd, cdep2], writes=[psd[6]])
                            g.op("act", lambda: nc.scalar.copy(out=hbTok[:, i, :], in_=psb), reads=[psd[6]], writes=[hbtokd[i]])
                            pb = 0
                            for c in range(NCH):
                                g.op("pe", lambda: nc.tensor.matmul(ps[pb][:, 0:NE], lhsT=h32[:, c, sub * 128:(sub + 1) * 128], rhs=wrt[:, c, :],
                                                                    start=(c == 0), stop=False),
                                     reads=[h32d, rtd], writes=[psd[pb]])
                            g.op("pe", lambda: nc.tensor.matmul(ps[pb][:, 0:NE], lhsT=ones32[0:1, :], rhs=brt[0:1, :], start=False, stop=True),
                                 reads=[rtd, onesd], writes=[psd[pb]])
                            g.op("act", lambda: nc.scalar.copy(out=lg[:], in_=ps[pb][:, 0:NE]), reads=[psd[pb]], writes=[rd])
                            g.op("dve", lambda: nc.vector.max(out=t8[:], in_=lg[:]), reads=[rd], writes=[rd])
                            g.op("dve", lambda: nc.vector.tensor_scalar(out=sm[:, 0:1], in0=t8[:, 0:1], scalar1=-1.0, scalar2=None, op0=ALU.mult),
                                 reads=[rd], writes=[rd])
                            g.op("act", lambda: nc.scalar.activation(out=ex[:], in_=lg[:], func=AF.Exp, bias=sm[:, 0:1]), reads=[rd], writes=[rd])
                            g.op("dve", lambda: nc.vector.tensor_scalar(out=mk[:], in0=lg[:], scalar1=t8[:, 3:4], scalar2=None, op0=ALU.is_ge),
                                 reads=[rd, mkd], writes=[rd, mkd])
                            g.op("dve", lambda: nc.vector.tensor_tensor(out=ex[:], in0=ex[:], in1=mk[:], op=ALU.mult), reads=[rd], writes=[rd])
                            g.op("dve", lambda: nc.vector.tensor_reduce(out=sm[:, 1:2], in_=ex[:], axis=AX.X, op=ALU.add), reads=[rd], writes=[rd])
                            g.op("dve", lambda: nc.vector.reciprocal(out=sm[:, 2:3], in_=sm[:, 1:2]), reads=[rd], writes=[rd])
                            g.op("dve", lambda: nc.vector.tensor_scalar(out=gt_[:], in0=ex[:], scalar1=sm[:, 2:3], scalar2=None, op0=ALU.mult),
                                 reads=[rd], writes=[rd])
                            g.op("pe", lambda: nc.tensor.transpose(out=ps[1][0:NE, 0:128], in_=gt_[:], identity=ident[:]),
                                 reads=[rd, cdep], writes=[psd[1]])
                            g.op("act", lambda: nc.scalar.copy(out=gT[:, t0:t0 + 128], in_=ps[1][0:NE, 0:128]), reads=[psd[1]], writes=[gTd])
                            g.op("pe", lambda: nc.tensor.matmul(ps[2][:, 0:NE], lhsT=utri[:], rhs=mk[:], start=True, stop=True),
                                 reads=[mkd, cdep2], writes=[psd[2]])
                            g.op("pe", lambda: nc.tensor.matmul(ps[3][:, 0:NE], lhsT=ones32[:], rhs=mk[:], start=True, stop=True),
                                 reads=[mkd, onesd], writes=[psd[3]])
                            g.op("dve", lambda: nc.vector.tensor_tensor(out=ex[:], in0=ps[2][:, 0:NE], in1=carry[:], op=ALU.add), reads=[psd[2], card, rd], writes=[rd])
                            g.op("dve", lambda: nc.vector.tensor_tensor(out=ex[:], in0=ex[:], in1=offrow[:], op=ALU.add), reads=[rd, cdep2], writes=[rd])
                            g.op("dve", lambda: nc.vector.tensor_tensor(out=ex[:], in0=ex[:], in1=mk[:], op=ALU.mult), reads=[rd], writes=[rd])
                            g.op("dve", lambda: nc.vector.tensor_scalar(out=ngd[:], in0=mk[:], scalar1=1.0e6, scalar2=-1.0e6, op0=ALU.mult, op1=ALU.add), reads=[rd], writes=[rd])
                            g.op("dve", lambda: nc.vector.tensor_tensor(out=ngd[:], in0=ngd[:], in1=ex[:], op=ALU.subtract), reads=[rd], writes=[rd])
                            g.op("dve", lambda: nc.vector.tensor_tensor(out=carry[:], in0=ps[3][:, 0:NE], in1=carry[:], op=ALU.add), reads=[psd[3], rd], writes=[card])
                            g.op("dve", lambda: nc.vector.max(out=t8b[:], in_=ngd[:]), reads=[rd], writes=[rd])
                            si = ixc % NIX
                            ixc += 1
                            g.op("dve", lambda: nc.vector.tensor_scalar(out=idx4[si][:], in0=t8b[:, 0:4], scalar1=-1.0, scalar2=None, op0=ALU.mult), reads=[rd], writes=[ixd[si]])
                            g.op("dve", lambda: nc.vector.tensor_scalar(out=val4[si][:], in0=tok4f[:], scalar1=float(4 * t0), scalar2=None, op0=ALU.add), reads=[cdep2], writes=[ixd[si]])
                            for k in range(4):
                                g.op("dve", lambda: nc.vector.scalar_tensor_tensor(out=ex[:], in0=ngd[:], scalar=t8b[:, k:k + 1], in1=gt_[:], op0=ALU.is_equal, op1=ALU.mult,
                                                                                   accum_out=GK[:, i, k:k + 1]), reads=[rd], writes=[rd, GKd])
                            for k in range(4):
                                g.idma(hs_d[:, :], idx4[si][:, k:k + 1], hbTok[:, i, :], bnd_hs, reads=[ixd[si], hbtokd[i]], writes=[hs_dep])
                                g.idma(idx_d[:, :], idx4[si][:, k:k + 1], val4[si][:, k:k + 1], bnd_hs, reads=[ixd[si]], writes=[idx_dep])
                    g.op("dve", lambda: nc.vector.tensor_copy(out=fli[:], in_=carry[:]), reads=[card, rd], writes=[rd])
                    g.dma("sp", cnt_d[l:l + 1, :], fli[0:1, :], reads=[rd], writes=[flag_dep[l]])
                    it = 0
                    for tt in tiles:
                        n = TN[tt]
                        for oc in range(NCH):
                            pb = 2 + it % 2
                            it += 1
                            g.op("pe", lambda: nc.tensor.matmul(ps[pb][:, 0:n], lhsT=b2n[:, oc * 128:(oc + 1) * 128], rhs=gT[:, tsl(tt)], start=True, stop=True),
                                 reads=[rtd, gTd], writes=[psd[pb]])
                            g.op("dve", lambda: nc.vector.scalar_tensor_tensor(out=x[:, oc, tsl(tt)], in0=ps[pb][:, 0:n], scalar=mod(l, 5, oc, 1 if tt == 0 else 0),
                                                                               in1=x[:, oc, tsl(tt)], op0=ALU.mult, op1=ALU.add),
                                 reads=[psd[pb], modd], writes=[xd[oc][tt]])
                    g.barrier()
                EE = ExitStack()
                with EE:
                    NSLOT = 4
                    hbflat = hb[:].rearrange("p c t -> p (c t)")
                    ring = [hbflat[:, k * 4096:(k + 1) * 4096].rearrange("p (c f) -> p c f", f=512) for k in range(NSLOT)]
                    ringd = [g.dep(dma=True) for _ in range(NSLOT)]
                    pf = [sb(EE, "pf%d" % i, [128, NCH, 512], BF16) for i in range(4)]
                    pfd = [g.dep(dma=True) for _ in range(4)]
                    hgToks = [sb(EE, "hgTok%d" % i, [128, 3, D], BF16) for i in range(3)]
                    hgds = [g.dep(dma=True) for _ in range(3)]
                    idxts = [sb(EE, "idxt%d" % i, [128, 3, 1], I32) for i in range(3)]
                    idxtds = [g.dep(dma=True) for _ in range(3)]

                    def gather_load(e, c0, hsel):
                        r0 = e * T + c0 * CAP
                        g.dma("sp", hgToks[hsel][:], hs_d[r0:r0 + CAP, :].rearrange("(j p) f -> p j f", p=128), reads=[hs_dep], writes=[hgds[hsel]])
                        for j3 in range(3):
                            g.dma("sp", idxts[hsel][:, j3, :], idx_d[r0 + j3 * 128:r0 + (j3 + 1) * 128, :], reads=[idx_dep], writes=[idxtds[hsel]])
                    hbg = sb(EE, "hbg", [128, NCH, CAP], BF16)
                    hbgd = [g.dep() for _ in range(NCH)]
                    actT = sb(EE, "actT", [128, NCH, CAP], BF16)
                    actd = [g.dep() for _ in range(NCH)]
                    yTok = sb(EE, "yTok", [128, 3, D], F32)
                    yTd = [g.dep() for _ in range(3)]
                    At = [sb(EE, "mA%d" % i, [128, CAP], F32) for i in range(2)]
                    St = [sb(EE, "mS%d" % i, [128, CAP], F32) for i in range(2)]
                    Lt = [sb(EE, "mL%d" % i, [128, CAP], F32) for i in range(2)]
                    Ad = [g.dep(), g.dep()]
                    Sdp = [g.dep(), g.dep()]
                    Ld = [g.dep(), g.dep()]
                    rc = [0]
                    ec = [0]
                    pc_ = [0]

                    def wsrc(ap):
                        return ap.rearrange("(c p) f -> p c f", p=128)

                    def expert_loads(e):
                        g.dma("pool", ring[0][:], wsrc(moe_w1[l, e, :, 512:1024]), writes=[ringd[0]])
                        g.dma("pool", ring[1][:], wsrc(moe_w1[l, e, :, D + 512:D + 1024]), writes=[ringd[1]])
                        g.dma("pool", ring[2][:], wsrc(moe_w2[l, e, :, 0:512]), writes=[ringd[2]])
                        g.dma("pool", ring[3][:], wsrc(moe_w2[l, e, :, 512:1024]), writes=[ringd[3]])

                    def prefetchA(e):
                        par = (e % 2) * 2
                        g.dma("pool", pf[par][:], wsrc(moe_w1[l, e, :, 0:512]), writes=[pfd[par]])
                        g.dma("pool", pf[par + 1][:], wsrc(moe_w1[l, e, :, D:D + 512]), writes=[pfd[par + 1]])

                    def sparse_chunk(e, c0):
                        b1o = voff["b1"] + (l * NE + e) * 16
                        r0 = e * T + c0 * CAP
                        if c0 == 0:
                            hsel = e % 2
                        else:
                            hsel = 2
                            gather_load(e, c0, hsel)
                        hgTok, hgd, idxt, idxtd = hgToks[hsel], hgds[hsel], idxts[hsel], idxtds[hsel]
                        par = (e % 2) * 2
                        w1s = {0: ((pf[par], pfd[par]), (pf[par + 1], pfd[par + 1])), 1: ((ring[0], ringd[0]), (ring[1], ringd[1]))}
                        w2s = [(ring[2], ringd[2]), (ring[3], ringd[3])]
                        for c in range(NCH):
                            pb = 6 + pc_[0] % 2
                            pc_[0] += 1
                            psb = ps[pb][:, :].bitcast(BF16)
                            for j3 in range(3):
                                g.op("pe", lambda: nc.tensor.transpose(out=psb[:, j3 * 128:(j3 + 1) * 128], in_=hgTok[:, j3, c * 128:(c + 1) * 128], identity=identb[:]),
                                     reads=[hgd, cdep2], writes=[psd[pb]])
                            if c % 2 == 0:
                                g.op("act", lambda: nc.scalar.copy(out=hbg[:, c, :], in_=psb[:, 0:CAP]), reads=[psd[pb]], writes=[hbgd[c]])
                            else:
                                g.op("dve", lambda: nc.vector.tensor_copy(out=hbg[:, c, :], in_=psb[:, 0:CAP]), reads=[psd[pb]], writes=[hbgd[c]])
                        pend = []
                        for u in range(2):
                            (sgA, sgD), (slA, slD) = w1s[u]
                            for jj in range(4):
                                j = u * 4 + jj
                                s = ec[0] % 2
                                s3 = ec[0] % 3
                                ec[0] += 1
                                pg = s3
                                pl = 3 + s3
                                for c in range(NCH):
                                    g.op("pe", lambda: nc.tensor.matmul(ps[pg][:, 0:CAP], lhsT=sgA[:, c, jj * 128:(jj + 1) * 128], rhs=hbg[:, c, :],
                                                                        start=(c == 0), stop=(c == NCH - 1)),
                                         reads=[sgD, hbgd[c]], writes=[psd[pg]])
                                for c in range(NCH):
                                    g.op("pe", lambda: nc.tensor.matmul(ps[pl][:, 0:CAP], lhsT=slA[:, c, jj * 128:(jj + 1) * 128], rhs=hbg[:, c, :],
                                                                        start=(c == 0), stop=(c == NCH - 1)),
                                         reads=[slD, hbgd[c]], writes=[psd[pl]])
                                g.op("dve", lambda: nc.vector.tensor_scalar(out=At[s][:], in0=ps[pg][:, 0:CAP], scalar1=V[:, b1o + j:b1o + j + 1], scalar2=7.0,
                                                                            op0=ALU.add, op1=ALU.min), reads=[psd[pg], Vd], writes=[Ad[s]])
                                g.op("act", lambda: nc.scalar.activation(out=St[s][:], in_=At[s][:], func=AF.Sigmoid, scale=1.702),
                                     reads=[Ad[s]], writes=[Sdp[s]])
                                g.op("dve", lambda: nc.vector.tensor_scalar(out=Lt[s][:], in0=ps[pl][:, 0:CAP], scalar1=V[:, b1o + 8 + j:b1o + 8 + j + 1], scalar2=-6.0,
                                                                            op0=ALU.add, op1=ALU.max), reads=[psd[pl], Vd], writes=[Ld[s]])
                                if pend:
                                    pend.pop()()

                                def _fin(s=s, j=j):
                                    g.op("dve", lambda: nc.vector.tensor_tensor(out=St[s][:], in0=At[s][:], in1=St[s][:], op=ALU.mult),
                                         reads=[Ad[s], Sdp[s]], writes=[Sdp[s]])
                                    g.op("dve", lambda: nc.vector.scalar_tensor_tensor(out=actT[:, j, :], in0=Lt[s][:], scalar=8.0, in1=St[s][:],
                                                                                       op0=ALU.min, op1=ALU.mult), reads=[Ld[s], Sdp[s]], writes=[actd[j]])
                                pend.append(_fin)
                        if pend:
                            pend.pop()()
                        for j3 in range(3):
                            for hh in range(2):
                                swA, swD = w2s[hh]
                                pb = 6 + pc_[0] % 2
                                pc_[0] += 1
                                for fc in range(NCH):
                                    g.op("pe", lambda: nc.tensor.matmul(ps[pb][:, :], lhsT=actT[:, fc, j3 * 128:(j3 + 1) * 128], rhs=swA[:, fc, :],
                                                                        start=(fc == 0), stop=(fc == NCH - 1)),
                                         reads=[swD, actd[fc]], writes=[psd[pb]])
                                g.op("act", lambda: nc.scalar.copy(out=yTok[:, j3, hh * 512:(hh + 1) * 512], in_=ps[pb][:, :]), reads=[psd[pb]], writes=[yTd[j3]])
                            g.idma(y4_d[:, :], idxt[:, j3, :], yTok[:, j3, :], bnd_y4, reads=[idxtd, yTd[j3]], writes=[y4_dep])

                    prefetchA(0)
                    gather_load(0, 0, 0)
                    for e in range(NE):
                        expert_loads(e)
                        if e + 1 < NE:
                            prefetchA(e + 1)
                            gather_load(e + 1, 0, (e + 1) % 2)
                        for reg in flag_regs:
                            ek = ENG_KEY[reg.engine]
                            g._wait(ek, [(flag_dep[l].sem, flag_dep[l].tot)])
                            g.eng[ek].reg_load(reg, cnt_d[l:l + 1, e:e + 1])
                        def emit_chunks(c0):
                            snap = g.snapshot()
                            with nc.If_cmp(flag_regs, c0 * CAP, "IS_GT"):
                                sparse_chunk(e, c0)
                                if c0 + 1 < NCHUNK:
                                    emit_chunks(c0 + 1)
                            big = g.snapshot()
                            g.restore(snap)
                            with nc.Else():
                                g.pad_to(big)
                            g.restore(big)
                            g.known = {k: dict(v) for k, v in snap[2].items()}

                        emit_chunks(0)
                    g.barrier()
                CB = ExitStack()
                with CB:
                    y4t = [sb(CB, "y4t%d" % i, [128, 4, D], F32) for i in range(2)]
                    y4td = [g.dep(dma=True) for _ in range(2)]
                    acc = [sb(CB, "cacc%d" % i, [128, D], F32) for i in range(2)]
                    accd = [g.dep(), g.dep()]
                    for n_, (i, tt, sub) in enumerate(subs):
                        s = n_ % 2
                        t0 = i * 128
                        cls = 1 if tt == 0 else 0
                        g.dma("sp", y4t[s][:], y4_d[4 * t0:4 * t0 + 512, :].rearrange("(p k) f -> p k f", k=4), reads=[y4_dep], writes=[y4td[s]])
                        g.op("dve", lambda: nc.vector.tensor_scalar(out=acc[s][:], in0=y4t[s][:, 0, :], scalar1=GK[:, i, 0:1], scalar2=None, op0=ALU.mult),
                             reads=[y4td[s], GKd], writes=[accd[s]])
                        for k in range(1, 4):
                            g.op("dve", lambda: nc.vector.scalar_tensor_tensor(out=acc[s][:], in0=y4t[s][:, k, :], scalar=GK[:, i, k:k + 1], in1=acc[s][:],
                                                                               op0=ALU.mult, op1=ALU.add), reads=[y4td[s], GKd, accd[s]], writes=[accd[s]])
                        for half in range(2):
                            pbt = 2 * s + half
                            for cc in range(4):
                                c = half * 4 + cc
                                g.op("pe", lambda: nc.tensor.transpose(out=ps[pbt][:, cc * 128:(cc + 1) * 128], in_=acc[s][:, c * 128:(c + 1) * 128], identity=ident[:]),
                                     reads=[accd[s], cdep], writes=[psd[pbt]])
                            for cc in range(4):
                                c = half * 4 + cc
                                g.op("dve", lambda: nc.vector.scalar_tensor_tensor(out=x[:, c, t0:t0 + 128], in0=ps[pbt][:, cc * 128:(cc + 1) * 128], scalar=mod(l, 5, c, cls),
                                                                                   in1=x[:, c, t0:t0 + 128], op0=ALU.mult, op1=ALU.add),
                                     reads=[psd[pbt], modd], writes=[xd[c][tt]])
                    g.barrier()

        moe_layer(0, list(range(NTT)))

        if dbg == "x1":
            for c in range(NCH):
                g.dma("sp", dbg_d[:, c, :], xstate["x"][:, c, :], reads=[xstate["d"][c][tt] for tt in range(NTT)], writes=[out_dep])

        LAT = [1, 2, 3, 4]
        L1 = ExitStack()
        with L1:
            cat1 = sb(L1, "cat1", [128, NCH, TL], BF16)
            cat1d = [[g.dep() for _ in range(NTT)] for _ in range(NCH)]
            x = xstate["x"]
            xd = xstate["d"]
            A1 = ExitStack()
            with A1:
                nb = norm_bufs(A1)
                for tt in range(NTT):
                    rmsnorm_tile(nb, lambda c: x[:, c, tsl(tt)], lambda c: xd[c][tt], tt, "gs1", 1, 0)
                for c in range(NCH):
                    g.dma("sp", xs_d[:, c, :], x[:, c, :], reads=[xd[c][tt] for tt in range(NTT)], writes=[xs_dep])
                g.barrier()
            xes.close()
            M1 = ExitStack()
            with M1:
                gw = sb(M1, "gw", [128, 16, 2, 256], BF16)
                gwd = g.dep(dma=True)
                for gi_, src in ((0, od_ga), (1, od_gx)):
                    for d_ in range(2):
                        g.dma("pool", gw[:, gi_ * 8 + d_ * 4:gi_ * 8 + d_ * 4 + 4, :, :],
                              src[d_].rearrange("b (c p) o -> p b c o", p=128), writes=[gwd])
                wr1 = [sb(M1, "w1r%d" % i, [128, NCH, 512], BF16) for i in range(2)]
                wr1d = [g.dep(dma=True) for _ in range(2)]
                ug = sb(M1, "ug", [128, 2, T], F32)
                ugd = [g.dep(), g.dep()]
                xc = sb(M1, "xc", [128, 2, T], F32)
                xcd = [g.dep(), g.dep()]
                xcb = sb(M1, "xcb", [128, 2, T], BF16)
                xcbd = [g.dep(), g.dep()]
                rec = sb(M1, "rec", [128, 2, T], F32)
                recd = [[g.dep() for _ in range(NTT)] for _ in range(2)]
                NT_ = 9
                tb = [[sb(M1, "tb%d_%d" % (k, i), [128, 512], F32) for i in range(2 if k < 7 else 1)] for k in range(NT_)]
                tb[7].append(tb[7][0])
                tb[8].append(tb[8][0])
                tbd = [[g.dep(), g.dep()] for _ in range(NT_)]
                tbd[7][1] = tbd[7][0]
                tbd[8][1] = tbd[8][0]
                cnt1 = [0]
                pcnt = [0]
                for blk in range(4):
                    sW = blk % 2
                    g.dma("pool", wr1[sW][:, :, 0:256], od_w_in[:, blk * 256:(blk + 1) * 256].rearrange("(c p) f -> p c f", p=128), writes=[wr1d[sW]])
                    g.dma("pool", wr1[sW][:, :, 256:512], od_w_in[:, D + blk * 256:D + (blk + 1) * 256].rearrange("(c p) f -> p c f", p=128), writes=[wr1d[sW]])
                    for cc in range(2):
                        for tt in range(NTT):
                            n = TN[tt]
                            pb = pcnt[0] % 2
                            pcnt[0] += 1
                            for c in range(NCH):
                                g.op("pe", lambda: nc.tensor.matmul(ps[pb][:, 0:n], lhsT=wr1[sW][:, c, 256 + cc * 128:256 + (cc + 1) * 128], rhs=hb[:, c, tsl(tt)],
                                                                    start=(c == 0), stop=(c == NCH - 1)),
                                     reads=[wr1d[sW], hbd[c][tt]], writes=[psd[pb]])
                            g.op("act", lambda: nc.scalar.copy(out=ug[:, cc, tsl(tt)], in_=ps[pb][:, 0:n]), reads=[psd[pb]], writes=[ugd[cc]])
                    for d_ in range(2):
                        for cc in range(2):
                            ch = blk * 2 + cc
                            wv = lambda k: Vcol("odconvw", (d_ * 4 + k) * 8 + ch)
                            bv = Vcol("odconvb", d_ * 8 + ch)
                            for (a, b) in ((0, TC), (TC, T)):
                                if d_ == 0:
                                    g.op("dve", lambda: nc.vector.tensor_scalar(out=xc[:, cc, a:b], in0=ug[:, cc, a:b], scalar1=wv(3), scalar2=bv, op0=ALU.mult, op1=ALU.add),
                                         reads=[ugd[cc], Vd], writes=[xcd[cc]])
                                    for k in range(3):
                                        sh = 3 - k
                                        g.op("dve", lambda: nc.vector.scalar_tensor_tensor(out=xc[:, cc, a + sh:b], in0=ug[:, cc, a:b - sh], scalar=wv(k), in1=xc[:, cc, a + sh:b],
                                                                                           op0=ALU.mult, op1=ALU.add), reads=[ugd[cc], Vd, xcd[cc]], writes=[xcd[cc]])
                                else:
                                    g.op("dve", lambda: nc.vector.tensor_scalar(out=xc[:, cc, a:b], in0=ug[:, cc, a:b], scalar1=wv(0), scalar2=bv, op0=ALU.mult, op1=ALU.add),
                                         reads=[ugd[cc], Vd], writes=[xcd[cc]])
                                    for k in range(1, 4):
                                        g.op("dve", lambda: nc.vector.scalar_tensor_tensor(out=xc[:, cc, a:b - k], in0=ug[:, cc, a + k:b], scalar=wv(k), in1=xc[:, cc, a:b - k],
                                                                                           op0=ALU.mult, op1=ALU.add), reads=[ugd[cc], Vd, xcd[cc]], writes=[xcd[cc]])
                            g.op("act", lambda: nc.scalar.copy(out=xcb[:, cc, :], in_=xc[:, cc, :]), reads=[xcd[cc]], writes=[xcbd[cc]])
                        order = [0, 1, 2, 3, 4] if d_ == 0 else [0, 4, 3, 2, 1]
                        for oc in range(2):
                            ch = blk * 2 + oc
                            prev = None
                            for tt in order:
                                n = TN[tt]
                                s = cnt1[0] % 2
                                cnt1[0] += 1
                                pa = 2 + s
                                px = 4 + s
                                for gi_, pb in ((0, pa), (1, px)):
                                    for ic in range(2):
                                        g.op("pe", lambda: nc.tensor.matmul(ps[pb][:, 0:n], lhsT=gw[:, gi_ * 8 + d_ * 4 + blk, ic, oc * 128:(oc + 1) * 128], rhs=xcb[:, ic, tsl(tt)],
                                                                            start=(ic == 0), stop=(ic == 1)),
                                             reads=[gwd, xcbd[ic]], writes=[psd[pb]])
                                R, I_, A_, A2, TH, GI, HS = 0, 1, 2, 3, 4, 5, 6
                                c8 = Scol("c8", d_ * 8 + ch)
                                c8x2 = Scol("c8x2", d_ * 8 + ch)
                                g.op("act", lambda: nc.scalar.activation(out=tb[R][s][:, 0:n], in_=ps[pa][:, 0:n], func=AF.Sigmoid, bias=Vcol("odgab", d_ * 8 + ch)),
                                     reads=[psd[pa], Vd], writes=[tbd[R][s]])
                                g.op("act", lambda: nc.scalar.activation(out=tb[I_][s][:, 0:n], in_=ps[px][:, 0:n], func=AF.Sigmoid, bias=Vcol("odgxb", d_ * 8 + ch)),
                                     reads=[psd[px], Vd], writes=[tbd[I_][s]])
                                g.op("act", lambda: nc.scalar.activation(out=tb[A_][s][:, 0:n], in_=tb[R][s][:, 0:n], func=AF.Exp, scale=c8),
                                     reads=[tbd[R][s], Sd], writes=[tbd[A_][s]])
                                g.op("act", lambda: nc.scalar.activation(out=tb[A2][s][:, 0:n], in_=tb[R][s][:, 0:n], func=AF.Exp, scale=c8x2),
                                     reads=[tbd[R][s], Sd], writes=[tbd[A2][s]])
                                g.op("act", lambda: nc.scalar.activation(out=tb[TH][s][:, 0:n], in_=tb[R][s][:, 0:n], func=AF.Tanh, scale=c8),
                                     reads=[tbd[R][s], Sd], writes=[tbd[TH][s]])
                                g.op("dve", lambda: nc.vector.scalar_tensor_tensor(out=tb[A2][s][:, 0:n], in0=tb[A2][s][:, 0:n], scalar=1.0, in1=tb[TH][s][:, 0:n],
                                                                                   op0=ALU.add, op1=ALU.mult), reads=[tbd[A2][s], tbd[TH][s]], writes=[tbd[A2][s]])
                                g.op("act", lambda: nc.scalar.activation(out=tb[A2][s][:, 0:n], in_=tb[A2][s][:, 0:n], func=AF.Sqrt, scale=-1.0),
                                     reads=[tbd[A2][s]], writes=[tbd[A2][s]])
                                g.op("pool", lambda: nc.gpsimd.tensor_tensor(out=tb[GI][s][:, 0:n], in0=tb[I_][s][:, 0:n], in1=xc[:, oc, tsl(tt)], op=ALU.mult),
                                     reads=[tbd[I_][s], xcd[oc]], writes=[tbd[GI][s]])
                                g.op("dve", lambda: nc.vector.tensor_tensor(out=tb[GI][s][:, 0:n], in0=tb[GI][s][:, 0:n], in1=tb[A2][s][:, 0:n], op=ALU.mult),
                                     reads=[tbd[GI][s], tbd[A2][s]], writes=[tbd[GI][s]])
                                if d_ == 0:
                                    dst = rec[:, oc, tsl(tt)]
                                    dstd = recd[oc][tt]
                                    init = 0.0 if prev is None else prev[0][:, TN[prev[2]] - 1:TN[prev[2]]]
                                    g.op("dve", lambda: nc.vector.tensor_tensor_scan(out=dst, data0=tb[A_][s][:, 0:n], data1=tb[GI][s][:, 0:n], initial=init,
                                                                                     op0=ALU.mult, op1=ALU.add),
                                         reads=[tbd[A_][s], tbd[GI][s]] + ([prev[1]] if prev else []), writes=[dstd])
                                    prev = (dst, dstd, tt)
                                else:
                                    dst = tb[HS][s][:, 0:n]
                                    dstd = tbd[HS][s]
                                    init = 0.0 if prev is None else prev[0][:, 0:1]
                                    g.op("dve", lambda: nc.vector.tensor_tensor_scan(out=dst[:, ::-1], data0=tb[A_][s][:, 0:n][:, ::-1], data1=tb[GI][s][:, 0:n][:, ::-1],
                                                                                     initial=init, op0=ALU.mult, op1=ALU.add),
                                         reads=[tbd[A_][s], tbd[GI][s]] + ([prev[1]] if prev else []), writes=[dstd])
                                    prev = (dst, dstd, tt)
                                    if tt != 0:
                                        pgt = 6 + s
                                        for c in range(NCH):
                                            g.op("pe", lambda: nc.tensor.matmul(ps[pgt][:, 0:n], lhsT=wr1[sW][:, c, oc * 128:(oc + 1) * 128], rhs=hb[:, c, tsl(tt)],
                                                                                start=(c == 0), stop=(c == NCH - 1)),
                                                 reads=[wr1d[sW], hbd[c][tt]], writes=[psd[pgt]])
                                        GL, SM = 7, 8
                                        g.op("act", lambda: nc.scalar.activation(out=tb[GL][s][:, 0:n], in_=ps[pgt][:, 0:n], func=AF.Gelu), reads=[psd[pgt]], writes=[tbd[GL][s]])
                                        g.op("pool", lambda: nc.gpsimd.tensor_tensor(out=tb[SM][s][:, 0:n], in0=dst, in1=rec[:, oc, tsl(tt)], op=ALU.add),
                                             reads=[dstd, recd[oc][tt]], writes=[tbd[SM][s]])
                                        g.op("dve", lambda: nc.vector.tensor_tensor(out=cat1[:, ch, TOFF[tt] - TC:TOFF[tt] - TC + n], in0=tb[SM][s][:, 0:n], in1=tb[GL][s][:, 0:n], op=ALU.mult),
                                             reads=[tbd[SM][s], tbd[GL][s]], writes=[cat1d[ch][tt]])
                g.barrier()
            alloc_x()
            C1 = ExitStack()
            with C1:
                xst = [sb(C1, "x1st%d" % i, [128, NCH, 512], F32) for i in range(2)]
                xstd = [g.dep(dma=True), g.dep(dma=True)]
                c1c = [0]

                def xold1(tt):
                    s = c1c[0] % 2
                    c1c[0] += 1
                    for c in range(NCH):
                        g.dma("sp", xst[s][:, c, 0:TN[tt]], xs_d[:, c, tsl(tt)], reads=[xs_dep], writes=[xstd[s]])
                    return xst[s], xstd[s]

                out_proj(1, od_w_out, lambda c, tt: cat1[:, c, TOFF[tt] - TC:TOFF[tt] - TC + TN[tt]], lambda c, tt: cat1d[c][tt], LAT, xold1, C1)
                g.barrier()
        moe_layer(1, LAT)

        FN = ExitStack()
        with FN:
            x = xstate["x"]
            xd = xstate["d"]
            nb = norm_bufs(FN)
            sq, sqd, tmp, tmpd, rs, rsd, cnt = nb
            yb = [sb(FN, "yb%d" % i, [128, NCH, 128], F32) for i in range(2)]
            ybd = [g.dep(), g.dep()]
            ot = [sb(FN, "ot%d" % i, [128, D], F32) for i in range(2)]
            otd = [g.dep(dma=True), g.dep(dma=True)]
            oc_ = [0]
            for tt in LAT:
                n = TN[tt]
                pb = 5
                for c in range(NCH):
                    s = cnt[0] % 2
                    cnt[0] += 1
                    g.op("act", lambda: nc.scalar.activation(out=sq[s][:, 0:n], in_=x[:, c, tsl(tt)], func=AF.Square), reads=[xd[c][tt]], writes=[sqd[s]])
                    g.op("pe", lambda: nc.tensor.matmul(ps[pb][:, 0:n], lhsT=ones32[:], rhs=sq[s][:, 0:n], start=(c == 0), stop=(c == NCH - 1)),
                         reads=[sqd[s], onesd], writes=[psd[pb]])
                g.op("act", lambda: nc.scalar.activation(out=rs[:, 0:n], in_=ps[pb][:, 0:n], func=AF.Sqrt, scale=1.0 / D, bias=EPS), reads=[psd[pb]], writes=[rsd])
                g.op("dve", lambda: nc.vector.reciprocal(out=rs[:, 0:n], in_=rs[:, 0:n]), reads=[rsd], writes=[rsd])
                for sub in range(n // 128):
                    s = oc_[0] % 2
                    oc_[0] += 1
                    for c in range(NCH):
                        g.op("dve", lambda: nc.vector.scalar_tensor_tensor(out=yb[s][:, c, :], in0=x[:, c, TOFF[tt] + sub * 128:TOFF[tt] + (sub + 1) * 128],
                                                                           scalar=Vcol("fnorm", c), in1=rs[:, sub * 128:(sub + 1) * 128], op0=ALU.mult, op1=ALU.mult),
                             reads=[xd[c][tt], rsd, Vd], writes=[ybd[s]])
                    for half in range(2):
                        pbt = 6 + half
                        for cc in range(4):
                            c = half * 4 + cc
                            g.op("pe", lambda: nc.tensor.transpose(out=ps[pbt][:, cc * 128:(cc + 1) * 128], in_=yb[s][:, c, :], identity=ident[:]),
                                 reads=[ybd[s], cdep], writes=[psd[pbt]])
                        if half == 0:
                            g.op("act", lambda: nc.scalar.copy(out=ot[s][:, 0:512], in_=ps[pbt][:, :]), reads=[psd[pbt]], writes=[otd[s]])
                        else:
                            g.op("dve", lambda: nc.vector.tensor_copy(out=ot[s][:, 512:1024], in_=ps[pbt][:, :]), reads=[psd[pbt]], writes=[otd[s]])
                    r0 = TOFF[tt] - TC + sub * 128
                    g.dma("sp", out_d[r0:r0 + 128, :], ot[s][:], reads=[otd[s]], writes=[out_dep])
            g.barrier()
        xes.close()
    return nc


def _consts():
    ident = np.eye(128, dtype=np.float32)
    perm = np.zeros((128, 128), np.float32)
    for m in range(128):
        blk = m // 16
        partner = (blk ^ 1) * 16 + (m % 16)
        perm[partner, m] = 1.0
    n_rows = TL // 64
    rows, cols = np.meshgrid(np.arange(n_rows), np.arange(64), indexing="ij")
    pos = np.stack([rows.reshape(-1), cols.reshape(-1)], axis=-1).astype(np.float32)
    inv = (np.float32(10000.0) ** (-np.arange(16, dtype=np.float32) / np.float32(16))).astype(np.float32)
    ang = (pos[:, :, None] * inv).astype(np.float32)
    cos = np.cos(ang).astype(np.float32)
    sin = np.sin(ang).astype(np.float32)
    cosT = np.ones((128, T), np.float32)
    sinT = np.zeros((128, T), np.float32)
    for p in range(128):
        dd = p % 64
        axis = dd // 32
        half = (dd % 32) // 16
        f = dd % 16
        cosT[p, TC:] = cos[:, axis, f]
        sinT[p, TC:] = (-sin[:, axis, f]) if half == 0 else sin[:, axis, f]
    return ident, perm, cosT, sinT


def _consts2(NE):
    utri = np.triu(np.ones((128, 128), np.float32), k=1)
    offrow = np.tile((np.arange(NE, dtype=np.float32) * T)[None, :], (128, 1))
    tok4f = (4.0 * np.arange(128, dtype=np.float32)[:, None] + np.arange(4, dtype=np.float32)[None, :]).astype(np.float32)
    return utri, offrow, tok4f


def make_in_maps(inp, NE, ncores):
    voff, vrows, vpad = vec_layout(NE)
    ident, perm, cosT, sinT = _consts()
    f = lambda a: np.ascontiguousarray(np.asarray(a, dtype=np.float32))
    shared = {
        "w_mod": f(inp["w_mod"]), "ev_w_in": f(inp["ev_w_in"][0]), "ev_w_out": f(inp["ev_w_out"][0]),
        "od_w_in": f(inp["od_w_in"][0]), "od_w_out": f(inp["od_w_out"][0]),
        "od_ga": f(inp["od_gate_a_w"][0]), "od_gx": f(inp["od_gate_x_w"][0]),
        "w_router": f(inp["moe_w_router"]), "b_router": f(inp["moe_b_router"]),
        "moe_w1": f(inp["moe_w1"]), "moe_w2": f(inp["moe_w2"]), "moe_b2": f(inp["moe_b2"]),
        "lams": f(np.stack([inp["ev_lambda_q1"][0], inp["ev_lambda_k1"][0], inp["ev_lambda_q2"][0], inp["ev_lambda_k2"][0]])),
        "ident": ident, "perm": perm, "cosT": cosT, "sinT": sinT,
    }
    shared["utri"], shared["offrow"], shared["tok4f"] = _consts2(NE)
    maps = []
    for b in range(ncores):
        rows = [
            f(inp["c"][b]).reshape(8, 128), f(inp["c_ctx"]).reshape(8, 128), f(inp["b_mod"]).reshape(96, 128),
            f(inp["norm_mix"]).reshape(16, 128), f(inp["norm_ffn"]).reshape(16, 128), f(inp["final_norm"]).reshape(8, 128),
            f(inp["ev_conv_w"][0]).reshape(12, 128), f(inp["ev_subln"][0]).reshape(1, 128),
            f(inp["od_conv_w"][0]).reshape(64, 128), f(inp["od_conv_b"][0]).reshape(16, 128),
            f(inp["od_gate_a_b"][0]).reshape(16, 128), f(inp["od_gate_x_b"][0]).reshape(16, 128),
            f(inp["od_lru_lambda"][0]).reshape(16, 128), f(inp["moe_b1"]).reshape(2 * NE * 16, 128),
        ]
        v = np.concatenate(rows, axis=0)
        assert v.shape[0] == vrows
        vp = np.zeros((vpad, 128), np.float32)
        vp[:vrows] = v
        m = dict(shared)
        m["xin"] = f(inp["x"][b])
        m["ctxin"] = f(inp["ctx"][b])
        m["vecs"] = vp
        maps.append(m)
    return maps


def kernel(**inputs):
    NE = inputs["moe_w1"].shape[1]
    B = inputs["x"].shape[0]
    nc = build(NE)
    maps = make_in_maps(inputs, NE, B)
    res = run_bass_kernel_spmd(nc, maps, core_ids=list(range(B)))
    return np.stack([np.asarray(r["out"], dtype=np.float32) for r in res.results], axis=0)
```

```python
import math
from contextlib import ExitStack

import numpy as np
import concourse.bass as bass
import concourse.mybir as mybir
from concourse.bass_utils import run_bass_kernel_spmd

F32 = mybir.dt.float32
F32R = mybir.dt.float32r
BF16 = mybir.dt.bfloat16
AF = mybir.ActivationFunctionType
ALU = mybir.AluOpType
AX = mybir.AxisListType

D = 1024
NCH = 8
TC = 256
TL = 2048
T = TC + TL
TOFF = [0, 256, 768, 1280, 1792]
TN = [256, 512, 512, 512, 512]
NTT = 5
NKT = T // 128
EPS = 1e-6
SELF_SYNC = True
CAP = 384
NCHUNK = (T + CAP - 1) // CAP
I32 = mybir.dt.int32
ET = mybir.EngineType
ENG_KEY = {ET.PE: "pe", ET.Activation: "act", ET.DVE: "dve", ET.Pool: "pool", ET.SP: "sp"}


def tsl(tt):
    return slice(TOFF[tt], TOFF[tt] + TN[tt])


def vec_layout(NE):
    ents = [("c", 8), ("cctx", 8), ("bmod", 96), ("nmix", 16), ("nffn", 16), ("fnorm", 8), ("evconv", 12),
            ("subln", 1), ("odconvw", 64), ("odconvb", 16), ("odgab", 16), ("odgxb", 16), ("odlam", 16),
            ("b1", 2 * NE * 16)]
    off = {}
    r = 0
    for name, n in ents:
        off[name] = r
        r += n
    rpad = ((r + 127) // 128) * 128
    return off, r, rpad


class Dep:
    __slots__ = ("w", "r", "sem", "tot")

    def __init__(self):
        self.w = None
        self.r = {}
        self.sem = None
        self.tot = 0


class G:
    def __init__(self, nc, es):
        self.nc = nc
        self.es = es
        self.eng = {"pe": nc.tensor, "act": nc.scalar, "dve": nc.vector, "pool": nc.gpsimd, "sp": nc.sync}
        self.semh = []
        self.esem = {}
        self.cnt = {}
        self.known = {}
        for k in self.eng:
            self.esem[k] = self.new_sem("e_" + k)
            self.cnt[k] = 0
            self.known[k] = {}
        self.dma_sems = []
        self.nsem = 0
        self.alldeps = []

    def new_sem(self, name):
        h = self.es.enter_context(self.nc.semaphore(name))
        self.semh.append(h)
        return len(self.semh) - 1

    def snapshot(self):
        deps = [(d, d.w, dict(d.r), d.tot) for d in self.alldeps]
        return (deps, dict(self.cnt), {k: dict(v) for k, v in self.known.items()})

    def restore(self, snap):
        deps, cnt, known = snap
        for d, w, r, tot in deps:
            d.w = w
            d.r = dict(r)
            d.tot = tot
        self.cnt = dict(cnt)
        self.known = {k: dict(v) for k, v in known.items()}

    def pad_to(self, big):
        deps, cnt, _ = big
        for e in self.eng:
            diff = cnt[e] - self.cnt[e]
            assert diff >= 0, (e, diff)
            if diff > 0:
                self.eng[e].wait_ge(self.semh[self.esem[e]], self.cnt[e])
                self.eng[e].sem_inc(self.semh[self.esem[e]], diff)
                self.cnt[e] = cnt[e]
        for d, w, r, tot in deps:
            if d.sem is None:
                continue
            diff = tot - d.tot
            assert diff >= 0
            if diff > 0:
                self.eng["sp"].wait_ge(self.semh[d.sem], d.tot)
                self.eng["sp"].sem_inc(self.semh[d.sem], diff)
                d.tot = tot

    def dep(self, dma=False):
        d = Dep()
        self.alldeps.append(d)
        if dma:
            self.nsem += 1
            d.sem = self.new_sem("d%d" % self.nsem)
            self.dma_sems.append(d)
        return d

    def _wait(self, e, deps):
        need = {}
        kn = self.known[e]
        for d in deps:
            if d is None:
                continue
            s, v = d
            if s == self.esem[e] and (e == "pe" or not SELF_SYNC):
                continue
            if kn.get(s, 0) >= v:
                continue
            if need.get(s, 0) < v:
                need[s] = v
        for s, v in need.items():
            self.eng[e].wait_ge(self.semh[s], v)
            kn[s] = v

    def op(self, e, fn, reads=(), writes=()):
        deps = []
        for t in reads:
            deps.append(t.w)
        for t in writes:
            deps.append(t.w)
            deps.extend(t.r.items())
        self._wait(e, deps)
        ins = fn()
        self.cnt[e] += 1
        s = self.esem[e]
        ins.then_inc(self.semh[s], 1)
        me = (s, self.cnt[e])
        for t in reads:
            t.r[s] = self.cnt[e]
        for t in writes:
            t.w = me
            t.r = {}
        return ins

    def dma(self, q, out, in_, reads=(), writes=(), sem_dep=None):
        deps = []
        for t in reads:
            deps.append(t.w)
        for t in writes:
            deps.append(t.w)
            deps.extend(t.r.items())
        self._wait(q, deps)
        sd = sem_dep if sem_dep is not None else writes[0]
        assert sd.sem is not None
        ins = self.eng[q].dma_start(out=out, in_=in_)
        sd.tot += 16
        ins.then_inc(self.semh[sd.sem], 16)
        me = (sd.sem, sd.tot)
        for t in reads:
            t.r[sd.sem] = sd.tot
        for t in writes:
            t.w = me
            t.r = {}
        return ins

    def idma(self, out, off_ap, in_, bound, reads=(), writes=()):
        deps = []
        for t in reads:
            deps.append(t.w)
        for t in writes:
            deps.append(t.w)
            deps.extend(t.r.items())
        self._wait("pool", deps)
        sd = writes[0]
        ins = self.nc.gpsimd.indirect_dma_start(out=out, out_offset=bass.IndirectOffsetOnAxis(ap=off_ap, axis=0), in_=in_, in_offset=None,
                                                bounds_check=bound, oob_is_err=False)
        sd.tot += 16
        ins.then_inc(self.semh[sd.sem], 16)
        me = (sd.sem, sd.tot)
        for t in reads:
            t.r[sd.sem] = sd.tot
        for t in writes:
            t.w = me
            t.r = {}
        return ins

    def barrier(self):
        for e in self.eng:
            deps = [(self.esem[o], self.cnt[o]) for o in self.eng if o != e and self.cnt[o] > 0]
            deps += [(d.sem, d.tot) for d in self.dma_sems if d.tot > 0]
            self._wait(e, deps)


def build(NE=32, dbg=None):
    nc = bass.Bass("TRN2", target_bir_lowering=False)
    voff, vrows, vpad = vec_layout(NE)
    NVB = vpad // 128

    def din(name, shape, dt=F32):
        return nc.dram_tensor(name, list(shape), dt, kind="ExternalInput").ap()

    xin = din("xin", [TL, D])
    ctxin = din("ctxin", [TC, D])
    vecs = din("vecs", [vpad, 128])
    w_mod = din("w_mod", [2, D, 6 * D])
    ev_w_in = din("ev_w_in", [D, 3 * D])
    ev_w_out = din("ev_w_out", [D, D])
    od_w_in = din("od_w_in", [D, 2 * D])
    od_w_out = din("od_w_out", [D, D])
    od_ga = din("od_ga", [2, 4, 256, 256])
    od_gx = din("od_gx", [2, 4, 256, 256])
    w_router = din("w_router", [2, D, NE])
    b_router = din("b_router", [2, NE])
    moe_w1 = din("moe_w1", [2, NE, D, 2 * D])
    moe_w2 = din("moe_w2", [2, NE, D, D])
    moe_b2 = din("moe_b2", [2, NE, D])
    lams = din("lams", [4, 64])
    ident_d = din("ident", [128, 128])
    perm_d = din("perm", [128, 128])
    cos_d = din("cosT", [128, T])
    sin_d = din("sinT", [128, T])
    utri_d = din("utri", [128, 128])
    offrow_d = din("offrow", [128, NE])
    tok4f_d = din("tok4f", [128, 4])
    out_d = nc.dram_tensor("out", [TL, D], F32, kind="ExternalOutput").ap()
    xs_d = nc.dram_tensor("xs_scratch", [128, NCH, T], F32, kind="Internal").ap()
    gsc_d = nc.dram_tensor("g_scratch", [2, NE, T], F32, kind="Internal").ap()
    psc_d = nc.dram_tensor("p_scratch", [2, NE, T], F32, kind="Internal").ap()
    cnt_d = nc.dram_tensor("cnt_scratch", [2, NE], I32, kind="Internal").ap()
    hs_d = nc.dram_tensor("hs_scratch", [NE * T, D], BF16, kind="Internal").ap()
    idx_d = nc.dram_tensor("idx_scratch", [NE * T, 1], I32, kind="Internal").ap()
    y4_d = nc.dram_tensor("y4_scratch", [4 * T, D], F32, kind="Internal").ap()
    dbg_d = None
    if dbg is not None:
        dbg_d = nc.dram_tensor("dbg", [128, NCH, T], F32, kind="ExternalOutput").ap()

    es = ExitStack()
    with es:
        g = G(nc, es)
        xs_dep = g.dep(dma=True)
        gsc_dep = [g.dep(dma=True), g.dep(dma=True)]
        psc_dep = [g.dep(dma=True), g.dep(dma=True)]
        flag_dep = [g.dep(dma=True), g.dep(dma=True)]
        hs_dep = g.dep(dma=True)
        bnd_hs = nc.gpsimd.alloc_register("bnd_hs")
        nc.gpsimd.reg_mov(bnd_hs, NE * T - 1)
        bnd_y4 = nc.gpsimd.alloc_register("bnd_y4")
        nc.gpsimd.reg_mov(bnd_y4, 4 * T - 1)
        idx_dep = g.dep(dma=True)
        y4_dep = g.dep(dma=True)
        flag_regs = nc.alloc_registers("ovf", [ET.PE, ET.Activation, ET.DVE, ET.Pool, ET.SP])
        out_dep = g.dep(dma=True)

        _uid = [0]

        def sb(es_, name, shape, dt, side=None):
            _uid[0] += 1
            return es_.enter_context(nc.sbuf_tensor("sb%d_%s" % (_uid[0], name), list(shape), dt, side=side))

        ps = [es.enter_context(nc.psum_tensor("ps%d" % i, [128, 512], F32)) for i in range(8)]
        psd = [g.dep() for _ in range(8)]

        ident = sb(es, "ident", [128, 128], F32)
        perm = sb(es, "perm", [128, 128], F32)
        ones32 = sb(es, "ones32", [128, 128], F32)
        ones16 = sb(es, "ones16", [128, 128], BF16)
        V = sb(es, "V", [128, vpad], F32)
        modT = sb(es, "modT", [128, 2, 48, 2], F32)
        S = sb(es, "S", [128, 192], F32)
        cdep = g.dep(dma=True)
        Vd = g.dep()
        modd = g.dep()
        Sd = g.dep()
        onesd = g.dep()
        g.dma("sp", ident[:], ident_d, writes=[cdep])
        g.dma("sp", perm[:], perm_d, writes=[cdep])
        identb = sb(es, "identb", [128, 128], BF16)
        utri = sb(es, "utri", [128, 128], F32)
        offrow = sb(es, "offrow", [128, NE], F32)
        tok4f = sb(es, "tok4f", [128, 4], F32)
        cdep2 = g.dep(dma=True)
        g.dma("pool", identb[:], ident_d, writes=[cdep2])
        g.dma("sp", utri[:], utri_d, writes=[cdep2])
        g.dma("sp", offrow[:], offrow_d, writes=[cdep2])
        g.dma("sp", tok4f[:], tok4f_d, writes=[cdep2])
        g.op("dve", lambda: nc.vector.memset(ones32[:], 1.0), writes=[onesd])
        g.op("dve", lambda: nc.vector.memset(ones16[:], 1.0), writes=[onesd])

        SC = {}
        _sc = [0]

        def scol(name, n):
            SC[name] = _sc[0]
            _sc[0] += n
            return SC[name]

        for l in range(2):
            for cls in range(2):
                scol("gs1_%d_%d" % (l, cls), 8)
                scol("gs2_%d_%d" % (l, cls), 8)
        scol("sg", 1)
        scol("neglam", 1)
        scol("c8", 16)
        scol("c8x2", 16)
        scol("tmp", 16)
        scol("c8h", 16)
        scol("hgab", 16)
        scol("hgxb", 16)
        assert _sc[0] <= 192

        def Scol(name, i=0):
            return S[:, SC[name] + i:SC[name] + i + 1]

        def Vcol(name, i=0):
            return V[:, voff[name] + i:voff[name] + i + 1]

        def mod(l, k, c, cls):
            return modT[:, l, k * 8 + c, cls:cls + 1]

        pes = ExitStack()
        with pes:
            vstg = [sb(pes, "vstg%d" % i, [128, 128], F32) for i in range(2)]
            vstd = [g.dep(dma=True) for _ in range(2)]
            for blk in range(NVB):
                s = blk % 2
                g.dma("sp", vstg[s][:], vecs[blk * 128:(blk + 1) * 128, :], writes=[vstd[s]])
                pb = blk % 2
                g.op("pe", lambda: nc.tensor.transpose(out=ps[pb][:, 0:128], in_=vstg[s][:], identity=ident[:]),
                     reads=[vstd[s], cdep], writes=[psd[pb]])
                g.op("act", lambda: nc.scalar.copy(out=V[:, blk * 128:(blk + 1) * 128], in_=ps[pb][:, 0:128]),
                     reads=[psd[pb]], writes=[Vd])
            b1v = V[:, voff["b1"]:voff["b1"] + 2 * NE * 16].rearrange("p (a j) -> p a j", j=16)
            g.op("dve", lambda: nc.vector.tensor_scalar(out=b1v[:, :, 8:16], in0=b1v[:, :, 8:16], scalar1=1.0,
                                                        scalar2=None, op0=ALU.add), reads=[Vd], writes=[Vd])
            scT = sb(pes, "scT", [128, 8, 2], F32R)
            scd = g.dep()
            g.op("act", lambda: nc.scalar.activation(out=scT[:, :, 0], in_=V[:, voff["c"]:voff["c"] + 8],
                                                     func=AF.Silu), reads=[Vd], writes=[scd])
            g.op("act", lambda: nc.scalar.activation(out=scT[:, :, 1], in_=V[:, voff["cctx"]:voff["cctx"] + 8],
                                                     func=AF.Silu), reads=[Vd], writes=[scd])
            wm = [sb(pes, "wm%d" % i, [128, 8, 512], F32R) for i in range(2)]
            wmd = [g.dep(dma=True) for _ in range(2)]
            it = 0
            for l in range(2):
                for blk in range(12):
                    s = it % 2
                    it += 1
                    src = w_mod[l, :, blk * 512:(blk + 1) * 512].rearrange("(c p) f -> p c f", p=128)
                    g.dma("pool", wm[s][:], src, writes=[wmd[s]])
                    for fcl in range(4):
                        pb = (blk * 4 + fcl) % 2
                        for dc in range(8):
                            g.op("pe", lambda: nc.tensor.matmul(ps[pb][:, 0:2], lhsT=wm[s][:, dc, fcl * 128:(fcl + 1) * 128],
                                                                rhs=scT[:, dc, :], start=(dc == 0), stop=(dc == 7)),
                                 reads=[wmd[s], scd], writes=[psd[pb]])
                        if True:
                            kk = blk * 4 + fcl
                            g.op("dve", lambda: nc.vector.tensor_scalar(out=modT[:, l, kk, :], in0=ps[pb][:, 0:2],
                                                                        scalar1=Vcol("bmod", l * 48 + kk), scalar2=None,
                                                                        op0=ALU.add), reads=[psd[pb], Vd], writes=[modd])
            for l in range(2):
                for cls in range(2):
                    for (nm, kc, vn) in (("gs1", 1, "nmix"), ("gs2", 4, "nffn")):
                        c0 = SC["%s_%d_%d" % (nm, l, cls)]
                        g.op("dve", lambda: nc.vector.scalar_tensor_tensor(
                            out=S[:, c0:c0 + 8], in0=modT[:, l, kc * 8:kc * 8 + 8, cls], scalar=1.0,
                            in1=V[:, voff[vn] + l * 8:voff[vn] + l * 8 + 8], op0=ALU.add, op1=ALU.mult),
                            reads=[modd, Vd], writes=[Sd])
            lam_init0 = 0.8 - 0.6 * math.exp(-0.3 * 0)
            g.op("dve", lambda: nc.vector.tensor_scalar(out=Scol("sg"), in0=Vcol("subln"), scalar1=(1.0 - lam_init0),
                                                        scalar2=None, op0=ALU.mult), reads=[Vd], writes=[Sd])
            lb = sb(pes, "lamb", [128, 4, 64], F32)
            lbd = g.dep(dma=True)
            for i in range(4):
                g.dma("sp", lb[:, i, :], lams[i:i + 1, :].to_broadcast([128, 64]), writes=[lbd])
            lt = sb(pes, "lamt", [128, 2, 64], F32)
            ltd = g.dep()
            g.op("dve", lambda: nc.vector.tensor_tensor(out=lt[:, 0, :], in0=lb[:, 0, :], in1=lb[:, 1, :], op=ALU.mult),
                 reads=[lbd], writes=[ltd])
            g.op("dve", lambda: nc.vector.tensor_tensor(out=lt[:, 1, :], in0=lb[:, 2, :], in1=lb[:, 3, :], op=ALU.mult),
                 reads=[lbd], writes=[ltd])
            tm = SC["tmp"]
            g.op("dve", lambda: nc.vector.tensor_reduce(out=S[:, tm:tm + 2], in_=lt[:], axis=AX.X, op=ALU.add),
                 reads=[ltd], writes=[Sd])
            g.op("act", lambda: nc.scalar.activation(out=S[:, tm + 2:tm + 4], in_=S[:, tm:tm + 2], func=AF.Exp),
                 reads=[Sd], writes=[Sd])
            g.op("dve", lambda: nc.vector.scalar_tensor_tensor(out=Scol("neglam"), in0=S[:, tm + 3:tm + 4],
                                                               scalar=-lam_init0, in1=S[:, tm + 2:tm + 3],
                                                               op0=ALU.add, op1=ALU.subtract), reads=[Sd], writes=[Sd])
            g.op("act", lambda: nc.scalar.activation(out=S[:, tm:tm + 16], in_=V[:, voff["odlam"]:voff["odlam"] + 16],
                                                     func=AF.Exp, scale=-1.0), reads=[Vd, Sd], writes=[Sd])
            g.op("act", lambda: nc.scalar.activation(out=S[:, tm:tm + 16], in_=S[:, tm:tm + 16], func=AF.Ln, bias=1.0),
                 reads=[Sd], writes=[Sd])
            g.op("dve", lambda: nc.vector.tensor_scalar(out=S[:, SC["c8"]:SC["c8"] + 16], in0=S[:, tm:tm + 16],
                                                        scalar1=-8.0, scalar2=None, op0=ALU.mult), reads=[Sd], writes=[Sd])
            g.op("dve", lambda: nc.vector.tensor_scalar(out=S[:, SC["c8x2"]:SC["c8x2"] + 16], in0=S[:, tm:tm + 16],
                                                        scalar1=-16.0, scalar2=None, op0=ALU.mult), reads=[Sd], writes=[Sd])
            g.op("dve", lambda: nc.vector.tensor_scalar(out=S[:, SC["c8h"]:SC["c8h"] + 16], in0=S[:, tm:tm + 16],
                                                        scalar1=-4.0, scalar2=None, op0=ALU.mult), reads=[Sd], writes=[Sd])
            g.op("dve", lambda: nc.vector.tensor_scalar(out=S[:, SC["hgab"]:SC["hgab"] + 16], in0=V[:, voff["odgab"]:voff["odgab"] + 16],
                                                        scalar1=0.5, scalar2=None, op0=ALU.mult), reads=[Sd, Vd], writes=[Sd])
            g.op("dve", lambda: nc.vector.tensor_scalar(out=S[:, SC["hgxb"]:SC["hgxb"] + 16], in0=V[:, voff["odgxb"]:voff["odgxb"] + 16],
                                                        scalar1=0.5, scalar2=None, op0=ALU.mult), reads=[Sd, Vd], writes=[Sd])
            g.barrier()
        hb = sb(es, "hb", [128, NCH, T], BF16)
        hbd = [[g.dep() for _ in range(NTT)] for _ in range(NCH)]
        xes = ExitStack()
        xstate = {}

        def alloc_x():
            xstate["x"] = xes.enter_context(nc.sbuf_tensor("xres%d" % len(xstate), [128, NCH, T], F32, side="right"))
            xstate["d"] = [[g.dep() for _ in range(NTT)] for _ in range(NCH)]

        def load_tok_tile(pes_bufs, tt, l0_src=True):
            xst, xstd, tstg, tstgd, cnt = pes_bufs
            s = cnt[0] % 2
            cnt[0] += 1
            n = TN[tt]
            for sub in range(n // 128):
                k = cnt[1] % 2
                cnt[1] += 1
                if tt == 0:
                    src = ctxin[sub * 128:(sub + 1) * 128, :]
                else:
                    r0 = TOFF[tt] - TC + sub * 128
                    src = xin[r0:r0 + 128, :]
                g.dma("sp", tstg[k][:], src, writes=[tstgd[k]])
                for half in range(2):
                    pb = 6 + half
                    for cc in range(4):
                        c = half * 4 + cc
                        g.op("pe", lambda: nc.tensor.transpose(out=ps[pb][:, cc * 128:(cc + 1) * 128],
                                                               in_=tstg[k][:, c * 128:(c + 1) * 128], identity=ident[:]),
                             reads=[tstgd[k], cdep], writes=[psd[pb]])
                    dst = xst[s][:, half * 4:half * 4 + 4, sub * 128:(sub + 1) * 128]
                    srcp = ps[pb][:, :].rearrange("p (c t) -> p c t", t=128)
                    if half == 0:
                        g.op("act", lambda: nc.scalar.copy(out=dst, in_=srcp), reads=[psd[pb]], writes=[xstd[s]])
                    else:
                        g.op("dve", lambda: nc.vector.tensor_copy(out=dst, in_=srcp), reads=[psd[pb]], writes=[xstd[s]])
            return xst[s], xstd[s]

        def rmsnorm_tile(nb, xsrc, xdeps, tt, gsname, l, k_sh, out32=None, out32d=None, hbout=None):
            sq, sqd, tmp, tmpd, rs, rsd, cnt = nb
            n = TN[tt]
            cls = 1 if tt == 0 else 0
            pb = 5
            for c in range(NCH):
                s = cnt[0] % 2
                cnt[0] += 1
                g.op("act", lambda: nc.scalar.activation(out=sq[s][:, 0:n], in_=xsrc(c), func=AF.Square),
                     reads=[xdeps(c)], writes=[sqd[s]])
                g.op("pe", lambda: nc.tensor.matmul(ps[pb][:, 0:n], lhsT=ones32[:], rhs=sq[s][:, 0:n],
                                                    start=(c == 0), stop=(c == NCH - 1)),
                     reads=[sqd[s], onesd], writes=[psd[pb]])
            g.op("act", lambda: nc.scalar.activation(out=rs[:, 0:n], in_=ps[pb][:, 0:n], func=AF.Sqrt,
                                                     scale=1.0 / D, bias=EPS), reads=[psd[pb]], writes=[rsd])
            g.op("dve", lambda: nc.vector.reciprocal(out=rs[:, 0:n], in_=rs[:, 0:n]), reads=[rsd], writes=[rsd])
            c0 = SC["%s_%d_%d" % (gsname, l, cls)]
            for c in range(NCH):
                s = cnt[1] % 2
                cnt[1] += 1
                g.op("dve", lambda: nc.vector.tensor_tensor(out=tmp[s][:, 0:n], in0=xsrc(c), in1=rs[:, 0:n], op=ALU.mult),
                     reads=[xdeps(c), rsd], writes=[tmpd[s]])
                ho, hod = (hb[:, c, tsl(tt)], hbd[c][tt]) if hbout is None else hbout(c)
                g.op("act", lambda: nc.scalar.activation(out=ho, in_=tmp[s][:, 0:n], func=AF.Identity,
                                                         scale=S[:, c0 + c:c0 + c + 1], bias=mod(l, k_sh, c, cls)),
                     reads=[tmpd[s], Sd, modd], writes=[hod])
                if out32 is not None:
                    g.op("dve", lambda: nc.vector.tensor_scalar(out=out32[:, c, 0:n], in0=tmp[s][:, 0:n],
                                                                scalar1=S[:, c0 + c:c0 + c + 1],
                                                                scalar2=mod(l, k_sh, c, cls), op0=ALU.mult, op1=ALU.add),
                         reads=[tmpd[s], Sd, modd], writes=[out32d])

        def norm_bufs(es_):
            sq = [sb(es_, "nsq%d" % i, [128, 512], F32) for i in range(2)]
            tmp = [sb(es_, "ntmp%d" % i, [128, 512], F32) for i in range(2)]
            rs = sb(es_, "nrs", [128, 512], F32)
            return (sq, [g.dep(), g.dep()], tmp, [g.dep(), g.dep()], rs, g.dep(), [0, 0])

        def stage_bufs(es_):
            xst = [sb(es_, "xst%d" % i, [128, NCH, 512], F32) for i in range(2)]
            tstg = [sb(es_, "tstg%d" % i, [128, D], F32) for i in range(2)]
            return (xst, [g.dep(dma=True), g.dep(dma=True)], tstg, [g.dep(dma=True), g.dep(dma=True)], [0, 0])

        def out_proj(l, wout_d, catfn, catdeps, tiles, xold_fn, es_):
            wo = sb(es_, "wo%d" % l, [128, NCH, D], BF16)
            wod = g.dep(dma=True)
            for hh in range(2):
                g.dma("pool", wo[:, :, hh * 512:(hh + 1) * 512],
                      wout_d[:, hh * 512:(hh + 1) * 512].rearrange("(c p) f -> p c f", p=128), writes=[wod])
            x = xstate["x"]
            xd = xstate["d"]
            it = 0
            for tt in tiles:
                n = TN[tt]
                cls = 1 if tt == 0 else 0
                xo, xod = xold_fn(tt)
                for oc in range(NCH):
                    pb = it % 2
                    it += 1
                    for c in range(NCH):
                        g.op("pe", lambda: nc.tensor.matmul(ps[pb][:, 0:n], lhsT=wo[:, c, oc * 128:(oc + 1) * 128],
                                                            rhs=catfn(c, tt), start=(c == 0), stop=(c == NCH - 1)),
                             reads=[wod, catdeps(c, tt)], writes=[psd[pb]])
                    g.op("dve", lambda: nc.vector.scalar_tensor_tensor(out=x[:, oc, tsl(tt)], in0=ps[pb][:, 0:n],
                                                                       scalar=mod(l, 2, oc, cls), in1=xo[:, oc, 0:n],
                                                                       op0=ALU.mult, op1=ALU.add),
                         reads=[psd[pb], xod, modd], writes=[xd[oc][tt]])

        L0 = ExitStack()
        with L0:
            catc = sb(L0, "catc", [128, 4, T], BF16)
            catcd = [[g.dep() for _ in range(NTT)] for _ in range(4)]
            A0 = ExitStack()
            with A0:
                stg = stage_bufs(A0)
                nb = norm_bufs(A0)
                for tt in range(NTT):
                    xt, xtd = load_tok_tile(stg, tt)
                    rmsnorm_tile(nb, lambda c: xt[:, c, 0:TN[tt]], lambda c: xtd, tt, "gs1", 0, 0)
                g.barrier()
            M0 = ExitStack()
            with M0:
                wr = [sb(M0, "w0r%d" % i, [128, NCH, 512], BF16) for i in range(3)]
                wrd = [g.dep(dma=True) for _ in range(3)]

                def load_win(slot, blk):
                    g.dma("pool", wr[slot][:], ev_w_in[:, blk * 512:(blk + 1) * 512].rearrange("(c p) f -> p c f", p=128),
                          writes=[wrd[slot]])

                load_win(0, 3)
                load_win(1, 4)
                load_win(2, 5)
                CV = ExitStack()
                with CV:
                    pj = sb(CV, "pj", [128, T], F32)
                    accj = sb(CV, "accj", [128, T], F32)
                    ctmp = [sb(CV, "ctmp%d" % i, [128, 512], F32) for i in range(2)]
                    pjd = g.dep()
                    accd = g.dep()
                    ctd = [g.dep(), g.dep()]
                    it = 0
                    for j in range(4):
                        for tt in range(NTT):
                            n = TN[tt]
                            for which, pb in ((1, 0), (2, 1)):
                                for c in range(NCH):
                                    g.op("pe", lambda: nc.tensor.matmul(ps[pb][:, 0:n], lhsT=wr[which][:, c, j * 128:(j + 1) * 128],
                                                                        rhs=hb[:, c, tsl(tt)], start=(c == 0), stop=(c == NCH - 1)),
                                         reads=[wrd[which], hbd[c][tt]], writes=[psd[pb]])
                            s = it % 2
                            it += 1
                            g.op("act", lambda: nc.scalar.copy(out=ctmp[s][:, 0:n], in_=ps[0][:, 0:n]), reads=[psd[0]], writes=[ctd[s]])
                            g.op("dve", lambda: nc.vector.tensor_tensor(out=pj[:, tsl(tt)], in0=ps[1][:, 0:n], in1=ctmp[s][:, 0:n],
                                                                        op=ALU.mult), reads=[psd[1], ctd[s]], writes=[pjd])
                        for (a, b) in ((0, TC), (TC, T)):
                            g.op("dve", lambda: nc.vector.tensor_scalar(out=accj[:, a:b], in0=pj[:, a:b], scalar1=Vcol("evconv", 1 * 4 + j),
                                                                        scalar2=None, op0=ALU.mult), reads=[pjd, Vd], writes=[accd])
                            g.op("dve", lambda: nc.vector.scalar_tensor_tensor(out=accj[:, a + 1:b], in0=pj[:, a:b - 1],
                                                                               scalar=Vcol("evconv", 0 * 4 + j), in1=accj[:, a + 1:b],
                                                                               op0=ALU.mult, op1=ALU.add), reads=[pjd, Vd, accd], writes=[accd])
                            g.op("dve", lambda: nc.vector.scalar_tensor_tensor(out=accj[:, a:b - 1], in0=pj[:, a + 1:b],
                                                                               scalar=Vcol("evconv", 2 * 4 + j), in1=accj[:, a:b - 1],
                                                                               op0=ALU.mult, op1=ALU.add), reads=[pjd, Vd, accd], writes=[accd])
                        for tt in range(NTT):
                            n = TN[tt]
                            pb = 2 + (tt % 2)
                            for c in range(NCH):
                                g.op("pe", lambda: nc.tensor.matmul(ps[pb][:, 0:n], lhsT=wr[0][:, c, j * 128:(j + 1) * 128],
                                                                    rhs=hb[:, c, tsl(tt)], start=(c == 0), stop=(c == NCH - 1)),
                                     reads=[wrd[0], hbd[c][tt]], writes=[psd[pb]])
                            g.op("dve", lambda: nc.vector.tensor_tensor(out=catc[:, j, tsl(tt)], in0=ps[pb][:, 0:n], in1=accj[:, tsl(tt)],
                                                                        op=ALU.mult), reads=[psd[pb], accd], writes=[catcd[j][tt]])
                    g.barrier()
                load_win(0, 0)
                load_win(1, 1)
                load_win(2, 2)
                qk = [sb(M0, "q", [128, 4, T], BF16), sb(M0, "k", [128, 4, T], BF16)]
                qkd = [[[g.dep() for _ in range(NTT)] for _ in range(4)] for _ in range(2)]
                vt = sb(M0, "v", [128, NKT, 512], BF16)
                vtd = [g.dep() for _ in range(NKT)]
                cosT = sb(M0, "cosT", [128, T], F32)
                sinT = sb(M0, "sinT", [128, T], F32)
                tabd = g.dep(dma=True)
                g.dma("sp", cosT[:], cos_d, writes=[tabd])
                g.dma("sp", sinT[:], sin_d, writes=[tabd])
                RP = ExitStack()
                with RP:
                    qf = [sb(RP, "qf%d" % i, [128, 512], F32) for i in range(2)]
                    qfd = [g.dep(), g.dep()]
                    t1 = [sb(RP, "rt1%d" % i, [128, 512], F32) for i in range(2)]
                    t1d = [g.dep(), g.dep()]
                    t2 = [sb(RP, "rt2%d" % i, [128, 512], F32) for i in range(2)]
                    t2d = [g.dep(), g.dep()]
                    it = 0
                    for which in range(2):
                        for hc in range(4):
                            for tt in range(NTT):
                                n = TN[tt]
                                s = it % 2
                                it += 1
                                pb = s
                                pr = 2 + s
                                for c in range(NCH):
                                    g.op("pe", lambda: nc.tensor.matmul(ps[pb][:, 0:n], lhsT=wr[which][:, c, hc * 128:(hc + 1) * 128],
                                                                        rhs=hb[:, c, tsl(tt)], start=(c == 0), stop=(c == NCH - 1)),
                                         reads=[wrd[which], hbd[c][tt]], writes=[psd[pb]])
                                g.op("act", lambda: nc.scalar.copy(out=qf[s][:, 0:n], in_=ps[pb][:, 0:n]), reads=[psd[pb]], writes=[qfd[s]])
                                g.op("pe", lambda: nc.tensor.matmul(ps[pr][:, 0:n], lhsT=perm[:], rhs=qf[s][:, 0:n], start=True, stop=True),
                                     reads=[qfd[s], cdep], writes=[psd[pr]])
                                g.op("dve", lambda: nc.vector.tensor_tensor(out=t1[s][:, 0:n], in0=qf[s][:, 0:n], in1=cosT[:, tsl(tt)], op=ALU.mult),
                                     reads=[qfd[s], tabd], writes=[t1d[s]])
                                g.op("dve", lambda: nc.vector.tensor_tensor(out=t2[s][:, 0:n], in0=ps[pr][:, 0:n], in1=sinT[:, tsl(tt)], op=ALU.mult),
                                     reads=[psd[pr], tabd], writes=[t2d[s]])
                                g.op("pool", lambda: nc.gpsimd.tensor_tensor(out=qk[which][:, hc, tsl(tt)], in0=t1[s][:, 0:n], in1=t2[s][:, 0:n], op=ALU.add),
                                     reads=[t1d[s], t2d[s]], writes=[qkd[which][hc][tt]])
                    for kt in range(NKT):
                        tt = 0 if kt < 2 else 1 + (kt - 2) // 4
                        pb = 4 + kt % 2
                        for c in range(NCH):
                            g.op("pe", lambda: nc.tensor.matmul(ps[pb][:, :], lhsT=hb[:, c, kt * 128:(kt + 1) * 128], rhs=wr[2][:, c, :],
                                                                start=(c == 0), stop=(c == NCH - 1)),
                                 reads=[wrd[2], hbd[c][tt]], writes=[psd[pb]])
                        g.op("act", lambda: nc.scalar.copy(out=vt[:, kt, :], in_=ps[pb][:, :]), reads=[psd[pb]], writes=[vtd[kt]])
                    g.barrier()
                AT = ExitStack()
                with AT:
                    eb = [[sb(AT, "e%d_%d" % (m, i), [128, 512], BF16) for i in range(2)] for m in range(2)]
                    ebd = [[g.dep(), g.dep()] for _ in range(2)]
                    rz = [sb(AT, "rz%d" % m, [128, 512], F32) for m in range(2)]
                    rzd = [g.dep(), g.dep()]
                    to = [sb(AT, "to%d" % m, [128, 512], F32) for m in range(2)]
                    tod = [g.dep(), g.dep()]
                    osb = sb(AT, "osb", [128, 512], F32)
                    osd = g.dep()
                    osq = sb(AT, "osq", [128, 512], F32)
                    osqd = g.dep()
                    ors = sb(AT, "ors", [128, 512], F32)
                    orsd = g.dep()
                    for h in range(4):
                        for qt in range(NTT):
                            n = TN[qt]
                            nkt = 2 if qt == 0 else NKT
                            def scores(kt):
                                ktt = 0 if kt < 2 else 1 + (kt - 2) // 4
                                sl = kt % 2
                                for m in range(2):
                                    pbs = 4 + 2 * m + sl
                                    g.op("pe", lambda: nc.tensor.matmul(ps[pbs][:, 0:n], lhsT=qk[1][m * 64:(m + 1) * 64, h, kt * 128:(kt + 1) * 128],
                                                                        rhs=qk[0][m * 64:(m + 1) * 64, h, tsl(qt)], start=True, stop=True),
                                         reads=[qkd[1][h][ktt], qkd[0][h][qt]], writes=[psd[pbs]])
                                    g.op("act", lambda: nc.scalar.activation(out=eb[m][sl][:, 0:n], in_=ps[pbs][:, 0:n], func=AF.Exp, scale=0.125),
                                         reads=[psd[pbs]], writes=[ebd[m][sl]])

                            scores(0)
                            for kt in range(nkt):
                                sl = kt % 2
                                if kt + 1 < nkt:
                                    scores(kt + 1)
                                for m in range(2):
                                    g.op("pe", lambda: nc.tensor.matmul(ps[2 * m][:, 0:n], lhsT=vt[:, kt, h * 128:(h + 1) * 128], rhs=eb[m][sl][:, 0:n],
                                                                        start=(kt == 0), stop=(kt == nkt - 1)),
                                         reads=[vtd[kt], ebd[m][sl]], writes=[psd[2 * m]])
                                    g.op("pe", lambda: nc.tensor.matmul(ps[2 * m + 1][:, 0:n], lhsT=ones16[:], rhs=eb[m][sl][:, 0:n],
                                                                        start=(kt == 0), stop=(kt == nkt - 1)),
                                         reads=[onesd, ebd[m][sl]], writes=[psd[2 * m + 1]])
                            for m in range(2):
                                g.op("dve", lambda: nc.vector.reciprocal(out=rz[m][:, 0:n], in_=ps[2 * m + 1][:, 0:n]), reads=[psd[2 * m + 1]], writes=[rzd[m]])
                                g.op("dve", lambda: nc.vector.tensor_tensor(out=to[m][:, 0:n], in0=ps[2 * m][:, 0:n], in1=rz[m][:, 0:n], op=ALU.mult),
                                     reads=[psd[2 * m], rzd[m]], writes=[tod[m]])
                            g.op("dve", lambda: nc.vector.scalar_tensor_tensor(out=osb[:, 0:n], in0=to[1][:, 0:n], scalar=Scol("neglam"), in1=to[0][:, 0:n],
                                                                               op0=ALU.mult, op1=ALU.add), reads=[tod[0], tod[1], Sd], writes=[osd])
                            g.op("act", lambda: nc.scalar.activation(out=osq[:, 0:n], in_=osb[:, 0:n], func=AF.Square), reads=[osd], writes=[osqd])
                            g.op("pe", lambda: nc.tensor.matmul(ps[4][:, 0:n], lhsT=ones32[:], rhs=osq[:, 0:n], start=True, stop=True),
                                 reads=[osqd, onesd], writes=[psd[4]])
                            g.op("act", lambda: nc.scalar.activation(out=ors[:, 0:n], in_=ps[4][:, 0:n], func=AF.Sqrt, scale=1.0 / 128, bias=EPS),
                                 reads=[psd[4]], writes=[orsd])
                            g.op("dve", lambda: nc.vector.reciprocal(out=ors[:, 0:n], in_=ors[:, 0:n]), reads=[orsd], writes=[orsd])
                            g.op("dve", lambda: nc.vector.tensor_tensor(out=osb[:, 0:n], in0=osb[:, 0:n], in1=ors[:, 0:n], op=ALU.mult),
                                 reads=[orsd, osd], writes=[osd])
                            g.op("act", lambda: nc.scalar.activation(out=hb[:, h, tsl(qt)], in_=osb[:, 0:n], func=AF.Identity, scale=Scol("sg")),
                                 reads=[osd, Sd], writes=[hbd[h][qt]])
                    g.barrier()
            alloc_x()
            C0 = ExitStack()
            with C0:
                stg = stage_bufs(C0)

                def xold0(tt):
                    return load_tok_tile(stg, tt)

                def cat0(c, tt):
                    return hb[:, c, tsl(tt)] if c < 4 else catc[:, c - 4, tsl(tt)]

                def cat0d(c, tt):
                    return hbd[c][tt] if c < 4 else catcd[c - 4][tt]

                out_proj(0, ev_w_out, cat0, cat0d, range(NTT), xold0, C0)
                g.barrier()

        def moe_layer(l, tiles):
            x = xstate["x"]
            xd = xstate["d"]
            subs = []
            for tt in tiles:
                for sub in range(TN[tt] // 128):
                    subs.append((TOFF[tt] // 128 + sub, tt, sub))
            hbTok = hb[:].rearrange("p c t -> p (c t)").rearrange("p (i f) -> p i f", f=D)
            hbtokd = [g.dep() for _ in range(NKT)]
            ML = ExitStack()
            with ML:
                GK = sb(ML, "GK", [128, NKT, 4], F32)
                GKd = g.dep()
                DD = ExitStack()
                with DD:
                    nb = norm_bufs(DD)
                    h32 = sb(DD, "h32", [128, NCH, 512], F32)
                    h32d = g.dep()
                    hbt = sb(DD, "hbt", [128, NCH, 512], BF16)
                    hbtd = g.dep()
                    wrt = sb(DD, "wrt", [128, NCH, NE], F32)
                    brt = sb(DD, "brt", [1, NE], F32)
                    b2n = sb(DD, "b2n", [NE, D], F32)
                    rtd = g.dep(dma=True)
                    g.dma("sp", wrt[:], w_router[l].rearrange("(c p) e -> p c e", p=128), writes=[rtd])
                    g.dma("sp", brt[:], b_router[l:l + 1, :], writes=[rtd])
                    g.dma("sp", b2n[:], moe_b2[l], writes=[rtd])
                    gT = sb(DD, "gT", [NE, T], F32)
                    gTd = g.dep()
                    carry = sb(DD, "carry", [128, NE], F32)
                    card = g.dep()
                    g.op("dve", lambda: nc.vector.memset(carry[:], 0.0), writes=[card])
                    oob = sb(DD, "oob", [128, NE * T // 128], I32)
                    oobd = g.dep()
                    g.op("pool", lambda: nc.gpsimd.memset(oob[:], 2000000000), writes=[oobd])
                    g.dma("sp", idx_d[:, :].rearrange("(p r) o -> p (r o)", p=128), oob[:], reads=[oobd], writes=[idx_dep])
                    lg = sb(DD, "lg", [128, NE], F32)
                    ex = sb(DD, "ex", [128, NE], F32)
                    mk = sb(DD, "mk", [128, NE], F32)
                    gt_ = sb(DD, "gt_", [128, NE], F32)
                    ngd = sb(DD, "ngd", [128, NE], F32)
                    t8 = sb(DD, "t8", [128, 8], F32)
                    t8b = sb(DD, "t8b", [128, 8], F32)
                    sm = sb(DD, "sm", [128, 4], F32)
                    fli = sb(DD, "fli", [128, NE], I32)
                    NIX = 3
                    idx4 = [sb(DD, "idx4_%d" % i, [128, 4], I32) for i in range(NIX)]
                    val4 = [sb(DD, "val4_%d" % i, [128, 4], I32) for i in range(NIX)]
                    ixd = [g.dep() for _ in range(NIX)]
                    rd = g.dep()
                    mkd = g.dep()
                    ixc = 0
                    for tt in tiles:
                        n = TN[tt]
                        rmsnorm_tile(nb, lambda c: x[:, c, tsl(tt)], lambda c: xd[c][tt], tt, "gs2", l, 3, out32=h32, out32d=h32d,
                                     hbout=lambda c: (hbt[:, c, 0:n], hbtd))
                        for sub in range(n // 128):
                            t0 = TOFF[tt] + sub * 128
                            i = t0 // 128
                            psb = ps[6][:, :].bitcast(BF16)
                            for c in range(NCH):
                                g.op("pe", lambda: nc.tensor.transpose(out=psb[:, c * 128:(c + 1) * 128], in_=hbt[:, c, sub * 128:(sub + 1) * 128], identity=identb[:]),
                                     reads=[hbtd, cdep2], writes=[psd[6]])
                            g.op("act", lambda: nc.scalar.copy(out=hbTok[:, i, :], in_=psb), reads=[psd[6]], writes=[hbtokd[i]])
                            pb = 0
                            for c in range(NCH):
                                g.op("pe", lambda: nc.tensor.matmul(ps[pb][:, 0:NE], lhsT=h32[:, c, sub * 128:(sub + 1) * 128], rhs=wrt[:, c, :],
                                                                    start=(c == 0), stop=False),
                                     reads=[h32d, rtd], writes=[psd[pb]])
                            g.op("pe", lambda: nc.tensor.matmul(ps[pb][:, 0:NE], lhsT=ones32[0:1, :], rhs=brt[0:1, :], start=False, stop=True),
                                 reads=[rtd, onesd], writes=[psd[pb]])
                            g.op("act", lambda: nc.scalar.copy(out=lg[:], in_=ps[pb][:, 0:NE]), reads=[psd[pb]], writes=[rd])
                            g.op("dve", lambda: nc.vector.max(out=t8[:], in_=lg[:]), reads=[rd], writes=[rd])
                            g.op("dve", lambda: nc.vector.tensor_scalar(out=sm[:, 0:1], in0=t8[:, 0:1], scalar1=-1.0, scalar2=None, op0=ALU.mult),
                                 reads=[rd], writes=[rd])
                            g.op("act", lambda: nc.scalar.activation(out=ex[:], in_=lg[:], func=AF.Exp, bias=sm[:, 0:1]), reads=[rd], writes=[rd])
                            g.op("dve", lambda: nc.vector.tensor_scalar(out=mk[:], in0=lg[:], scalar1=t8[:, 3:4], scalar2=None, op0=ALU.is_ge),
                                 reads=[rd, mkd], writes=[rd, mkd])
                            g.op("dve", lambda: nc.vector.tensor_tensor(out=ex[:], in0=ex[:], in1=mk[:], op=ALU.mult), reads=[rd], writes=[rd])
                            g.op("dve", lambda: nc.vector.tensor_reduce(out=sm[:, 1:2], in_=ex[:], axis=AX.X, op=ALU.add), reads=[rd], writes=[rd])
                            g.op("dve", lambda: nc.vector.reciprocal(out=sm[:, 2:3], in_=sm[:, 1:2]), reads=[rd], writes=[rd])
                            g.op("dve", lambda: nc.vector.tensor_scalar(out=gt_[:], in0=ex[:], scalar1=sm[:, 2:3], scalar2=None, op0=ALU.mult),
                                 reads=[rd], writes=[rd])
                            g.op("pe", lambda: nc.tensor.transpose(out=ps[1][0:NE, 0:128], in_=gt_[:], identity=ident[:]),
                                 reads=[rd, cdep], writes=[psd[1]])
                            g.op("act", lambda: nc.scalar.copy(out=gT[:, t0:t0 + 128], in_=ps[1][0:NE, 0:128]), reads=[psd[1]], writes=[gTd])
                            g.op("pe", lambda: nc.tensor.matmul(ps[2][:, 0:NE], lhsT=utri[:], rhs=mk[:], start=True, stop=True),
                                 reads=[mkd, cdep2], writes=[psd[2]])
                            g.op("pe", lambda: nc.tensor.matmul(ps[3][:, 0:NE], lhsT=ones32[:], rhs=mk[:], start=True, stop=True),
                                 reads=[mkd, onesd], writes=[psd[3]])
                            g.op("dve", lambda: nc.vector.tensor_tensor(out=ex[:], in0=ps[2][:, 0:NE], in1=carry[:], op=ALU.add), reads=[psd[2], card, rd], writes=[rd])
                            g.op("dve", lambda: nc.vector.tensor_tensor(out=ex[:], in0=ex[:], in1=offrow[:], op=ALU.add), reads=[rd, cdep2], writes=[rd])
                            g.op("dve", lambda: nc.vector.tensor_tensor(out=ex[:], in0=ex[:], in1=mk[:], op=ALU.mult), reads=[rd], writes=[rd])
                            g.op("dve", lambda: nc.vector.tensor_scalar(out=ngd[:], in0=mk[:], scalar1=1.0e6, scalar2=-1.0e6, op0=ALU.mult, op1=ALU.add), reads=[rd], writes=[rd])
                            g.op("dve", lambda: nc.vector.tensor_tensor(out=ngd[:], in0=ngd[:], in1=ex[:], op=ALU.subtract), reads=[rd], writes=[rd])
                            g.op("dve", lambda: nc.vector.tensor_tensor(out=carry[:], in0=ps[3][:, 0:NE], in1=carry[:], op=ALU.add), reads=[psd[3], rd], writes=[card])
                            g.op("dve", lambda: nc.vector.max(out=t8b[:], in_=ngd[:]), reads=[rd], writes=[rd])
                            si = ixc % NIX
                            ixc += 1
                            g.op("dve", lambda: nc.vector.tensor_scalar(out=idx4[si][:], in0=t8b[:, 0:4], scalar1=-1.0, scalar2=None, op0=ALU.mult), reads=[rd], writes=[ixd[si]])
                            g.op("dve", lambda: nc.vector.tensor_scalar(out=val4[si][:], in0=tok4f[:], scalar1=float(4 * t0), scalar2=None, op0=ALU.add), reads=[cdep2], writes=[ixd[si]])
                            for k in range(4):
                                g.op("dve", lambda: nc.vector.scalar_tensor_tensor(out=ex[:], in0=ngd[:], scalar=t8b[:, k:k + 1], in1=gt_[:], op0=ALU.is_equal, op1=ALU.mult,
                                                                                   accum_out=GK[:, i, k:k + 1]), reads=[rd], writes=[rd, GKd])
                            for k in range(4):
                                g.idma(hs_d[:, :], idx4[si][:, k:k + 1], hbTok[:, i, :], bnd_hs, reads=[ixd[si], hbtokd[i]], writes=[hs_dep])
                                g.idma(idx_d[:, :], idx4[si][:, k:k + 1], val4[si][:, k:k + 1], bnd_hs, reads=[ixd[si]], writes=[idx_dep])
                    g.op("dve", lambda: nc.vector.tensor_copy(out=fli[:], in_=carry[:]), reads=[card, rd], writes=[rd])
                    g.dma("sp", cnt_d[l:l + 1, :], fli[0:1, :], reads=[rd], writes=[flag_dep[l]])
                    it = 0
                    for tt in tiles:
                        n = TN[tt]
                        for oc in range(NCH):
                            pb = 2 + it % 2
                            it += 1
                            g.op("pe", lambda: nc.tensor.matmul(ps[pb][:, 0:n], lhsT=b2n[:, oc * 128:(oc + 1) * 128], rhs=gT[:, tsl(tt)], start=True, stop=True),
                                 reads=[rtd, gTd], writes=[psd[pb]])
                            g.op("dve", lambda: nc.vector.scalar_tensor_tensor(out=x[:, oc, tsl(tt)], in0=ps[pb][:, 0:n], scalar=mod(l, 5, oc, 1 if tt == 0 else 0),
                                                                               in1=x[:, oc, tsl(tt)], op0=ALU.mult, op1=ALU.add),
                                 reads=[psd[pb], modd], writes=[xd[oc][tt]])
                    g.barrier()
                EE = ExitStack()
                with EE:
                    NSLOT = 4
                    hbflat = hb[:].rearrange("p c t -> p (c t)")
                    ring = [hbflat[:, k * 4096:(k + 1) * 4096].rearrange("p (c f) -> p c f", f=512) for k in range(NSLOT)]
                    ringd = [g.dep(dma=True) for _ in range(NSLOT)]
                    pf = [sb(EE, "pf%d" % i, [128, NCH, 512], BF16) for i in range(4)]
                    pfd = [g.dep(dma=True) for _ in range(4)]
                    hgToks = [sb(EE, "hgTok%d" % i, [128, 3, D], BF16) for i in range(3)]
                    hgds = [g.dep(dma=True) for _ in range(3)]
                    idxts = [sb(EE, "idxt%d" % i, [128, 3, 1], I32) for i in range(3)]
                    idxtds = [g.dep(dma=True) for _ in range(3)]

                    def gather_load(e, c0, hsel):
                        r0 = e * T + c0 * CAP
                        g.dma("sp", hgToks[hsel][:], hs_d[r0:r0 + CAP, :].rearrange("(j p) f -> p j f", p=128), reads=[hs_dep], writes=[hgds[hsel]])
                        for j3 in range(3):
                            g.dma("sp", idxts[hsel][:, j3, :], idx_d[r0 + j3 * 128:r0 + (j3 + 1) * 128, :], reads=[idx_dep], writes=[idxtds[hsel]])
                    hbg = sb(EE, "hbg", [128, NCH, CAP], BF16)
                    hbgd = [g.dep() for _ in range(NCH)]
                    actT = sb(EE, "actT", [128, NCH, CAP], BF16)
                    actd = [g.dep() for _ in range(NCH)]
                    yTok = sb(EE, "yTok", [128, 3, D], F32)
                    yTd = [g.dep() for _ in range(3)]
                    At = [sb(EE, "mA%d" % i, [128, CAP], F32) for i in range(2)]
                    St = [sb(EE, "mS%d" % i, [128, CAP], F32) for i in range(2)]
                    Lt = [sb(EE, "mL%d" % i, [128, CAP], F32) for i in range(2)]
                    Ad = [g.dep(), g.dep()]
                    Sdp = [g.dep(), g.dep()]
                    Ld = [g.dep(), g.dep()]
                    rc = [0]
                    ec = [0]
                    pc_ = [0]

                    def wsrc(ap):
                        return ap.rearrange("(c p) f -> p c f", p=128)

                    def expert_loads(e):
                        g.dma("pool", ring[0][:], wsrc(moe_w1[l, e, :, 512:1024]), writes=[ringd[0]])
                        g.dma("pool", ring[1][:], wsrc(moe_w1[l, e, :, D + 512:D + 1024]), writes=[ringd[1]])
                        g.dma("pool", ring[2][:], wsrc(moe_w2[l, e, :, 0:512]), writes=[ringd[2]])
                        g.dma("pool", ring[3][:], wsrc(moe_w2[l, e, :, 512:1024]), writes=[ringd[3]])

                    def prefetchA(e):
                        par = (e % 2) * 2
                        g.dma("pool", pf[par][:], wsrc(moe_w1[l, e, :, 0:512]), writes=[pfd[par]])
                        g.dma("pool", pf[par + 1][:], wsrc(moe_w1[l, e, :, D:D + 512]), writes=[pfd[par + 1]])

                    def sparse_chunk(e, c0):
                        b1o = voff["b1"] + (l * NE + e) * 16
                        r0 = e * T + c0 * CAP
                        if c0 == 0:
                            hsel = e % 2
                        else:
                            hsel = 2
                            gather_load(e, c0, hsel)
                        hgTok, hgd, idxt, idxtd = hgToks[hsel], hgds[hsel], idxts[hsel], idxtds[hsel]
                        par = (e % 2) * 2
                        w1s = {0: ((pf[par], pfd[par]), (pf[par + 1], pfd[par + 1])), 1: ((ring[0], ringd[0]), (ring[1], ringd[1]))}
                        w2s = [(ring[2], ringd[2]), (ring[3], ringd[3])]
                        for c in range(NCH):
                            pb = 6 + pc_[0] % 2
                            pc_[0] += 1
                            psb = ps[pb][:, :].bitcast(BF16)
                            for j3 in range(3):
                                g.op("pe", lambda: nc.tensor.transpose(out=psb[:, j3 * 128:(j3 + 1) * 128], in_=hgTok[:, j3, c * 128:(c + 1) * 128], identity=identb[:]),
                                     reads=[hgd, cdep2], writes=[psd[pb]])
                            if c % 2 == 0:
                                g.op("act", lambda: nc.scalar.copy(out=hbg[:, c, :], in_=psb[:, 0:CAP]), reads=[psd[pb]], writes=[hbgd[c]])
                            else:
                                g.op("dve", lambda: nc.vector.tensor_copy(out=hbg[:, c, :], in_=psb[:, 0:CAP]), reads=[psd[pb]], writes=[hbgd[c]])
                        pend = []
                        for u in range(2):
                            (sgA, sgD), (slA, slD) = w1s[u]
                            for jj in range(4):
                                j = u * 4 + jj
                                s = ec[0] % 2
                                s3 = ec[0] % 3
                                ec[0] += 1
                                pg = s3
                                pl = 3 + s3
                                for c in range(NCH):
                                    g.op("pe", lambda: nc.tensor.matmul(ps[pg][:, 0:CAP], lhsT=sgA[:, c, jj * 128:(jj + 1) * 128], rhs=hbg[:, c, :],
                                                                        start=(c == 0), stop=(c == NCH - 1)),
                                         reads=[sgD, hbgd[c]], writes=[psd[pg]])
                                for c in range(NCH):
                                    g.op("pe", lambda: nc.tensor.matmul(ps[pl][:, 0:CAP], lhsT=slA[:, c, jj * 128:(jj + 1) * 128], rhs=hbg[:, c, :],
                                                                        start=(c == 0), stop=(c == NCH - 1)),
                                         reads=[slD, hbgd[c]], writes=[psd[pl]])
                                g.op("dve", lambda: nc.vector.tensor_scalar(out=At[s][:], in0=ps[pg][:, 0:CAP], scalar1=V[:, b1o + j:b1o + j + 1], scalar2=7.0,
                                                                            op0=ALU.add, op1=ALU.min), reads=[psd[pg], Vd], writes=[Ad[s]])
                                g.op("act", lambda: nc.scalar.activation(out=St[s][:], in_=At[s][:], func=AF.Sigmoid, scale=1.702),
                                     reads=[Ad[s]], writes=[Sdp[s]])
                                g.op("dve", lambda: nc.vector.tensor_scalar(out=Lt[s][:], in0=ps[pl][:, 0:CAP], scalar1=V[:, b1o + 8 + j:b1o + 8 + j + 1], scalar2=-6.0,
                                                                            op0=ALU.add, op1=ALU.max), reads=[psd[pl], Vd], writes=[Ld[s]])
                                if pend:
                                    pend.pop()()

                                def _fin(s=s, j=j):
                                    g.op("dve", lambda: nc.vector.tensor_tensor(out=St[s][:], in0=At[s][:], in1=St[s][:], op=ALU.mult),
                                         reads=[Ad[s], Sdp[s]], writes=[Sdp[s]])
                                    g.op("dve", lambda: nc.vector.scalar_tensor_tensor(out=actT[:, j, :], in0=Lt[s][:], scalar=8.0, in1=St[s][:],
                                                                                       op0=ALU.min, op1=ALU.mult), reads=[Ld[s], Sdp[s]], writes=[actd[j]])
                                pend.append(_fin)
                        if pend:
                            pend.pop()()
                        for j3 in range(3):
                            for hh in range(2):
                                swA, swD = w2s[hh]
                                pb = 6 + pc_[0] % 2
                                pc_[0] += 1
                                for fc in range(NCH):
                                    g.op("pe", lambda: nc.tensor.matmul(ps[pb][:, :], lhsT=actT[:, fc, j3 * 128:(j3 + 1) * 128], rhs=swA[:, fc, :],
                                                                        start=(fc == 0), stop=(fc == NCH - 1)),
                                         reads=[swD, actd[fc]], writes=[psd[pb]])
                                g.op("act", lambda: nc.scalar.copy(out=yTok[:, j3, hh * 512:(hh + 1) * 512], in_=ps[pb][:, :]), reads=[psd[pb]], writes=[yTd[j3]])
                            g.idma(y4_d[:, :], idxt[:, j3, :], yTok[:, j3, :], bnd_y4, reads=[idxtd, yTd[j3]], writes=[y4_dep])

                    prefetchA(0)
                    gather_load(0, 0, 0)
                    for e in range(NE):
                        expert_loads(e)
                        if e + 1 < NE:
                            prefetchA(e + 1)
                            gather_load(e + 1, 0, (e + 1) % 2)
                        for reg in flag_regs:
                            ek = ENG_KEY[reg.engine]
                            g._wait(ek, [(flag_dep[l].sem, flag_dep[l].tot)])
                            g.eng[ek].reg_load(reg, cnt_d[l:l + 1, e:e + 1])
                        def emit_chunks(c0):
                            snap = g.snapshot()
                            with nc.If_cmp(flag_regs, c0 * CAP, "IS_GT"):
                                sparse_chunk(e, c0)
                                if c0 + 1 < NCHUNK:
                                    emit_chunks(c0 + 1)
                            big = g.snapshot()
                            g.restore(snap)
                            with nc.Else():
                                g.pad_to(big)
                            g.restore(big)
                            g.known = {k: dict(v) for k, v in snap[2].items()}

                        emit_chunks(0)
                    g.barrier()
                CB = ExitStack()
                with CB:
                    y4t = [sb(CB, "y4t%d" % i, [128, 4, D], F32) for i in range(2)]
                    y4td = [g.dep(dma=True) for _ in range(2)]
                    acc = [sb(CB, "cacc%d" % i, [128, D], F32) for i in range(2)]
                    accd = [g.dep(), g.dep()]
                    for n_, (i, tt, sub) in enumerate(subs):
                        s = n_ % 2
                        t0 = i * 128
                        cls = 1 if tt == 0 else 0
                        g.dma("sp", y4t[s][:], y4_d[4 * t0:4 * t0 + 512, :].rearrange("(p k) f -> p k f", k=4), reads=[y4_dep], writes=[y4td[s]])
                        g.op("dve", lambda: nc.vector.tensor_scalar(out=acc[s][:], in0=y4t[s][:, 0, :], scalar1=GK[:, i, 0:1], scalar2=None, op0=ALU.mult),
                             reads=[y4td[s], GKd], writes=[accd[s]])
                        for k in range(1, 4):
                            g.op("dve", lambda: nc.vector.scalar_tensor_tensor(out=acc[s][:], in0=y4t[s][:, k, :], scalar=GK[:, i, k:k + 1], in1=acc[s][:],
                                                                               op0=ALU.mult, op1=ALU.add), reads=[y4td[s], GKd, accd[s]], writes=[accd[s]])
                        for half in range(2):
                            pbt = 2 * s + half
                            for cc in range(4):
                                c = half * 4 + cc
                                g.op("pe", lambda: nc.tensor.transpose(out=ps[pbt][:, cc * 128:(cc + 1) * 128], in_=acc[s][:, c * 128:(c + 1) * 128], identity=ident[:]),
                                     reads=[accd[s], cdep], writes=[psd[pbt]])
                            for cc in range(4):
                                c = half * 4 + cc
                                g.op("dve", lambda: nc.vector.scalar_tensor_tensor(out=x[:, c, t0:t0 + 128], in0=ps[pbt][:, cc * 128:(cc + 1) * 128], scalar=mod(l, 5, c, cls),
                                                                                   in1=x[:, c, t0:t0 + 128], op0=ALU.mult, op1=ALU.add),
                                     reads=[psd[pbt], modd], writes=[xd[c][tt]])
                    g.barrier()

        moe_layer(0, list(range(NTT)))

        if dbg == "x1":
            for c in range(NCH):
                g.dma("sp", dbg_d[:, c, :], xstate["x"][:, c, :], reads=[xstate["d"][c][tt] for tt in range(NTT)], writes=[out_dep])

        LAT = [1, 2, 3, 4]
        L1 = ExitStack()
        with L1:
            cat1 = sb(L1, "cat1", [128, NCH, TL], BF16)
            cat1d = [[g.dep() for _ in range(NTT)] for _ in range(NCH)]
            x = xstate["x"]
            xd = xstate["d"]
            A1 = ExitStack()
            with A1:
                nb = norm_bufs(A1)
                for tt in range(NTT):
                    rmsnorm_tile(nb, lambda c: x[:, c, tsl(tt)], lambda c: xd[c][tt], tt, "gs1", 1, 0)
                for c in range(NCH):
                    g.dma("sp", xs_d[:, c, :], x[:, c, :], reads=[xd[c][tt] for tt in range(NTT)], writes=[xs_dep])
                g.barrier()
            xes.close()
            M1 = ExitStack()
            with M1:
                gw = sb(M1, "gw", [128, 16, 2, 256], BF16)
                gwd = g.dep(dma=True)
                for gi_, src in ((0, od_ga), (1, od_gx)):
                    for d_ in range(2):
                        g.dma("pool", gw[:, gi_ * 8 + d_ * 4:gi_ * 8 + d_ * 4 + 4, :, :],
                              src[d_].rearrange("b (c p) o -> p b c o", p=128), writes=[gwd])
                wr1 = [sb(M1, "w1r%d" % i, [128, NCH, 512], BF16) for i in range(2)]
                wr1d = [g.dep(dma=True) for _ in range(2)]
                ug = sb(M1, "ug", [128, 2, T], F32)
                ugd = [g.dep(), g.dep()]
                xc = sb(M1, "xc", [128, 2, T], F32)
                xcd = [g.dep(), g.dep()]
                xcb = sb(M1, "xcb", [128, 2, T], BF16)
                xcbd = [g.dep(), g.dep()]
                rec = sb(M1, "rec", [128, 2, T], F32)
                recd = [[g.dep() for _ in range(NTT)] for _ in range(2)]
                NT_ = 9
                tb = [[sb(M1, "tb%d_%d" % (k, i), [128, 512], F32) for i in range(2 if k < 7 else 1)] for k in range(NT_)]
                tb[7].append(tb[7][0])
                tb[8].append(tb[8][0])
                tbd = [[g.dep(), g.dep()] for _ in range(NT_)]
                tbd[7][1] = tbd[7][0]
                tbd[8][1] = tbd[8][0]
                cnt1 = [0]
                pcnt = [0]
                for blk in range(4):
                    sW = blk % 2
                    g.dma("pool", wr1[sW][:, :, 0:256], od_w_in[:, blk * 256:(blk + 1) * 256].rearrange("(c p) f -> p c f", p=128), writes=[wr1d[sW]])
                    g.dma("pool", wr1[sW][:, :, 256:512], od_w_in[:, D + blk * 256:D + (blk + 1) * 256].rearrange("(c p) f -> p c f", p=128), writes=[wr1d[sW]])
                    for cc in range(2):
                        for tt in range(NTT):
                            n = TN[tt]
                            pb = pcnt[0] % 2
                            pcnt[0] += 1
                            for c in range(NCH):
                                g.op("pe", lambda: nc.tensor.matmul(ps[pb][:, 0:n], lhsT=wr1[sW][:, c, 256 + cc * 128:256 + (cc + 1) * 128], rhs=hb[:, c, tsl(tt)],
                                                                    start=(c == 0), stop=(c == NCH - 1)),
                                     reads=[wr1d[sW], hbd[c][tt]], writes=[psd[pb]])
                            g.op("act", lambda: nc.scalar.copy(out=ug[:, cc, tsl(tt)], in_=ps[pb][:, 0:n]), reads=[psd[pb]], writes=[ugd[cc]])
                    for d_ in range(2):
                        for cc in range(2):
                            ch = blk * 2 + cc
                            wv = lambda k: Vcol("odconvw", (d_ * 4 + k) * 8 + ch)
                            bv = Vcol("odconvb", d_ * 8 + ch)
                            for (a, b) in ((0, TC), (TC, T)):
                                if d_ == 0:
                                    g.op("dve", lambda: nc.vector.tensor_scalar(out=xc[:, cc, a:b], in0=ug[:, cc, a:b], scalar1=wv(3), scalar2=bv, op0=ALU.mult, op1=ALU.add),
                                         reads=[ugd[cc], Vd], writes=[xcd[cc]])
                                    for k in range(3):
                                        sh = 3 - k
                                        g.op("dve", lambda: nc.vector.scalar_tensor_tensor(out=xc[:, cc, a + sh:b], in0=ug[:, cc, a:b - sh], scalar=wv(k), in1=xc[:, cc, a + sh:b],
                                                                                           op0=ALU.mult, op1=ALU.add), reads=[ugd[cc], Vd, xcd[cc]], writes=[xcd[cc]])
                                else:
                                    g.op("dve", lambda: nc.vector.tensor_scalar(out=xc[:, cc, a:b], in0=ug[:, cc, a:b], scalar1=wv(0), scalar2=bv, op0=ALU.mult, op1=ALU.add),
                                         reads=[ugd[cc], Vd], writes=[xcd[cc]])
                                    for k in range(1, 4):
                                        g.op("dve", lambda: nc.vector.scalar_tensor_tensor(out=xc[:, cc, a:b - k], in0=ug[:, cc, a + k:b], scalar=wv(k), in1=xc[:, cc, a:b - k],
                                                                                           op0=ALU.mult, op1=ALU.add), reads=[ugd[cc], Vd, xcd[cc]], writes=[xcd[cc]])
                            g.op("act", lambda: nc.scalar.copy(out=xcb[:, cc, :], in_=xc[:, cc, :]), reads=[xcd[cc]], writes=[xcbd[cc]])
                        order = [0, 1, 2, 3, 4] if d_ == 0 else [0, 4, 3, 2, 1]
                        for oc in range(2):
                            ch = blk * 2 + oc
                            prev = None
                            for tt in order:
                                n = TN[tt]
                                s = cnt1[0] % 2
                                cnt1[0] += 1
                                pa = 2 + s
                                px = 4 + s
                                for gi_, pb in ((0, pa), (1, px)):
                                    for ic in range(2):
                                        g.op("pe", lambda: nc.tensor.matmul(ps[pb][:, 0:n], lhsT=gw[:, gi_ * 8 + d_ * 4 + blk, ic, oc * 128:(oc + 1) * 128], rhs=xcb[:, ic, tsl(tt)],
                                                                            start=(ic == 0), stop=(ic == 1)),
                                             reads=[gwd, xcbd[ic]], writes=[psd[pb]])
                                R, I_, A_, A2, TH, GI, HS = 0, 1, 2, 3, 4, 5, 6
                                c8 = Scol("c8", d_ * 8 + ch)
                                c8h = Scol("c8h", d_ * 8 + ch)
                                g.op("act", lambda: nc.scalar.activation(out=tb[R][s][:, 0:n], in_=ps[pa][:, 0:n], func=AF.Tanh, scale=0.5, bias=Scol("hgab", d_ * 8 + ch)),
                                     reads=[psd[pa], Sd], writes=[tbd[R][s]])
                                g.op("act", lambda: nc.scalar.activation(out=tb[I_][s][:, 0:n], in_=ps[px][:, 0:n], func=AF.Tanh, scale=0.5, bias=Scol("hgxb", d_ * 8 + ch)),
                                     reads=[psd[px], Sd], writes=[tbd[I_][s]])
                                g.op("act", lambda: nc.scalar.activation(out=tb[A_][s][:, 0:n], in_=tb[R][s][:, 0:n], func=AF.Exp, scale=c8h, bias=c8h),
                                     reads=[tbd[R][s], Sd], writes=[tbd[A_][s]])
                                g.op("act", lambda: nc.scalar.activation(out=tb[A2][s][:, 0:n], in_=tb[R][s][:, 0:n], func=AF.Exp, scale=c8, bias=c8),
                                     reads=[tbd[R][s], Sd], writes=[tbd[A2][s]])
                                g.op("act", lambda: nc.scalar.activation(out=tb[TH][s][:, 0:n], in_=tb[R][s][:, 0:n], func=AF.Tanh, scale=c8h, bias=c8h),
                                     reads=[tbd[R][s], Sd], writes=[tbd[TH][s]])
                                g.op("dve", lambda: nc.vector.scalar_tensor_tensor(out=tb[A2][s][:, 0:n], in0=tb[A2][s][:, 0:n], scalar=1.0, in1=tb[TH][s][:, 0:n],
                                                                                   op0=ALU.add, op1=ALU.mult), reads=[tbd[A2][s], tbd[TH][s]], writes=[tbd[A2][s]])
                                g.op("act", lambda: nc.scalar.activation(out=tb[A2][s][:, 0:n], in_=tb[A2][s][:, 0:n], func=AF.Ln, scale=-1.0),
                                     reads=[tbd[A2][s]], writes=[tbd[A2][s]])
                                g.op("act", lambda: nc.scalar.activation(out=tb[A2][s][:, 0:n], in_=tb[A2][s][:, 0:n], func=AF.Exp, scale=0.5, bias=math.log(0.5)),
                                     reads=[tbd[A2][s]], writes=[tbd[A2][s]])
                                g.op("dve", lambda: nc.vector.scalar_tensor_tensor(out=tb[GI][s][:, 0:n], in0=tb[I_][s][:, 0:n], scalar=1.0, in1=xc[:, oc, tsl(tt)],
                                                                                   op0=ALU.add, op1=ALU.mult), reads=[tbd[I_][s], xcd[oc]], writes=[tbd[GI][s]])
                                g.op("dve", lambda: nc.vector.tensor_tensor(out=tb[GI][s][:, 0:n], in0=tb[GI][s][:, 0:n], in1=tb[A2][s][:, 0:n], op=ALU.mult),
                                     reads=[tbd[GI][s], tbd[A2][s]], writes=[tbd[GI][s]])
                                if d_ == 0:
                                    dst = rec[:, oc, tsl(tt)]
                                    dstd = recd[oc][tt]
                                    init = 0.0 if prev is None else prev[0][:, TN[prev[2]] - 1:TN[prev[2]]]
                                    g.op("dve", lambda: nc.vector.tensor_tensor_scan(out=dst, data0=tb[A_][s][:, 0:n], data1=tb[GI][s][:, 0:n], initial=init,
                                                                                     op0=ALU.mult, op1=ALU.add),
                                         reads=[tbd[A_][s], tbd[GI][s]] + ([prev[1]] if prev else []), writes=[dstd])
                                    prev = (dst, dstd, tt)
                                else:
                                    dst = tb[HS][s][:, 0:n]
                                    dstd = tbd[HS][s]
                                    init = 0.0 if prev is None else prev[0][:, 0:1]
                                    g.op("dve", lambda: nc.vector.tensor_tensor_scan(out=dst[:, ::-1], data0=tb[A_][s][:, 0:n][:, ::-1], data1=tb[GI][s][:, 0:n][:, ::-1],
                                                                                     initial=init, op0=ALU.mult, op1=ALU.add),
                                         reads=[tbd[A_][s], tbd[GI][s]] + ([prev[1]] if prev else []), writes=[dstd])
                                    prev = (dst, dstd, tt)
                                    if tt != 0:
                                        pgt = 6 + s
                                        for c in range(NCH):
                                            g.op("pe", lambda: nc.tensor.matmul(ps[pgt][:, 0:n], lhsT=wr1[sW][:, c, oc * 128:(oc + 1) * 128], rhs=hb[:, c, tsl(tt)],
                                                                                start=(c == 0), stop=(c == NCH - 1)),
                                                 reads=[wr1d[sW], hbd[c][tt]], writes=[psd[pgt]])
                                        GL, SM = 7, 8
                                        g.op("act", lambda: nc.scalar.activation(out=tb[GL][s][:, 0:n], in_=ps[pgt][:, 0:n], func=AF.Gelu), reads=[psd[pgt]], writes=[tbd[GL][s]])
                                        g.op("pool", lambda: nc.gpsimd.tensor_tensor(out=tb[SM][s][:, 0:n], in0=dst, in1=rec[:, oc, tsl(tt)], op=ALU.add),
                                             reads=[dstd, recd[oc][tt]], writes=[tbd[SM][s]])
                                        g.op("dve", lambda: nc.vector.tensor_tensor(out=cat1[:, ch, TOFF[tt] - TC:TOFF[tt] - TC + n], in0=tb[SM][s][:, 0:n], in1=tb[GL][s][:, 0:n], op=ALU.mult),
                                             reads=[tbd[SM][s], tbd[GL][s]], writes=[cat1d[ch][tt]])
                g.barrier()
            alloc_x()
            C1 = ExitStack()
            with C1:
                xst = [sb(C1, "x1st%d" % i, [128, NCH, 512], F32) for i in range(2)]
                xstd = [g.dep(dma=True), g.dep(dma=True)]
                c1c = [0]

                def xold1(tt):
                    s = c1c[0] % 2
                    c1c[0] += 1
                    for c in range(NCH):
                        g.dma("sp", xst[s][:, c, 0:TN[tt]], xs_d[:, c, tsl(tt)], reads=[xs_dep], writes=[xstd[s]])
                    return xst[s], xstd[s]

                out_proj(1, od_w_out, lambda c, tt: cat1[:, c, TOFF[tt] - TC:TOFF[tt] - TC + TN[tt]], lambda c, tt: cat1d[c][tt], LAT, xold1, C1)
                g.barrier()
        moe_layer(1, LAT)

        FN = ExitStack()
        with FN:
            x = xstate["x"]
            xd = xstate["d"]
            nb = norm_bufs(FN)
            sq, sqd, tmp, tmpd, rs, rsd, cnt = nb
            yb = [sb(FN, "yb%d" % i, [128, NCH, 128], F32) for i in range(2)]
            ybd = [g.dep(), g.dep()]
            ot = [sb(FN, "ot%d" % i, [128, D], F32) for i in range(2)]
            otd = [g.dep(dma=True), g.dep(dma=True)]
            oc_ = [0]
            for tt in LAT:
                n = TN[tt]
                pb = 5
                for c in range(NCH):
                    s = cnt[0] % 2
                    cnt[0] += 1
                    g.op("act", lambda: nc.scalar.activation(out=sq[s][:, 0:n], in_=x[:, c, tsl(tt)], func=AF.Square), reads=[xd[c][tt]], writes=[sqd[s]])
                    g.op("pe", lambda: nc.tensor.matmul(ps[pb][:, 0:n], lhsT=ones32[:], rhs=sq[s][:, 0:n], start=(c == 0), stop=(c == NCH - 1)),
                         reads=[sqd[s], onesd], writes=[psd[pb]])
                g.op("act", lambda: nc.scalar.activation(out=rs[:, 0:n], in_=ps[pb][:, 0:n], func=AF.Sqrt, scale=1.0 / D, bias=EPS), reads=[psd[pb]], writes=[rsd])
                g.op("dve", lambda: nc.vector.reciprocal(out=rs[:, 0:n], in_=rs[:, 0:n]), reads=[rsd], writes=[rsd])
                for sub in range(n // 128):
                    s = oc_[0] % 2
                    oc_[0] += 1
                    for c in range(NCH):
                        g.op("dve", lambda: nc.vector.scalar_tensor_tensor(out=yb[s][:, c, :], in0=x[:, c, TOFF[tt] + sub * 128:TOFF[tt] + (sub + 1) * 128],
                                                                           scalar=Vcol("fnorm", c), in1=rs[:, sub * 128:(sub + 1) * 128], op0=ALU.mult, op1=ALU.mult),
                             reads=[xd[c][tt], rsd, Vd], writes=[ybd[s]])
                    for half in range(2):
                        pbt = 6 + half
                        for cc in range(4):
                            c = half * 4 + cc
                            g.op("pe", lambda: nc.tensor.transpose(out=ps[pbt][:, cc * 128:(cc + 1) * 128], in_=yb[s][:, c, :], identity=ident[:]),
                                 reads=[ybd[s], cdep], writes=[psd[pbt]])
                        if half == 0:
                            g.op("act", lambda: nc.scalar.copy(out=ot[s][:, 0:512], in_=ps[pbt][:, :]), reads=[psd[pbt]], writes=[otd[s]])
                        else:
                            g.op("dve", lambda: nc.vector.tensor_copy(out=ot[s][:, 512:1024], in_=ps[pbt][:, :]), reads=[psd[pbt]], writes=[otd[s]])
                    r0 = TOFF[tt] - TC + sub * 128
                    g.dma("sp", out_d[r0:r0 + 128, :], ot[s][:], reads=[otd[s]], writes=[out_dep])
            g.barrier()
        xes.close()
    return nc


def _consts():
    ident = np.eye(128, dtype=np.float32)
    perm = np.zeros((128, 128), np.float32)
    for m in range(128):
        blk = m // 16
        partner = (blk ^ 1) * 16 + (m % 16)
        perm[partner, m] = 1.0
    n_rows = TL // 64
    rows, cols = np.meshgrid(np.arange(n_rows), np.arange(64), indexing="ij")
    pos = np.stack([rows.reshape(-1), cols.reshape(-1)], axis=-1).astype(np.float32)
    inv = (np.float32(10000.0) ** (-np.arange(16, dtype=np.float32) / np.float32(16))).astype(np.float32)
    ang = (pos[:, :, None] * inv).astype(np.float32)
    cos = np.cos(ang).astype(np.float32)
    sin = np.sin(ang).astype(np.float32)
    cosT = np.ones((128, T), np.float32)
    sinT = np.zeros((128, T), np.float32)
    for p in range(128):
        dd = p % 64
        axis = dd // 32
        half = (dd % 32) // 16
        f = dd % 16
        cosT[p, TC:] = cos[:, axis, f]
        sinT[p, TC:] = (-sin[:, axis, f]) if half == 0 else sin[:, axis, f]
    return ident, perm, cosT, sinT


def _consts2(NE):
    utri = np.triu(np.ones((128, 128), np.float32), k=1)
    offrow = np.tile((np.arange(NE, dtype=np.float32) * T)[None, :], (128, 1))
    tok4f = (4.0 * np.arange(128, dtype=np.float32)[:, None] + np.arange(4, dtype=np.float32)[None, :]).astype(np.float32)
    return utri, offrow, tok4f


def make_in_maps(inp, NE, ncores):
    voff, vrows, vpad = vec_layout(NE)
    ident, perm, cosT, sinT = _consts()
    f = lambda a: np.ascontiguousarray(np.asarray(a, dtype=np.float32))
    shared = {
        "w_mod": f(inp["w_mod"]), "ev_w_in": f(inp["ev_w_in"][0]), "ev_w_out": f(inp["ev_w_out"][0]),
        "od_w_in": f(inp["od_w_in"][0]), "od_w_out": f(inp["od_w_out"][0]),
        "od_ga": f(inp["od_gate_a_w"][0]), "od_gx": f(inp["od_gate_x_w"][0]),
        "w_router": f(inp["moe_w_router"]), "b_router": f(inp["moe_b_router"]),
        "moe_w1": f(inp["moe_w1"]), "moe_w2": f(inp["moe_w2"]), "moe_b2": f(inp["moe_b2"]),
        "lams": f(np.stack([inp["ev_lambda_q1"][0], inp["ev_lambda_k1"][0], inp["ev_lambda_q2"][0], inp["ev_lambda_k2"][0]])),
        "ident": ident, "perm": perm, "cosT": cosT, "sinT": sinT,
    }
    shared["utri"], shared["offrow"], shared["tok4f"] = _consts2(NE)
    maps = []
    for b in range(ncores):
        rows = [
            f(inp["c"][b]).reshape(8, 128), f(inp["c_ctx"]).reshape(8, 128), f(inp["b_mod"]).reshape(96, 128),
            f(inp["norm_mix"]).reshape(16, 128), f(inp["norm_ffn"]).reshape(16, 128), f(inp["final_norm"]).reshape(8, 128),
            f(inp["ev_conv_w"][0]).reshape(12, 128), f(inp["ev_subln"][0]).reshape(1, 128),
            f(inp["od_conv_w"][0]).reshape(64, 128), f(inp["od_conv_b"][0]).reshape(16, 128),
            f(inp["od_gate_a_b"][0]).reshape(16, 128), f(inp["od_gate_x_b"][0]).reshape(16, 128),
            f(inp["od_lru_lambda"][0]).reshape(16, 128), f(inp["moe_b1"]).reshape(2 * NE * 16, 128),
        ]
        v = np.concatenate(rows, axis=0)
        assert v.shape[0] == vrows
        vp = np.zeros((vpad, 128), np.float32)
        vp[:vrows] = v
        m = dict(shared)
        m["xin"] = f(inp["x"][b])
        m["ctxin"] = f(inp["ctx"][b])
        m["vecs"] = vp
        maps.append(m)
    return maps


def kernel(**inputs):
    NE = inputs["moe_w1"].shape[1]
    B = inputs["x"].shape[0]
    nc = build(NE)
    maps = make_in_maps(inputs, NE, B)
    res = run_bass_kernel_spmd(nc, maps, core_ids=list(range(B)))
    return np.stack([np.asarray(r["out"], dtype=np.float32) for r in res.results], axis=0)
```
